# Optimizing a Trainium2 kernel written in Bass

```python
import math
import jax
import jax.numpy as jnp
from jax import lax
import numpy as np

D_MODEL = 1024
BATCH = 4
SEQ = 8192
DEPTH = 4

GRID_W = 64
CTX_LEN = 256
N_MIXERS = 3
DN_ALPHA = (2 * DEPTH) ** 0.25
DN_BETA = (8 * DEPTH) ** -0.25
LN_EPS = 1e-5

RK_HEAD = 64
RK_HEADS = D_MODEL // RK_HEAD
RK_DECAY_LORA = 64
RK_ICLR_LORA = 64
RK_GATE_LORA = 128
RK_DECAY_SCALE = math.exp(-0.5)
RK_GN_EPS = 64e-5

POOL_WINDOWS = (2, 4, 8, 16)
POOL_GROUP = D_MODEL // len(POOL_WINDOWS)

GDN_HEAD = 128
GDN_HEADS = D_MODEL // GDN_HEAD
GDN_CONV = 4
GDN_CHUNK = 64
GDN_NORM_EPS = 1e-6
GDN_IN = 4 * D_MODEL + 4 * GDN_HEADS

D_FF = 2816
N_EXPERTS = 8
TOP_K = 2
D_FF_EXPERT = 1408

N_RWKV_LAYERS = len(range(0, DEPTH, N_MIXERS))
N_POOL_LAYERS = len(range(1, DEPTH, N_MIXERS))
N_GDN_LAYERS = len(range(2, DEPTH, N_MIXERS))
N_DENSE_LAYERS = len(range(0, DEPTH, 2))
N_MOE_LAYERS = len(range(1, DEPTH, 2))

kernel_name = 'hybrid_rwkv7_pool_gdn_moe_diffusion_trunk'


def layer_norm(x, g, b):
    xf = x.astype(jnp.float32)
    mean = jnp.mean(xf, axis=-1, keepdims=True)
    var = jnp.mean(jnp.square(xf - mean), axis=-1, keepdims=True)
    return ((xf - mean) * lax.rsqrt(var + LN_EPS) * g + b).astype(x.dtype)


def flip_time(t):
    return jnp.flip(t, axis=1)


def identity(t):
    return t


def qshift_grid(h, n_rows):
    b, n, d = h.shape
    q = d // 4
    grid = h.reshape(b, n_rows, GRID_W, d)
    left = jnp.pad(grid[:, :, :-1, :q], ((0, 0), (0, 0), (1, 0), (0, 0)))
    right = jnp.pad(grid[:, :, 1:, q:2 * q], ((0, 0), (0, 0), (0, 1), (0, 0)))
    up = jnp.pad(grid[:, :-1, :, 2 * q:3 * q], ((0, 0), (1, 0), (0, 0), (0, 0)))
    down = jnp.pad(grid[:, 1:, :, 3 * q:], ((0, 0), (0, 1), (0, 0), (0, 0)))
    return jnp.concatenate([left, right, up, down], axis=-1).reshape(b, n, d)


def bishift_seq(h):
    half = h.shape[-1] // 2
    prev = jnp.pad(h[:, :-1, :half], ((0, 0), (1, 0), (0, 0)))
    nxt = jnp.pad(h[:, 1:, half:], ((0, 0), (0, 1), (0, 0)))
    return jnp.concatenate([prev, nxt], axis=-1)


def to_heads(t, n_heads, head_dim):
    return t.reshape(*t.shape[:-1], n_heads, head_dim)


def rwkv7_branch(h, shifted, mu, w_rkv, w0, w1, w2, a0, a1, a2, k_k, k_a):
    f32 = jnp.float32
    xx = shifted - h
    xr, xw, xk, xv, xa, xg = (h + xx * mu[m] for m in range(6))
    r = (xr @ w_rkv[0]).astype(f32)
    k = (xk @ w_rkv[1]).astype(f32)
    v = (xv @ w_rkv[2]).astype(f32)
    d_lora = jnp.einsum('zbtr,zrd->zbtd', jnp.tanh(jnp.einsum('btd,zdr->zbtr', xw, w1)), w2)
    log_w = -RK_DECAY_SCALE * jax.nn.sigmoid((w0[:, None, None, :] + d_lora).astype(f32))
    a_lora = jnp.einsum('zbtr,zrd->zbtd', jnp.einsum('btd,zdr->zbtr', xa, a1), a2)
    a = jax.nn.sigmoid((a0[:, None, None, :] + a_lora).astype(f32))
    kk = to_heads(k * k_k.astype(f32), RK_HEADS, RK_HEAD)
    kk = kk / jnp.maximum(jnp.sqrt(jnp.sum(kk * kk, axis=-1, keepdims=True)), 1e-12)
    k_dir = k[None] * (1.0 + (a - 1.0) * k_a.astype(f32))
    hd = lambda t: to_heads(t, RK_HEADS, RK_HEAD)
    return (hd(r), hd(log_w), kk, hd(a), hd(k_dir), hd(v), xg)


def wkv7_scan(s0, r, w, kk, a, k, v):
    def step(s, inp):
        r_t, w_t, kk_t, a_t, k_t, v_t = inp
        s_kk = jnp.einsum('bhvk,bhk->bhv', s, kk_t)
        s = (s * w_t[:, :, None, :] - s_kk[..., None] * (kk_t * a_t)[:, :, None, :]
             + v_t[..., None] * k_t[:, :, None, :])
        return s, jnp.einsum('bhvk,bhk->bhv', s, r_t)
    xs = tuple(jnp.moveaxis(t, 1, 0) for t in (r, w, kk, a, k, v))
    s_final, ys = lax.scan(step, s0, xs)
    return jnp.moveaxis(ys, 0, 1), s_final


def rwkv7_dir_inputs(feats, d, flip):
    r, log_w, kk, a, k_dir, v, _ = feats
    return [flip(t) for t in (r, jnp.exp(log_w[d]), kk, a[d], k_dir[d], v)]


def rwkv7_out(y, feats, r_k, g1, g2, lnx_g, lnx_b, w_o):
    r, _, _, _, k_dir, v, xg = feats
    b, n = y.shape[:2]
    mean = jnp.mean(y, axis=-1, keepdims=True)
    var = jnp.mean(jnp.square(y - mean), axis=-1, keepdims=True)
    yn = ((y - mean) * lax.rsqrt(var + RK_GN_EPS)).reshape(b, n, D_MODEL) * lnx_g + lnx_b
    coef = jnp.einsum('bthn,zbthn,hn->bth', r, k_dir, r_k.astype(jnp.float32))
    bonus = (coef[..., None] * v).reshape(b, n, D_MODEL)
    gate = jax.nn.sigmoid(xg @ g1) @ g2
    return ((yn + bonus).astype(xg.dtype) * gate) @ w_o


def rwkv7_mixer(h, hc, n_rows, mu, w_rkv, w0, w1, w2, a0, a1, a2, g1, g2, k_k, k_a, r_k,
                lnx_g, lnx_b, w_o, want_ctx):
    lat = rwkv7_branch(h, qshift_grid(h, n_rows), mu, w_rkv, w0, w1, w2, a0, a1, a2, k_k, k_a)
    cx = rwkv7_branch(hc, bishift_seq(hc), mu, w_rkv, w0, w1, w2, a0, a1, a2, k_k, k_a)
    zero_state = jnp.zeros((h.shape[0], RK_HEADS, RK_HEAD, RK_HEAD), jnp.float32)
    y_lat = 0.0
    y_ctx = 0.0
    for d in range(2):
        flip = flip_time if d == 1 else identity
        yc, s_ctx = wkv7_scan(zero_state, *rwkv7_dir_inputs(cx, d, flip))
        yl, _ = wkv7_scan(s_ctx, *rwkv7_dir_inputs(lat, d, flip))
        y_lat = y_lat + flip(yl)
        y_ctx = y_ctx + flip(yc)
    out_lat = rwkv7_out(y_lat, lat, r_k, g1, g2, lnx_g, lnx_b, w_o)
    out_ctx = rwkv7_out(y_ctx, cx, r_k, g1, g2, lnx_g, lnx_b, w_o) if want_ctx else None
    return out_lat, out_ctx


def centred_pool_minus_self(x, window):
    n = x.shape[-2]
    xf = x.astype(jnp.float32)
    csum = jnp.cumsum(xf, axis=-2)
    csum = jnp.concatenate([jnp.zeros_like(csum[..., :1, :]), csum], axis=-2)
    t = jnp.arange(n)
    lo = jnp.clip(t - window // 2, 0, n)
    hi = jnp.clip(t + window // 2, 0, n)
    total = jnp.take(csum, hi, axis=-2) - jnp.take(csum, lo, axis=-2)
    count = (hi - lo).astype(jnp.float32)[:, None]
    return (total / count - xf).astype(x.dtype)


def pool_mixer(h, pool_w, pool_scale):
    outs = []
    for gi, win in enumerate(POOL_WINDOWS):
        xg = h[..., gi * POOL_GROUP:(gi + 1) * POOL_GROUP]
        outs.append(centred_pool_minus_self(xg, win) @ pool_w[gi])
    return jnp.concatenate(outs, axis=-1) * pool_scale


def depthwise_conv_centred(x, w):
    left = GDN_CONV // 2
    right = GDN_CONV - 1 - left
    return lax.conv_general_dilated(x, w[:, None, :], window_strides=(1,), padding=[(left, right)],
                                    dimension_numbers=('NWC', 'WIO', 'NWC'),
                                    feature_group_count=x.shape[-1])


def l2_normalize(t, eps):
    return t * lax.rsqrt(jnp.sum(t * t, axis=-1, keepdims=True) + eps)


def gdn_features(h, w_in, conv_w, a_log, dt_bias):
    f32 = jnp.float32
    b, n, d = h.shape
    proj = h @ w_in
    qkv = jax.nn.silu(depthwise_conv_centred(proj[..., :3 * d], conv_w)).astype(f32)
    q, k, v = (to_heads(t, GDN_HEADS, GDN_HEAD) for t in jnp.split(qkv, 3, axis=-1))
    q = l2_normalize(q, 1e-6) * GDN_HEAD ** -0.5
    k = l2_normalize(k, 1e-6)
    z = proj[..., 3 * d:4 * d]
    a_pre = proj[..., 4 * d:4 * d + 2 * GDN_HEADS].astype(f32).reshape(b, n, 2, GDN_HEADS)
    b_pre = proj[..., 4 * d + 2 * GDN_HEADS:].astype(f32).reshape(b, n, 2, GDN_HEADS)
    g = -jnp.exp(a_log.astype(f32)) * jax.nn.softplus(a_pre + dt_bias.astype(f32))
    beta = jax.nn.sigmoid(b_pre)
    return q, k, v, z, g, beta


def gated_delta_chunked(q, k, v, g, beta, s0):
    b, n, nh, dk = k.shape
    c = GDN_CHUNK
    nc = n // c

    def chunks(t):
        t = jnp.moveaxis(t, 2, 1)
        return t.reshape(b, nh, nc, c, *t.shape[3:])

    q, k, v, g, beta = chunks(q), chunks(k), chunks(v), chunks(g), chunks(beta)
    gc = jnp.cumsum(g, axis=-1)
    causal = jnp.tril(jnp.ones((c, c), bool))
    strict = jnp.tril(jnp.ones((c, c), bool), -1)
    decay = jnp.exp(jnp.where(causal, gc[..., :, None] - gc[..., None, :], -jnp.inf))
    kb = k * beta[..., None]
    vb = v * beta[..., None]
    lower = jnp.where(strict, jnp.einsum('bhnid,bhnjd->bhnij', kb, k) * decay, 0.0)
    eye = jnp.eye(c, dtype=lower.dtype)
    tmat = lax.linalg.triangular_solve(eye + lower, jnp.broadcast_to(eye, lower.shape),
                                       left_side=True, lower=True)
    u = tmat @ vb
    w = tmat @ (kb * jnp.exp(gc)[..., None])
    a_qk = jnp.where(causal, jnp.einsum('bhnid,bhnjd->bhnij', q, k) * decay, 0.0)
    q_dec = q * jnp.exp(gc)[..., None]
    k_dec = k * jnp.exp(gc[..., -1:] - gc)[..., None]
    g_last = jnp.exp(gc[..., -1])

    def step(s, inp):
        u_i, w_i, q_i, k_i, a_i, gl_i = inp
        v_new = u_i - w_i @ s
        o_i = q_i @ s + a_i @ v_new
        s = s * gl_i[..., None, None] + jnp.swapaxes(k_i, -1, -2) @ v_new
        return s, o_i

    xs = tuple(jnp.moveaxis(t, 2, 0) for t in (u, w, q_dec, k_dec, a_qk, g_last))
    s_final, o = lax.scan(step, s0, xs)
    o = jnp.moveaxis(o, 0, 2).reshape(b, nh, n, -1)
    return jnp.moveaxis(o, 1, 2), s_final


def gdn_out(o, z, norm_w, w_o):
    b, n = z.shape[:2]
    on = o * lax.rsqrt(jnp.mean(o * o, axis=-1, keepdims=True) + GDN_NORM_EPS) * norm_w.astype(jnp.float32)
    zf = to_heads(z.astype(jnp.float32), GDN_HEADS, GDN_HEAD)
    y = (on * jax.nn.silu(zf)).reshape(b, n, D_MODEL).astype(z.dtype)
    return y @ w_o


def gdn_mixer(h, hc, w_in, conv_w, a_log, dt_bias, norm_w, w_o, want_ctx):
    lq, lk, lv, lz, lg, lb = gdn_features(h, w_in, conv_w, a_log, dt_bias)
    cq, ck, cv, cz, cg, cb = gdn_features(hc, w_in, conv_w, a_log, dt_bias)
    zero_state = jnp.zeros((h.shape[0], GDN_HEADS, GDN_HEAD, GDN_HEAD), jnp.float32)
    o_lat = 0.0
    o_ctx = 0.0
    for d in range(2):
        flip = flip_time if d == 1 else identity
        oc, s_ctx = gated_delta_chunked(flip(cq), flip(ck), flip(cv), flip(cg[:, :, d]), flip(cb[:, :, d]),
                                        zero_state)
        ol, _ = gated_delta_chunked(flip(lq), flip(lk), flip(lv), flip(lg[:, :, d]), flip(lb[:, :, d]), s_ctx)
        o_lat = o_lat + flip(ol)
        o_ctx = o_ctx + flip(oc)
    y_lat = gdn_out(o_lat, lz, norm_w, w_o)
    y_ctx = gdn_out(o_ctx, cz, norm_w, w_o) if want_ctx else None
    return y_lat, y_ctx


def swiglu(h, w1, w3, w2):
    return (jax.nn.silu(h @ w1) * (h @ w3)) @ w2


def moe_swiglu(h, router_w, router_b, w1, w3, w2):
    logits = (h @ router_w).astype(jnp.float32) + router_b.astype(jnp.float32)
    top_logit, top_idx = lax.top_k(logits, TOP_K)
    top_p = jax.nn.softmax(top_logit, axis=-1)
    gates = jnp.einsum('btk,btke->bte', top_p,
                       jax.nn.one_hot(top_idx, N_EXPERTS, dtype=jnp.float32)).astype(h.dtype)
    y = jnp.zeros_like(h)
    for e in range(N_EXPERTS):
        y = y + gates[..., e:e + 1] * swiglu(h, w1[e], w3[e], w2[e])
    return y


def setup_inputs(seed: int = 0) -> dict:
    key = jax.random.key(seed)
    ks = iter(jax.random.split(key, 64))
    f32 = jnp.float32

    def nrm(shape, std):
        return jax.random.normal(next(ks), shape, f32) * std

    def near_one(shape):
        return 1.0 + nrm(shape, 0.1)

    D, H, N = D_MODEL, RK_HEADS, RK_HEAD
    NA, NB, NC, ND, NM = N_RWKV_LAYERS, N_POOL_LAYERS, N_GDN_LAYERS, N_DENSE_LAYERS, N_MOE_LAYERS
    dt = jnp.exp(jax.random.uniform(next(ks), (NC, 2, GDN_HEADS), f32, math.log(1e-3), math.log(1e-1)))
    gdn_dt_bias = dt + jnp.log(-jnp.expm1(-dt))
    return {
        'x': nrm((BATCH, SEQ, D), 1.0),
        'c': nrm((BATCH, D), 1.0),
        'ctx': nrm((BATCH, CTX_LEN, D), 1.0),
        'c_ctx': nrm((D,), 1.0),
        'ada_w': nrm((DEPTH, D, 6 * D), D ** -0.5),
        'ada_b': nrm((DEPTH, 6 * D), 0.02),
        'ln_g': near_one((DEPTH, 2, D)),
        'ln_b': nrm((DEPTH, 2, D), 0.02),
        'rk_mu': jax.random.uniform(next(ks), (NA, 6, D), f32),
        'rk_w_rkv': nrm((NA, 3, D, D), D ** -0.5),
        'rk_w0': nrm((NA, 2, D), 1.0),
        'rk_w1': nrm((NA, 2, D, RK_DECAY_LORA), D ** -0.5),
        'rk_w2': nrm((NA, 2, RK_DECAY_LORA, D), 0.5 * RK_DECAY_LORA ** -0.5),
        'rk_a0': nrm((NA, 2, D), 0.5),
        'rk_a1': nrm((NA, 2, D, RK_ICLR_LORA), D ** -0.5),
        'rk_a2': nrm((NA, 2, RK_ICLR_LORA, D), 0.5 * RK_ICLR_LORA ** -0.5),
        'rk_g1': nrm((NA, D, RK_GATE_LORA), D ** -0.5),
        'rk_g2': nrm((NA, RK_GATE_LORA, D), RK_GATE_LORA ** -0.5),
        'rk_k_k': near_one((NA, D)),
        'rk_k_a': near_one((NA, D)),
        'rk_r_k': nrm((NA, H, N), 0.1),
        'rk_lnx_g': near_one((NA, D)),
        'rk_lnx_b': nrm((NA, D), 0.02),
        'rk_w_o': nrm((NA, D, D), DN_BETA * D ** -0.5),
        'pool_w': nrm((NB, len(POOL_WINDOWS), POOL_GROUP, POOL_GROUP), DN_BETA * POOL_GROUP ** -0.5),
        'pool_scale': near_one((NB, D)),
        'gdn_w_in': nrm((NC, D, GDN_IN), D ** -0.5),
        'gdn_conv_w': nrm((NC, GDN_CONV, 3 * D), GDN_CONV ** -0.5),
        'gdn_a_log': jnp.log(jax.random.uniform(next(ks), (NC, 2, GDN_HEADS), f32, 1.0, 16.0)),
        'gdn_dt_bias': gdn_dt_bias,
        'gdn_norm_w': near_one((NC, GDN_HEAD)),
        'gdn_w_o': nrm((NC, D, D), DN_BETA * D ** -0.5),
        'ffn_w1': nrm((ND, D, D_FF), D ** -0.5),
        'ffn_w3': nrm((ND, D, D_FF), D ** -0.5),
        'ffn_w2': nrm((ND, D_FF, D), DN_BETA * D_FF ** -0.5),
        'moe_router_w': nrm((NM, D, N_EXPERTS), D ** -0.5),
        'moe_router_b': nrm((NM, N_EXPERTS), 0.01),
        'moe_w1': nrm((NM, N_EXPERTS, D, D_FF_EXPERT), D ** -0.5),
        'moe_w3': nrm((NM, N_EXPERTS, D, D_FF_EXPERT), D ** -0.5),
        'moe_w2': nrm((NM, N_EXPERTS, D_FF_EXPERT, D), DN_BETA * D_FF_EXPERT ** -0.5),
    }


def reference(x, c, ctx, c_ctx, ada_w, ada_b, ln_g, ln_b,
              rk_mu, rk_w_rkv, rk_w0, rk_w1, rk_w2, rk_a0, rk_a1, rk_a2, rk_g1, rk_g2,
              rk_k_k, rk_k_a, rk_r_k, rk_lnx_g, rk_lnx_b, rk_w_o,
              pool_w, pool_scale,
              gdn_w_in, gdn_conv_w, gdn_a_log, gdn_dt_bias, gdn_norm_w, gdn_w_o,
              ffn_w1, ffn_w3, ffn_w2,
              moe_router_w, moe_router_b, moe_w1, moe_w3, moe_w2):
    b, n, d = x.shape
    n_rows = n // GRID_W
    for i in range(DEPTH):
        want_ctx = i < DEPTH - 1
        mod = jax.nn.silu(c) @ ada_w[i] + ada_b[i]
        mod_c = jax.nn.silu(c_ctx) @ ada_w[i] + ada_b[i]
        sh1, sc1, gt1, sh2, sc2, gt2 = jnp.split(mod[:, None, :], 6, axis=-1)
        csh1, csc1, cgt1, csh2, csc2, cgt2 = jnp.split(mod_c, 6, axis=-1)
        h = x * (1.0 + sc1) + sh1
        hc = ctx * (1.0 + csc1) + csh1
        kind, j = i % N_MIXERS, i // N_MIXERS
        if kind == 0:
            y, yc = rwkv7_mixer(h, hc, n_rows, rk_mu[j], rk_w_rkv[j], rk_w0[j], rk_w1[j], rk_w2[j],
                                rk_a0[j], rk_a1[j], rk_a2[j], rk_g1[j], rk_g2[j], rk_k_k[j], rk_k_a[j],
                                rk_r_k[j], rk_lnx_g[j], rk_lnx_b[j], rk_w_o[j], want_ctx)
        elif kind == 1:
            y = pool_mixer(h.reshape(b, n_rows, GRID_W, d), pool_w[j], pool_scale[j]).reshape(b, n, d)
            yc = pool_mixer(hc, pool_w[j], pool_scale[j]) if want_ctx else None
        else:
            y, yc = gdn_mixer(h, hc, gdn_w_in[j], gdn_conv_w[j], gdn_a_log[j], gdn_dt_bias[j],
                              gdn_norm_w[j], gdn_w_o[j], want_ctx)
        if i % 2 == 0:
            e = i // 2
            channel_mixer = lambda t, e=e: swiglu(t, ffn_w1[e], ffn_w3[e], ffn_w2[e])
        else:
            e = i // 2
            channel_mixer = lambda t, e=e: moe_swiglu(t, moe_router_w[e], moe_router_b[e],
                                                      moe_w1[e], moe_w3[e], moe_w2[e])
        x = layer_norm(DN_ALPHA * x + gt1 * y, ln_g[i, 0], ln_b[i, 0])
        x = layer_norm(DN_ALPHA * x + gt2 * channel_mixer(x * (1.0 + sc2) + sh2), ln_g[i, 1], ln_b[i, 1])
        if want_ctx:
            ctx = layer_norm(DN_ALPHA * ctx + cgt1 * yc, ln_g[i, 0], ln_b[i, 0])
            ctx = layer_norm(DN_ALPHA * ctx + cgt2 * channel_mixer(ctx * (1.0 + csc2) + csh2),
                             ln_g[i, 1], ln_b[i, 1])
    return x
```

```python
import contextlib
import numpy as np
import concourse.bass as bass
import concourse.mybir as mybir
from concourse.bass_utils import run_bass_kernel_spmd

F32 = mybir.dt.float32
BF16 = mybir.dt.bfloat16
AF = mybir.ActivationFunctionType
ALU = mybir.AluOpType
AX = mybir.AxisListType

D = 1024
NT = 8448
TC = 256
DEPTH = 4
ALPHA = (2 * DEPTH) ** 0.25
LN_EPS = 1e-5
NSLOT = 6


class KB:
    EPOCH = 30000
    NDMA = 24

    def __init__(self, nc):
        self.nc = nc
        self._ctx = []
        self.engs = {'pe': nc.tensor, 'dve': nc.vector, 'act': nc.scalar, 'pool': nc.gpsimd, 'sp': nc.sync}
        self.sems = []
        self.esem = {}
        self.ecnt = {}
        for e in ('pe', 'dve', 'act', 'pool'):
            self.esem[e] = self._newsem('c_' + e)
            self.ecnt[e] = 0
        self.dsem = [self._newsem('d%d' % i) for i in range(self.NDMA)]
        self.dcnt = [0] * self.NDMA
        self.dnext = 0
        self.waited = {e: {} for e in self.engs}
        self.res = {}
        self.nins = 0
        self.allsems = {}
        self.excl = set()

    def _newsem(self, name):
        cm = self.nc.semaphore(name + '_%d' % len(self.sems))
        h = cm.__enter__()
        self._ctx.append(cm)
        self.sems.append(h)
        return len(self.sems) - 1

    def _r(self, key):
        r = self.res.get(key)
        if r is None:
            r = {'w': None, 'r': {}}
            self.res[key] = r
        return r

    def _waits(self, eng, reads, writes):
        need = {}

        def add(tok):
            if tok is None:
                return
            s, v = tok
            if need.get(s, 0) < v:
                need[s] = v
        for k in reads:
            add(self._r(k)['w'])
        for k in writes:
            r = self._r(k)
            add(r['w'])
            for s, v in r['r'].items():
                add((s, v))
        wd = self.waited[eng]
        for s, v in need.items():
            if wd.get(s, 0) >= v:
                continue
            self.engs[eng].wait_ge(self.sems[s], v)
            self.nins += 1
            wd[s] = v

    def _mark(self, tok, reads, writes):
        s, v = tok
        self.allsems[s] = v
        for k in writes:
            r = self._r(k)
            r['w'] = tok
            r['r'] = {}
        for k in reads:
            if k in writes:
                continue
            r = self._r(k)
            if r['r'].get(s, 0) < v:
                r['r'][s] = v

    def op(self, eng, fn, reads=(), writes=()):
        ex = [k for k in reads if k in self.excl and k not in writes]
        if ex:
            writes = list(writes) + ex
        self._waits(eng, reads, writes)
        ins = fn()
        if self.ecnt[eng] >= self.EPOCH:
            self.esem[eng] = self._newsem('c_' + eng)
            self.ecnt[eng] = 0
        s = self.esem[eng]
        ins.then_inc(self.sems[s], 1)
        self.ecnt[eng] += 1
        self.nins += 1
        tok = (s, self.ecnt[eng])
        self._mark(tok, reads, writes)
        return tok

    def dma(self, q, out, in_, reads=(), writes=(), **kw):
        j = self.dnext
        self.dnext = (self.dnext + 1) % self.NDMA
        wd = self.waited[q]
        if self.dcnt[j] > 0 and wd.get(self.dsem[j], 0) < 16 * self.dcnt[j]:
            self.engs[q].wait_ge(self.sems[self.dsem[j]], 16 * self.dcnt[j])
            wd[self.dsem[j]] = 16 * self.dcnt[j]
        self._waits(q, reads, writes)
        self.engs[q].dma_start(out=out, in_=in_, **kw).then_inc(self.sems[self.dsem[j]], 16)
        self.dcnt[j] += 1
        self.nins += 1
        tok = (self.dsem[j], 16 * self.dcnt[j])
        self._mark(tok, reads, writes)
        return tok

    def barrier(self):
        for e in self.engs:
            wd = self.waited[e]
            for s, v in self.allsems.items():
                if wd.get(s, 0) >= v:
                    continue
                self.engs[e].wait_ge(self.sems[s], v)
                self.nins += 1
                wd[s] = v
        self.res = {}


class Stage:
    _n = [0]

    def __init__(self, kb):
        self.kb = kb
        self.nc = kb.nc
        self.es = contextlib.ExitStack()
        Stage._n[0] += 1
        self.sid = Stage._n[0]

    def __enter__(self):
        self.es.__enter__()
        return self

    def sb(self, name, shape, dt):
        return self.es.enter_context(self.nc.sbuf_tensor('%s_s%d' % (name, self.sid), shape, dt))

    def ps(self, name, shape, dt):
        self.kb.excl.add(name)
        return self.es.enter_context(self.nc.psum_tensor('%s_s%d' % (name, self.sid), shape, dt))

    def __exit__(self, *a):
        self.kb.barrier()
        return self.es.__exit__(*a)


def tiles_of(total, w, start=0):
    out = []
    t = start
    while t < total:
        out.append((t, min(w, total - t)))
        t += w
    return out


class Prog:
    def __init__(self, layers, debug_outs=()):
        self.layers = layers
        nc = bass.Bass("TRN2", target_bir_lowering=False)
        self.nc = nc
        self.kb = KB(nc)
        self.inp = {}
        self.debug_outs = debug_outs

    def din(self, name, shape, dt=F32):
        t = self.nc.dram_tensor(name, list(shape), dt, kind="ExternalInput").ap()
        self.inp[name] = t
        return t

    def dscr(self, name, shape, dt=F32):
        return self.nc.dram_tensor(name, list(shape), dt, kind="Internal").ap()

    def dout(self, name, shape, dt=F32):
        return self.nc.dram_tensor(name, list(shape), dt, kind="ExternalOutput").ap()

    def bcast_load(self, st, name, row_ap, n, dt=F32):
        t = st.sb(name, [128, n], dt)
        self.kb.dma('sp', t[:], row_ap.partition_broadcast(128), writes=[name])
        return t

    def stage_mod(self, i):
        nc, kb = self.nc, self.kb
        with Stage(kb) as st:
            cT = st.sb("cT", [128, 8, 2], F32)
            sT = st.sb("sT", [128, 8, 2], F32)
            modsb = st.sb("modsb", [2, 6144], F32)
            adab = st.sb("adab", [2, 6144], F32)
            wst = [st.sb("wst%d" % j, [128, 8, 512], F32) for j in range(2)]
            pm_ = [st.ps("pm%d" % j, [128, 512], F32) for j in range(2)]
            pm = [t[0:2, :] for t in pm_]
            for r in range(2):
                kb.dma('sp', cT[:, :, r], self.cvec[r, :].rearrange("(k p) -> p k", p=128),
                       reads=['cT'] if r else [], writes=['cT'], allow_slow_non_contiguous=True)
                kb.dma('sp', adab[r:r + 1, :], self.ada_b[i:i + 1, :], reads=['adab'] if r else [], writes=['adab'])
            kb.op('act', lambda: nc.scalar.activation(out=sT[:], in_=cT[:], func=AF.Silu), reads=['cT'], writes=['sT'])
            for g in range(12):
                j = g % 2
                kb.dma('sp', wst[j][:], self.ada_w[i, :, g * 512:(g + 1) * 512].rearrange("(k p) n -> p k n", p=128),
                       writes=['wst%d' % j])

                def mm(j=j):
                    for k in range(8):
                        ins = nc.tensor.matmul(pm[j][:], lhsT=sT[:, k, :], rhs=wst[j][:, k, :], start=(k == 0), stop=(k == 7))
                    return ins
                kb.op('pe', mm, reads=['sT', 'wst%d' % j], writes=['pm%d' % j])
                kb.op('dve', lambda j=j, g=g: nc.vector.tensor_tensor(out=modsb[:, g * 512:(g + 1) * 512], in0=pm[j][:],
                                                                     in1=adab[:, g * 512:(g + 1) * 512], op=ALU.add),
                      reads=['pm%d' % j, 'adab', 'modsb'], writes=['modsb'])
            for c0 in (1024, 4096):
                kb.op('dve', lambda c0=c0: nc.vector.tensor_scalar(out=modsb[:, c0:c0 + 1024], in0=modsb[:, c0:c0 + 1024],
                                                                 scalar1=1.0, scalar2=None, op0=ALU.add),
                      reads=['modsb'], writes=['modsb'])
            kb.dma('sp', self.MODD[i], modsb[:], reads=['modsb'], writes=['MODD'])

    def mod_tiles(self, st, i, slots):
        out = {}
        for s in slots:
            for r in range(2):
                out[(s, r)] = self.bcast_load(st, "mod%d_%d" % (s, r), self.MODD[i, r, s * 1024:(s + 1) * 1024], 1024)
        return out

    def stage_prep(self, i, sub, router=None):
        nc, kb = self.nc, self.kb
        with Stage(kb) as st:
            sh_s, sc_s = (0, 1) if sub == 1 else (3, 4)
            md = self.mod_tiles(st, i, [sh_s, sc_s])
            identf = st.sb("identf", [128, 128], F32)
            kb.dma('sp', identf[:], self.cst_identf, writes=['identf'])
            NB = 2
            xt = [st.sb("xt%d" % j, [128, 1024], F32) for j in range(NB)]
            hf = [st.sb("hf%d" % j, [128, 1024], F32) for j in range(NB)]
            hTb = [st.sb("hTb%d" % j, [128, 8, 128], BF16) for j in range(NB)]
            pT = [st.ps("pT%d" % j, [128, 8, 128], F32) for j in range(NB)]
            if router is not None:
                e = router
                rw = st.sb("rw", [128, 8, 8], F32)
                kb.dma('sp', rw[:], self.moe_router_w[e].rearrange("(k p) n -> p k n", p=128), writes=['rw'])
                rb = self.bcast_load(st, "rb", self.moe_router_b[e, :], 8)
                hTf = [st.sb("hTf%d" % j, [128, 8, 128], F32) for j in range(NB)]
                pl_ = [st.ps("pl%d" % j, [128, 512], F32) for j in range(NB)]
                pl = [t[:, 0:8] for t in pl_]
                lg = [st.sb("lg%d" % j, [128, 8], F32) for j in range(NB)]
                m1 = [st.sb("m1_%d" % j, [128, 8], F32) for j in range(NB)]
                m2 = [st.sb("m2_%d" % j, [128, 8], F32) for j in range(NB)]
                l2 = [st.sb("l2_%d" % j, [128, 8], F32) for j in range(NB)]
                mx = [st.sb("mx%d" % j, [128, 4], F32) for j in range(NB)]
                gt = [st.sb("gt%d" % j, [128, 8], F32) for j in range(NB)]
            for ti in range(NT // 128):
                j = ti % NB
                isc = 1 if ti < TC // 128 else 0
                t0 = ti * 128
                X, H, HB, PT = 'xt%d' % j, 'hf%d' % j, 'hTb%d' % j, 'pT%d' % j
                kb.dma('sp', xt[j][:], self.XS[t0:t0 + 128, :], writes=[X])
                kb.op('dve', lambda j=j, isc=isc: nc.vector.tensor_tensor(out=hf[j][:], in0=xt[j][:], in1=md[(sc_s, isc)][:], op=ALU.mult),
                      reads=[X, 'mod%d_%d' % (sc_s, isc)], writes=[H])
                kb.op('dve', lambda j=j, isc=isc: nc.vector.tensor_tensor(out=hf[j][:], in0=hf[j][:], in1=md[(sh_s, isc)][:], op=ALU.add),
                      reads=[H, 'mod%d_%d' % (sh_s, isc)], writes=[H])

                def tr(j=j):
                    for k in range(8):
                        ins = nc.tensor.transpose(pT[j][:, k, :], hf[j][:, k * 128:(k + 1) * 128], identf[:])
                    return ins
                kb.op('pe', tr, reads=[H, 'identf'], writes=[PT])
                if router is None:
                    kb.op('act', lambda j=j: nc.scalar.copy(out=hTb[j][:], in_=pT[j][:]), reads=[PT], writes=[HB])
                else:
                    kb.op('dve', lambda j=j: nc.vector.tensor_copy(out=hTf[j][:], in_=pT[j][:]), reads=[PT], writes=['hTf%d' % j])
                    kb.op('act', lambda j=j: nc.scalar.copy(out=hTb[j][:], in_=hTf[j][:]), reads=['hTf%d' % j], writes=[HB])
                kb.dma('sp', self.HT[:, :, t0:t0 + 128], hTb[j][:], reads=[HB], writes=['HT%d' % ti])
                import os
                RD = int(os.environ.get('RD', '9'))
                if router is not None and RD >= 1:
                    HF, PL, LG = 'hTf%d' % j, 'pl%d' % j, 'lg%d' % j

                    def rmm(j=j):
                        for k in range(8):
                            ins = nc.tensor.matmul(pl[j][:], lhsT=hTf[j][:, k, :], rhs=rw[:, k, :], start=(k == 0), stop=(k == 7))
                        return ins
                    kb.op('pe', rmm, reads=[HF, 'rw'], writes=[PL])
                    G = 'g%d' % j
                    if RD < 2:
                        continue
                    kb.op('dve', lambda j=j: nc.vector.tensor_tensor(out=lg[j][:], in0=pl[j][:], in1=rb[:], op=ALU.add), reads=[PL, 'rb', G], writes=[G])
                    if RD < 3:
                        kb.dma('sp', self.GATES[t0:t0 + 128, :], lg[j][:], reads=[G], writes=['GATES%d' % ti])
                        continue
                    kb.op('dve', lambda j=j: nc.vector.tensor_reduce(out=mx[j][:, 0:1], in_=lg[j][:], axis=AX.X, op=ALU.max), reads=[G], writes=[G])
                    kb.op('dve', lambda j=j: nc.vector.tensor_scalar(out=m1[j][:], in0=lg[j][:], scalar1=mx[j][:, 0:1], scalar2=None, op0=ALU.is_equal), reads=[G], writes=[G])
                    kb.op('dve', lambda j=j: nc.vector.scalar_tensor_tensor(out=l2[j][:], in0=m1[j][:], scalar=-1e30, in1=lg[j][:], op0=ALU.mult, op1=ALU.add), reads=[G], writes=[G])
                    kb.op('dve', lambda j=j: nc.vector.tensor_reduce(out=mx[j][:, 1:2], in_=l2[j][:], axis=AX.X, op=ALU.max), reads=[G], writes=[G])
                    kb.op('dve', lambda j=j: nc.vector.tensor_scalar(out=m2[j][:], in0=l2[j][:], scalar1=mx[j][:, 1:2], scalar2=None, op0=ALU.is_equal), reads=[G], writes=[G])
                    kb.op('dve', lambda j=j: nc.vector.tensor_tensor(out=mx[j][:, 2:3], in0=mx[j][:, 0:1], in1=mx[j][:, 1:2], op=ALU.subtract), reads=[G], writes=[G])
                    kb.op('act', lambda j=j: nc.scalar.activation(out=mx[j][:, 2:3], in_=mx[j][:, 2:3], func=AF.Sigmoid), reads=[G], writes=[G])
                    kb.op('dve', lambda j=j: nc.vector.tensor_scalar(out=mx[j][:, 3:4], in0=mx[j][:, 2:3], scalar1=-1.0, scalar2=1.0, op0=ALU.mult, op1=ALU.add), reads=[G], writes=[G])
                    kb.op('dve', lambda j=j: nc.vector.tensor_scalar(out=gt[j][:], in0=m1[j][:], scalar1=mx[j][:, 2:3], scalar2=None, op0=ALU.mult), reads=[G], writes=[G])
                    kb.op('dve', lambda j=j: nc.vector.scalar_tensor_tensor(out=gt[j][:], in0=m2[j][:], scalar=mx[j][:, 3:4], in1=gt[j][:], op0=ALU.mult, op1=ALU.add), reads=[G], writes=[G])
                    kb.dma('sp', self.GATES[t0:t0 + 128, :], gt[j][:], reads=[G], writes=['GATES%d' % ti])

    def stage_ffnpass(self, w1, w3, w2, gate_col, first):
        nc, kb = self.nc, self.kb
        with Stage(kb) as st:
            w1b = st.sb("w1b", [128, 8, 1408], BF16)
            w3b = st.sb("w3b", [128, 8, 1408], BF16)
            w2b = st.sb("w2b", [128, 11, 1024], BF16)
            stg = [st.sb("stg%d" % j, [128, 1408], F32) for j in range(3)]
            n = 0
            for (dst, src, nk, key) in ((w1b, w1, 8, 'w1b'), (w3b, w3, 8, 'w3b'), (w2b, w2, 11, 'w2b')):
                width = src.shape[1]
                for k in range(nk):
                    j = n % 3
                    n += 1
                    kb.dma('sp', stg[j][:, 0:width], src[k * 128:(k + 1) * 128, :], writes=['stg%d' % j])
                    if n % 2:
                        kb.op('act', lambda dst=dst, k=k, j=j, width=width: nc.scalar.copy(out=dst[:, k, :], in_=stg[j][:, 0:width]),
                              reads=['stg%d' % j, key], writes=[key])
                    else:
                        kb.op('dve', lambda dst=dst, k=k, j=j, width=width: nc.vector.tensor_copy(out=dst[:, k, :], in_=stg[j][:, 0:width]),
                              reads=['stg%d' % j, key], writes=[key])
            hT = [st.sb("hT%d" % j, [128, 8, 512], BF16) for j in range(2)]
            act = [st.sb("act%d" % j, [128, 11, 512], BF16) for j in range(2)]
            sg = [st.sb("sg%d" % j, [128, 512], F32) for j in range(2)]
            pA = [st.ps("pA%d" % j, [128, 512], F32) for j in range(2)]
            pB = [st.ps("pB%d" % j, [128, 512], F32) for j in range(2)]
            pY = [st.ps("pY%d" % j, [128, 512], F32) for j in range(2)]
            yo = [st.sb("yo%d" % j, [128, 1024], F32) for j in range(2)]
            ya = [st.sb("ya%d" % j, [128, 1024], F32) for j in range(2)]
            gtl = [st.sb("gtl%d" % j, [128, 8], F32) for j in range(2)]
            nf = 0
            ny = 0
            for it, (t0, W) in enumerate(tiles_of(NT, 512)):
                j = it % 2
                kb.dma('sp', hT[j][:, :, 0:W], self.HT[:, :, t0:t0 + W], reads=['HT%d' % q for q in range(t0 // 128, (t0 + W) // 128)],
                       writes=['hT%d' % j])
                for f in range(11):
                    jf = nf % 2
                    nf += 1

                    def mmA(jf=jf, f=f, j=j, W=W, wb=w1b, pp=pA):
                        for k in range(8):
                            ins = nc.tensor.matmul(pp[jf][:, 0:W], lhsT=wb[:, k, f * 128:(f + 1) * 128], rhs=hT[j][:, k, 0:W],
                                                   start=(k == 0), stop=(k == 7))
                        return ins
                    kb.op('pe', mmA, reads=['hT%d' % j, 'w1b'], writes=['pA%d' % jf])
                    kb.op('pe', lambda jf=jf, f=f, j=j, W=W: mmA(jf, f, j, W, w3b, pB), reads=['hT%d' % j, 'w3b'], writes=['pB%d' % jf])
                    kb.op('act', lambda jf=jf, W=W: nc.scalar.activation(out=sg[jf][:, 0:W], in_=pA[jf][:, 0:W], func=AF.Silu),
                          reads=['pA%d' % jf], writes=['sg%d' % jf])
                    kb.op('dve', lambda jf=jf, f=f, j=j, W=W: nc.vector.tensor_tensor(out=act[j][:, f, 0:W], in0=sg[jf][:, 0:W], in1=pB[jf][:, 0:W], op=ALU.mult),
                          reads=['sg%d' % jf, 'pB%d' % jf, 'act%d' % j], writes=['act%d' % j])
                for sub in range(W // 128):
                    jy = ny % 2
                    ny += 1
                    ti = t0 // 128 + sub
                    ts = t0 + sub * 128
                    if not first:
                        kb.dma('sp', ya[jy][:], self.ACC[ts:ts + 128, :], reads=['ACC%d' % ti], writes=['ya%d' % jy])
                    if gate_col is not None:
                        kb.dma('sp', gtl[jy][:], self.GATES[ts:ts + 128, :], reads=['GATES%d' % ti], writes=['gtl%d' % jy])
                    for half in range(2):
                        jp = half

                        def mmY(jp=jp, half=half, sub=sub, j=j):
                            for f in range(11):
                                ins = nc.tensor.matmul(pY[jp][:], lhsT=act[j][:, f, sub * 128:(sub + 1) * 128],
                                                       rhs=w2b[:, f, half * 512:(half + 1) * 512], start=(f == 0), stop=(f == 10))
                            return ins
                        kb.op('pe', mmY, reads=['act%d' % j, 'w2b'], writes=['pY%d' % jp])
                        osl = slice(half * 512, (half + 1) * 512)
                        rd = ['pY%d' % jp, 'yo%d' % jy]
                        if gate_col is not None and not first:
                            kb.op('dve', lambda jp=jp, jy=jy, osl=osl: nc.vector.scalar_tensor_tensor(
                                out=yo[jy][:, osl], in0=pY[jp][:], scalar=gtl[jy][:, gate_col:gate_col + 1], in1=ya[jy][:, osl],
                                op0=ALU.mult, op1=ALU.add), reads=rd + ['gtl%d' % jy, 'ya%d' % jy], writes=['yo%d' % jy])
                        elif gate_col is not None:
                            kb.op('dve', lambda jp=jp, jy=jy, osl=osl: nc.vector.tensor_scalar(
                                out=yo[jy][:, osl], in0=pY[jp][:], scalar1=gtl[jy][:, gate_col:gate_col + 1], scalar2=None, op0=ALU.mult),
                                reads=rd + ['gtl%d' % jy], writes=['yo%d' % jy])
                        elif not first:
                            kb.op('dve', lambda jp=jp, jy=jy, osl=osl: nc.vector.tensor_tensor(
                                out=yo[jy][:, osl], in0=pY[jp][:], in1=ya[jy][:, osl], op=ALU.add),
                                reads=rd + ['ya%d' % jy], writes=['yo%d' % jy])
                        else:
                            kb.op('act', lambda jp=jp, jy=jy, osl=osl: nc.scalar.copy(out=yo[jy][:, osl], in_=pY[jp][:]),
                                  reads=rd, writes=['yo%d' % jy])
                    kb.dma('sp', self.ACC[ts:ts + 128, :], yo[jy][:], reads=['yo%d' % jy], writes=['ACC%d' % ti])

    def stage_finish(self, i, sub, out_final=None):
        nc, kb = self.nc, self.kb
        with Stage(kb) as st:
            gs = 2 if sub == 1 else 5
            md = self.mod_tiles(st, i, [gs])
            lng = self.bcast_load(st, "lng", self.ln_g[i, sub - 1, :], 1024)
            lnb = self.bcast_load(st, "lnb", self.ln_b[i, sub - 1, :], 1024)
            NB = 2
            xt = [st.sb("xt%d" % j, [128, 1024], F32) for j in range(NB)]
            at = [st.sb("at%d" % j, [128, 1024], F32) for j in range(NB)]
            stt = [st.sb("stt%d" % j, [128, 2, 6], F32) for j in range(NB)]
            mv = [st.sb("mv%d" % j, [128, 4], F32) for j in range(NB)]
            for ti in range(NT // 128):
                j = ti % NB
                isc = 1 if ti < TC // 128 else 0
                t0 = ti * 128
                X, A, S = 'xt%d' % j, 'at%d' % j, 'st%d' % j
                kb.dma('sp', xt[j][:], self.XS[t0:t0 + 128, :], writes=[X])
                kb.dma('sp', at[j][:], self.ACC[t0:t0 + 128, :], reads=['ACC%d' % ti], writes=[A])
                kb.op('dve', lambda j=j, isc=isc: nc.vector.tensor_tensor(out=at[j][:], in0=at[j][:], in1=md[(gs, isc)][:], op=ALU.mult),
                      reads=[A, 'mod%d_%d' % (gs, isc)], writes=[A])
                kb.op('dve', lambda j=j: nc.vector.scalar_tensor_tensor(out=xt[j][:], in0=xt[j][:], scalar=float(ALPHA), in1=at[j][:], op0=ALU.mult, op1=ALU.add),
                      reads=[X, A], writes=[X])

                def bn(j=j):
                    nc.vector.bn_stats(out=stt[j][:, 0, :], in_=xt[j][:, 0:512])
                    return nc.vector.bn_stats(out=stt[j][:, 1, :], in_=xt[j][:, 512:1024])
                kb.op('dve', bn, reads=[X, S], writes=[S])
                kb.op('dve', lambda j=j: nc.vector.bn_aggr(out=mv[j][:, 0:2], in_=stt[j][:]), reads=[S], writes=[S])
                kb.op('dve', lambda j=j: nc.vector.tensor_scalar(out=mv[j][:, 2:3], in0=mv[j][:, 1:2], scalar1=LN_EPS, scalar2=None, op0=ALU.add), reads=[S], writes=[S])
                kb.op('act', lambda j=j: nc.scalar.activation(out=mv[j][:, 2:3], in_=mv[j][:, 2:3], func=AF.Sqrt), reads=[S], writes=[S])
                kb.op('dve', lambda j=j: nc.vector.reciprocal(out=mv[j][:, 3:4], in_=mv[j][:, 2:3]), reads=[S], writes=[S])
                kb.op('dve', lambda j=j: nc.vector.tensor_scalar(out=xt[j][:], in0=xt[j][:], scalar1=mv[j][:, 0:1], scalar2=mv[j][:, 3:4],
                                                             op0=ALU.subtract, op1=ALU.mult), reads=[X, S], writes=[X])
                kb.op('dve', lambda j=j: nc.vector.tensor_tensor(out=xt[j][:], in0=xt[j][:], in1=lng[:], op=ALU.mult), reads=[X, 'lng'], writes=[X])
                kb.op('dve', lambda j=j: nc.vector.tensor_tensor(out=xt[j][:], in0=xt[j][:], in1=lnb[:], op=ALU.add), reads=[X, 'lnb'], writes=[X])
                kb.dma('sp', self.XS[t0:t0 + 128, :], xt[j][:], reads=[X], writes=['XS%d' % ti])
                if out_final is not None and ti >= TC // 128:
                    kb.dma('sp', out_final[t0 - TC:t0 - TC + 128, :], xt[j][:], reads=[X], writes=['OUT%d' % ti])

    def stage_pool(self, j_layer):
        nc, kb = self.nc, self.kb
        with Stage(kb) as st:
            pw = st.sb("pw", [128, 4, 2, 256], BF16)
            pst = [st.sb("pst%d" % j, [128, 256], F32) for j in range(2)]
            n = 0
            for gi in range(4):
                for kk in range(2):
                    j = n % 2
                    n += 1
                    kb.dma('sp', pst[j][:], self.pool_w[j_layer, gi, kk * 128:(kk + 1) * 128, :], writes=['pst%d' % j])
                    kb.op('dve', lambda gi=gi, kk=kk, j=j: nc.vector.tensor_copy(out=pw[:, gi, kk, :], in_=pst[j][:]), reads=['pst%d' % j, 'pw'], writes=['pw'])
            psc = self.bcast_load(st, "psc", self.pool_scale[j_layer, :], 1024)
            invc = st.sb("invc", [128, 4, 64], F32)
            kb.dma('sp', invc[:], self.cst_invc64, writes=['invc'])
            invcc = st.sb("invcc", [128, 4, 256], F32)
            kb.dma('sp', invcc[:], self.cst_invc256, writes=['invcc'])
            hT = [st.sb("hT%d" % j, [128, 8, 512], BF16) for j in range(2)]
            P = st.sb("P", [128, 8, 8, 80], F32)
            S2 = st.sb("S2", [128, 8, 8, 80], F32)
            S4 = st.sb("S4", [128, 8, 8, 80], F32)
            S8 = st.sb("S8", [128, 8, 8, 80], F32)
            S16 = st.sb("S16", [128, 8, 8, 80], F32)
            pl = [st.sb("pl%d" % j, [128, 8, 512], BF16) for j in range(2)]
            tmp = st.sb("tmp", [128, 2, 8, 64], F32)
            pY = [st.ps("pY%d" % j, [128, 1024], F32) for j in range(2)]
            yo = [st.sb("yo%d" % j, [128, 1024], F32) for j in range(2)]
            for b_ in (P, S2, S4, S8, S16):
                pass
            kb.op('pool', lambda: nc.gpsimd.memset(P[:], 0.0), writes=['P'])
            ny = 0
            for it, (t0, W) in enumerate([(0, 256)] + tiles_of(NT, 512, 256)):
                j = it % 2
                isc = (t0 == 0)
                kb.dma('sp', hT[j][:, :, 0:W], self.HT[:, :, t0:t0 + W], reads=['HT%d' % q for q in range(t0 // 128, (t0 + W) // 128)],
                       writes=['hT%d' % j])
                if isc:
                    Pv = P[:].rearrange("p k r c -> p k (r c)")[:, :, 0:272].rearrange("p k (r c) -> p k r c", r=1)
                    views = [b_[:].rearrange("p k r c -> p k (r c)")[:, :, 0:272].rearrange("p k (r c) -> p k r c", r=1) for b_ in (P, S2, S4, S8, S16)]
                    L = 256
                    R = 1
                else:
                    if it == 1:
                        kb.op('pool', lambda: nc.gpsimd.memset(P[:], 0.0), reads=['P'], writes=['P'])
                    views = [b_[:] for b_ in (P, S2, S4, S8, S16)]
                    L = 64
                    R = 8
                Pv, S2v, S4v, S8v, S16v = views
                LP = L + 16
                kb.op('act', lambda Pv=Pv, j=j, L=L, R=R, W=W: nc.scalar.copy(out=Pv[:, :, :, 8:8 + L], in_=hT[j][:, :, 0:W].rearrange("p k (r c) -> p k r c", r=R)),
                      reads=['hT%d' % j, 'P'], writes=['P'])
                kb.op('dve', lambda: nc.vector.tensor_tensor(out=S2v[:, :, :, 0:LP - 1], in0=Pv[:, :, :, 0:LP - 1], in1=Pv[:, :, :, 1:LP], op=ALU.add), reads=['P', 'S2'], writes=['S2'])
                kb.op('dve', lambda: nc.vector.tensor_tensor(out=S4v[:, :, :, 0:LP - 3], in0=S2v[:, :, :, 0:LP - 3], in1=S2v[:, :, :, 2:LP - 1], op=ALU.add), reads=['S2', 'S4'], writes=['S4'])
                kb.op('dve', lambda: nc.vector.tensor_tensor(out=S8v[:, :, :, 0:LP - 7], in0=S4v[:, :, :, 0:LP - 7], in1=S4v[:, :, :, 4:LP - 3], op=ALU.add), reads=['S4', 'S8'], writes=['S8'])
                kb.op('dve', lambda: nc.vector.tensor_tensor(out=S16v[:, :, :, 0:LP - 15], in0=S8v[:, :, :, 0:LP - 15], in1=S8v[:, :, :, 8:LP - 7], op=ALU.add), reads=['S8', 'S16'], writes=['S16'])
                for gi, (win, Sv, key) in enumerate(((2, S2v, 'S2'), (4, S4v, 'S4'), (8, S8v, 'S8'), (16, S16v, 'S16'))):
                    o = 8 - win // 2
                    ic = (invcc if isc else invc)
                    icv = ic[:, gi, :].unsqueeze(1).unsqueeze(1).to_broadcast([128, 2, R, L])
                    tv = tmp[:].rearrange("p k r c -> p k (r c)")[:, :, 0:W].rearrange("p k (r c) -> p k r c", r=R)
                    kb.op('dve', lambda Sv=Sv, gi=gi, o=o, icv=icv, tv=tv, L=L: nc.vector.tensor_tensor(out=tv, in0=Sv[:, 2 * gi:2 * gi + 2, :, o:o + L], in1=icv, op=ALU.mult),
                          reads=[key, 'invc', 'invcc', 'tmp'], writes=['tmp'])
                    kb.op('dve', lambda gi=gi, tv=tv, j=j, Pv=Pv, L=L, R=R, W=W: nc.vector.tensor_tensor(
                        out=pl[j][:, 2 * gi:2 * gi + 2, 0:W].rearrange("p k (r c) -> p k r c", r=R), in0=tv, in1=Pv[:, 2 * gi:2 * gi + 2, :, 8:8 + L], op=ALU.subtract),
                        reads=['tmp', 'P', 'pl%d' % j], writes=['pl%d' % j])
                for sub in range(W // 128):
                    jy = ny % 2
                    ny += 1
                    ti = t0 // 128 + sub
                    ts = t0 + sub * 128

                    def mm(jy=jy, sub=sub, j=j):
                        for gi in range(4):
                            for kk in range(2):
                                ins = nc.tensor.matmul(pY[jy][:, gi * 256:(gi + 1) * 256], lhsT=pl[j][:, 2 * gi + kk, sub * 128:(sub + 1) * 128],
                                                       rhs=pw[:, gi, kk, :], start=(kk == 0), stop=(kk == 1))
                        return ins
                    kb.op('pe', mm, reads=['pl%d' % j, 'pw'], writes=['pY%d' % jy])
                    kb.op('dve', lambda jy=jy: nc.vector.tensor_tensor(out=yo[jy][:], in0=pY[jy][:], in1=psc[:], op=ALU.mult), reads=['pY%d' % jy, 'psc', 'yo%d' % jy], writes=['yo%d' % jy])
                    kb.dma('sp', self.ACC[ts:ts + 128, :], yo[jy][:], reads=['yo%d' % jy], writes=['ACC%d' % ti])

    def scan_consts(self, st, z):
        kb = self.kb
        c = {}
        for nm in ('SB', 'SBI', 'SBT', 'BLK'):
            t = st.sb(nm, [128, 128], F32)
            kb.dma('sp', t[:], self.cst_masks[z, {'SB': 0, 'SBI': 1, 'SBT': 2, 'BLK': 3}[nm]], writes=[nm])
            c[nm] = t
        idf = st.sb("identf", [128, 128], F32)
        kb.dma('sp', idf[:], self.cst_identf, writes=['identf'])
        idb = st.sb("identb", [128, 128], BF16)
        kb.op('dve', lambda: self.nc.vector.tensor_copy(out=idb[:], in_=idf[:]), reads=['identf'], writes=['identb'])
        c['identf'] = idf
        c['identb'] = idb
        return c

    def head_scan(self, st, C, hid, dk, dv, pb, KAP, RM, BCt, KCt, V, KAPT, BPT, KPT, RMT, Dx, Di, DxT, wc, T, yout, opk, z, bufs, slot=0):
        nc, kb = self.nc, self.kb
        B = bufs
        par = slot
        ps = B['ps']

        def nps():
            return ps[slot], 'hps%d' % slot
        sfx = '_%d' % par

        def S(name):
            return B[name][par], name + sfx
        idf, idb = C['identf'], C['identb']

        def mm_ev(name, lhsT, rhs, mul=None, mulkey=None, eng='dve', rows=128, cols=128, extra=None, reads=()):
            p, pk = nps()
            dst, dk_ = S(name)

            def f():
                ins = nc.tensor.matmul(p[0:rows, 0:cols], lhsT=lhsT, rhs=rhs, start=True, stop=(extra is None))
                if extra is not None:
                    for qi, (l2, r2) in enumerate(extra):
                        ins = nc.tensor.matmul(p[0:rows, 0:cols], lhsT=l2, rhs=r2, start=False, stop=(qi == len(extra) - 1))
                return ins
            kb.op('pe', f, reads=list(reads), writes=[pk])
            if mul is not None:
                kb.op('dve', lambda: nc.vector.tensor_tensor(out=dst[0:rows, 0:cols], in0=p[0:rows, 0:cols], in1=mul, op=ALU.mult),
                      reads=[pk, mulkey, dk_], writes=[dk_])
            elif eng == 'act':
                kb.op('act', lambda: nc.scalar.copy(out=dst[0:rows, 0:cols], in_=p[0:rows, 0:cols]), reads=[pk, dk_], writes=[dk_])
            else:
                kb.op('dve', lambda: nc.vector.tensor_copy(out=dst[0:rows, 0:cols], in_=p[0:rows, 0:cols]), reads=[pk, dk_], writes=[dk_])
            return dst, dk_
        OK = list(opk)
        N_, Nk = mm_ev('N', BPT, KAPT, mul=Dx[0], mulkey=Dx[1], reads=OK)
        yield
        NT_, NTk = mm_ev('NT', KAPT, BPT, mul=DxT[0], mulkey=DxT[1], reads=OK)
        yield
        BtT, BtTk = mm_ev('BtT', KPT, KAPT, mul=Dx[0], mulkey=Dx[1], reads=OK)
        yield
        AbT, AbTk = mm_ev('AbT', BPT, RMT, mul=Di[0], mulkey=Di[1], reads=OK)
        yield
        AkT, AkTk = mm_ev('AkT', KPT, RMT, mul=Di[0], mulkey=Di[1], reads=OK)
        yield
        R_, Rk = S('R')
        kb.op('dve', lambda: nc.vector.tensor_tensor(out=R_[:], in0=N_[:], in1=idf[:], op=ALU.add), reads=[Nk, 'identf', Rk], writes=[Rk])
        yield
        P, Pk, PT, PTk = N_, Nk, NT_, NTk
        for it in range(5):
            P2, P2k = mm_ev('P%d' % (it % 2), PT[:], P[:], reads=[Pk, PTk])
            yield
            PT2, PT2k = mm_ev('PT%d' % (it % 2), P[:], PT[:], eng='act', reads=[Pk, PTk])
            yield
            p, pk = nps()
            kb.op('pe', lambda p=p, PT2=PT2: nc.tensor.matmul(p[:, 0:128], lhsT=PT2[:], rhs=R_[:], start=True, stop=True), reads=[PT2k, Rk], writes=[pk])
            yield
            kb.op('dve', lambda p=p: nc.vector.tensor_tensor(out=R_[:], in0=p[:, 0:128], in1=R_[:], op=ALU.add), reads=[pk, Rk], writes=[Rk])
            yield
            P, Pk, PT, PTk = P2, P2k, PT2, PT2k
        MTb, MTbk = S('MTb')
        kb.op('act', lambda: nc.scalar.copy(out=MTb[:], in_=R_[:]), reads=[Rk, MTbk], writes=[MTbk])
        yield
        X0, X0k = mm_ev('X0', BtT[:], V, cols=dv, reads=[BtTk] + OK)
        yield
        U0, U0k = mm_ev('U0', MTb[:], X0[:, 0:dv], cols=dv, eng='act', reads=[MTbk, X0k])
        yield
        MK, MKk = mm_ev('MK', MTb[:], KAP, cols=dk, reads=[MTbk] + OK)
        yield
        RstT, RstTk = mm_ev('RstT', MK[:, 0:dk], AbT[:], rows=dk, extra=[(RM, idb[:])], eng='act', reads=[MKk, AbTk, 'identb'] + OK)
        yield
        Y0, Y0k = mm_ev('Y0', AbT[:], U0[:, 0:dv], cols=dv, extra=[(AkT[:], V)], reads=[AbTk, U0k, AkTk] + OK)
        yield
        Phi = {}
        Z0 = {}
        for c in range(2):
            rs = slice(c * 64, (c + 1) * 64)
            p, pk = nps()
            kb.op('pe', lambda p=p, rs=rs: nc.tensor.matmul(p[0:dk, 0:dk], lhsT=MK[rs, 0:dk], rhs=BCt[rs, :], start=True, stop=True), reads=[MKk] + OK, writes=[pk])
            yield
            dst, dkey = S('Phi%d' % c)
            kb.op('dve', lambda p=p, dst=dst, c=c: nc.vector.scalar_tensor_tensor(out=dst[0:dk, 0:dk], in0=idf[0:dk, 0:dk], scalar=wc[0][:, c:c + 1], in1=p[0:dk, 0:dk],
                                                                               op0=ALU.mult, op1=ALU.add), reads=[pk, 'identf', wc[1], dkey], writes=[dkey])
            Phi[c] = (dst, dkey)
            Z0[c] = mm_ev('Z0%d' % c, BCt[rs, :], U0[rs, 0:dv], rows=dk, cols=dv, extra=[(KCt[rs, :], V[rs, :])], eng='act', reads=[U0k] + OK)
            yield
        Tt, Tk = T
        for c in ((0, 1) if z == 0 else (1, 0)):
            rs = slice(c * 64, (c + 1) * 64)
            p, pk = nps()
            kb.op('pe', lambda p=p: nc.tensor.matmul(p[:, 0:dv], lhsT=RstT[0:dk, :], rhs=Tt[:], start=True, stop=True), reads=[RstTk, Tk], writes=[pk])
            yield
            kb.op('dve', lambda p=p, rs=rs: nc.vector.tensor_tensor(out=yout[0][rs, :], in0=p[rs, 0:dv], in1=Y0[rs, 0:dv], op=ALU.add), reads=[pk, Y0k, yout[1]], writes=[yout[1]])
            yield
            p2, pk2 = nps()

            def tm(p2=p2, c=c):
                nc.tensor.matmul(p2[0:dk, 0:dv], lhsT=Phi[c][0][0:dk, 0:dk], rhs=Tt[:], start=True, stop=False)
                return nc.tensor.matmul(p2[0:dk, 0:dv], lhsT=idf[0:dk, 0:dk], rhs=Z0[c][0][0:dk, 0:dv], start=False, stop=True)
            kb.op('pe', tm, reads=[Phi[c][1], Z0[c][1], Tk, 'identf'], writes=[pk2])
            yield
            kb.op('act', lambda p2=p2: nc.scalar.copy(out=Tt[:], in_=p2[0:dk, 0:dv]), reads=[pk2, Tk], writes=[Tk])
            yield

    def run_heads(self, gens):
        gens = list(gens)
        active = {}
        free = list(range(NSLOT))
        while gens or active:
            while gens and free:
                sl = free.pop(0)
                active[sl] = gens.pop(0)(sl)
            for sl in list(active.keys()):
                try:
                    next(active[sl])
                except StopIteration:
                    del active[sl]
                    free.append(sl)

    def head_bufs(self, st):
        B = {}
        for nm, dt in (('N', F32), ('NT', F32), ('R', F32), ('P0', F32), ('P1', F32), ('PT0', F32), ('PT1', F32),
                       ('BtT', BF16), ('AbT', BF16), ('AkT', BF16), ('MTb', BF16), ('X0', BF16), ('U0', BF16), ('MK', BF16),
                       ('RstT', F32), ('Y0', F32), ('Phi0', F32), ('Phi1', F32), ('Z00', F32), ('Z01', F32)):
            B[nm] = [st.sb("%s_%d" % (nm, q), [128, 128], dt) for q in range(NSLOT)]
        B['ps'] = [st.ps("hps%d" % q, [128, 512], F32) for q in range(NSLOT)]
        return B

    def scan_order(self, z):
        nt = NT // 128
        nct = TC // 128
        if z == 0:
            return list(range(nt))
        return list(range(nct - 1, -1, -1)) + list(range(nt - 1, nct - 1, -1))

    def stage_rwkv_proj(self, jl):
        nc, kb = self.nc, self.kb
        with Stage(kb) as st:
            NCOL = 3072 + 384
            Wb = st.sb("Wb", [128, 8, NCOL], BF16)
            stg = [st.sb("stg%d" % j, [128, 1024], F32) for j in range(2)]
            n = 0
            srcs = [(self.rk_w_rkv[jl, 0], 0, 1024), (self.rk_w_rkv[jl, 1], 1024, 1024), (self.rk_w_rkv[jl, 2], 2048, 1024),
                    (self.rk_w1[jl, 0], 3072, 64), (self.rk_w1[jl, 1], 3136, 64), (self.rk_a1[jl, 0], 3200, 64), (self.rk_a1[jl, 1], 3264, 64),
                    (self.rk_g1[jl], 3328, 128)]
            for (src, c0, wd) in srcs:
                for k in range(8):
                    j = n % 2
                    n += 1
                    kb.dma('sp', stg[j][:, 0:wd], src[k * 128:(k + 1) * 128, :], writes=['stg%d' % j])
                    kb.op('act' if n % 2 else 'dve', (lambda k=k, j=j, c0=c0, wd=wd: nc.scalar.copy(out=Wb[:, k, c0:c0 + wd], in_=stg[j][:, 0:wd])) if n % 2 else
                          (lambda k=k, j=j, c0=c0, wd=wd: nc.vector.tensor_copy(out=Wb[:, k, c0:c0 + wd], in_=stg[j][:, 0:wd])), reads=['stg%d' % j, 'Wb'], writes=['Wb'])
            W2 = st.sb("W2", [128, 3, 1024], BF16)
            for q, srcl in enumerate(([self.rk_w2[jl, 0], self.rk_w2[jl, 1]], [self.rk_a2[jl, 0], self.rk_a2[jl, 1]], [self.rk_g2[jl]])):
                j = q % 2
                r0 = 0
                for si, src in enumerate(srcl):
                    rws = src.shape[0]
                    kb.dma('sp', stg[j][r0:r0 + rws, :], src, reads=['stg%d' % j] if si else [], writes=['stg%d' % j])
                    r0 += rws
                kb.op('dve', lambda q=q, j=j: nc.vector.tensor_copy(out=W2[:, q, :], in_=stg[j][:]), reads=['stg%d' % j, 'W2'], writes=['W2'])
            mu = st.sb("mu", [128, 6, 8], F32)
            for m in range(6):
                kb.dma('sp', mu[:, m, :], self.rk_mu[jl, m, :].rearrange("(k p) -> p k", p=128), reads=['mu'] if m else [], writes=['mu'], allow_slow_non_contiguous=True)
            HW_ = 512 + 128
            ht = st.sb("ht", [128, 8, HW_], BF16)
            xx = st.sb("xx", [128, 8, 512], BF16)
            xm = st.sb("xm", [128, 6, 8, 512], BF16)
            l1 = st.sb("l1", [128, 3, 512], BF16)
            pp = [st.ps("pp%d" % q, [128, 512], F32) for q in range(4)]
            ob = [st.sb("ob%d" % q, [128, 1024], F32) for q in range(4)]
            npp = [0]
            nob = [0]
            tiles = [(0, TC)] + tiles_of(NT, 512, TC)
            for it, (t0, W) in enumerate(tiles):
                isc = (t0 == 0)
                lo = t0 - 64
                hi = t0 + W + 64
                zl = isc or t0 == TC
                zr = isc or (t0 + W == NT)
                if zl:
                    kb.op('dve', lambda: nc.vector.memset(ht[:, :, 0:64], 0.0), reads=['ht'], writes=['ht'])
                if zr:
                    kb.op('dve', lambda W=W: nc.vector.memset(ht[:, :, 64 + W:128 + W], 0.0), reads=['ht'], writes=['ht'])
                a = t0 if zl else lo
                b = t0 + W if zr else hi
                kb.dma('sp', ht[:, :, 64 + (a - t0):64 + (b - t0)], self.HT[:, :, a:b], reads=['HT%d' % q for q in range(a // 128, (b + 127) // 128)] + ['ht'], writes=['ht'])
                for k in range(8):
                    if isc:
                        off = 63 if k < 4 else 65
                    else:
                        off = (63, 63, 65, 65, 0, 0, 128, 128)[k]
                    kb.op('dve', lambda k=k, off=off, W=W: nc.vector.tensor_tensor(out=xx[:, k, 0:W], in0=ht[:, k, off:off + W], in1=ht[:, k, 64:64 + W], op=ALU.subtract),
                          reads=['ht', 'xx'], writes=['xx'])
                    if not isc and k < 4:
                        col = 0 if k < 2 else 63
                        kb.op('dve', lambda k=k, col=col, W=W: nc.vector.tensor_scalar(
                            out=xx[:, k, 0:W].rearrange("p (r c) -> p r c", c=64)[:, :, col], in0=ht[:, k, 64:64 + W].rearrange("p (r c) -> p r c", c=64)[:, :, col],
                            scalar1=-1.0, scalar2=None, op0=ALU.mult), reads=['ht', 'xx'], writes=['xx'])
                for m in range(6):
                    for k in range(8):
                        eng = 'dve'
                        E = nc.vector if eng == 'dve' else nc.gpsimd
                        kb.op(eng, lambda m=m, k=k, W=W, E=E: E.scalar_tensor_tensor(out=xm[:, m, k, 0:W], in0=xx[:, k, 0:W], scalar=mu[:, m, k:k + 1], in1=ht[:, k, 64:64 + W],
                                                                                 op0=ALU.mult, op1=ALU.add), reads=['xx', 'ht', 'mu', 'xm%d' % m], writes=['xm%d' % m])
                for c, (m, fn) in enumerate(((1, AF.Tanh), (4, AF.Copy), (5, AF.Sigmoid))):
                    q = npp[0] % 4
                    npp[0] += 1

                    def f(q=q, c=c, m=m, W=W):
                        for k in range(8):
                            ins = nc.tensor.matmul(pp[q][:, 0:W], lhsT=Wb[:, k, 3072 + c * 128:3072 + (c + 1) * 128], rhs=xm[:, m, k, 0:W], start=(k == 0), stop=(k == 7))
                        return ins
                    kb.op('pe', f, reads=['Wb', 'xm%d' % m], writes=['pp%d' % q])
                    kb.op('act', lambda q=q, c=c, fn=fn, W=W: nc.scalar.activation(out=l1[:, c, 0:W], in_=pp[q][:, 0:W], func=fn), reads=['pp%d' % q, 'l1'], writes=['l1'])
                for sub in range(W // 128):
                    ts = t0 + sub * 128
                    ti = ts // 128
                    ss = slice(sub * 128, (sub + 1) * 128)
                    outs = [('RR', 0, 0, None), ('KK0', 2, 1024, None), ('VV', 3, 2048, None),
                            ('DL0', None, 0, (0, 0, 64)), ('DL1', None, 0, (0, 64, 128)), ('AL0', None, 1, (1, 0, 64)), ('AL1', None, 1, (1, 64, 128)), ('GG', None, 2, (2, 0, 128))]
                    for (nm, m, c0, l2) in outs:
                        o = nob[0] % 4
                        nob[0] += 1
                        for half in range(2):
                            q = npp[0] % 4
                            npp[0] += 1
                            if l2 is None:
                                def f(q=q, m=m, c0=c0, half=half, ss=ss):
                                    for k in range(8):
                                        ins = nc.tensor.matmul(pp[q][:], lhsT=xm[:, m, k, ss], rhs=Wb[:, k, c0 + half * 512:c0 + (half + 1) * 512], start=(k == 0), stop=(k == 7))
                                    return ins
                                kb.op('pe', f, reads=['Wb', 'xm%d' % m], writes=['pp%d' % q])
                            else:
                                c, r0, r1 = l2
                                kb.op('pe', lambda q=q, c=c, r0=r0, r1=r1, half=half, ss=ss: nc.tensor.matmul(pp[q][:], lhsT=l1[r0:r1, c, ss], rhs=W2[r0:r1, c, half * 512:(half + 1) * 512],
                                                                                                           start=True, stop=True), reads=['l1', 'W2'], writes=['pp%d' % q])
                            if half == 0:
                                kb.op('act', lambda q=q, o=o: nc.scalar.copy(out=ob[o][:, 0:512], in_=pp[q][:]), reads=['pp%d' % q, 'ob%d' % o], writes=['ob%d' % o])
                            else:
                                kb.op('dve', lambda q=q, o=o: nc.vector.tensor_copy(out=ob[o][:, 512:1024], in_=pp[q][:]), reads=['pp%d' % q, 'ob%d' % o], writes=['ob%d' % o])
                        kb.dma('sp', self.RK[nm][ts:ts + 128, :], ob[o][:], reads=['ob%d' % o], writes=['%s%d' % (nm, ti)])

    def stage_rwkv_scan(self, jl, z):
        nc, kb = self.nc, self.kb
        with Stage(kb) as st:
            C = self.scan_consts(st, z)
            B = self.head_bufs(st)
            w0 = self.bcast_load(st, "w0", self.rk_w0[jl, z, :], 1024)
            a0 = self.bcast_load(st, "a0", self.rk_a0[jl, z, :], 1024)
            k_k = self.bcast_load(st, "k_k", self.rk_k_k[jl, :], 1024)
            k_a = self.bcast_load(st, "k_a", self.rk_k_a[jl, :], 1024)
            T = [st.sb("T%d" % h, [64, 64], F32) for h in range(16)]
            for h in range(16):
                kb.op('dve', lambda h=h: nc.vector.memset(T[h][:], 0.0), writes=['T%d' % h])
            ld = {nm: st.sb("ld_" + nm, [128, 1024], F32) for nm in ('RR', 'KK0', 'VV', 'DL', 'AL')}
            f = {nm: st.sb("f_" + nm, [128, 1024], F32) for nm in ('sw', 'a', 'kk', 'kd', 'b', 'eG', 'enG', 'eGx', 'eRev', 'eTot', 'Gs', 't1')}
            sm = st.sb("sm", [128, 3, 16], F32)
            tb = {nm: st.sb("tb_" + nm, [128, 1024], BF16) for nm in ('KAP', 'RM', 'BC', 'KC', 'V', 'BP', 'KP')}
            fT = {nm: st.sb("fT_" + nm, [128, 8, 128], BF16) for nm in ('KAP', 'BP', 'KP', 'RM')}
            eTT = st.sb("eTT", [128, 8, 128], F32)
            wc = st.sb("wc", [64, 16, 2], F32)
            pG = [st.ps("pG%d" % q, [128, 512], F32) for q in range(2)]
            yt = st.sb("yt", [128, 1024], F32)
            DEC = 0.6065306597126334
            for ti in self.scan_order(z):
                t0 = ti * 128
                for nm, src in (('RR', self.RK['RR']), ('KK0', self.RK['KK0']), ('VV', self.RK['VV']), ('DL', self.RK['DL%d' % z]), ('AL', self.RK['AL%d' % z])):
                    kb.dma('sp', ld[nm][:], src[t0:t0 + 128, :], reads=['%s%d' % (nm if nm in ('RR', 'KK0', 'VV') else nm + str(z), ti)], writes=['ld_' + nm])
                V_ = nc.vector
                kb.op('dve', lambda: V_.tensor_tensor(out=f['t1'][:], in0=ld['DL'][:], in1=w0[:], op=ALU.add), reads=['ld_DL', 'w0', 'f_t1'], writes=['f_t1'])
                kb.op('act', lambda: nc.scalar.activation(out=f['sw'][:], in_=f['t1'][:], func=AF.Sigmoid), reads=['f_t1', 'f_sw'], writes=['f_sw'])
                kb.op('dve', lambda: V_.tensor_tensor(out=f['t1'][:], in0=ld['AL'][:], in1=a0[:], op=ALU.add), reads=['ld_AL', 'a0', 'f_t1'], writes=['f_t1'])
                kb.op('act', lambda: nc.scalar.activation(out=f['a'][:], in_=f['t1'][:], func=AF.Sigmoid), reads=['f_t1', 'f_a'], writes=['f_a'])
                kb.op('dve', lambda: V_.tensor_tensor(out=f['kk'][:], in0=ld['KK0'][:], in1=k_k[:], op=ALU.mult), reads=['ld_KK0', 'k_k', 'f_kk'], writes=['f_kk'])
                kb.op('dve', lambda: V_.tensor_tensor(out=f['t1'][:], in0=f['kk'][:], in1=f['kk'][:], op=ALU.mult), reads=['f_kk', 'f_t1'], writes=['f_t1'])
                kb.op('dve', lambda: V_.tensor_reduce(out=sm[:, 0, :], in_=f['t1'][:].rearrange("p (h n) -> p h n", n=64), axis=AX.X, op=ALU.add), reads=['f_t1', 'sm'], writes=['sm'])
                kb.op('act', lambda: nc.scalar.activation(out=sm[:, 1, :], in_=sm[:, 0, :], func=AF.Sqrt), reads=['sm'], writes=['sm'])
                kb.op('dve', lambda: V_.tensor_scalar(out=sm[:, 1, :], in0=sm[:, 1, :], scalar1=1e-12, scalar2=None, op0=ALU.max), reads=['sm'], writes=['sm'])
                kb.op('dve', lambda: V_.reciprocal(out=sm[:, 2, :], in_=sm[:, 1, :]), reads=['sm'], writes=['sm'])
                kb.op('dve', lambda: V_.tensor_tensor(out=f['kk'][:].rearrange("p (h n) -> p h n", n=64), in0=f['kk'][:].rearrange("p (h n) -> p h n", n=64),
                                                      in1=sm[:, 2, :].unsqueeze(2).to_broadcast([128, 16, 64]), op=ALU.mult), reads=['sm', 'f_kk'], writes=['f_kk'])
                kb.op('dve', lambda: V_.scalar_tensor_tensor(out=f['t1'][:], in0=f['a'][:], scalar=-1.0, in1=k_a[:], op0=ALU.add, op1=ALU.mult), reads=['f_a', 'k_a', 'f_t1'], writes=['f_t1'])
                kb.op('dve', lambda: V_.scalar_tensor_tensor(out=f['kd'][:], in0=f['t1'][:], scalar=1.0, in1=ld['KK0'][:], op0=ALU.add, op1=ALU.mult), reads=['f_t1', 'ld_KK0', 'f_kd'], writes=['f_kd'])
                kb.op('dve', lambda: V_.tensor_tensor(out=f['b'][:], in0=f['kk'][:], in1=f['a'][:], op=ALU.mult), reads=['f_kk', 'f_a', 'f_b'], writes=['f_b'])
                for half in range(2):
                    hs = slice(half * 512, (half + 1) * 512)
                    kb.op('pe', lambda hs=hs: nc.tensor.matmul(pG[0][:], lhsT=C['SBI'][:], rhs=f['sw'][:, hs], start=True, stop=True), reads=['SBI', 'f_sw'], writes=['pG0'])
                    kb.op('pe', lambda hs=hs: nc.tensor.matmul(pG[1][:], lhsT=C['BLK'][:], rhs=f['sw'][:, hs], start=True, stop=True), reads=['BLK', 'f_sw'], writes=['pG1'])
                    kb.op('act', lambda hs=hs: nc.scalar.activation(out=f['eG'][:, hs], in_=pG[0][:], func=AF.Exp, scale=-DEC), reads=['pG0', 'f_eG'], writes=['f_eG'])
                    kb.op('act', lambda hs=hs: nc.scalar.activation(out=f['enG'][:, hs], in_=pG[0][:], func=AF.Exp, scale=DEC), reads=['pG0', 'f_enG'], writes=['f_enG'])
                    kb.op('dve', lambda hs=hs: V_.tensor_copy(out=f['Gs'][:, hs], in_=pG[0][:]), reads=['pG0', 'f_Gs'], writes=['f_Gs'])
                    kb.op('act', lambda hs=hs: nc.scalar.activation(out=f['eTot'][:, hs], in_=pG[1][:], func=AF.Exp, scale=-DEC), reads=['pG1', 'f_eTot'], writes=['f_eTot'])
                    kb.op('dve', lambda hs=hs: V_.tensor_tensor(out=f['eRev'][:, hs], in0=pG[1][:], in1=f['Gs'][:, hs], op=ALU.subtract), reads=['pG1', 'f_Gs', 'f_eRev'], writes=['f_eRev'])
                kb.op('act', lambda: nc.scalar.activation(out=f['eRev'][:], in_=f['eRev'][:], func=AF.Exp, scale=-DEC), reads=['f_eRev'], writes=['f_eRev'])
                kb.op('dve', lambda: V_.tensor_tensor(out=f['eGx'][:], in0=f['Gs'][:], in1=f['sw'][:], op=ALU.subtract), reads=['f_Gs', 'f_sw', 'f_eGx'], writes=['f_eGx'])
                kb.op('act', lambda: nc.scalar.activation(out=f['eGx'][:], in_=f['eGx'][:], func=AF.Exp, scale=-DEC), reads=['f_eGx'], writes=['f_eGx'])
                kb.op('dve', lambda: V_.scalar_tensor_tensor(out=tb['KAP'][:], in0=f['kk'][:], scalar=-1.0, in1=f['eGx'][:], op0=ALU.mult, op1=ALU.mult), reads=['f_kk', 'f_eGx', 'tb_KAP'], writes=['tb_KAP'])
                kb.op('pool', lambda: nc.gpsimd.tensor_tensor(out=tb['RM'][:], in0=ld['RR'][:], in1=f['eG'][:], op=ALU.mult), reads=['ld_RR', 'f_eG', 'tb_RM'], writes=['tb_RM'])
                kb.op('dve', lambda: V_.tensor_tensor(out=tb['BP'][:], in0=f['b'][:], in1=f['enG'][:], op=ALU.mult), reads=['f_b', 'f_enG', 'tb_BP'], writes=['tb_BP'])
                kb.op('pool', lambda: nc.gpsimd.tensor_tensor(out=tb['KP'][:], in0=f['kd'][:], in1=f['enG'][:], op=ALU.mult), reads=['f_kd', 'f_enG', 'tb_KP'], writes=['tb_KP'])
                kb.op('dve', lambda: V_.tensor_tensor(out=tb['BC'][:], in0=f['b'][:], in1=f['eRev'][:], op=ALU.mult), reads=['f_b', 'f_eRev', 'tb_BC'], writes=['tb_BC'])
                kb.op('pool', lambda: nc.gpsimd.tensor_tensor(out=tb['KC'][:], in0=f['kd'][:], in1=f['eRev'][:], op=ALU.mult), reads=['f_kd', 'f_eRev', 'tb_KC'], writes=['tb_KC'])
                kb.op('act', lambda: nc.scalar.copy(out=tb['V'][:], in_=ld['VV'][:]), reads=['ld_VV', 'tb_V'], writes=['tb_V'])
                for nm in ('KAP', 'BP', 'KP', 'RM'):
                    pT = pG[0][:].bitcast(BF16)[:, 0:1024].rearrange("p (k t) -> p k t", t=128)

                    def tr(nm=nm, pT=pT):
                        for k in range(8):
                            ins = nc.tensor.transpose(pT[:, k, :], tb[nm][:, k * 128:(k + 1) * 128], C['identb'][:])
                        return ins
                    kb.op('pe', tr, reads=['tb_' + nm, 'identb'], writes=['pG0'])
                    kb.op('act', lambda nm=nm, pT=pT: nc.scalar.copy(out=fT[nm][:], in_=pT), reads=['pG0', 'fT_' + nm], writes=['fT_' + nm])
                for half in range(2):
                    pT = pG[1][:].rearrange("p (k t) -> p k t", t=128)

                    def tr2(half=half, pT=pT):
                        for k in range(4):
                            kk_ = half * 4 + k
                            ins = nc.tensor.transpose(pT[:, k, :], f['eTot'][:, kk_ * 128:(kk_ + 1) * 128], C['identf'][:])
                        return ins
                    kb.op('pe', tr2, reads=['f_eTot', 'identf'], writes=['pG1'])
                    kb.op('dve', lambda half=half, pT=pT: V_.tensor_copy(out=eTT[:, half * 4:(half + 1) * 4, :], in_=pT), reads=['pG1', 'eTT'], writes=['eTT'])
                for par in range(2):
                    kb.op('dve', lambda par=par: V_.tensor_copy(out=wc[:, par::2, :], in_=eTT[par * 64:(par + 1) * 64, :, 0:128:64]), reads=['eTT', 'wc'], writes=['wc'])
                opk = ['tb_KAP', 'tb_RM', 'tb_BC', 'tb_KC', 'tb_V', 'fT_KAP', 'fT_BP', 'fT_KP', 'fT_RM']
                gens = []
                for h in range(16):
                    def mk(h):
                        cs = slice(h * 64, (h + 1) * 64)
                        pb = (h % 2) * 64
                        k8 = h // 2
                        return lambda sl: self.head_scan(st, C, h, 64, 64, pb, tb['KAP'][:, cs], tb['RM'][:, cs], tb['BC'][:, cs], tb['KC'][:, cs], tb['V'][:, cs],
                                   fT['KAP'][pb:pb + 64, k8, :], fT['BP'][pb:pb + 64, k8, :], fT['KP'][pb:pb + 64, k8, :], fT['RM'][pb:pb + 64, k8, :],
                                   (C['SB'][:], 'SB'), (C['SBI'][:], 'SBI'), (C['SBT'][:], 'SBT'), (wc[:, h, :], 'wc'), (T[h], 'T%d' % h), (yt[:, cs], 'yt%d' % h), opk, z, B, sl)
                    gens.append(mk(h))
                self.run_heads(gens)
                kb.dma('sp', self.YZ[z][t0:t0 + 128, :], yt[:], reads=['yt%d' % h for h in range(16)], writes=['YZ%d_%d' % (z, ti)])

    def stage_rwkv_out(self, jl):
        nc, kb = self.nc, self.kb
        with Stage(kb) as st:
            V_ = nc.vector
            idf = st.sb("identf", [128, 128], F32)
            kb.dma('sp', idf[:], self.cst_identf, writes=['identf'])
            idb = st.sb("identb", [128, 128], BF16)
            kb.op('dve', lambda: V_.tensor_copy(out=idb[:], in_=idf[:]), reads=['identf'], writes=['identb'])
            wo = st.sb("wo", [128, 8, 1024], BF16)
            stg = [st.sb("stg%d" % j, [128, 1024], F32) for j in range(2)]
            for k in range(8):
                j = k % 2
                kb.dma('sp', stg[j][:], self.rk_w_o[jl, k * 128:(k + 1) * 128, :], writes=['stg%d' % j])
                kb.op('dve', lambda k=k, j=j: V_.tensor_copy(out=wo[:, k, :], in_=stg[j][:]), reads=['stg%d' % j, 'wo'], writes=['wo'])
            bc = {}
            for nm, src in (('a00', self.rk_a0[jl, 0, :]), ('a01', self.rk_a0[jl, 1, :]), ('k_a', self.rk_k_a[jl, :]), ('r_k', self.rk_r_k[jl].rearrange("h n -> (h n)")),
                            ('lg', self.rk_lnx_g[jl, :]), ('lb', self.rk_lnx_b[jl, :])):
                bc[nm] = self.bcast_load(st, nm, src, 1024)
            ld = {nm: st.sb("ld_" + nm, [128, 1024], F32) for nm in ('Y0', 'Y1', 'RR', 'KK0', 'VV', 'AL0', 'AL1', 'GG')}
            t1 = st.sb("t1", [128, 1024], F32)
            t2 = st.sb("t2", [128, 1024], F32)
            zb = st.sb("zb", [128, 1024], BF16)
            zT = st.sb("zT", [128, 8, 128], BF16)
            sm = st.sb("sm", [128, 4, 16], F32)
            pT = st.ps("pT", [128, 512], F32)
            pO = [st.ps("pO%d" % q, [128, 512], F32) for q in range(2)]
            yo = st.sb("yo", [128, 1024], F32)
            H3 = lambda ap: ap.rearrange("p (h n) -> p h n", n=64)
            B3 = lambda ap: ap.unsqueeze(2).to_broadcast([128, 16, 64])
            for ti in range(NT // 128):
                t0 = ti * 128
                for nm, src, key in (('Y0', self.YZ[0], 'YZ0_%d' % ti), ('Y1', self.YZ[1], 'YZ1_%d' % ti), ('RR', self.RK['RR'], 'RR%d' % ti), ('KK0', self.RK['KK0'], 'KK0%d' % ti),
                                     ('VV', self.RK['VV'], 'VV%d' % ti), ('AL0', self.RK['AL0'], 'AL0%d' % ti), ('AL1', self.RK['AL1'], 'AL1%d' % ti), ('GG', self.RK['GG'], 'GG%d' % ti)):
                    kb.dma('sp', ld[nm][:], src[t0:t0 + 128, :], reads=[key], writes=['ld_' + nm])
                kb.op('dve', lambda: V_.tensor_tensor(out=t1[:], in0=ld['Y0'][:], in1=ld['Y1'][:], op=ALU.add), reads=['ld_Y0', 'ld_Y1', 't1'], writes=['t1'])
                kb.op('dve', lambda: V_.tensor_reduce(out=sm[:, 0, :], in_=H3(t1[:]), axis=AX.X, op=ALU.add), reads=['t1', 'sm'], writes=['sm'])
                kb.op('dve', lambda: V_.tensor_scalar(out=sm[:, 0, :], in0=sm[:, 0, :], scalar1=1.0 / 64, scalar2=None, op0=ALU.mult), reads=['sm'], writes=['sm'])
                kb.op('dve', lambda: V_.tensor_tensor(out=H3(t1[:]), in0=H3(t1[:]), in1=B3(sm[:, 0, :]), op=ALU.subtract), reads=['sm', 't1'], writes=['t1'])
                kb.op('dve', lambda: V_.tensor_tensor(out=t2[:], in0=t1[:], in1=t1[:], op=ALU.mult), reads=['t1', 't2'], writes=['t2'])
                kb.op('dve', lambda: V_.tensor_reduce(out=sm[:, 1, :], in_=H3(t2[:]), axis=AX.X, op=ALU.add), reads=['t2', 'sm'], writes=['sm'])
                kb.op('dve', lambda: V_.tensor_scalar(out=sm[:, 1, :], in0=sm[:, 1, :], scalar1=1.0 / 64, scalar2=64e-5, op0=ALU.mult, op1=ALU.add), reads=['sm'], writes=['sm'])
                kb.op('act', lambda: nc.scalar.activation(out=sm[:, 1, :], in_=sm[:, 1, :], func=AF.Sqrt), reads=['sm'], writes=['sm'])
                kb.op('dve', lambda: V_.reciprocal(out=sm[:, 2, :], in_=sm[:, 1, :]), reads=['sm'], writes=['sm'])
                kb.op('dve', lambda: V_.tensor_tensor(out=H3(t1[:]), in0=H3(t1[:]), in1=B3(sm[:, 2, :]), op=ALU.mult), reads=['sm', 't1'], writes=['t1'])
                kb.op('dve', lambda: V_.tensor_tensor(out=t1[:], in0=t1[:], in1=bc['lg'][:], op=ALU.mult), reads=['t1', 'lg'], writes=['t1'])
                kb.op('dve', lambda: V_.tensor_tensor(out=t1[:], in0=t1[:], in1=bc['lb'][:], op=ALU.add), reads=['t1', 'lb'], writes=['t1'])
                kb.op('dve', lambda: V_.tensor_tensor(out=t2[:], in0=ld['AL0'][:], in1=bc['a00'][:], op=ALU.add), reads=['ld_AL0', 'a00', 't2'], writes=['t2'])
                kb.op('act', lambda: nc.scalar.activation(out=t2[:], in_=t2[:], func=AF.Sigmoid), reads=['t2'], writes=['t2'])
                kb.op('dve', lambda: V_.tensor_tensor(out=ld['AL1'][:], in0=ld['AL1'][:], in1=bc['a01'][:], op=ALU.add), reads=['ld_AL1', 'a01'], writes=['ld_AL1'])
                kb.op('act', lambda: nc.scalar.activation(out=ld['AL1'][:], in_=ld['AL1'][:], func=AF.Sigmoid), reads=['ld_AL1'], writes=['ld_AL1'])
                kb.op('dve', lambda: V_.tensor_tensor(out=t2[:], in0=t2[:], in1=ld['AL1'][:], op=ALU.add), reads=['t2', 'ld_AL1'], writes=['t2'])
                kb.op('dve', lambda: V_.scalar_tensor_tensor(out=t2[:], in0=t2[:], scalar=-2.0, in1=bc['k_a'][:], op0=ALU.add, op1=ALU.mult), reads=['t2', 'k_a'], writes=['t2'])
                kb.op('dve', lambda: V_.scalar_tensor_tensor(out=t2[:], in0=t2[:], scalar=2.0, in1=ld['KK0'][:], op0=ALU.add, op1=ALU.mult), reads=['t2', 'ld_KK0'], writes=['t2'])
                kb.op('dve', lambda: V_.tensor_tensor(out=t2[:], in0=t2[:], in1=ld['RR'][:], op=ALU.mult), reads=['t2', 'ld_RR'], writes=['t2'])
                kb.op('dve', lambda: V_.tensor_tensor(out=t2[:], in0=t2[:], in1=bc['r_k'][:], op=ALU.mult), reads=['t2', 'r_k'], writes=['t2'])
                kb.op('dve', lambda: V_.tensor_reduce(out=sm[:, 3, :], in_=H3(t2[:]), axis=AX.X, op=ALU.add), reads=['t2', 'sm'], writes=['sm'])
                kb.op('dve', lambda: V_.tensor_tensor(out=H3(t2[:]), in0=H3(ld['VV'][:]), in1=B3(sm[:, 3, :]), op=ALU.mult), reads=['sm', 'ld_VV', 't2'], writes=['t2'])
                kb.op('dve', lambda: V_.tensor_tensor(out=t1[:], in0=t1[:], in1=t2[:], op=ALU.add), reads=['t1', 't2'], writes=['t1'])
                kb.op('dve', lambda: V_.tensor_tensor(out=zb[:], in0=t1[:], in1=ld['GG'][:], op=ALU.mult), reads=['t1', 'ld_GG', 'zb'], writes=['zb'])
                pTv = pT[:].bitcast(BF16)[:, 0:1024].rearrange("p (k t) -> p k t", t=128)

                def tr(pTv=pTv):
                    for k in range(8):
                        ins = nc.tensor.transpose(pTv[:, k, :], zb[:, k * 128:(k + 1) * 128], idb[:])
                    return ins
                kb.op('pe', tr, reads=['zb', 'identb'], writes=['pT'])
                kb.op('act', lambda pTv=pTv: nc.scalar.copy(out=zT[:], in_=pTv), reads=['pT', 'zT'], writes=['zT'])
                for half in range(2):
                    def mm(half=half):
                        for k in range(8):
                            ins = nc.tensor.matmul(pO[half][:], lhsT=zT[:, k, :], rhs=wo[:, k, half * 512:(half + 1) * 512], start=(k == 0), stop=(k == 7))
                        return ins
                    kb.op('pe', mm, reads=['zT', 'wo'], writes=['pO%d' % half])
                    kb.op('act' if half else 'dve', (lambda half=half: nc.scalar.copy(out=yo[:, 512:1024], in_=pO[1][:])) if half else
                          (lambda half=half: V_.tensor_copy(out=yo[:, 0:512], in_=pO[0][:])), reads=['pO%d' % half, 'yo'], writes=['yo'])
                kb.dma('sp', self.ACC[t0:t0 + 128, :], yo[:], reads=['yo'], writes=['ACC%d' % ti])

    def stage_rwkv(self, i, jl):
        self.stage_rwkv_proj(jl)
        for z in range(2):
            self.stage_rwkv_scan(jl, z)
        self.stage_rwkv_out(jl)

    def stage_gdn_proj(self, jl):
        nc, kb = self.nc, self.kb
        V_ = nc.vector
        for blk in range(4):
            with Stage(kb) as st:
                ntap = 4 if blk < 3 else 1
                ncol = 1024 if blk < 3 else 1056
                c0 = blk * 1024
                Wc = st.sb("Wc", [128, ntap, 8, ncol], BF16)
                stg = [st.sb("stg%d" % j, [128, 1056], F32) for j in range(2)]
                cw = [self.bcast_load(st, "cw%d" % tp, self.gdn_conv_w[jl, tp, c0:c0 + 1024], 1024) for tp in range(ntap)] if blk < 3 else None
                for k in range(8):
                    j = k % 2
                    kb.dma('sp', stg[j][:, 0:ncol], self.gdn_w_in[jl, k * 128:(k + 1) * 128, c0:c0 + ncol], writes=['stg%d' % j])
                    for tp in range(ntap):
                        if blk < 3:
                            kb.op('dve', lambda k=k, j=j, tp=tp: V_.tensor_tensor(out=Wc[:, tp, k, :], in0=stg[j][:, 0:1024], in1=cw[tp][:], op=ALU.mult), reads=['stg%d' % j, 'cw%d' % tp, 'Wc'], writes=['Wc'])
                        else:
                            kb.op('dve', lambda k=k, j=j: V_.tensor_copy(out=Wc[:, 0, k, :], in_=stg[j][:, 0:ncol]), reads=['stg%d' % j, 'Wc'], writes=['Wc'])
                ht = st.sb("ht", [128, 8, 640], BF16)
                pp = [st.ps("pp%d" % q, [128, 512], F32) for q in range(4)]
                ob = [st.sb("ob%d" % q, [128, 1056], F32) for q in range(2)]
                sq = st.sb("sq", [128, 1024], F32)
                sm = st.sb("sm", [128, 2, 8], F32)
                npp = [0]
                nob = [0]
                for it, (t0, W) in enumerate([(0, TC)] + tiles_of(NT, 512, TC)):
                    isc = (t0 == 0)
                    zl = isc or t0 == TC
                    zr = isc or (t0 + W == NT)
                    if zl:
                        kb.op('dve', lambda: V_.memset(ht[:, :, 0:64], 0.0), reads=['ht'], writes=['ht'])
                    if zr:
                        kb.op('dve', lambda W=W: V_.memset(ht[:, :, 64 + W:128 + W], 0.0), reads=['ht'], writes=['ht'])
                    a = t0 if zl else t0 - 64
                    b = t0 + W if zr else t0 + W + 64
                    kb.dma('sp', ht[:, :, 64 + (a - t0):64 + (b - t0)], self.HT[:, :, a:b], reads=['HT%d' % q for q in range(a // 128, (b + 127) // 128)] + ['ht'], writes=['ht'])
                    for sub in range(W // 128):
                        ts = t0 + sub * 128
                        ti = ts // 128
                        o = nob[0] % 2
                        nob[0] += 1
                        segs = [(0, 512), (512, 1024)] + ([(1024, 1056)] if blk == 3 else [])
                        for (s0, s1) in segs:
                            q = npp[0] % 4
                            npp[0] += 1

                            def f(q=q, s0=s0, s1=s1, sub=sub):
                                n_ = ntap * 8
                                i_ = 0
                                for tp in range(ntap):
                                    off = 64 + sub * 128 + (tp - 2 if blk < 3 else 0)
                                    for k in range(8):
                                        ins = nc.tensor.matmul(pp[q][:, 0:s1 - s0], lhsT=ht[:, k, off:off + 128], rhs=Wc[:, tp, k, s0:s1], start=(i_ == 0), stop=(i_ == n_ - 1))
                                        i_ += 1
                                return ins
                            kb.op('pe', f, reads=['ht', 'Wc'], writes=['pp%d' % q])
                            if blk < 3:
                                kb.op('act', lambda q=q, o=o, s0=s0, s1=s1: nc.scalar.activation(out=ob[o][:, s0:s1], in_=pp[q][:, 0:s1 - s0], func=AF.Silu), reads=['pp%d' % q, 'ob%d' % o], writes=['ob%d' % o])
                            else:
                                kb.op('act', lambda q=q, o=o, s0=s0, s1=s1: nc.scalar.copy(out=ob[o][:, s0:s1], in_=pp[q][:, 0:s1 - s0]), reads=['pp%d' % q, 'ob%d' % o], writes=['ob%d' % o])
                        if blk < 2:
                            O3 = ob[o][:, 0:1024].rearrange("p (h n) -> p h n", n=128)
                            kb.op('dve', lambda o=o: V_.tensor_tensor(out=sq[:], in0=ob[o][:, 0:1024], in1=ob[o][:, 0:1024], op=ALU.mult), reads=['ob%d' % o, 'sq'], writes=['sq'])
                            kb.op('dve', lambda: V_.tensor_reduce(out=sm[:, 0, :], in_=sq[:].rearrange("p (h n) -> p h n", n=128), axis=AX.X, op=ALU.add), reads=['sq', 'sm'], writes=['sm'])
                            kb.op('dve', lambda: V_.tensor_scalar(out=sm[:, 0, :], in0=sm[:, 0, :], scalar1=1e-6, scalar2=None, op0=ALU.add), reads=['sm'], writes=['sm'])
                            kb.op('act', lambda: nc.scalar.activation(out=sm[:, 0, :], in_=sm[:, 0, :], func=AF.Sqrt), reads=['sm'], writes=['sm'])
                            kb.op('dve', lambda: V_.reciprocal(out=sm[:, 1, :], in_=sm[:, 0, :]), reads=['sm'], writes=['sm'])
                            if blk == 0:
                                kb.op('dve', lambda: V_.tensor_scalar(out=sm[:, 1, :], in0=sm[:, 1, :], scalar1=128 ** -0.5, scalar2=None, op0=ALU.mult), reads=['sm'], writes=['sm'])
                            kb.op('dve', lambda O3=O3: V_.tensor_tensor(out=O3, in0=O3, in1=sm[:, 1, :].unsqueeze(2).to_broadcast([128, 8, 128]), op=ALU.mult), reads=['sm', 'ob%d' % o], writes=['ob%d' % o])
                        nm = ('GQ', 'GK', 'GV', 'GZ')[blk]
                        kb.dma('sp', self.GD[nm][ts:ts + 128, :], ob[o][:, 0:1024], reads=['ob%d' % o], writes=['%s%d' % (nm, ti)])
                        if blk == 3:
                            kb.dma('sp', self.GD['GAB'][ts:ts + 128, :], ob[o][:, 1024:1056], reads=['ob%d' % o], writes=['GAB%d' % ti])

    def stage_gdn_scan(self, jl, z):
        nc, kb = self.nc, self.kb
        V_ = nc.vector
        with Stage(kb) as st:
            C = self.scan_consts(st, z)
            B = self.head_bufs(st)
            ones = st.sb("ones", [128, 128], F32)
            kb.op('dve', lambda: V_.memset(ones[:], 1.0), writes=['ones'])
            alog = self.bcast_load(st, "alog", self.gdn_a_log[jl, z, :], 8)
            dtb = self.bcast_load(st, "dtb", self.gdn_dt_bias[jl, z, :], 8)
            kb.op('act', lambda: nc.scalar.activation(out=alog[:], in_=alog[:], func=AF.Exp), reads=['alog'], writes=['alog'])
            T = [st.sb("T%d" % h, [128, 128], F32) for h in range(8)]
            for h in range(8):
                kb.op('dve', lambda h=h: V_.memset(T[h][:], 0.0), writes=['T%d' % h])
            ld = {nm: st.sb("ld_" + nm, [128, 1024], F32) for nm in ('GQ', 'GK', 'GV')}
            ab = st.sb("ab", [128, 32], F32)
            s8 = {nm: st.sb("s8_" + nm, [128, 8], F32) for nm in ('g', 'beta', 'G', 'Gx', 'eGx', 'eG', 'eRev', 'bk', 'bkg', 'bkr', 't', 'eTot', 'nG')}
            tb = {nm: st.sb("tb_" + nm, [128, 1024], BF16) for nm in ('KAP', 'RM', 'BC', 'KC', 'V', 'KU', 'BU', 'KD', 'QU')}
            fT = {nm: st.sb("fT_" + nm, [128, 8, 128], BF16) for nm in ('KU', 'BU', 'KD', 'QU')}
            wc = st.sb("wc", [128, 8, 2], F32)
            pG = [st.ps("pG%d" % q, [128, 512], F32) for q in range(2)]
            dg = st.sb("dg", [128, 128], F32)
            Dm = [{nm: st.sb("D_%s%d" % (nm, h), [128, 128], F32) for nm in ('x', 'i', 'xT')} for h in range(8)]
            yt = st.sb("yt", [128, 1024], F32)
            H3 = lambda ap: ap.rearrange("p (h n) -> p h n", n=128)
            B3 = lambda ap: ap.unsqueeze(2).to_broadcast([128, 8, 128])
            S8 = ['s8']
            for ti in self.scan_order(z):
                t0 = ti * 128
                for nm in ('GQ', 'GK', 'GV'):
                    kb.dma('sp', ld[nm][:], self.GD[nm][t0:t0 + 128, :], reads=['%s%d' % (nm, ti)], writes=['ld_' + nm])
                kb.dma('sp', ab[:], self.GD['GAB'][t0:t0 + 128, :], reads=['GAB%d' % ti], writes=['ab'])
                kb.op('dve', lambda: V_.tensor_tensor(out=s8['t'][:], in0=ab[:, z * 8:(z + 1) * 8], in1=dtb[:], op=ALU.add), reads=['ab', 'dtb'] + S8, writes=S8)
                kb.op('act', lambda: nc.scalar.activation(out=s8['t'][:], in_=s8['t'][:], func=AF.Exp), reads=S8, writes=S8)
                kb.op('act', lambda: nc.scalar.activation(out=s8['t'][:], in_=s8['t'][:], func=AF.Ln, bias=1.0), reads=S8, writes=S8)
                kb.op('dve', lambda: V_.scalar_tensor_tensor(out=s8['g'][:], in0=s8['t'][:], scalar=-1.0, in1=alog[:], op0=ALU.mult, op1=ALU.mult), reads=S8 + ['alog'], writes=S8)
                kb.op('act', lambda: nc.scalar.activation(out=s8['beta'][:], in_=ab[:, 16 + z * 8:16 + (z + 1) * 8], func=AF.Sigmoid), reads=['ab'] + S8, writes=S8)
                kb.op('pe', lambda: nc.tensor.matmul(pG[0][:, 0:8], lhsT=C['SBI'][:], rhs=s8['g'][:], start=True, stop=True), reads=['SBI'] + S8, writes=['pG0'])
                kb.op('pe', lambda: nc.tensor.matmul(pG[1][:, 0:8], lhsT=C['BLK'][:], rhs=s8['g'][:], start=True, stop=True), reads=['BLK'] + S8, writes=['pG1'])
                kb.op('dve', lambda: V_.tensor_copy(out=s8['G'][:], in_=pG[0][:, 0:8]), reads=['pG0'] + S8, writes=S8)
                kb.op('dve', lambda: V_.tensor_tensor(out=s8['Gx'][:], in0=s8['G'][:], in1=s8['g'][:], op=ALU.subtract), reads=S8, writes=S8)
                kb.op('dve', lambda: V_.tensor_tensor(out=s8['eRev'][:], in0=pG[1][:, 0:8], in1=s8['G'][:], op=ALU.subtract), reads=['pG1'] + S8, writes=S8)
                kb.op('act', lambda: nc.scalar.activation(out=s8['eTot'][:], in_=pG[1][:, 0:8], func=AF.Exp), reads=['pG1'] + S8, writes=S8)
                for nm_o, nm_i in (('eRev', 'eRev'), ('eGx', 'Gx'), ('eG', 'G'), ('bkg', 'g')):
                    kb.op('act', lambda nm_o=nm_o, nm_i=nm_i: nc.scalar.activation(out=s8[nm_o][:], in_=s8[nm_i][:], func=AF.Exp), reads=S8, writes=S8)
                kb.op('dve', lambda: V_.tensor_scalar(out=s8['nG'][:], in0=s8['G'][:], scalar1=-1.0, scalar2=None, op0=ALU.mult), reads=S8, writes=S8)
                kb.op('dve', lambda: V_.tensor_tensor(out=s8['bkg'][:], in0=s8['bkg'][:], in1=s8['beta'][:], op=ALU.mult), reads=S8, writes=S8)
                kb.op('dve', lambda: V_.tensor_tensor(out=s8['bk'][:], in0=s8['bkg'][:], in1=s8['eRev'][:], op=ALU.mult), reads=S8, writes=S8)
                kb.op('dve', lambda: V_.tensor_tensor(out=s8['bkr'][:], in0=s8['beta'][:], in1=s8['eRev'][:], op=ALU.mult), reads=S8, writes=S8)
                kb.op('dve', lambda: V_.tensor_scalar(out=s8['eGx'][:], in0=s8['eGx'][:], scalar1=-1.0, scalar2=None, op0=ALU.mult), reads=S8, writes=S8)
                K3 = H3(ld['GK'][:])
                for nm, sc in (('KAP', 'eGx'), ('BC', 'bk'), ('KC', 'bkr'), ('BU', 'bkg'), ('KD', 'beta')):
                    kb.op('dve', lambda nm=nm, sc=sc: V_.tensor_tensor(out=H3(tb[nm][:]), in0=K3, in1=B3(s8[sc][:]), op=ALU.mult), reads=S8 + ['ld_GK', 'tb_' + nm], writes=['tb_' + nm])
                kb.op('dve', lambda: V_.tensor_scalar(out=tb['KU'][:], in0=ld['GK'][:], scalar1=-1.0, scalar2=None, op0=ALU.mult), reads=['ld_GK', 'tb_KU'], writes=['tb_KU'])
                kb.op('dve', lambda: V_.tensor_tensor(out=H3(tb['RM'][:]), in0=H3(ld['GQ'][:]), in1=B3(s8['eG'][:]), op=ALU.mult), reads=S8 + ['ld_GQ', 'tb_RM'], writes=['tb_RM'])
                kb.op('act', lambda: nc.scalar.copy(out=tb['QU'][:], in_=ld['GQ'][:]), reads=['ld_GQ', 'tb_QU'], writes=['tb_QU'])
                kb.op('act', lambda: nc.scalar.copy(out=tb['V'][:], in_=ld['GV'][:]), reads=['ld_GV', 'tb_V'], writes=['tb_V'])
                for nm in ('KU', 'BU', 'KD', 'QU'):
                    pT = pG[0][:].bitcast(BF16)[:, 0:1024].rearrange("p (k t) -> p k t", t=128)

                    def tr(nm=nm, pT=pT):
                        for k in range(8):
                            ins = nc.tensor.transpose(pT[:, k, :], tb[nm][:, k * 128:(k + 1) * 128], C['identb'][:])
                        return ins
                    kb.op('pe', tr, reads=['tb_' + nm, 'identb'], writes=['pG0'])
                    kb.op('act', lambda nm=nm, pT=pT: nc.scalar.copy(out=fT[nm][:], in_=pT), reads=['pG0', 'fT_' + nm], writes=['fT_' + nm])
                for c in range(2):
                    kb.op('pe', lambda c=c: nc.tensor.matmul(pG[1][:, 0:8], lhsT=C['BLK'][c * 64:(c + 1) * 64, c * 64:c * 64 + 1].to_broadcast([64, 128]) if False else ones[c * 64:(c + 1) * 64, :],
                                                           rhs=s8['g'][c * 64:(c + 1) * 64, :], start=True, stop=True), reads=['ones'] + S8, writes=['pG1'])
                    kb.op('act', lambda c=c: nc.scalar.activation(out=wc[:, :, c], in_=pG[1][:, 0:8], func=AF.Exp), reads=['pG1', 'wc'], writes=['wc'])
                opk = ['tb_KAP', 'tb_RM', 'tb_BC', 'tb_KC', 'tb_V', 'fT_KU', 'fT_BU', 'fT_KD', 'fT_QU']
                gens = []
                for h in range(8):
                    cs = slice(h * 128, (h + 1) * 128)
                    for (src, outs) in (('G', (('i', 'G', 'SBI', False),)), ('Gx', (('x', 'G', 'SB', False),)), ('G', (('xT', 'Gx', 'SBT', True),))):
                        kb.op('dve', lambda h=h, src=src: V_.tensor_scalar(out=dg[:], in0=C['identf'][:], scalar1=s8[src][:, h:h + 1], scalar2=None, op0=ALU.mult), reads=['identf', 'dg'] + S8, writes=['dg'])
                        kb.op('pe', lambda: nc.tensor.matmul(pG[1][:, 0:128], lhsT=ones[:], rhs=dg[:], start=True, stop=True), reads=['ones', 'dg'], writes=['pG1'])
                        for (dn, sub_, mk, flip) in outs:
                            d_ = Dm[h][dn]
                            dn = dn + str(h)
                            if not flip:
                                kb.op('dve', lambda d_=d_, h=h, sub_=sub_: V_.tensor_scalar(out=d_[:], in0=pG[1][:, 0:128], scalar1=s8[sub_][:, h:h + 1], scalar2=0.0, op0=ALU.subtract, op1=ALU.min),
                                      reads=['pG1', 'D_' + dn] + S8, writes=['D_' + dn])
                            else:
                                kb.op('dve', lambda d_=d_, h=h, sub_=sub_: V_.tensor_scalar(out=d_[:], in0=pG[1][:, 0:128], scalar1=-1.0, scalar2=s8[sub_][:, h:h + 1], op0=ALU.mult, op1=ALU.add),
                                      reads=['pG1', 'D_' + dn] + S8, writes=['D_' + dn])
                                kb.op('dve', lambda d_=d_: V_.tensor_scalar(out=d_[:], in0=d_[:], scalar1=0.0, scalar2=None, op0=ALU.min), reads=['D_' + dn], writes=['D_' + dn])
                            kb.op('act', lambda d_=d_: nc.scalar.activation(out=d_[:], in_=d_[:], func=AF.Exp), reads=['D_' + dn], writes=['D_' + dn])
                            kb.op('dve', lambda d_=d_, mk=mk: V_.tensor_tensor(out=d_[:], in0=d_[:], in1=C[mk][:], op=ALU.mult), reads=['D_' + dn, mk], writes=['D_' + dn])
                    def mk(h, cs):
                        dmh = Dm[h]
                        return lambda sl: self.head_scan(st, C, h, 128, 128, 0, tb['KAP'][:, cs], tb['RM'][:, cs], tb['BC'][:, cs], tb['KC'][:, cs], tb['V'][:, cs],
                                   fT['KU'][:, h, :], fT['BU'][:, h, :], fT['KD'][:, h, :], fT['QU'][:, h, :],
                                   (dmh['x'][:], 'D_x%d' % h), (dmh['i'][:], 'D_i%d' % h), (dmh['xT'][:], 'D_xT%d' % h), (wc[:, h, :], 'wc'), (T[h], 'T%d' % h), (yt[:, cs], 'yt%d' % h), opk, z, B, sl)
                    gens.append(mk(h, cs))
                self.run_heads(gens)
                kb.dma('sp', self.YZ[z][t0:t0 + 128, :], yt[:], reads=['yt%d' % h for h in range(8)], writes=['YZ%d_%d' % (z, ti)])

    def stage_gdn_out(self, jl):
        nc, kb = self.nc, self.kb
        V_ = nc.vector
        with Stage(kb) as st:
            idf = st.sb("identf", [128, 128], F32)
            kb.dma('sp', idf[:], self.cst_identf, writes=['identf'])
            idb = st.sb("identb", [128, 128], BF16)
            kb.op('dve', lambda: V_.tensor_copy(out=idb[:], in_=idf[:]), reads=['identf'], writes=['identb'])
            wo = st.sb("wo", [128, 8, 1024], BF16)
            stg = [st.sb("stg%d" % j, [128, 1024], F32) for j in range(2)]
            for k in range(8):
                j = k % 2
                kb.dma('sp', stg[j][:], self.gdn_w_o[jl, k * 128:(k + 1) * 128, :], writes=['stg%d' % j])
                kb.op('dve', lambda k=k, j=j: V_.tensor_copy(out=wo[:, k, :], in_=stg[j][:]), reads=['stg%d' % j, 'wo'], writes=['wo'])
            nw = self.bcast_load(st, "nw", self.gdn_norm_w[jl, :], 128)
            ld = {nm: st.sb("ld_" + nm, [128, 1024], F32) for nm in ('Y0', 'Y1', 'GZ')}
            t2 = st.sb("t2", [128, 1024], F32)
            zb = st.sb("zb", [128, 1024], BF16)
            zT = st.sb("zT", [128, 8, 128], BF16)
            sm = st.sb("sm", [128, 2, 8], F32)
            pT = st.ps("pT", [128, 512], F32)
            pO = [st.ps("pO%d" % q, [128, 512], F32) for q in range(2)]
            yo = st.sb("yo", [128, 1024], F32)
            H3 = lambda ap: ap.rearrange("p (h n) -> p h n", n=128)
            for ti in range(NT // 128):
                t0 = ti * 128
                for nm, src, key in (('Y0', self.YZ[0], 'YZ0_%d' % ti), ('Y1', self.YZ[1], 'YZ1_%d' % ti), ('GZ', self.GD['GZ'], 'GZ%d' % ti)):
                    kb.dma('sp', ld[nm][:], src[t0:t0 + 128, :], reads=[key], writes=['ld_' + nm])
                kb.op('dve', lambda: V_.tensor_tensor(out=ld['Y0'][:], in0=ld['Y0'][:], in1=ld['Y1'][:], op=ALU.add), reads=['ld_Y0', 'ld_Y1'], writes=['ld_Y0'])
                kb.op('dve', lambda: V_.tensor_tensor(out=t2[:], in0=ld['Y0'][:], in1=ld['Y0'][:], op=ALU.mult), reads=['ld_Y0', 't2'], writes=['t2'])
                kb.op('dve', lambda: V_.tensor_reduce(out=sm[:, 0, :], in_=H3(t2[:]), axis=AX.X, op=ALU.add), reads=['t2', 'sm'], writes=['sm'])
                kb.op('dve', lambda: V_.tensor_scalar(out=sm[:, 0, :], in0=sm[:, 0, :], scalar1=1.0 / 128, scalar2=1e-6, op0=ALU.mult, op1=ALU.add), reads=['sm'], writes=['sm'])
                kb.op('act', lambda: nc.scalar.activation(out=sm[:, 0, :], in_=sm[:, 0, :], func=AF.Sqrt), reads=['sm'], writes=['sm'])
                kb.op('dve', lambda: V_.reciprocal(out=sm[:, 1, :], in_=sm[:, 0, :]), reads=['sm'], writes=['sm'])
                kb.op('dve', lambda: V_.tensor_tensor(out=H3(ld['Y0'][:]), in0=H3(ld['Y0'][:]), in1=sm[:, 1, :].unsqueeze(2).to_broadcast([128, 8, 128]), op=ALU.mult), reads=['sm', 'ld_Y0'], writes=['ld_Y0'])
                kb.op('dve', lambda: V_.tensor_tensor(out=H3(ld['Y0'][:]), in0=H3(ld['Y0'][:]), in1=nw[:].unsqueeze(1).to_broadcast([128, 8, 128]), op=ALU.mult), reads=['nw', 'ld_Y0'], writes=['ld_Y0'])
                kb.op('act', lambda: nc.scalar.activation(out=t2[:], in_=ld['GZ'][:], func=AF.Silu), reads=['ld_GZ', 't2'], writes=['t2'])
                kb.op('dve', lambda: V_.tensor_tensor(out=zb[:], in0=ld['Y0'][:], in1=t2[:], op=ALU.mult), reads=['ld_Y0', 't2', 'zb'], writes=['zb'])
                pTv = pT[:].bitcast(BF16)[:, 0:1024].rearrange("p (k t) -> p k t", t=128)

                def tr(pTv=pTv):
                    for k in range(8):
                        ins = nc.tensor.transpose(pTv[:, k, :], zb[:, k * 128:(k + 1) * 128], idb[:])
                    return ins
                kb.op('pe', tr, reads=['zb', 'identb'], writes=['pT'])
                kb.op('act', lambda pTv=pTv: nc.scalar.copy(out=zT[:], in_=pTv), reads=['pT', 'zT'], writes=['zT'])
                for half in range(2):
                    def mm(half=half):
                        for k in range(8):
                            ins = nc.tensor.matmul(pO[half][:], lhsT=zT[:, k, :], rhs=wo[:, k, half * 512:(half + 1) * 512], start=(k == 0), stop=(k == 7))
                        return ins
                    kb.op('pe', mm, reads=['zT', 'wo'], writes=['pO%d' % half])
                    kb.op('dve', lambda half=half: V_.tensor_copy(out=yo[:, half * 512:(half + 1) * 512], in_=pO[half][:]), reads=['pO%d' % half, 'yo'], writes=['yo'])
                kb.dma('sp', self.ACC[t0:t0 + 128, :], yo[:], reads=['yo'], writes=['ACC%d' % ti])

    def stage_gdn(self, i, jl):
        self.stage_gdn_proj(jl)
        for z in range(2):
            self.stage_gdn_scan(jl, z)
        self.stage_gdn_out(jl)

    def decls(self):
        nc, kb = self.nc, self.kb
        self.xin = self.din("xin", [NT, D])
        self.cvec = self.din("cvec", [2, D])
        self.ada_w = self.din("ada_w", [4, D, 6 * D])
        self.ada_b = self.din("ada_b", [4, 6 * D])
        self.ln_g = self.din("ln_g", [4, 2, D])
        self.ln_b = self.din("ln_b", [4, 2, D])
        self.pool_w = self.din("pool_w", [1, 4, 256, 256])
        self.pool_scale = self.din("pool_scale", [1, D])
        self.ffn_w1 = self.din("ffn_w1", [2, D, 2816])
        self.ffn_w3 = self.din("ffn_w3", [2, D, 2816])
        self.ffn_w2 = self.din("ffn_w2", [2, 2816, D])
        self.moe_router_w = self.din("moe_router_w", [2, D, 8])
        self.moe_router_b = self.din("moe_router_b", [2, 8])
        self.moe_w1 = self.din("moe_w1", [2, 8, D, 1408])
        self.moe_w3 = self.din("moe_w3", [2, 8, D, 1408])
        self.moe_w2 = self.din("moe_w2", [2, 8, 1408, D])
        for nm, shp in (("rk_mu", [2, 6, D]), ("rk_w_rkv", [2, 3, D, D]), ("rk_w0", [2, 2, D]), ("rk_w1", [2, 2, D, 64]), ("rk_w2", [2, 2, 64, D]),
                        ("rk_a0", [2, 2, D]), ("rk_a1", [2, 2, D, 64]), ("rk_a2", [2, 2, 64, D]), ("rk_g1", [2, D, 128]), ("rk_g2", [2, 128, D]),
                        ("rk_k_k", [2, D]), ("rk_k_a", [2, D]), ("rk_r_k", [2, 16, 64]), ("rk_lnx_g", [2, D]), ("rk_lnx_b", [2, D]), ("rk_w_o", [2, D, D])):
            setattr(self, nm, self.din(nm, shp))
        for nm, shp in (("gdn_w_in", [1, D, 4128]), ("gdn_conv_w", [1, 4, 3072]), ("gdn_a_log", [1, 2, 8]), ("gdn_dt_bias", [1, 2, 8]),
                        ("gdn_norm_w", [1, 128]), ("gdn_w_o", [1, D, D])):
            setattr(self, nm, self.din(nm, shp))
        self.GD = {nm: self.dscr("GD_" + nm, [NT, D]) for nm in ("GQ", "GK", "GV", "GZ")}
        self.GD["GAB"] = self.dscr("GD_GAB", [NT, 32])
        self.cst_masks = self.din("cst_masks", [2, 4, 128, 128])
        self.RK = {nm: self.dscr("RK_" + nm, [NT, D]) for nm in ("RR", "KK0", "VV", "DL0", "DL1", "AL0", "AL1", "GG")}
        self.YZ = [self.dscr("YZ%d" % z, [NT, D]) for z in range(2)]
        self.cst_identf = self.din("cst_identf", [128, 128])
        self.cst_invc64 = self.din("cst_invc64", [128, 4, 64])
        self.cst_invc256 = self.din("cst_invc256", [128, 4, 256])
        self.out = self.dout("out", [NT - TC, D])
        self.XS = self.dscr("XS", [NT, D])
        self.ACC = self.dscr("ACC", [NT, D])
        self.HT = self.dscr("HT", [128, 8, NT], BF16)
        self.GATES = self.dscr("GATES", [NT, 8])
        self.MODD = self.dscr("MODD", [4, 2, 6 * D])

    def stage_init(self):
        kb = self.kb
        for (t0, W) in tiles_of(NT, 128):
            kb.dma('sp', self.XS[t0:t0 + W, :], self.xin[t0:t0 + W, :], writes=['XSinit%d' % t0])
        kb.barrier()

    def build(self):
        nc, kb = self.nc, self.kb
        self.decls()
        self.stage_init()
        for i in self.layers:
            last = (i == self.layers[-1])
            self.stage_mod(i)
            self.stage_prep(i, 1)
            kind, jl = i % 3, i // 3
            if kind == 1:
                self.stage_pool(jl)
            elif kind == 0:
                self.stage_rwkv(i, jl)
            else:
                self.stage_gdn(i, jl)
            self.stage_finish(i, 1)
            e = i // 2
            if i % 2 == 0:
                self.stage_prep(i, 2)
                for hf in range(2):
                    sl = slice(hf * 1408, (hf + 1) * 1408)
                    self.stage_ffnpass(self.ffn_w1[e, :, sl], self.ffn_w3[e, :, sl], self.ffn_w2[e, sl, :], None, hf == 0)
            else:
                self.stage_prep(i, 2, router=e)
                for x in range(8):
                    self.stage_ffnpass(self.moe_w1[e, x], self.moe_w3[e, x], self.moe_w2[e, x], x, x == 0)
            self.stage_finish(i, 2, out_final=self.out if last else None)
        kb.barrier()
        return nc


def _pool_invc(L):
    t = np.arange(L)
    out = np.zeros((4, L), np.float32)
    for gi, w in enumerate((2, 4, 8, 16)):
        lo = np.clip(t - w // 2, 0, L)
        hi = np.clip(t + w // 2, 0, L)
        out[gi] = 1.0 / (hi - lo)
    return np.ascontiguousarray(np.broadcast_to(out[None], (128, 4, L))).astype(np.float32)


def _masks():
    s = np.arange(128)[:, None]
    t = np.arange(128)[None, :]
    same = (s // 64) == (t // 64)
    m = np.zeros((2, 4, 128, 128), np.float32)
    for z in range(2):
        sb = ((s < t) if z == 0 else (s > t)) & same
        sbi = ((s <= t) if z == 0 else (s >= t)) & same
        m[z, 0] = sb
        m[z, 1] = sbi
        m[z, 2] = sb.T
        m[z, 3] = same
    return m


def make_consts():
    return {
        "cst_masks": _masks(),
        "cst_identf": np.eye(128, dtype=np.float32),
        "cst_invc64": _pool_invc(64),
        "cst_invc256": _pool_invc(256),
    }


WEIGHT_KEYS = ["ada_w", "ada_b", "ln_g", "ln_b", "pool_w", "pool_scale", "ffn_w1", "ffn_w3", "ffn_w2",
               "moe_router_w", "moe_router_b", "moe_w1", "moe_w3", "moe_w2"]


def run(inputs, layers=(0, 1, 2, 3), n_cores=4, xin_override=None):
    prog = Prog(list(layers))
    nc = prog.build()
    print("instructions:", prog.kb.nins, "sems:", len(prog.kb.sems))
    cst = make_consts()
    in_maps = []
    for cidx in range(n_cores):
        b = cidx % 4
        m = {}
        if xin_override is not None:
            m["xin"] = xin_override[b]
        else:
            m["xin"] = np.ascontiguousarray(np.concatenate([inputs["ctx"][b], inputs["x"][b]], axis=0))
        m["cvec"] = np.ascontiguousarray(np.stack([inputs["c"][b], inputs["c_ctx"]], axis=0))
        for k in prog.inp:
            if k in m:
                continue
            m[k] = cst[k] if k in cst else np.ascontiguousarray(inputs[k])
        in_maps.append(m)
    res = run_bass_kernel_spmd(nc, in_maps, core_ids=list(range(n_cores)))
    return res


def kernel(**inputs):
    inputs = {k: np.asarray(v) for k, v in inputs.items()}
    res = run(inputs)
    out = np.stack([res.results[b]["out"] for b in range(4)], axis=0)
    return out.astype(np.float32)
```

```python
import contextlib
import numpy as np
import concourse.bass as bass
import concourse.mybir as mybir
from concourse.bass_utils import run_bass_kernel_spmd

F32 = mybir.dt.float32
BF16 = mybir.dt.bfloat16
AF = mybir.ActivationFunctionType
ALU = mybir.AluOpType
AX = mybir.AxisListType

D = 1024
NT = 8448
TC = 256
DEPTH = 4
ALPHA = (2 * DEPTH) ** 0.25
LN_EPS = 1e-5
NSLOT = 6


class KB:
    EPOCH = 30000
    NDMA = 24

    def __init__(self, nc):
        self.nc = nc
        self._ctx = []
        self.engs = {'pe': nc.tensor, 'dve': nc.vector, 'act': nc.scalar, 'pool': nc.gpsimd, 'sp': nc.sync}
        self.sems = []
        self.esem = {}
        self.ecnt = {}
        for e in ('pe', 'dve', 'act', 'pool'):
            self.esem[e] = self._newsem('c_' + e)
            self.ecnt[e] = 0
        self.dsem = [self._newsem('d%d' % i) for i in range(self.NDMA)]
        self.dcnt = [0] * self.NDMA
        self.dnext = 0
        self.waited = {e: {} for e in self.engs}
        self.res = {}
        self.nins = 0
        self.allsems = {}
        self.excl = set()

    def _newsem(self, name):
        cm = self.nc.semaphore(name + '_%d' % len(self.sems))
        h = cm.__enter__()
        self._ctx.append(cm)
        self.sems.append(h)
        return len(self.sems) - 1

    def _r(self, key):
        r = self.res.get(key)
        if r is None:
            r = {'w': None, 'r': {}}
            self.res[key] = r
        return r

    def _waits(self, eng, reads, writes):
        need = {}

        def add(tok):
            if tok is None:
                return
            s, v = tok
            if need.get(s, 0) < v:
                need[s] = v
        for k in reads:
            add(self._r(k)['w'])
        for k in writes:
            r = self._r(k)
            add(r['w'])
            for s, v in r['r'].items():
                add((s, v))
        wd = self.waited[eng]
        for s, v in need.items():
            if wd.get(s, 0) >= v:
                continue
            self.engs[eng].wait_ge(self.sems[s], v)
            self.nins += 1
            wd[s] = v

    def _mark(self, tok, reads, writes):
        s, v = tok
        self.allsems[s] = v
        for k in writes:
            r = self._r(k)
            r['w'] = tok
            r['r'] = {}
        for k in reads:
            if k in writes:
                continue
            r = self._r(k)
            if r['r'].get(s, 0) < v:
                r['r'][s] = v

    def op(self, eng, fn, reads=(), writes=()):
        ex = [k for k in reads if k in self.excl and k not in writes]
        if ex:
            writes = list(writes) + ex
        self._waits(eng, reads, writes)
        ins = fn()
        if self.ecnt[eng] >= self.EPOCH:
            self.esem[eng] = self._newsem('c_' + eng)
            self.ecnt[eng] = 0
        s = self.esem[eng]
        ins.then_inc(self.sems[s], 1)
        self.ecnt[eng] += 1
        self.nins += 1
        tok = (s, self.ecnt[eng])
        self._mark(tok, reads, writes)
        return tok

    def dma(self, q, out, in_, reads=(), writes=(), **kw):
        j = self.dnext
        self.dnext = (self.dnext + 1) % self.NDMA
        wd = self.waited[q]
        if self.dcnt[j] > 0 and wd.get(self.dsem[j], 0) < 16 * self.dcnt[j]:
            self.engs[q].wait_ge(self.sems[self.dsem[j]], 16 * self.dcnt[j])
            wd[self.dsem[j]] = 16 * self.dcnt[j]
        self._waits(q, reads, writes)
        self.engs[q].dma_start(out=out, in_=in_, **kw).then_inc(self.sems[self.dsem[j]], 16)
        self.dcnt[j] += 1
        self.nins += 1
        tok = (self.dsem[j], 16 * self.dcnt[j])
        self._mark(tok, reads, writes)
        return tok

    def barrier(self):
        for e in self.engs:
            wd = self.waited[e]
            for s, v in self.allsems.items():
                if wd.get(s, 0) >= v:
                    continue
                self.engs[e].wait_ge(self.sems[s], v)
                self.nins += 1
                wd[s] = v
        self.res = {}


class Stage:
    _n = [0]

    def __init__(self, kb):
        self.kb = kb
        self.nc = kb.nc
        self.es = contextlib.ExitStack()
        Stage._n[0] += 1
        self.sid = Stage._n[0]

    def __enter__(self):
        self.es.__enter__()
        return self

    def sb(self, name, shape, dt):
        return self.es.enter_context(self.nc.sbuf_tensor('%s_s%d' % (name, self.sid), shape, dt))

    def ps(self, name, shape, dt):
        self.kb.excl.add(name)
        return self.es.enter_context(self.nc.psum_tensor('%s_s%d' % (name, self.sid), shape, dt))

    def __exit__(self, *a):
        self.kb.barrier()
        return self.es.__exit__(*a)


def tiles_of(total, w, start=0):
    out = []
    t = start
    while t < total:
        out.append((t, min(w, total - t)))
        t += w
    return out


class Prog:
    def __init__(self, layers, debug_outs=()):
        self.layers = layers
        nc = bass.Bass("TRN2", target_bir_lowering=False)
        self.nc = nc
        self.kb = KB(nc)
        self.inp = {}
        self.debug_outs = debug_outs

    def din(self, name, shape, dt=F32):
        t = self.nc.dram_tensor(name, list(shape), dt, kind="ExternalInput").ap()
        self.inp[name] = t
        return t

    def dscr(self, name, shape, dt=F32):
        return self.nc.dram_tensor(name, list(shape), dt, kind="Internal").ap()

    def dout(self, name, shape, dt=F32):
        return self.nc.dram_tensor(name, list(shape), dt, kind="ExternalOutput").ap()

    def bcast_load(self, st, name, row_ap, n, dt=F32):
        t = st.sb(name, [128, n], dt)
        self.kb.dma('sp', t[:], row_ap.partition_broadcast(128), writes=[name])
        return t

    def stage_mod(self, i):
        nc, kb = self.nc, self.kb
        with Stage(kb) as st:
            cT = st.sb("cT", [128, 8, 2], F32)
            sT = st.sb("sT", [128, 8, 2], F32)
            modsb = st.sb("modsb", [2, 6144], F32)
            adab = st.sb("adab", [2, 6144], F32)
            wst = [st.sb("wst%d" % j, [128, 8, 512], F32) for j in range(2)]
            pm_ = [st.ps("pm%d" % j, [128, 512], F32) for j in range(2)]
            pm = [t[0:2, :] for t in pm_]
            for r in range(2):
                kb.dma('sp', cT[:, :, r], self.cvec[r, :].rearrange("(k p) -> p k", p=128),
                       reads=['cT'] if r else [], writes=['cT'], allow_slow_non_contiguous=True)
                kb.dma('sp', adab[r:r + 1, :], self.ada_b[i:i + 1, :], reads=['adab'] if r else [], writes=['adab'])
            kb.op('act', lambda: nc.scalar.activation(out=sT[:], in_=cT[:], func=AF.Silu), reads=['cT'], writes=['sT'])
            for g in range(12):
                j = g % 2
                kb.dma('sp', wst[j][:], self.ada_w[i, :, g * 512:(g + 1) * 512].rearrange("(k p) n -> p k n", p=128),
                       writes=['wst%d' % j])

                def mm(j=j):
                    for k in range(8):
                        ins = nc.tensor.matmul(pm[j][:], lhsT=sT[:, k, :], rhs=wst[j][:, k, :], start=(k == 0), stop=(k == 7))
                    return ins
                kb.op('pe', mm, reads=['sT', 'wst%d' % j], writes=['pm%d' % j])
                kb.op('dve', lambda j=j, g=g: nc.vector.tensor_tensor(out=modsb[:, g * 512:(g + 1) * 512], in0=pm[j][:],
                                                                     in1=adab[:, g * 512:(g + 1) * 512], op=ALU.add),
                      reads=['pm%d' % j, 'adab', 'modsb'], writes=['modsb'])
            for c0 in (1024, 4096):
                kb.op('dve', lambda c0=c0: nc.vector.tensor_scalar(out=modsb[:, c0:c0 + 1024], in0=modsb[:, c0:c0 + 1024],
                                                                 scalar1=1.0, scalar2=None, op0=ALU.add),
                      reads=['modsb'], writes=['modsb'])
            kb.dma('sp', self.MODD[i], modsb[:], reads=['modsb'], writes=['MODD'])

    def mod_tiles(self, st, i, slots):
        out = {}
        for s in slots:
            for r in range(2):
                out[(s, r)] = self.bcast_load(st, "mod%d_%d" % (s, r), self.MODD[i, r, s * 1024:(s + 1) * 1024], 1024)
        return out

    def stage_prep(self, i, sub, router=None):
        nc, kb = self.nc, self.kb
        with Stage(kb) as st:
            sh_s, sc_s = (0, 1) if sub == 1 else (3, 4)
            md = self.mod_tiles(st, i, [sh_s, sc_s])
            identf = st.sb("identf", [128, 128], F32)
            kb.dma('sp', identf[:], self.cst_identf, writes=['identf'])
            NB = 2
            xt = [st.sb("xt%d" % j, [128, 1024], F32) for j in range(NB)]
            hf = [st.sb("hf%d" % j, [128, 1024], F32) for j in range(NB)]
            hTb = [st.sb("hTb%d" % j, [128, 8, 128], BF16) for j in range(NB)]
            pT = [st.ps("pT%d" % j, [128, 8, 128], F32) for j in range(NB)]
            if router is not None:
                e = router
                rw = st.sb("rw", [128, 8, 8], F32)
                kb.dma('sp', rw[:], self.moe_router_w[e].rearrange("(k p) n -> p k n", p=128), writes=['rw'])
                rb = self.bcast_load(st, "rb", self.moe_router_b[e, :], 8)
                hTf = [st.sb("hTf%d" % j, [128, 8, 128], F32) for j in range(NB)]
                pl_ = [st.ps("pl%d" % j, [128, 512], F32) for j in range(NB)]
                pl = [t[:, 0:8] for t in pl_]
                lg = [st.sb("lg%d" % j, [128, 8], F32) for j in range(NB)]
                m1 = [st.sb("m1_%d" % j, [128, 8], F32) for j in range(NB)]
                m2 = [st.sb("m2_%d" % j, [128, 8], F32) for j in range(NB)]
                l2 = [st.sb("l2_%d" % j, [128, 8], F32) for j in range(NB)]
                mx = [st.sb("mx%d" % j, [128, 4], F32) for j in range(NB)]
                gt = [st.sb("gt%d" % j, [128, 8], F32) for j in range(NB)]
            for ti in range(NT // 128):
                j = ti % NB
                isc = 1 if ti < TC // 128 else 0
                t0 = ti * 128
                X, H, HB, PT = 'xt%d' % j, 'hf%d' % j, 'hTb%d' % j, 'pT%d' % j
                kb.dma('sp', xt[j][:], self.XS[t0:t0 + 128, :], writes=[X])
                kb.op('dve', lambda j=j, isc=isc: nc.vector.tensor_tensor(out=hf[j][:], in0=xt[j][:], in1=md[(sc_s, isc)][:], op=ALU.mult),
                      reads=[X, 'mod%d_%d' % (sc_s, isc)], writes=[H])
                kb.op('dve', lambda j=j, isc=isc: nc.vector.tensor_tensor(out=hf[j][:], in0=hf[j][:], in1=md[(sh_s, isc)][:], op=ALU.add),
                      reads=[H, 'mod%d_%d' % (sh_s, isc)], writes=[H])

                def tr(j=j):
                    for k in range(8):
                        ins = nc.tensor.transpose(pT[j][:, k, :], hf[j][:, k * 128:(k + 1) * 128], identf[:])
                    return ins
                kb.op('pe', tr, reads=[H, 'identf'], writes=[PT])
                if router is None:
                    kb.op('act', lambda j=j: nc.scalar.copy(out=hTb[j][:], in_=pT[j][:]), reads=[PT], writes=[HB])
                else:
                    kb.op('dve', lambda j=j: nc.vector.tensor_copy(out=hTf[j][:], in_=pT[j][:]), reads=[PT], writes=['hTf%d' % j])
                    kb.op('act', lambda j=j: nc.scalar.copy(out=hTb[j][:], in_=hTf[j][:]), reads=['hTf%d' % j], writes=[HB])
                kb.dma('sp', self.HT[:, :, t0:t0 + 128], hTb[j][:], reads=[HB], writes=['HT%d' % ti])
                import os
                RD = int(os.environ.get('RD', '9'))
                if router is not None and RD >= 1:
                    HF, PL, LG = 'hTf%d' % j, 'pl%d' % j, 'lg%d' % j

                    def rmm(j=j):
                        for k in range(8):
                            ins = nc.tensor.matmul(pl[j][:], lhsT=hTf[j][:, k, :], rhs=rw[:, k, :], start=(k == 0), stop=(k == 7))
                        return ins
                    kb.op('pe', rmm, reads=[HF, 'rw'], writes=[PL])
                    G = 'g%d' % j
                    if RD < 2:
                        continue
                    kb.op('dve', lambda j=j: nc.vector.tensor_tensor(out=lg[j][:], in0=pl[j][:], in1=rb[:], op=ALU.add), reads=[PL, 'rb', G], writes=[G])
                    if RD < 3:
                        kb.dma('sp', self.GATES[t0:t0 + 128, :], lg[j][:], reads=[G], writes=['GATES%d' % ti])
                        continue
                    kb.op('dve', lambda j=j: nc.vector.tensor_reduce(out=mx[j][:, 0:1], in_=lg[j][:], axis=AX.X, op=ALU.max), reads=[G], writes=[G])
                    kb.op('dve', lambda j=j: nc.vector.tensor_scalar(out=m1[j][:], in0=lg[j][:], scalar1=mx[j][:, 0:1], scalar2=None, op0=ALU.is_equal), reads=[G], writes=[G])
                    kb.op('dve', lambda j=j: nc.vector.scalar_tensor_tensor(out=l2[j][:], in0=m1[j][:], scalar=-1e30, in1=lg[j][:], op0=ALU.mult, op1=ALU.add), reads=[G], writes=[G])
                    kb.op('dve', lambda j=j: nc.vector.tensor_reduce(out=mx[j][:, 1:2], in_=l2[j][:], axis=AX.X, op=ALU.max), reads=[G], writes=[G])
                    kb.op('dve', lambda j=j: nc.vector.tensor_scalar(out=m2[j][:], in0=l2[j][:], scalar1=mx[j][:, 1:2], scalar2=None, op0=ALU.is_equal), reads=[G], writes=[G])
                    kb.op('dve', lambda j=j: nc.vector.tensor_tensor(out=mx[j][:, 2:3], in0=mx[j][:, 0:1], in1=mx[j][:, 1:2], op=ALU.subtract), reads=[G], writes=[G])
                    kb.op('act', lambda j=j: nc.scalar.activation(out=mx[j][:, 2:3], in_=mx[j][:, 2:3], func=AF.Sigmoid), reads=[G], writes=[G])
                    kb.op('dve', lambda j=j: nc.vector.tensor_scalar(out=mx[j][:, 3:4], in0=mx[j][:, 2:3], scalar1=-1.0, scalar2=1.0, op0=ALU.mult, op1=ALU.add), reads=[G], writes=[G])
                    kb.op('dve', lambda j=j: nc.vector.tensor_scalar(out=gt[j][:], in0=m1[j][:], scalar1=mx[j][:, 2:3], scalar2=None, op0=ALU.mult), reads=[G], writes=[G])
                    kb.op('dve', lambda j=j: nc.vector.scalar_tensor_tensor(out=gt[j][:], in0=m2[j][:], scalar=mx[j][:, 3:4], in1=gt[j][:], op0=ALU.mult, op1=ALU.add), reads=[G], writes=[G])
                    kb.dma('sp', self.GATES[t0:t0 + 128, :], gt[j][:], reads=[G], writes=['GATES%d' % ti])

    def stage_ffnpass(self, w1, w3, w2, gate_col, first):
        nc, kb = self.nc, self.kb
        with Stage(kb) as st:
            w1b = st.sb("w1b", [128, 8, 1408], BF16)
            w3b = st.sb("w3b", [128, 8, 1408], BF16)
            w2b = st.sb("w2b", [128, 11, 1024], BF16)
            stg = [st.sb("stg%d" % j, [128, 1408], F32) for j in range(3)]
            n = 0
            for (dst, src, nk, key) in ((w1b, w1, 8, 'w1b'), (w3b, w3, 8, 'w3b'), (w2b, w2, 11, 'w2b')):
                width = src.shape[1]
                for k in range(nk):
                    j = n % 3
                    n += 1
                    kb.dma('sp', stg[j][:, 0:width], src[k * 128:(k + 1) * 128, :], writes=['stg%d' % j])
                    if n % 2:
                        kb.op('act', lambda dst=dst, k=k, j=j, width=width: nc.scalar.copy(out=dst[:, k, :], in_=stg[j][:, 0:width]),
                              reads=['stg%d' % j, key], writes=[key])
                    else:
                        kb.op('dve', lambda dst=dst, k=k, j=j, width=width: nc.vector.tensor_copy(out=dst[:, k, :], in_=stg[j][:, 0:width]),
                              reads=['stg%d' % j, key], writes=[key])
            hT = [st.sb("hT%d" % j, [128, 8, 512], BF16) for j in range(2)]
            act = [st.sb("act%d" % j, [128, 11, 512], BF16) for j in range(2)]
            sg = [st.sb("sg%d" % j, [128, 512], F32) for j in range(2)]
            pA = [st.ps("pA%d" % j, [128, 512], F32) for j in range(2)]
            pB = [st.ps("pB%d" % j, [128, 512], F32) for j in range(2)]
            pY = [st.ps("pY%d" % j, [128, 512], F32) for j in range(2)]
            yo = [st.sb("yo%d" % j, [128, 1024], F32) for j in range(2)]
            ya = [st.sb("ya%d" % j, [128, 1024], F32) for j in range(2)]
            gtl = [st.sb("gtl%d" % j, [128, 8], F32) for j in range(2)]
            nf = 0
            ny = 0
            for it, (t0, W) in enumerate(tiles_of(NT, 512)):
                j = it % 2
                kb.dma('sp', hT[j][:, :, 0:W], self.HT[:, :, t0:t0 + W], reads=['HT%d' % q for q in range(t0 // 128, (t0 + W) // 128)],
                       writes=['hT%d' % j])
                for f in range(11):
                    jf = nf % 2
                    nf += 1

                    def mmA(jf=jf, f=f, j=j, W=W, wb=w1b, pp=pA):
                        for k in range(8):
                            ins = nc.tensor.matmul(pp[jf][:, 0:W], lhsT=wb[:, k, f * 128:(f + 1) * 128], rhs=hT[j][:, k, 0:W],
                                                   start=(k == 0), stop=(k == 7))
                        return ins
                    kb.op('pe', mmA, reads=['hT%d' % j, 'w1b'], writes=['pA%d' % jf])
                    kb.op('pe', lambda jf=jf, f=f, j=j, W=W: mmA(jf, f, j, W, w3b, pB), reads=['hT%d' % j, 'w3b'], writes=['pB%d' % jf])
                    kb.op('act', lambda jf=jf, W=W: nc.scalar.activation(out=sg[jf][:, 0:W], in_=pA[jf][:, 0:W], func=AF.Silu),
                          reads=['pA%d' % jf], writes=['sg%d' % jf])
                    kb.op('dve', lambda jf=jf, f=f, j=j, W=W: nc.vector.tensor_tensor(out=act[j][:, f, 0:W], in0=sg[jf][:, 0:W], in1=pB[jf][:, 0:W], op=ALU.mult),
                          reads=['sg%d' % jf, 'pB%d' % jf, 'act%d' % j], writes=['act%d' % j])
                for sub in range(W // 128):
                    jy = ny % 2
                    ny += 1
                    ti = t0 // 128 + sub
                    ts = t0 + sub * 128
                    if not first:
                        kb.dma('sp', ya[jy][:], self.ACC[ts:ts + 128, :], reads=['ACC%d' % ti], writes=['ya%d' % jy])
                    if gate_col is not None:
                        kb.dma('sp', gtl[jy][:], self.GATES[ts:ts + 128, :], reads=['GATES%d' % ti], writes=['gtl%d' % jy])
                    for half in range(2):
                        jp = half

                        def mmY(jp=jp, half=half, sub=sub, j=j):
                            for f in range(11):
                                ins = nc.tensor.matmul(pY[jp][:], lhsT=act[j][:, f, sub * 128:(sub + 1) * 128],
                                                       rhs=w2b[:, f, half * 512:(half + 1) * 512], start=(f == 0), stop=(f == 10))
                            return ins
                        kb.op('pe', mmY, reads=['act%d' % j, 'w2b'], writes=['pY%d' % jp])
                        osl = slice(half * 512, (half + 1) * 512)
                        rd = ['pY%d' % jp, 'yo%d' % jy]
                        if gate_col is not None and not first:
                            kb.op('dve', lambda jp=jp, jy=jy, osl=osl: nc.vector.scalar_tensor_tensor(
                                out=yo[jy][:, osl], in0=pY[jp][:], scalar=gtl[jy][:, gate_col:gate_col + 1], in1=ya[jy][:, osl],
                                op0=ALU.mult, op1=ALU.add), reads=rd + ['gtl%d' % jy, 'ya%d' % jy], writes=['yo%d' % jy])
                        elif gate_col is not None:
                            kb.op('dve', lambda jp=jp, jy=jy, osl=osl: nc.vector.tensor_scalar(
                                out=yo[jy][:, osl], in0=pY[jp][:], scalar1=gtl[jy][:, gate_col:gate_col + 1], scalar2=None, op0=ALU.mult),
                                reads=rd + ['gtl%d' % jy], writes=['yo%d' % jy])
                        elif not first:
                            kb.op('dve', lambda jp=jp, jy=jy, osl=osl: nc.vector.tensor_tensor(
                                out=yo[jy][:, osl], in0=pY[jp][:], in1=ya[jy][:, osl], op=ALU.add),
                                reads=rd + ['ya%d' % jy], writes=['yo%d' % jy])
                        else:
                            kb.op('act', lambda jp=jp, jy=jy, osl=osl: nc.scalar.copy(out=yo[jy][:, osl], in_=pY[jp][:]),
                                  reads=rd, writes=['yo%d' % jy])
                    kb.dma('sp', self.ACC[ts:ts + 128, :], yo[jy][:], reads=['yo%d' % jy], writes=['ACC%d' % ti])

    def stage_finish(self, i, sub, out_final=None):
        nc, kb = self.nc, self.kb
        with Stage(kb) as st:
            gs = 2 if sub == 1 else 5
            md = self.mod_tiles(st, i, [gs])
            lng = self.bcast_load(st, "lng", self.ln_g[i, sub - 1, :], 1024)
            lnb = self.bcast_load(st, "lnb", self.ln_b[i, sub - 1, :], 1024)
            NB = 2
            xt = [st.sb("xt%d" % j, [128, 1024], F32) for j in range(NB)]
            at = [st.sb("at%d" % j, [128, 1024], F32) for j in range(NB)]
            stt = [st.sb("stt%d" % j, [128, 2, 6], F32) for j in range(NB)]
            mv = [st.sb("mv%d" % j, [128, 4], F32) for j in range(NB)]
            for ti in range(NT // 128):
                j = ti % NB
                isc = 1 if ti < TC // 128 else 0
                t0 = ti * 128
                X, A, S = 'xt%d' % j, 'at%d' % j, 'st%d' % j
                kb.dma('sp', xt[j][:], self.XS[t0:t0 + 128, :], writes=[X])
                kb.dma('sp', at[j][:], self.ACC[t0:t0 + 128, :], reads=['ACC%d' % ti], writes=[A])
                kb.op('dve', lambda j=j, isc=isc: nc.vector.tensor_tensor(out=at[j][:], in0=at[j][:], in1=md[(gs, isc)][:], op=ALU.mult),
                      reads=[A, 'mod%d_%d' % (gs, isc)], writes=[A])
                kb.op('dve', lambda j=j: nc.vector.scalar_tensor_tensor(out=xt[j][:], in0=xt[j][:], scalar=float(ALPHA), in1=at[j][:], op0=ALU.mult, op1=ALU.add),
                      reads=[X, A], writes=[X])

                def bn(j=j):
                    nc.vector.bn_stats(out=stt[j][:, 0, :], in_=xt[j][:, 0:512])
                    return nc.vector.bn_stats(out=stt[j][:, 1, :], in_=xt[j][:, 512:1024])
                kb.op('dve', bn, reads=[X, S], writes=[S])
                kb.op('dve', lambda j=j: nc.vector.bn_aggr(out=mv[j][:, 0:2], in_=stt[j][:]), reads=[S], writes=[S])
                kb.op('dve', lambda j=j: nc.vector.tensor_scalar(out=mv[j][:, 2:3], in0=mv[j][:, 1:2], scalar1=LN_EPS, scalar2=None, op0=ALU.add), reads=[S], writes=[S])
                kb.op('act', lambda j=j: nc.scalar.activation(out=mv[j][:, 2:3], in_=mv[j][:, 2:3], func=AF.Sqrt), reads=[S], writes=[S])
                kb.op('dve', lambda j=j: nc.vector.reciprocal(out=mv[j][:, 3:4], in_=mv[j][:, 2:3]), reads=[S], writes=[S])
                kb.op('dve', lambda j=j: nc.vector.tensor_scalar(out=xt[j][:], in0=xt[j][:], scalar1=mv[j][:, 0:1], scalar2=mv[j][:, 3:4],
                                                             op0=ALU.subtract, op1=ALU.mult), reads=[X, S], writes=[X])
                kb.op('dve', lambda j=j: nc.vector.tensor_tensor(out=xt[j][:], in0=xt[j][:], in1=lng[:], op=ALU.mult), reads=[X, 'lng'], writes=[X])
                kb.op('dve', lambda j=j: nc.vector.tensor_tensor(out=xt[j][:], in0=xt[j][:], in1=lnb[:], op=ALU.add), reads=[X, 'lnb'], writes=[X])
                kb.dma('sp', self.XS[t0:t0 + 128, :], xt[j][:], reads=[X], writes=['XS%d' % ti])
                if out_final is not None and ti >= TC // 128:
                    kb.dma('sp', out_final[t0 - TC:t0 - TC + 128, :], xt[j][:], reads=[X], writes=['OUT%d' % ti])

    def stage_pool(self, j_layer):
        nc, kb = self.nc, self.kb
        with Stage(kb) as st:
            pw = st.sb("pw", [128, 4, 2, 256], BF16)
            pst = [st.sb("pst%d" % j, [128, 256], F32) for j in range(2)]
            n = 0
            for gi in range(4):
                for kk in range(2):
                    j = n % 2
                    n += 1
                    kb.dma('sp', pst[j][:], self.pool_w[j_layer, gi, kk * 128:(kk + 1) * 128, :], writes=['pst%d' % j])
                    kb.op('dve', lambda gi=gi, kk=kk, j=j: nc.vector.tensor_copy(out=pw[:, gi, kk, :], in_=pst[j][:]), reads=['pst%d' % j, 'pw'], writes=['pw'])
            psc = self.bcast_load(st, "psc", self.pool_scale[j_layer, :], 1024)
            invc = st.sb("invc", [128, 4, 64], F32)
            kb.dma('sp', invc[:], self.cst_invc64, writes=['invc'])
            invcc = st.sb("invcc", [128, 4, 256], F32)
            kb.dma('sp', invcc[:], self.cst_invc256, writes=['invcc'])
            hT = [st.sb("hT%d" % j, [128, 8, 512], BF16) for j in range(2)]
            P = st.sb("P", [128, 8, 8, 80], F32)
            S2 = st.sb("S2", [128, 8, 8, 80], F32)
            S4 = st.sb("S4", [128, 8, 8, 80], F32)
            S8 = st.sb("S8", [128, 8, 8, 80], F32)
            S16 = st.sb("S16", [128, 8, 8, 80], F32)
            pl = [st.sb("pl%d" % j, [128, 8, 512], BF16) for j in range(2)]
            tmp = st.sb("tmp", [128, 2, 8, 64], F32)
            pY = [st.ps("pY%d" % j, [128, 1024], F32) for j in range(2)]
            yo = [st.sb("yo%d" % j, [128, 1024], F32) for j in range(2)]
            for b_ in (P, S2, S4, S8, S16):
                pass
            kb.op('pool', lambda: nc.gpsimd.memset(P[:], 0.0), writes=['P'])
            ny = 0
            for it, (t0, W) in enumerate([(0, 256)] + tiles_of(NT, 512, 256)):
                j = it % 2
                isc = (t0 == 0)
                kb.dma('sp', hT[j][:, :, 0:W], self.HT[:, :, t0:t0 + W], reads=['HT%d' % q for q in range(t0 // 128, (t0 + W) // 128)],
                       writes=['hT%d' % j])
                if isc:
                    Pv = P[:].rearrange("p k r c -> p k (r c)")[:, :, 0:272].rearrange("p k (r c) -> p k r c", r=1)
                    views = [b_[:].rearrange("p k r c -> p k (r c)")[:, :, 0:272].rearrange("p k (r c) -> p k r c", r=1) for b_ in (P, S2, S4, S8, S16)]
                    L = 256
                    R = 1
                else:
                    if it == 1:
                        kb.op('pool', lambda: nc.gpsimd.memset(P[:], 0.0), reads=['P'], writes=['P'])
                    views = [b_[:] for b_ in (P, S2, S4, S8, S16)]
                    L = 64
                    R = 8
                Pv, S2v, S4v, S8v, S16v = views
                LP = L + 16
                kb.op('act', lambda Pv=Pv, j=j, L=L, R=R, W=W: nc.scalar.copy(out=Pv[:, :, :, 8:8 + L], in_=hT[j][:, :, 0:W].rearrange("p k (r c) -> p k r c", r=R)),
                      reads=['hT%d' % j, 'P'], writes=['P'])
                kb.op('dve', lambda: nc.vector.tensor_tensor(out=S2v[:, :, :, 0:LP - 1], in0=Pv[:, :, :, 0:LP - 1], in1=Pv[:, :, :, 1:LP], op=ALU.add), reads=['P', 'S2'], writes=['S2'])
                kb.op('dve', lambda: nc.vector.tensor_tensor(out=S4v[:, :, :, 0:LP - 3], in0=S2v[:, :, :, 0:LP - 3], in1=S2v[:, :, :, 2:LP - 1], op=ALU.add), reads=['S2', 'S4'], writes=['S4'])
                kb.op('dve', lambda: nc.vector.tensor_tensor(out=S8v[:, :, :, 0:LP - 7], in0=S4v[:, :, :, 0:LP - 7], in1=S4v[:, :, :, 4:LP - 3], op=ALU.add), reads=['S4', 'S8'], writes=['S8'])
                kb.op('dve', lambda: nc.vector.tensor_tensor(out=S16v[:, :, :, 0:LP - 15], in0=S8v[:, :, :, 0:LP - 15], in1=S8v[:, :, :, 8:LP - 7], op=ALU.add), reads=['S8', 'S16'], writes=['S16'])
                for gi, (win, Sv, key) in enumerate(((2, S2v, 'S2'), (4, S4v, 'S4'), (8, S8v, 'S8'), (16, S16v, 'S16'))):
                    o = 8 - win // 2
                    ic = (invcc if isc else invc)
                    icv = ic[:, gi, :].unsqueeze(1).unsqueeze(1).to_broadcast([128, 2, R, L])
                    tv = tmp[:].rearrange("p k r c -> p k (r c)")[:, :, 0:W].rearrange("p k (r c) -> p k r c", r=R)
                    kb.op('dve', lambda Sv=Sv, gi=gi, o=o, icv=icv, tv=tv, L=L: nc.vector.tensor_tensor(out=tv, in0=Sv[:, 2 * gi:2 * gi + 2, :, o:o + L], in1=icv, op=ALU.mult),
                          reads=[key, 'invc', 'invcc', 'tmp'], writes=['tmp'])
                    kb.op('dve', lambda gi=gi, tv=tv, j=j, Pv=Pv, L=L, R=R, W=W: nc.vector.tensor_tensor(
                        out=pl[j][:, 2 * gi:2 * gi + 2, 0:W].rearrange("p k (r c) -> p k r c", r=R), in0=tv, in1=Pv[:, 2 * gi:2 * gi + 2, :, 8:8 + L], op=ALU.subtract),
                        reads=['tmp', 'P', 'pl%d' % j], writes=['pl%d' % j])
                for sub in range(W // 128):
                    jy = ny % 2
                    ny += 1
                    ti = t0 // 128 + sub
                    ts = t0 + sub * 128

                    def mm(jy=jy, sub=sub, j=j):
                        for gi in range(4):
                            for kk in range(2):
                                ins = nc.tensor.matmul(pY[jy][:, gi * 256:(gi + 1) * 256], lhsT=pl[j][:, 2 * gi + kk, sub * 128:(sub + 1) * 128],
                                                       rhs=pw[:, gi, kk, :], start=(kk == 0), stop=(kk == 1))
                        return ins
                    kb.op('pe', mm, reads=['pl%d' % j, 'pw'], writes=['pY%d' % jy])
                    kb.op('dve', lambda jy=jy: nc.vector.tensor_tensor(out=yo[jy][:], in0=pY[jy][:], in1=psc[:], op=ALU.mult), reads=['pY%d' % jy, 'psc', 'yo%d' % jy], writes=['yo%d' % jy])
                    kb.dma('sp', self.ACC[ts:ts + 128, :], yo[jy][:], reads=['yo%d' % jy], writes=['ACC%d' % ti])

    def scan_consts(self, st, z):
        kb = self.kb
        c = {}
        for nm in ('SB', 'SBI', 'SBT', 'BLK'):
            t = st.sb(nm, [128, 128], F32)
            kb.dma('sp', t[:], self.cst_masks[z, {'SB': 0, 'SBI': 1, 'SBT': 2, 'BLK': 3}[nm]], writes=[nm])
            c[nm] = t
        idf = st.sb("identf", [128, 128], F32)
        kb.dma('sp', idf[:], self.cst_identf, writes=['identf'])
        idb = st.sb("identb", [128, 128], BF16)
        kb.op('dve', lambda: self.nc.vector.tensor_copy(out=idb[:], in_=idf[:]), reads=['identf'], writes=['identb'])
        c['identf'] = idf
        c['identb'] = idb
        return c

    def head_scan(self, st, C, hid, dk, dv, pb, KAP, RM, BCt, KCt, V, KAPT, BPT, KPT, RMT, Dx, Di, DxT, wc, T, yout, opk, z, bufs, slot=0, ndt=BF16):
        nc, kb = self.nc, self.kb
        B = bufs
        par = slot
        ps = B['ps']

        def nps():
            return ps[slot], 'hps%d' % slot
        sfx = '_%d' % par

        def S(name):
            return B[name][par], name + sfx
        idf, idb = C['identf'], C['identb']

        def mm_ev(name, lhsT, rhs, mul=None, mulkey=None, eng='dve', rows=128, cols=128, extra=None, reads=()):
            p, pk = nps()
            dst, dk_ = S(name)

            def f():
                ins = nc.tensor.matmul(p[0:rows, 0:cols], lhsT=lhsT, rhs=rhs, start=True, stop=(extra is None))
                if extra is not None:
                    for qi, (l2, r2) in enumerate(extra):
                        ins = nc.tensor.matmul(p[0:rows, 0:cols], lhsT=l2, rhs=r2, start=False, stop=(qi == len(extra) - 1))
                return ins
            kb.op('pe', f, reads=list(reads), writes=[pk])
            if mul is not None:
                kb.op('dve', lambda: nc.vector.tensor_tensor(out=dst[0:rows, 0:cols], in0=p[0:rows, 0:cols], in1=mul, op=ALU.mult),
                      reads=[pk, mulkey, dk_], writes=[dk_])
            elif eng == 'act':
                kb.op('act', lambda: nc.scalar.copy(out=dst[0:rows, 0:cols], in_=p[0:rows, 0:cols]), reads=[pk, dk_], writes=[dk_])
            else:
                kb.op('dve', lambda: nc.vector.tensor_copy(out=dst[0:rows, 0:cols], in_=p[0:rows, 0:cols]), reads=[pk, dk_], writes=[dk_])
            return dst, dk_
        OK = list(opk)
        N_, Nk = mm_ev('N', BPT, KAPT, mul=Dx[0], mulkey=Dx[1], reads=OK)
        yield
        NT_, NTk = mm_ev('NT', KAPT, BPT, mul=DxT[0], mulkey=DxT[1], reads=OK)
        yield
        BtT, BtTk = mm_ev('BtT', KPT, KAPT, mul=Dx[0], mulkey=Dx[1], reads=OK)
        yield
        AbT, AbTk = mm_ev('AbT', BPT, RMT, mul=Di[0], mulkey=Di[1], reads=OK)
        yield
        AkT, AkTk = mm_ev('AkT', KPT, RMT, mul=Di[0], mulkey=Di[1], reads=OK)
        yield
        R_, Rk = S('R')
        kb.op('dve', lambda: nc.vector.tensor_tensor(out=R_[:], in0=N_[:], in1=idf[:], op=ALU.add), reads=[Nk, 'identf', Rk], writes=[Rk])
        yield
        P, Pk, PT, PTk = N_, Nk, NT_, NTk
        for it in range(5):
            PT2, PT2k = mm_ev('PT%d' % (it % 2), P[:], PT[:], eng='act', reads=[Pk, PTk])
            yield
            if it < 4:
                P2, P2k = mm_ev('P%d' % (it % 2), PT[:], P[:], eng='act', reads=[Pk, PTk])
                yield
            R2_, R2k = S('R' if it % 2 else 'Rb')
            Rn, Rnk = mm_ev('Rb' if it % 2 == 0 else 'R', (idb if ndt == BF16 else idf)[:], R_[:], extra=[(PT2[:], R_[:])], eng='dve', reads=['identb', 'identf', PT2k, Rk])
            yield
            R_, Rk = Rn, Rnk
            if it < 4:
                P, Pk, PT, PTk = P2, P2k, PT2, PT2k
        if ndt == BF16:
            MTb, MTbk = R_, Rk
        else:
            MTb, MTbk = S('MTb')
            kb.op('act', lambda: nc.scalar.copy(out=MTb[:], in_=R_[:]), reads=[Rk, MTbk], writes=[MTbk])
            yield
        X0, X0k = mm_ev('X0', BtT[:], V, cols=dv, reads=[BtTk] + OK)
        yield
        U0, U0k = mm_ev('U0', MTb[:], X0[:, 0:dv], cols=dv, eng='act', reads=[MTbk, X0k])
        yield
        MK, MKk = mm_ev('MK', MTb[:], KAP, cols=dk, reads=[MTbk] + OK)
        yield
        RstT, RstTk = mm_ev('RstT', MK[:, 0:dk], AbT[:], rows=dk, extra=[(RM, idb[:])], eng='act', reads=[MKk, AbTk, 'identb'] + OK)
        yield
        Y0, Y0k = mm_ev('Y0', AbT[:], U0[:, 0:dv], cols=dv, extra=[(AkT[:], V)], reads=[AbTk, U0k, AkTk] + OK)
        yield
        Phi = {}
        Z0 = {}
        for c in range(2):
            rs = slice(c * 64, (c + 1) * 64)
            p, pk = nps()
            kb.op('pe', lambda p=p, rs=rs: nc.tensor.matmul(p[0:dk, 0:dk], lhsT=MK[rs, 0:dk], rhs=BCt[rs, :], start=True, stop=True), reads=[MKk] + OK, writes=[pk])
            yield
            dst, dkey = S('Phi%d' % c)
            kb.op('dve', lambda p=p, dst=dst, c=c: nc.vector.scalar_tensor_tensor(out=dst[0:dk, 0:dk], in0=idf[0:dk, 0:dk], scalar=wc[0][:, c:c + 1], in1=p[0:dk, 0:dk],
                                                                               op0=ALU.mult, op1=ALU.add), reads=[pk, 'identf', wc[1], dkey], writes=[dkey])
            Phi[c] = (dst, dkey)
            Z0[c] = mm_ev('Z0%d' % c, BCt[rs, :], U0[rs, 0:dv], rows=dk, cols=dv, extra=[(KCt[rs, :], V[rs, :])], eng='act', reads=[U0k] + OK)
            yield
        Tt, Tk = T
        for c in ((0, 1) if z == 0 else (1, 0)):
            rs = slice(c * 64, (c + 1) * 64)
            p, pk = nps()
            kb.op('pe', lambda p=p: nc.tensor.matmul(p[:, 0:dv], lhsT=RstT[0:dk, :], rhs=Tt[:], start=True, stop=True), reads=[RstTk, Tk], writes=[pk])
            yield
            kb.op('dve', lambda p=p, rs=rs: nc.vector.tensor_tensor(out=yout[0][rs, :], in0=p[rs, 0:dv], in1=Y0[rs, 0:dv], op=ALU.add), reads=[pk, Y0k, yout[1]], writes=[yout[1]])
            yield
            p2, pk2 = nps()

            def tm(p2=p2, c=c):
                nc.tensor.matmul(p2[0:dk, 0:dv], lhsT=Phi[c][0][0:dk, 0:dk], rhs=Tt[:], start=True, stop=False)
                return nc.tensor.matmul(p2[0:dk, 0:dv], lhsT=idf[0:dk, 0:dk], rhs=Z0[c][0][0:dk, 0:dv], start=False, stop=True)
            kb.op('pe', tm, reads=[Phi[c][1], Z0[c][1], Tk, 'identf'], writes=[pk2])
            yield
            kb.op('act', lambda p2=p2: nc.scalar.copy(out=Tt[:], in_=p2[0:dk, 0:dv]), reads=[pk2, Tk], writes=[Tk])
            yield

    def run_heads(self, gens):
        gens = list(gens)
        active = {}
        free = list(range(NSLOT))
        while gens or active:
            while gens and free:
                sl = free.pop(0)
                active[sl] = gens.pop(0)(sl)
            for sl in list(active.keys()):
                try:
                    next(active[sl])
                except StopIteration:
                    del active[sl]
                    free.append(sl)

    def head_bufs(self, st, ndt=BF16):
        B = {}
        for nm, dt in (('N', ndt), ('NT', ndt), ('R', ndt), ('Rb', ndt), ('P0', ndt), ('P1', ndt), ('PT0', ndt), ('PT1', ndt),
                       ('BtT', BF16), ('AbT', BF16), ('AkT', BF16), ('MTb', BF16), ('X0', BF16), ('U0', BF16), ('MK', BF16),
                       ('RstT', F32), ('Y0', F32), ('Phi0', F32), ('Phi1', F32), ('Z00', F32), ('Z01', F32)):
            B[nm] = [st.sb("%s_%d" % (nm, q), [128, 128], dt) for q in range(NSLOT)]
        B['ps'] = [st.ps("hps%d" % q, [128, 512], F32) for q in range(NSLOT)]
        return B

    def scan_order(self, z):
        nt = NT // 128
        nct = TC // 128
        if z == 0:
            return list(range(nt))
        return list(range(nct - 1, -1, -1)) + list(range(nt - 1, nct - 1, -1))

    def stage_rwkv_proj(self, jl):
        nc, kb = self.nc, self.kb
        with Stage(kb) as st:
            NCOL = 3072 + 384
            Wb = st.sb("Wb", [128, 8, NCOL], BF16)
            stg = [st.sb("stg%d" % j, [128, 1024], F32) for j in range(2)]
            n = 0
            srcs = [(self.rk_w_rkv[jl, 0], 0, 1024), (self.rk_w_rkv[jl, 1], 1024, 1024), (self.rk_w_rkv[jl, 2], 2048, 1024),
                    (self.rk_w1[jl, 0], 3072, 64), (self.rk_w1[jl, 1], 3136, 64), (self.rk_a1[jl, 0], 3200, 64), (self.rk_a1[jl, 1], 3264, 64),
                    (self.rk_g1[jl], 3328, 128)]
            for (src, c0, wd) in srcs:
                for k in range(8):
                    j = n % 2
                    n += 1
                    kb.dma('sp', stg[j][:, 0:wd], src[k * 128:(k + 1) * 128, :], writes=['stg%d' % j])
                    kb.op('act' if n % 2 else 'dve', (lambda k=k, j=j, c0=c0, wd=wd: nc.scalar.copy(out=Wb[:, k, c0:c0 + wd], in_=stg[j][:, 0:wd])) if n % 2 else
                          (lambda k=k, j=j, c0=c0, wd=wd: nc.vector.tensor_copy(out=Wb[:, k, c0:c0 + wd], in_=stg[j][:, 0:wd])), reads=['stg%d' % j, 'Wb'], writes=['Wb'])
            W2 = st.sb("W2", [128, 3, 1024], BF16)
            for q, srcl in enumerate(([self.rk_w2[jl, 0], self.rk_w2[jl, 1]], [self.rk_a2[jl, 0], self.rk_a2[jl, 1]], [self.rk_g2[jl]])):
                j = q % 2
                r0 = 0
                for si, src in enumerate(srcl):
                    rws = src.shape[0]
                    kb.dma('sp', stg[j][r0:r0 + rws, :], src, reads=['stg%d' % j] if si else [], writes=['stg%d' % j])
                    r0 += rws
                kb.op('dve', lambda q=q, j=j: nc.vector.tensor_copy(out=W2[:, q, :], in_=stg[j][:]), reads=['stg%d' % j, 'W2'], writes=['W2'])
            mu = st.sb("mu", [128, 6, 8], F32)
            for m in range(6):
                kb.dma('sp', mu[:, m, :], self.rk_mu[jl, m, :].rearrange("(k p) -> p k", p=128), reads=['mu'] if m else [], writes=['mu'], allow_slow_non_contiguous=True)
            HW_ = 512 + 128
            ht = st.sb("ht", [128, 8, HW_], BF16)
            xx = st.sb("xx", [128, 8, 512], BF16)
            xm = st.sb("xm", [128, 6, 8, 512], BF16)
            l1 = st.sb("l1", [128, 3, 512], BF16)
            pp = [st.ps("pp%d" % q, [128, 512], F32) for q in range(4)]
            ob = [st.sb("ob%d" % q, [128, 1024], F32) for q in range(4)]
            npp = [0]
            nob = [0]
            tiles = [(0, TC)] + tiles_of(NT, 512, TC)
            for it, (t0, W) in enumerate(tiles):
                isc = (t0 == 0)
                lo = t0 - 64
                hi = t0 + W + 64
                zl = isc or t0 == TC
                zr = isc or (t0 + W == NT)
                if zl:
                    kb.op('dve', lambda: nc.vector.memset(ht[:, :, 0:64], 0.0), reads=['ht'], writes=['ht'])
                if zr:
                    kb.op('dve', lambda W=W: nc.vector.memset(ht[:, :, 64 + W:128 + W], 0.0), reads=['ht'], writes=['ht'])
                a = t0 if zl else lo
                b = t0 + W if zr else hi
                kb.dma('sp', ht[:, :, 64 + (a - t0):64 + (b - t0)], self.HT[:, :, a:b], reads=['HT%d' % q for q in range(a // 128, (b + 127) // 128)] + ['ht'], writes=['ht'])
                for k in range(8):
                    if isc:
                        off = 63 if k < 4 else 65
                    else:
                        off = (63, 63, 65, 65, 0, 0, 128, 128)[k]
                    kb.op('dve', lambda k=k, off=off, W=W: nc.vector.tensor_tensor(out=xx[:, k, 0:W], in0=ht[:, k, off:off + W], in1=ht[:, k, 64:64 + W], op=ALU.subtract),
                          reads=['ht', 'xx'], writes=['xx'])
                    if not isc and k < 4:
                        col = 0 if k < 2 else 63
                        kb.op('dve', lambda k=k, col=col, W=W: nc.vector.tensor_scalar(
                            out=xx[:, k, 0:W].rearrange("p (r c) -> p r c", c=64)[:, :, col], in0=ht[:, k, 64:64 + W].rearrange("p (r c) -> p r c", c=64)[:, :, col],
                            scalar1=-1.0, scalar2=None, op0=ALU.mult), reads=['ht', 'xx'], writes=['xx'])
                for m in range(6):
                    for k in range(8):
                        eng = 'dve'
                        E = nc.vector if eng == 'dve' else nc.gpsimd
                        kb.op(eng, lambda m=m, k=k, W=W, E=E: E.scalar_tensor_tensor(out=xm[:, m, k, 0:W], in0=xx[:, k, 0:W], scalar=mu[:, m, k:k + 1], in1=ht[:, k, 64:64 + W],
                                                                                 op0=ALU.mult, op1=ALU.add), reads=['xx', 'ht', 'mu', 'xm%d' % m], writes=['xm%d' % m])
                for c, (m, fn) in enumerate(((1, AF.Tanh), (4, AF.Copy), (5, AF.Sigmoid))):
                    q = npp[0] % 4
                    npp[0] += 1

                    def f(q=q, c=c, m=m, W=W):
                        for k in range(8):
                            ins = nc.tensor.matmul(pp[q][:, 0:W], lhsT=Wb[:, k, 3072 + c * 128:3072 + (c + 1) * 128], rhs=xm[:, m, k, 0:W], start=(k == 0), stop=(k == 7))
                        return ins
                    kb.op('pe', f, reads=['Wb', 'xm%d' % m], writes=['pp%d' % q])
                    kb.op('act', lambda q=q, c=c, fn=fn, W=W: nc.scalar.activation(out=l1[:, c, 0:W], in_=pp[q][:, 0:W], func=fn), reads=['pp%d' % q, 'l1'], writes=['l1'])
                for sub in range(W // 128):
                    ts = t0 + sub * 128
                    ti = ts // 128
                    ss = slice(sub * 128, (sub + 1) * 128)
                    outs = [('RR', 0, 0, None), ('KK0', 2, 1024, None), ('VV', 3, 2048, None),
                            ('DL0', None, 0, (0, 0, 64)), ('DL1', None, 0, (0, 64, 128)), ('AL0', None, 1, (1, 0, 64)), ('AL1', None, 1, (1, 64, 128)), ('GG', None, 2, (2, 0, 128))]
                    for (nm, m, c0, l2) in outs:
                        o = nob[0] % 4
                        nob[0] += 1
                        for half in range(2):
                            q = npp[0] % 4
                            npp[0] += 1
                            if l2 is None:
                                def f(q=q, m=m, c0=c0, half=half, ss=ss):
                                    for k in range(8):
                                        ins = nc.tensor.matmul(pp[q][:], lhsT=xm[:, m, k, ss], rhs=Wb[:, k, c0 + half * 512:c0 + (half + 1) * 512], start=(k == 0), stop=(k == 7))
                                    return ins
                                kb.op('pe', f, reads=['Wb', 'xm%d' % m], writes=['pp%d' % q])
                            else:
                                c, r0, r1 = l2
                                kb.op('pe', lambda q=q, c=c, r0=r0, r1=r1, half=half, ss=ss: nc.tensor.matmul(pp[q][:], lhsT=l1[r0:r1, c, ss], rhs=W2[r0:r1, c, half * 512:(half + 1) * 512],
                                                                                                           start=True, stop=True), reads=['l1', 'W2'], writes=['pp%d' % q])
                            if half == 0:
                                kb.op('act', lambda q=q, o=o: nc.scalar.copy(out=ob[o][:, 0:512], in_=pp[q][:]), reads=['pp%d' % q, 'ob%d' % o], writes=['ob%d' % o])
                            else:
                                kb.op('dve', lambda q=q, o=o: nc.vector.tensor_copy(out=ob[o][:, 512:1024], in_=pp[q][:]), reads=['pp%d' % q, 'ob%d' % o], writes=['ob%d' % o])
                        kb.dma('sp', self.RK[nm][ts:ts + 128, :], ob[o][:], reads=['ob%d' % o], writes=['%s%d' % (nm, ti)])

    def stage_rwkv_scan(self, jl, z):
        nc, kb = self.nc, self.kb
        with Stage(kb) as st:
            C = self.scan_consts(st, z)
            B = self.head_bufs(st)
            w0 = self.bcast_load(st, "w0", self.rk_w0[jl, z, :], 1024)
            a0 = self.bcast_load(st, "a0", self.rk_a0[jl, z, :], 1024)
            k_k = self.bcast_load(st, "k_k", self.rk_k_k[jl, :], 1024)
            k_a = self.bcast_load(st, "k_a", self.rk_k_a[jl, :], 1024)
            T = [st.sb("T%d" % h, [64, 64], F32) for h in range(16)]
            for h in range(16):
                kb.op('dve', lambda h=h: nc.vector.memset(T[h][:], 0.0), writes=['T%d' % h])
            ld = {nm: st.sb("ld_" + nm, [128, 1024], F32) for nm in ('RR', 'KK0', 'VV', 'DL', 'AL')}
            f = {nm: st.sb("f_" + nm, [128, 1024], F32) for nm in ('sw', 'a', 'kk', 'kd', 'b', 'eG', 'enG', 'eGx', 'eRev', 'eTot', 'Gs', 't1')}
            sm = st.sb("sm", [128, 3, 16], F32)
            tb = {nm: st.sb("tb_" + nm, [128, 1024], BF16) for nm in ('KAP', 'RM', 'BC', 'KC', 'V', 'BP', 'KP')}
            fT = {nm: st.sb("fT_" + nm, [128, 8, 128], BF16) for nm in ('KAP', 'BP', 'KP', 'RM')}
            eTT = st.sb("eTT", [128, 8, 128], F32)
            wc = st.sb("wc", [64, 16, 2], F32)
            pG = [st.ps("pG%d" % q, [128, 512], F32) for q in range(2)]
            yt = st.sb("yt", [128, 1024], F32)
            DEC = 0.6065306597126334
            for ti in self.scan_order(z):
                t0 = ti * 128
                for nm, src in (('RR', self.RK['RR']), ('KK0', self.RK['KK0']), ('VV', self.RK['VV']), ('DL', self.RK['DL%d' % z]), ('AL', self.RK['AL%d' % z])):
                    kb.dma('sp', ld[nm][:], src[t0:t0 + 128, :], reads=['%s%d' % (nm if nm in ('RR', 'KK0', 'VV') else nm + str(z), ti)], writes=['ld_' + nm])
                V_ = nc.vector
                kb.op('dve', lambda: V_.tensor_tensor(out=f['t1'][:], in0=ld['DL'][:], in1=w0[:], op=ALU.add), reads=['ld_DL', 'w0', 'f_t1'], writes=['f_t1'])
                kb.op('act', lambda: nc.scalar.activation(out=f['sw'][:], in_=f['t1'][:], func=AF.Sigmoid), reads=['f_t1', 'f_sw'], writes=['f_sw'])
                kb.op('dve', lambda: V_.tensor_tensor(out=f['t1'][:], in0=ld['AL'][:], in1=a0[:], op=ALU.add), reads=['ld_AL', 'a0', 'f_t1'], writes=['f_t1'])
                kb.op('act', lambda: nc.scalar.activation(out=f['a'][:], in_=f['t1'][:], func=AF.Sigmoid), reads=['f_t1', 'f_a'], writes=['f_a'])
                kb.op('dve', lambda: V_.tensor_tensor(out=f['kk'][:], in0=ld['KK0'][:], in1=k_k[:], op=ALU.mult), reads=['ld_KK0', 'k_k', 'f_kk'], writes=['f_kk'])
                kb.op('dve', lambda: V_.tensor_tensor(out=f['t1'][:], in0=f['kk'][:], in1=f['kk'][:], op=ALU.mult), reads=['f_kk', 'f_t1'], writes=['f_t1'])
                kb.op('dve', lambda: V_.tensor_reduce(out=sm[:, 0, :], in_=f['t1'][:].rearrange("p (h n) -> p h n", n=64), axis=AX.X, op=ALU.add), reads=['f_t1', 'sm'], writes=['sm'])
                kb.op('act', lambda: nc.scalar.activation(out=sm[:, 1, :], in_=sm[:, 0, :], func=AF.Sqrt), reads=['sm'], writes=['sm'])
                kb.op('dve', lambda: V_.tensor_scalar(out=sm[:, 1, :], in0=sm[:, 1, :], scalar1=1e-12, scalar2=None, op0=ALU.max), reads=['sm'], writes=['sm'])
                kb.op('dve', lambda: V_.reciprocal(out=sm[:, 2, :], in_=sm[:, 1, :]), reads=['sm'], writes=['sm'])
                kb.op('dve', lambda: V_.tensor_tensor(out=f['kk'][:].rearrange("p (h n) -> p h n", n=64), in0=f['kk'][:].rearrange("p (h n) -> p h n", n=64),
                                                      in1=sm[:, 2, :].unsqueeze(2).to_broadcast([128, 16, 64]), op=ALU.mult), reads=['sm', 'f_kk'], writes=['f_kk'])
                kb.op('dve', lambda: V_.scalar_tensor_tensor(out=f['t1'][:], in0=f['a'][:], scalar=-1.0, in1=k_a[:], op0=ALU.add, op1=ALU.mult), reads=['f_a', 'k_a', 'f_t1'], writes=['f_t1'])
                kb.op('dve', lambda: V_.scalar_tensor_tensor(out=f['kd'][:], in0=f['t1'][:], scalar=1.0, in1=ld['KK0'][:], op0=ALU.add, op1=ALU.mult), reads=['f_t1', 'ld_KK0', 'f_kd'], writes=['f_kd'])
                kb.op('dve', lambda: V_.tensor_tensor(out=f['b'][:], in0=f['kk'][:], in1=f['a'][:], op=ALU.mult), reads=['f_kk', 'f_a', 'f_b'], writes=['f_b'])
                for half in range(2):
                    hs = slice(half * 512, (half + 1) * 512)
                    kb.op('pe', lambda hs=hs: nc.tensor.matmul(pG[0][:], lhsT=C['SBI'][:], rhs=f['sw'][:, hs], start=True, stop=True), reads=['SBI', 'f_sw'], writes=['pG0'])
                    kb.op('pe', lambda hs=hs: nc.tensor.matmul(pG[1][:], lhsT=C['BLK'][:], rhs=f['sw'][:, hs], start=True, stop=True), reads=['BLK', 'f_sw'], writes=['pG1'])
                    kb.op('act', lambda hs=hs: nc.scalar.activation(out=f['eG'][:, hs], in_=pG[0][:], func=AF.Exp, scale=-DEC), reads=['pG0', 'f_eG'], writes=['f_eG'])
                    kb.op('act', lambda hs=hs: nc.scalar.activation(out=f['enG'][:, hs], in_=pG[0][:], func=AF.Exp, scale=DEC), reads=['pG0', 'f_enG'], writes=['f_enG'])
                    kb.op('dve', lambda hs=hs: V_.tensor_copy(out=f['Gs'][:, hs], in_=pG[0][:]), reads=['pG0', 'f_Gs'], writes=['f_Gs'])
                    kb.op('act', lambda hs=hs: nc.scalar.activation(out=f['eTot'][:, hs], in_=pG[1][:], func=AF.Exp, scale=-DEC), reads=['pG1', 'f_eTot'], writes=['f_eTot'])
                    kb.op('dve', lambda hs=hs: V_.tensor_tensor(out=f['eRev'][:, hs], in0=pG[1][:], in1=f['Gs'][:, hs], op=ALU.subtract), reads=['pG1', 'f_Gs', 'f_eRev'], writes=['f_eRev'])
                kb.op('act', lambda: nc.scalar.activation(out=f['eRev'][:], in_=f['eRev'][:], func=AF.Exp, scale=-DEC), reads=['f_eRev'], writes=['f_eRev'])
                kb.op('dve', lambda: V_.tensor_tensor(out=f['eGx'][:], in0=f['Gs'][:], in1=f['sw'][:], op=ALU.subtract), reads=['f_Gs', 'f_sw', 'f_eGx'], writes=['f_eGx'])
                kb.op('act', lambda: nc.scalar.activation(out=f['eGx'][:], in_=f['eGx'][:], func=AF.Exp, scale=-DEC), reads=['f_eGx'], writes=['f_eGx'])
                kb.op('dve', lambda: V_.scalar_tensor_tensor(out=tb['KAP'][:], in0=f['kk'][:], scalar=-1.0, in1=f['eGx'][:], op0=ALU.mult, op1=ALU.mult), reads=['f_kk', 'f_eGx', 'tb_KAP'], writes=['tb_KAP'])
                kb.op('dve', lambda: nc.vector.tensor_tensor(out=tb['RM'][:], in0=ld['RR'][:], in1=f['eG'][:], op=ALU.mult), reads=['ld_RR', 'f_eG', 'tb_RM'], writes=['tb_RM'])
                kb.op('dve', lambda: V_.tensor_tensor(out=tb['BP'][:], in0=f['b'][:], in1=f['enG'][:], op=ALU.mult), reads=['f_b', 'f_enG', 'tb_BP'], writes=['tb_BP'])
                kb.op('dve', lambda: nc.vector.tensor_tensor(out=tb['KP'][:], in0=f['kd'][:], in1=f['enG'][:], op=ALU.mult), reads=['f_kd', 'f_enG', 'tb_KP'], writes=['tb_KP'])
                kb.op('dve', lambda: V_.tensor_tensor(out=tb['BC'][:], in0=f['b'][:], in1=f['eRev'][:], op=ALU.mult), reads=['f_b', 'f_eRev', 'tb_BC'], writes=['tb_BC'])
                kb.op('dve', lambda: nc.vector.tensor_tensor(out=tb['KC'][:], in0=f['kd'][:], in1=f['eRev'][:], op=ALU.mult), reads=['f_kd', 'f_eRev', 'tb_KC'], writes=['tb_KC'])
                kb.op('act', lambda: nc.scalar.copy(out=tb['V'][:], in_=ld['VV'][:]), reads=['ld_VV', 'tb_V'], writes=['tb_V'])
                for nm in ('KAP', 'BP', 'KP', 'RM'):
                    pT = pG[0][:].bitcast(BF16)[:, 0:1024].rearrange("p (k t) -> p k t", t=128)

                    def tr(nm=nm, pT=pT):
                        for k in range(8):
                            ins = nc.tensor.transpose(pT[:, k, :], tb[nm][:, k * 128:(k + 1) * 128], C['identb'][:])
                        return ins
                    kb.op('pe', tr, reads=['tb_' + nm, 'identb'], writes=['pG0'])
                    kb.op('act', lambda nm=nm, pT=pT: nc.scalar.copy(out=fT[nm][:], in_=pT), reads=['pG0', 'fT_' + nm], writes=['fT_' + nm])
                for half in range(2):
                    pT = pG[1][:].rearrange("p (k t) -> p k t", t=128)

                    def tr2(half=half, pT=pT):
                        for k in range(4):
                            kk_ = half * 4 + k
                            ins = nc.tensor.transpose(pT[:, k, :], f['eTot'][:, kk_ * 128:(kk_ + 1) * 128], C['identf'][:])
                        return ins
                    kb.op('pe', tr2, reads=['f_eTot', 'identf'], writes=['pG1'])
                    kb.op('dve', lambda half=half, pT=pT: V_.tensor_copy(out=eTT[:, half * 4:(half + 1) * 4, :], in_=pT), reads=['pG1', 'eTT'], writes=['eTT'])
                for par in range(2):
                    kb.op('dve', lambda par=par: V_.tensor_copy(out=wc[:, par::2, :], in_=eTT[par * 64:(par + 1) * 64, :, 0:128:64]), reads=['eTT', 'wc'], writes=['wc'])
                opk = ['tb_KAP', 'tb_RM', 'tb_BC', 'tb_KC', 'tb_V', 'fT_KAP', 'fT_BP', 'fT_KP', 'fT_RM']
                gens = []
                for h in range(16):
                    def mk(h):
                        cs = slice(h * 64, (h + 1) * 64)
                        pb = (h % 2) * 64
                        k8 = h // 2
                        return lambda sl: self.head_scan(st, C, h, 64, 64, pb, tb['KAP'][:, cs], tb['RM'][:, cs], tb['BC'][:, cs], tb['KC'][:, cs], tb['V'][:, cs],
                                   fT['KAP'][pb:pb + 64, k8, :], fT['BP'][pb:pb + 64, k8, :], fT['KP'][pb:pb + 64, k8, :], fT['RM'][pb:pb + 64, k8, :],
                                   (C['SB'][:], 'SB'), (C['SBI'][:], 'SBI'), (C['SBT'][:], 'SBT'), (wc[:, h, :], 'wc'), (T[h], 'T%d' % h), (yt[:, cs], 'yt%d' % h), opk, z, B, sl)
                    gens.append(mk(h))
                self.run_heads(gens)
                kb.dma('sp', self.YZ[z][t0:t0 + 128, :], yt[:], reads=['yt%d' % h for h in range(16)], writes=['YZ%d_%d' % (z, ti)])

    def stage_rwkv_out(self, jl):
        nc, kb = self.nc, self.kb
        with Stage(kb) as st:
            V_ = nc.vector
            idf = st.sb("identf", [128, 128], F32)
            kb.dma('sp', idf[:], self.cst_identf, writes=['identf'])
            idb = st.sb("identb", [128, 128], BF16)
            kb.op('dve', lambda: V_.tensor_copy(out=idb[:], in_=idf[:]), reads=['identf'], writes=['identb'])
            wo = st.sb("wo", [128, 8, 1024], BF16)
            stg = [st.sb("stg%d" % j, [128, 1024], F32) for j in range(2)]
            for k in range(8):
                j = k % 2
                kb.dma('sp', stg[j][:], self.rk_w_o[jl, k * 128:(k + 1) * 128, :], writes=['stg%d' % j])
                kb.op('dve', lambda k=k, j=j: V_.tensor_copy(out=wo[:, k, :], in_=stg[j][:]), reads=['stg%d' % j, 'wo'], writes=['wo'])
            bc = {}
            for nm, src in (('a00', self.rk_a0[jl, 0, :]), ('a01', self.rk_a0[jl, 1, :]), ('k_a', self.rk_k_a[jl, :]), ('r_k', self.rk_r_k[jl].rearrange("h n -> (h n)")),
                            ('lg', self.rk_lnx_g[jl, :]), ('lb', self.rk_lnx_b[jl, :])):
                bc[nm] = self.bcast_load(st, nm, src, 1024)
            ld = {nm: st.sb("ld_" + nm, [128, 1024], F32) for nm in ('Y0', 'Y1', 'RR', 'KK0', 'VV', 'AL0', 'AL1', 'GG')}
            t1 = st.sb("t1", [128, 1024], F32)
            t2 = st.sb("t2", [128, 1024], F32)
            zb = st.sb("zb", [128, 1024], BF16)
            zT = st.sb("zT", [128, 8, 128], BF16)
            sm = st.sb("sm", [128, 4, 16], F32)
            pT = st.ps("pT", [128, 512], F32)
            pO = [st.ps("pO%d" % q, [128, 512], F32) for q in range(2)]
            yo = st.sb("yo", [128, 1024], F32)
            H3 = lambda ap: ap.rearrange("p (h n) -> p h n", n=64)
            B3 = lambda ap: ap.unsqueeze(2).to_broadcast([128, 16, 64])
            for ti in range(NT // 128):
                t0 = ti * 128
                for nm, src, key in (('Y0', self.YZ[0], 'YZ0_%d' % ti), ('Y1', self.YZ[1], 'YZ1_%d' % ti), ('RR', self.RK['RR'], 'RR%d' % ti), ('KK0', self.RK['KK0'], 'KK0%d' % ti),
                                     ('VV', self.RK['VV'], 'VV%d' % ti), ('AL0', self.RK['AL0'], 'AL0%d' % ti), ('AL1', self.RK['AL1'], 'AL1%d' % ti), ('GG', self.RK['GG'], 'GG%d' % ti)):
                    kb.dma('sp', ld[nm][:], src[t0:t0 + 128, :], reads=[key], writes=['ld_' + nm])
                kb.op('dve', lambda: V_.tensor_tensor(out=t1[:], in0=ld['Y0'][:], in1=ld['Y1'][:], op=ALU.add), reads=['ld_Y0', 'ld_Y1', 't1'], writes=['t1'])
                kb.op('dve', lambda: V_.tensor_reduce(out=sm[:, 0, :], in_=H3(t1[:]), axis=AX.X, op=ALU.add), reads=['t1', 'sm'], writes=['sm'])
                kb.op('dve', lambda: V_.tensor_scalar(out=sm[:, 0, :], in0=sm[:, 0, :], scalar1=1.0 / 64, scalar2=None, op0=ALU.mult), reads=['sm'], writes=['sm'])
                kb.op('dve', lambda: V_.tensor_tensor(out=H3(t1[:]), in0=H3(t1[:]), in1=B3(sm[:, 0, :]), op=ALU.subtract), reads=['sm', 't1'], writes=['t1'])
                kb.op('dve', lambda: V_.tensor_tensor(out=t2[:], in0=t1[:], in1=t1[:], op=ALU.mult), reads=['t1', 't2'], writes=['t2'])
                kb.op('dve', lambda: V_.tensor_reduce(out=sm[:, 1, :], in_=H3(t2[:]), axis=AX.X, op=ALU.add), reads=['t2', 'sm'], writes=['sm'])
                kb.op('dve', lambda: V_.tensor_scalar(out=sm[:, 1, :], in0=sm[:, 1, :], scalar1=1.0 / 64, scalar2=64e-5, op0=ALU.mult, op1=ALU.add), reads=['sm'], writes=['sm'])
                kb.op('act', lambda: nc.scalar.activation(out=sm[:, 1, :], in_=sm[:, 1, :], func=AF.Sqrt), reads=['sm'], writes=['sm'])
                kb.op('dve', lambda: V_.reciprocal(out=sm[:, 2, :], in_=sm[:, 1, :]), reads=['sm'], writes=['sm'])
                kb.op('dve', lambda: V_.tensor_tensor(out=H3(t1[:]), in0=H3(t1[:]), in1=B3(sm[:, 2, :]), op=ALU.mult), reads=['sm', 't1'], writes=['t1'])
                kb.op('dve', lambda: V_.tensor_tensor(out=t1[:], in0=t1[:], in1=bc['lg'][:], op=ALU.mult), reads=['t1', 'lg'], writes=['t1'])
                kb.op('dve', lambda: V_.tensor_tensor(out=t1[:], in0=t1[:], in1=bc['lb'][:], op=ALU.add), reads=['t1', 'lb'], writes=['t1'])
                kb.op('dve', lambda: V_.tensor_tensor(out=t2[:], in0=ld['AL0'][:], in1=bc['a00'][:], op=ALU.add), reads=['ld_AL0', 'a00', 't2'], writes=['t2'])
                kb.op('act', lambda: nc.scalar.activation(out=t2[:], in_=t2[:], func=AF.Sigmoid), reads=['t2'], writes=['t2'])
                kb.op('dve', lambda: V_.tensor_tensor(out=ld['AL1'][:], in0=ld['AL1'][:], in1=bc['a01'][:], op=ALU.add), reads=['ld_AL1', 'a01'], writes=['ld_AL1'])
                kb.op('act', lambda: nc.scalar.activation(out=ld['AL1'][:], in_=ld['AL1'][:], func=AF.Sigmoid), reads=['ld_AL1'], writes=['ld_AL1'])
                kb.op('dve', lambda: V_.tensor_tensor(out=t2[:], in0=t2[:], in1=ld['AL1'][:], op=ALU.add), reads=['t2', 'ld_AL1'], writes=['t2'])
                kb.op('dve', lambda: V_.scalar_tensor_tensor(out=t2[:], in0=t2[:], scalar=-2.0, in1=bc['k_a'][:], op0=ALU.add, op1=ALU.mult), reads=['t2', 'k_a'], writes=['t2'])
                kb.op('dve', lambda: V_.scalar_tensor_tensor(out=t2[:], in0=t2[:], scalar=2.0, in1=ld['KK0'][:], op0=ALU.add, op1=ALU.mult), reads=['t2', 'ld_KK0'], writes=['t2'])
                kb.op('dve', lambda: V_.tensor_tensor(out=t2[:], in0=t2[:], in1=ld['RR'][:], op=ALU.mult), reads=['t2', 'ld_RR'], writes=['t2'])
                kb.op('dve', lambda: V_.tensor_tensor(out=t2[:], in0=t2[:], in1=bc['r_k'][:], op=ALU.mult), reads=['t2', 'r_k'], writes=['t2'])
                kb.op('dve', lambda: V_.tensor_reduce(out=sm[:, 3, :], in_=H3(t2[:]), axis=AX.X, op=ALU.add), reads=['t2', 'sm'], writes=['sm'])
                kb.op('dve', lambda: V_.tensor_tensor(out=H3(t2[:]), in0=H3(ld['VV'][:]), in1=B3(sm[:, 3, :]), op=ALU.mult), reads=['sm', 'ld_VV', 't2'], writes=['t2'])
                kb.op('dve', lambda: V_.tensor_tensor(out=t1[:], in0=t1[:], in1=t2[:], op=ALU.add), reads=['t1', 't2'], writes=['t1'])
                kb.op('dve', lambda: V_.tensor_tensor(out=zb[:], in0=t1[:], in1=ld['GG'][:], op=ALU.mult), reads=['t1', 'ld_GG', 'zb'], writes=['zb'])
                pTv = pT[:].bitcast(BF16)[:, 0:1024].rearrange("p (k t) -> p k t", t=128)

                def tr(pTv=pTv):
                    for k in range(8):
                        ins = nc.tensor.transpose(pTv[:, k, :], zb[:, k * 128:(k + 1) * 128], idb[:])
                    return ins
                kb.op('pe', tr, reads=['zb', 'identb'], writes=['pT'])
                kb.op('act', lambda pTv=pTv: nc.scalar.copy(out=zT[:], in_=pTv), reads=['pT', 'zT'], writes=['zT'])
                for half in range(2):
                    def mm(half=half):
                        for k in range(8):
                            ins = nc.tensor.matmul(pO[half][:], lhsT=zT[:, k, :], rhs=wo[:, k, half * 512:(half + 1) * 512], start=(k == 0), stop=(k == 7))
                        return ins
                    kb.op('pe', mm, reads=['zT', 'wo'], writes=['pO%d' % half])
                    kb.op('act' if half else 'dve', (lambda half=half: nc.scalar.copy(out=yo[:, 512:1024], in_=pO[1][:])) if half else
                          (lambda half=half: V_.tensor_copy(out=yo[:, 0:512], in_=pO[0][:])), reads=['pO%d' % half, 'yo'], writes=['yo'])
                kb.dma('sp', self.ACC[t0:t0 + 128, :], yo[:], reads=['yo'], writes=['ACC%d' % ti])

    def stage_rwkv(self, i, jl):
        self.stage_rwkv_proj(jl)
        for z in range(2):
            self.stage_rwkv_scan(jl, z)
        self.stage_rwkv_out(jl)

    def stage_gdn_proj(self, jl):
        nc, kb = self.nc, self.kb
        V_ = nc.vector
        for blk in range(4):
            with Stage(kb) as st:
                ntap = 4 if blk < 3 else 1
                ncol = 1024 if blk < 3 else 1056
                c0 = blk * 1024
                Wc = st.sb("Wc", [128, ntap, 8, ncol], BF16)
                stg = [st.sb("stg%d" % j, [128, 1056], F32) for j in range(2)]
                cw = [self.bcast_load(st, "cw%d" % tp, self.gdn_conv_w[jl, tp, c0:c0 + 1024], 1024) for tp in range(ntap)] if blk < 3 else None
                for k in range(8):
                    j = k % 2
                    kb.dma('sp', stg[j][:, 0:ncol], self.gdn_w_in[jl, k * 128:(k + 1) * 128, c0:c0 + ncol], writes=['stg%d' % j])
                    for tp in range(ntap):
                        if blk < 3:
                            kb.op('dve', lambda k=k, j=j, tp=tp: V_.tensor_tensor(out=Wc[:, tp, k, :], in0=stg[j][:, 0:1024], in1=cw[tp][:], op=ALU.mult), reads=['stg%d' % j, 'cw%d' % tp, 'Wc'], writes=['Wc'])
                        else:
                            kb.op('dve', lambda k=k, j=j: V_.tensor_copy(out=Wc[:, 0, k, :], in_=stg[j][:, 0:ncol]), reads=['stg%d' % j, 'Wc'], writes=['Wc'])
                ht = st.sb("ht", [128, 8, 640], BF16)
                pp = [st.ps("pp%d" % q, [128, 512], F32) for q in range(4)]
                ob = [st.sb("ob%d" % q, [128, 1056], F32) for q in range(2)]
                sq = st.sb("sq", [128, 1024], F32)
                sm = st.sb("sm", [128, 2, 8], F32)
                npp = [0]
                nob = [0]
                for it, (t0, W) in enumerate([(0, TC)] + tiles_of(NT, 512, TC)):
                    isc = (t0 == 0)
                    zl = isc or t0 == TC
                    zr = isc or (t0 + W == NT)
                    if zl:
                        kb.op('dve', lambda: V_.memset(ht[:, :, 0:64], 0.0), reads=['ht'], writes=['ht'])
                    if zr:
                        kb.op('dve', lambda W=W: V_.memset(ht[:, :, 64 + W:128 + W], 0.0), reads=['ht'], writes=['ht'])
                    a = t0 if zl else t0 - 64
                    b = t0 + W if zr else t0 + W + 64
                    kb.dma('sp', ht[:, :, 64 + (a - t0):64 + (b - t0)], self.HT[:, :, a:b], reads=['HT%d' % q for q in range(a // 128, (b + 127) // 128)] + ['ht'], writes=['ht'])
                    for sub in range(W // 128):
                        ts = t0 + sub * 128
                        ti = ts // 128
                        o = nob[0] % 2
                        nob[0] += 1
                        segs = [(0, 512), (512, 1024)] + ([(1024, 1056)] if blk == 3 else [])
                        for (s0, s1) in segs:
                            q = npp[0] % 4
                            npp[0] += 1

                            def f(q=q, s0=s0, s1=s1, sub=sub):
                                n_ = ntap * 8
                                i_ = 0
                                for tp in range(ntap):
                                    off = 64 + sub * 128 + (tp - 2 if blk < 3 else 0)
                                    for k in range(8):
                                        ins = nc.tensor.matmul(pp[q][:, 0:s1 - s0], lhsT=ht[:, k, off:off + 128], rhs=Wc[:, tp, k, s0:s1], start=(i_ == 0), stop=(i_ == n_ - 1))
                                        i_ += 1
                                return ins
                            kb.op('pe', f, reads=['ht', 'Wc'], writes=['pp%d' % q])
                            if blk < 3:
                                kb.op('act', lambda q=q, o=o, s0=s0, s1=s1: nc.scalar.activation(out=ob[o][:, s0:s1], in_=pp[q][:, 0:s1 - s0], func=AF.Silu), reads=['pp%d' % q, 'ob%d' % o], writes=['ob%d' % o])
                            else:
                                kb.op('act', lambda q=q, o=o, s0=s0, s1=s1: nc.scalar.copy(out=ob[o][:, s0:s1], in_=pp[q][:, 0:s1 - s0]), reads=['pp%d' % q, 'ob%d' % o], writes=['ob%d' % o])
                        if blk < 2:
                            O3 = ob[o][:, 0:1024].rearrange("p (h n) -> p h n", n=128)
                            kb.op('dve', lambda o=o: V_.tensor_tensor(out=sq[:], in0=ob[o][:, 0:1024], in1=ob[o][:, 0:1024], op=ALU.mult), reads=['ob%d' % o, 'sq'], writes=['sq'])
                            kb.op('dve', lambda: V_.tensor_reduce(out=sm[:, 0, :], in_=sq[:].rearrange("p (h n) -> p h n", n=128), axis=AX.X, op=ALU.add), reads=['sq', 'sm'], writes=['sm'])
                            kb.op('dve', lambda: V_.tensor_scalar(out=sm[:, 0, :], in0=sm[:, 0, :], scalar1=1e-6, scalar2=None, op0=ALU.add), reads=['sm'], writes=['sm'])
                            kb.op('act', lambda: nc.scalar.activation(out=sm[:, 0, :], in_=sm[:, 0, :], func=AF.Sqrt), reads=['sm'], writes=['sm'])
                            kb.op('dve', lambda: V_.reciprocal(out=sm[:, 1, :], in_=sm[:, 0, :]), reads=['sm'], writes=['sm'])
                            if blk == 0:
                                kb.op('dve', lambda: V_.tensor_scalar(out=sm[:, 1, :], in0=sm[:, 1, :], scalar1=128 ** -0.5, scalar2=None, op0=ALU.mult), reads=['sm'], writes=['sm'])
                            kb.op('dve', lambda O3=O3: V_.tensor_tensor(out=O3, in0=O3, in1=sm[:, 1, :].unsqueeze(2).to_broadcast([128, 8, 128]), op=ALU.mult), reads=['sm', 'ob%d' % o], writes=['ob%d' % o])
                        nm = ('GQ', 'GK', 'GV', 'GZ')[blk]
                        kb.dma('sp', self.GD[nm][ts:ts + 128, :], ob[o][:, 0:1024], reads=['ob%d' % o], writes=['%s%d' % (nm, ti)])
                        if blk == 3:
                            kb.dma('sp', self.GD['GAB'][ts:ts + 128, :], ob[o][:, 1024:1056], reads=['ob%d' % o], writes=['GAB%d' % ti])

    def stage_gdn_scan(self, jl, z):
        nc, kb = self.nc, self.kb
        V_ = nc.vector
        with Stage(kb) as st:
            C = self.scan_consts(st, z)
            B = self.head_bufs(st, F32)
            ones = st.sb("ones", [128, 128], F32)
            kb.op('dve', lambda: V_.memset(ones[:], 1.0), writes=['ones'])
            alog = self.bcast_load(st, "alog", self.gdn_a_log[jl, z, :], 8)
            dtb = self.bcast_load(st, "dtb", self.gdn_dt_bias[jl, z, :], 8)
            kb.op('act', lambda: nc.scalar.activation(out=alog[:], in_=alog[:], func=AF.Exp), reads=['alog'], writes=['alog'])
            T = [st.sb("T%d" % h, [128, 128], F32) for h in range(8)]
            for h in range(8):
                kb.op('dve', lambda h=h: V_.memset(T[h][:], 0.0), writes=['T%d' % h])
            ld = {nm: st.sb("ld_" + nm, [128, 1024], F32) for nm in ('GQ', 'GK', 'GV')}
            ab = st.sb("ab", [128, 32], F32)
            s8 = {nm: st.sb("s8_" + nm, [128, 8], F32) for nm in ('g', 'beta', 'G', 'Gx', 'eGx', 'eG', 'eRev', 'bk', 'bkg', 'bkr', 't', 'eTot', 'nG')}
            tb = {nm: st.sb("tb_" + nm, [128, 1024], BF16) for nm in ('KAP', 'RM', 'BC', 'KC', 'V', 'KU', 'BU', 'KD', 'QU')}
            fT = {nm: st.sb("fT_" + nm, [128, 8, 128], BF16) for nm in ('KU', 'BU', 'KD', 'QU')}
            wc = st.sb("wc", [128, 8, 2], F32)
            pG = [st.ps("pG%d" % q, [128, 512], F32) for q in range(2)]
            dg = st.sb("dg", [128, 128], F32)
            Dm = [{nm: st.sb("D_%s%d" % (nm, h), [128, 128], F32) for nm in ('x', 'i', 'xT')} for h in range(8)]
            yt = st.sb("yt", [128, 1024], F32)
            H3 = lambda ap: ap.rearrange("p (h n) -> p h n", n=128)
            B3 = lambda ap: ap.unsqueeze(2).to_broadcast([128, 8, 128])
            S8 = ['s8']
            for ti in self.scan_order(z):
                t0 = ti * 128
                for nm in ('GQ', 'GK', 'GV'):
                    kb.dma('sp', ld[nm][:], self.GD[nm][t0:t0 + 128, :], reads=['%s%d' % (nm, ti)], writes=['ld_' + nm])
                kb.dma('sp', ab[:], self.GD['GAB'][t0:t0 + 128, :], reads=['GAB%d' % ti], writes=['ab'])
                kb.op('dve', lambda: V_.tensor_tensor(out=s8['t'][:], in0=ab[:, z * 8:(z + 1) * 8], in1=dtb[:], op=ALU.add), reads=['ab', 'dtb'] + S8, writes=S8)
                kb.op('act', lambda: nc.scalar.activation(out=s8['t'][:], in_=s8['t'][:], func=AF.Exp), reads=S8, writes=S8)
                kb.op('act', lambda: nc.scalar.activation(out=s8['t'][:], in_=s8['t'][:], func=AF.Ln, bias=1.0), reads=S8, writes=S8)
                kb.op('dve', lambda: V_.scalar_tensor_tensor(out=s8['g'][:], in0=s8['t'][:], scalar=-1.0, in1=alog[:], op0=ALU.mult, op1=ALU.mult), reads=S8 + ['alog'], writes=S8)
                kb.op('act', lambda: nc.scalar.activation(out=s8['beta'][:], in_=ab[:, 16 + z * 8:16 + (z + 1) * 8], func=AF.Sigmoid), reads=['ab'] + S8, writes=S8)
                kb.op('pe', lambda: nc.tensor.matmul(pG[0][:, 0:8], lhsT=C['SBI'][:], rhs=s8['g'][:], start=True, stop=True), reads=['SBI'] + S8, writes=['pG0'])
                kb.op('pe', lambda: nc.tensor.matmul(pG[1][:, 0:8], lhsT=C['BLK'][:], rhs=s8['g'][:], start=True, stop=True), reads=['BLK'] + S8, writes=['pG1'])
                kb.op('dve', lambda: V_.tensor_copy(out=s8['G'][:], in_=pG[0][:, 0:8]), reads=['pG0'] + S8, writes=S8)
                kb.op('dve', lambda: V_.tensor_tensor(out=s8['Gx'][:], in0=s8['G'][:], in1=s8['g'][:], op=ALU.subtract), reads=S8, writes=S8)
                kb.op('dve', lambda: V_.tensor_tensor(out=s8['eRev'][:], in0=pG[1][:, 0:8], in1=s8['G'][:], op=ALU.subtract), reads=['pG1'] + S8, writes=S8)
                kb.op('act', lambda: nc.scalar.activation(out=s8['eTot'][:], in_=pG[1][:, 0:8], func=AF.Exp), reads=['pG1'] + S8, writes=S8)
                for nm_o, nm_i in (('eRev', 'eRev'), ('eGx', 'Gx'), ('eG', 'G'), ('bkg', 'g')):
                    kb.op('act', lambda nm_o=nm_o, nm_i=nm_i: nc.scalar.activation(out=s8[nm_o][:], in_=s8[nm_i][:], func=AF.Exp), reads=S8, writes=S8)
                kb.op('dve', lambda: V_.tensor_scalar(out=s8['nG'][:], in0=s8['G'][:], scalar1=-1.0, scalar2=None, op0=ALU.mult), reads=S8, writes=S8)
                kb.op('dve', lambda: V_.tensor_tensor(out=s8['bkg'][:], in0=s8['bkg'][:], in1=s8['beta'][:], op=ALU.mult), reads=S8, writes=S8)
                kb.op('dve', lambda: V_.tensor_tensor(out=s8['bk'][:], in0=s8['bkg'][:], in1=s8['eRev'][:], op=ALU.mult), reads=S8, writes=S8)
                kb.op('dve', lambda: V_.tensor_tensor(out=s8['bkr'][:], in0=s8['beta'][:], in1=s8['eRev'][:], op=ALU.mult), reads=S8, writes=S8)
                kb.op('dve', lambda: V_.tensor_scalar(out=s8['eGx'][:], in0=s8['eGx'][:], scalar1=-1.0, scalar2=None, op0=ALU.mult), reads=S8, writes=S8)
                K3 = H3(ld['GK'][:])
                for nm, sc in (('KAP', 'eGx'), ('BC', 'bk'), ('KC', 'bkr'), ('BU', 'bkg'), ('KD', 'beta')):
                    kb.op('dve', lambda nm=nm, sc=sc: V_.tensor_tensor(out=H3(tb[nm][:]), in0=K3, in1=B3(s8[sc][:]), op=ALU.mult), reads=S8 + ['ld_GK', 'tb_' + nm], writes=['tb_' + nm])
                kb.op('dve', lambda: V_.tensor_scalar(out=tb['KU'][:], in0=ld['GK'][:], scalar1=-1.0, scalar2=None, op0=ALU.mult), reads=['ld_GK', 'tb_KU'], writes=['tb_KU'])
                kb.op('dve', lambda: V_.tensor_tensor(out=H3(tb['RM'][:]), in0=H3(ld['GQ'][:]), in1=B3(s8['eG'][:]), op=ALU.mult), reads=S8 + ['ld_GQ', 'tb_RM'], writes=['tb_RM'])
                kb.op('act', lambda: nc.scalar.copy(out=tb['QU'][:], in_=ld['GQ'][:]), reads=['ld_GQ', 'tb_QU'], writes=['tb_QU'])
                kb.op('act', lambda: nc.scalar.copy(out=tb['V'][:], in_=ld['GV'][:]), reads=['ld_GV', 'tb_V'], writes=['tb_V'])
                for nm in ('KU', 'BU', 'KD', 'QU'):
                    pT = pG[0][:].bitcast(BF16)[:, 0:1024].rearrange("p (k t) -> p k t", t=128)

                    def tr(nm=nm, pT=pT):
                        for k in range(8):
                            ins = nc.tensor.transpose(pT[:, k, :], tb[nm][:, k * 128:(k + 1) * 128], C['identb'][:])
                        return ins
                    kb.op('pe', tr, reads=['tb_' + nm, 'identb'], writes=['pG0'])
                    kb.op('act', lambda nm=nm, pT=pT: nc.scalar.copy(out=fT[nm][:], in_=pT), reads=['pG0', 'fT_' + nm], writes=['fT_' + nm])
                for c in range(2):
                    kb.op('pe', lambda c=c: nc.tensor.matmul(pG[1][:, 0:8], lhsT=C['BLK'][c * 64:(c + 1) * 64, c * 64:c * 64 + 1].to_broadcast([64, 128]) if False else ones[c * 64:(c + 1) * 64, :],
                                                           rhs=s8['g'][c * 64:(c + 1) * 64, :], start=True, stop=True), reads=['ones'] + S8, writes=['pG1'])
                    kb.op('act', lambda c=c: nc.scalar.activation(out=wc[:, :, c], in_=pG[1][:, 0:8], func=AF.Exp), reads=['pG1', 'wc'], writes=['wc'])
                opk = ['tb_KAP', 'tb_RM', 'tb_BC', 'tb_KC', 'tb_V', 'fT_KU', 'fT_BU', 'fT_KD', 'fT_QU']
                gens = []
                for h in range(8):
                    cs = slice(h * 128, (h + 1) * 128)
                    for (src, outs) in (('G', (('i', 'G', 'SBI', False),)), ('Gx', (('x', 'G', 'SB', False),)), ('G', (('xT', 'Gx', 'SBT', True),))):
                        kb.op('dve', lambda h=h, src=src: V_.tensor_scalar(out=dg[:], in0=C['identf'][:], scalar1=s8[src][:, h:h + 1], scalar2=None, op0=ALU.mult), reads=['identf', 'dg'] + S8, writes=['dg'])
                        kb.op('pe', lambda: nc.tensor.matmul(pG[1][:, 0:128], lhsT=ones[:], rhs=dg[:], start=True, stop=True), reads=['ones', 'dg'], writes=['pG1'])
                        for (dn, sub_, mk, flip) in outs:
                            d_ = Dm[h][dn]
                            dn = dn + str(h)
                            if not flip:
                                kb.op('dve', lambda d_=d_, h=h, sub_=sub_: V_.tensor_scalar(out=d_[:], in0=pG[1][:, 0:128], scalar1=s8[sub_][:, h:h + 1], scalar2=0.0, op0=ALU.subtract, op1=ALU.min),
                                      reads=['pG1', 'D_' + dn] + S8, writes=['D_' + dn])
                            else:
                                kb.op('dve', lambda d_=d_, h=h, sub_=sub_: V_.tensor_scalar(out=d_[:], in0=pG[1][:, 0:128], scalar1=-1.0, scalar2=s8[sub_][:, h:h + 1], op0=ALU.mult, op1=ALU.add),
                                      reads=['pG1', 'D_' + dn] + S8, writes=['D_' + dn])
                                kb.op('dve', lambda d_=d_: V_.tensor_scalar(out=d_[:], in0=d_[:], scalar1=0.0, scalar2=None, op0=ALU.min), reads=['D_' + dn], writes=['D_' + dn])
                            kb.op('act', lambda d_=d_: nc.scalar.activation(out=d_[:], in_=d_[:], func=AF.Exp), reads=['D_' + dn], writes=['D_' + dn])
                            kb.op('dve', lambda d_=d_, mk=mk: V_.tensor_tensor(out=d_[:], in0=d_[:], in1=C[mk][:], op=ALU.mult), reads=['D_' + dn, mk], writes=['D_' + dn])
                    def mk(h, cs):
                        dmh = Dm[h]
                        return lambda sl: self.head_scan(st, C, h, 128, 128, 0, tb['KAP'][:, cs], tb['RM'][:, cs], tb['BC'][:, cs], tb['KC'][:, cs], tb['V'][:, cs],
                                   fT['KU'][:, h, :], fT['BU'][:, h, :], fT['KD'][:, h, :], fT['QU'][:, h, :],
                                   (dmh['x'][:], 'D_x%d' % h), (dmh['i'][:], 'D_i%d' % h), (dmh['xT'][:], 'D_xT%d' % h), (wc[:, h, :], 'wc'), (T[h], 'T%d' % h), (yt[:, cs], 'yt%d' % h), opk, z, B, sl, F32)
                    gens.append(mk(h, cs))
                self.run_heads(gens)
                kb.dma('sp', self.YZ[z][t0:t0 + 128, :], yt[:], reads=['yt%d' % h for h in range(8)], writes=['YZ%d_%d' % (z, ti)])

    def stage_gdn_out(self, jl):
        nc, kb = self.nc, self.kb
        V_ = nc.vector
        with Stage(kb) as st:
            idf = st.sb("identf", [128, 128], F32)
            kb.dma('sp', idf[:], self.cst_identf, writes=['identf'])
            idb = st.sb("identb", [128, 128], BF16)
            kb.op('dve', lambda: V_.tensor_copy(out=idb[:], in_=idf[:]), reads=['identf'], writes=['identb'])
            wo = st.sb("wo", [128, 8, 1024], BF16)
            stg = [st.sb("stg%d" % j, [128, 1024], F32) for j in range(2)]
            for k in range(8):
                j = k % 2
                kb.dma('sp', stg[j][:], self.gdn_w_o[jl, k * 128:(k + 1) * 128, :], writes=['stg%d' % j])
                kb.op('dve', lambda k=k, j=j: V_.tensor_copy(out=wo[:, k, :], in_=stg[j][:]), reads=['stg%d' % j, 'wo'], writes=['wo'])
            nw = self.bcast_load(st, "nw", self.gdn_norm_w[jl, :], 128)
            ld = {nm: st.sb("ld_" + nm, [128, 1024], F32) for nm in ('Y0', 'Y1', 'GZ')}
            t2 = st.sb("t2", [128, 1024], F32)
            zb = st.sb("zb", [128, 1024], BF16)
            zT = st.sb("zT", [128, 8, 128], BF16)
            sm = st.sb("sm", [128, 2, 8], F32)
            pT = st.ps("pT", [128, 512], F32)
            pO = [st.ps("pO%d" % q, [128, 512], F32) for q in range(2)]
            yo = st.sb("yo", [128, 1024], F32)
            H3 = lambda ap: ap.rearrange("p (h n) -> p h n", n=128)
            for ti in range(NT // 128):
                t0 = ti * 128
                for nm, src, key in (('Y0', self.YZ[0], 'YZ0_%d' % ti), ('Y1', self.YZ[1], 'YZ1_%d' % ti), ('GZ', self.GD['GZ'], 'GZ%d' % ti)):
                    kb.dma('sp', ld[nm][:], src[t0:t0 + 128, :], reads=[key], writes=['ld_' + nm])
                kb.op('dve', lambda: V_.tensor_tensor(out=ld['Y0'][:], in0=ld['Y0'][:], in1=ld['Y1'][:], op=ALU.add), reads=['ld_Y0', 'ld_Y1'], writes=['ld_Y0'])
                kb.op('dve', lambda: V_.tensor_tensor(out=t2[:], in0=ld['Y0'][:], in1=ld['Y0'][:], op=ALU.mult), reads=['ld_Y0', 't2'], writes=['t2'])
                kb.op('dve', lambda: V_.tensor_reduce(out=sm[:, 0, :], in_=H3(t2[:]), axis=AX.X, op=ALU.add), reads=['t2', 'sm'], writes=['sm'])
                kb.op('dve', lambda: V_.tensor_scalar(out=sm[:, 0, :], in0=sm[:, 0, :], scalar1=1.0 / 128, scalar2=1e-6, op0=ALU.mult, op1=ALU.add), reads=['sm'], writes=['sm'])
                kb.op('act', lambda: nc.scalar.activation(out=sm[:, 0, :], in_=sm[:, 0, :], func=AF.Sqrt), reads=['sm'], writes=['sm'])
                kb.op('dve', lambda: V_.reciprocal(out=sm[:, 1, :], in_=sm[:, 0, :]), reads=['sm'], writes=['sm'])
                kb.op('dve', lambda: V_.tensor_tensor(out=H3(ld['Y0'][:]), in0=H3(ld['Y0'][:]), in1=sm[:, 1, :].unsqueeze(2).to_broadcast([128, 8, 128]), op=ALU.mult), reads=['sm', 'ld_Y0'], writes=['ld_Y0'])
                kb.op('dve', lambda: V_.tensor_tensor(out=H3(ld['Y0'][:]), in0=H3(ld['Y0'][:]), in1=nw[:].unsqueeze(1).to_broadcast([128, 8, 128]), op=ALU.mult), reads=['nw', 'ld_Y0'], writes=['ld_Y0'])
                kb.op('act', lambda: nc.scalar.activation(out=t2[:], in_=ld['GZ'][:], func=AF.Silu), reads=['ld_GZ', 't2'], writes=['t2'])
                kb.op('dve', lambda: V_.tensor_tensor(out=zb[:], in0=ld['Y0'][:], in1=t2[:], op=ALU.mult), reads=['ld_Y0', 't2', 'zb'], writes=['zb'])
                pTv = pT[:].bitcast(BF16)[:, 0:1024].rearrange("p (k t) -> p k t", t=128)

                def tr(pTv=pTv):
                    for k in range(8):
                        ins = nc.tensor.transpose(pTv[:, k, :], zb[:, k * 128:(k + 1) * 128], idb[:])
                    return ins
                kb.op('pe', tr, reads=['zb', 'identb'], writes=['pT'])
                kb.op('act', lambda pTv=pTv: nc.scalar.copy(out=zT[:], in_=pTv), reads=['pT', 'zT'], writes=['zT'])
                for half in range(2):
                    def mm(half=half):
                        for k in range(8):
                            ins = nc.tensor.matmul(pO[half][:], lhsT=zT[:, k, :], rhs=wo[:, k, half * 512:(half + 1) * 512], start=(k == 0), stop=(k == 7))
                        return ins
                    kb.op('pe', mm, reads=['zT', 'wo'], writes=['pO%d' % half])
                    kb.op('dve', lambda half=half: V_.tensor_copy(out=yo[:, half * 512:(half + 1) * 512], in_=pO[half][:]), reads=['pO%d' % half, 'yo'], writes=['yo'])
                kb.dma('sp', self.ACC[t0:t0 + 128, :], yo[:], reads=['yo'], writes=['ACC%d' % ti])

    def stage_gdn(self, i, jl):
        self.stage_gdn_proj(jl)
        for z in range(2):
            self.stage_gdn_scan(jl, z)
        self.stage_gdn_out(jl)

    def decls(self):
        nc, kb = self.nc, self.kb
        self.xin = self.din("xin", [NT, D])
        self.cvec = self.din("cvec", [2, D])
        self.ada_w = self.din("ada_w", [4, D, 6 * D])
        self.ada_b = self.din("ada_b", [4, 6 * D])
        self.ln_g = self.din("ln_g", [4, 2, D])
        self.ln_b = self.din("ln_b", [4, 2, D])
        self.pool_w = self.din("pool_w", [1, 4, 256, 256])
        self.pool_scale = self.din("pool_scale", [1, D])
        self.ffn_w1 = self.din("ffn_w1", [2, D, 2816])
        self.ffn_w3 = self.din("ffn_w3", [2, D, 2816])
        self.ffn_w2 = self.din("ffn_w2", [2, 2816, D])
        self.moe_router_w = self.din("moe_router_w", [2, D, 8])
        self.moe_router_b = self.din("moe_router_b", [2, 8])
        self.moe_w1 = self.din("moe_w1", [2, 8, D, 1408])
        self.moe_w3 = self.din("moe_w3", [2, 8, D, 1408])
        self.moe_w2 = self.din("moe_w2", [2, 8, 1408, D])
        for nm, shp in (("rk_mu", [2, 6, D]), ("rk_w_rkv", [2, 3, D, D]), ("rk_w0", [2, 2, D]), ("rk_w1", [2, 2, D, 64]), ("rk_w2", [2, 2, 64, D]),
                        ("rk_a0", [2, 2, D]), ("rk_a1", [2, 2, D, 64]), ("rk_a2", [2, 2, 64, D]), ("rk_g1", [2, D, 128]), ("rk_g2", [2, 128, D]),
                        ("rk_k_k", [2, D]), ("rk_k_a", [2, D]), ("rk_r_k", [2, 16, 64]), ("rk_lnx_g", [2, D]), ("rk_lnx_b", [2, D]), ("rk_w_o", [2, D, D])):
            setattr(self, nm, self.din(nm, shp))
        for nm, shp in (("gdn_w_in", [1, D, 4128]), ("gdn_conv_w", [1, 4, 3072]), ("gdn_a_log", [1, 2, 8]), ("gdn_dt_bias", [1, 2, 8]),
                        ("gdn_norm_w", [1, 128]), ("gdn_w_o", [1, D, D])):
            setattr(self, nm, self.din(nm, shp))
        self.GD = {nm: self.dscr("GD_" + nm, [NT, D]) for nm in ("GQ", "GK", "GV", "GZ")}
        self.GD["GAB"] = self.dscr("GD_GAB", [NT, 32])
        self.cst_masks = self.din("cst_masks", [2, 4, 128, 128])
        self.RK = {nm: self.dscr("RK_" + nm, [NT, D]) for nm in ("RR", "KK0", "VV", "DL0", "DL1", "AL0", "AL1", "GG")}
        self.YZ = [self.dscr("YZ%d" % z, [NT, D]) for z in range(2)]
        self.cst_identf = self.din("cst_identf", [128, 128])
        self.cst_invc64 = self.din("cst_invc64", [128, 4, 64])
        self.cst_invc256 = self.din("cst_invc256", [128, 4, 256])
        self.out = self.dout("out", [NT - TC, D])
        self.XS = self.dscr("XS", [NT, D])
        self.ACC = self.dscr("ACC", [NT, D])
        self.HT = self.dscr("HT", [128, 8, NT], BF16)
        self.GATES = self.dscr("GATES", [NT, 8])
        self.MODD = self.dscr("MODD", [4, 2, 6 * D])

    def stage_init(self):
        kb = self.kb
        for (t0, W) in tiles_of(NT, 128):
            kb.dma('sp', self.XS[t0:t0 + W, :], self.xin[t0:t0 + W, :], writes=['XSinit%d' % t0])
        kb.barrier()

    def build(self):
        nc, kb = self.nc, self.kb
        self.decls()
        self.stage_init()
        for i in self.layers:
            last = (i == self.layers[-1])
            self.stage_mod(i)
            self.stage_prep(i, 1)
            kind, jl = i % 3, i // 3
            if kind == 1:
                self.stage_pool(jl)
            elif kind == 0:
                self.stage_rwkv(i, jl)
            else:
                self.stage_gdn(i, jl)
            self.stage_finish(i, 1)
            e = i // 2
            if i % 2 == 0:
                self.stage_prep(i, 2)
                for hf in range(2):
                    sl = slice(hf * 1408, (hf + 1) * 1408)
                    self.stage_ffnpass(self.ffn_w1[e, :, sl], self.ffn_w3[e, :, sl], self.ffn_w2[e, sl, :], None, hf == 0)
            else:
                self.stage_prep(i, 2, router=e)
                for x in range(8):
                    self.stage_ffnpass(self.moe_w1[e, x], self.moe_w3[e, x], self.moe_w2[e, x], x, x == 0)
            self.stage_finish(i, 2, out_final=self.out if last else None)
        kb.barrier()
        return nc


def _pool_invc(L):
    t = np.arange(L)
    out = np.zeros((4, L), np.float32)
    for gi, w in enumerate((2, 4, 8, 16)):
        lo = np.clip(t - w // 2, 0, L)
        hi = np.clip(t + w // 2, 0, L)
        out[gi] = 1.0 / (hi - lo)
    return np.ascontiguousarray(np.broadcast_to(out[None], (128, 4, L))).astype(np.float32)


def _masks():
    s = np.arange(128)[:, None]
    t = np.arange(128)[None, :]
    same = (s // 64) == (t // 64)
    m = np.zeros((2, 4, 128, 128), np.float32)
    for z in range(2):
        sb = ((s < t) if z == 0 else (s > t)) & same
        sbi = ((s <= t) if z == 0 else (s >= t)) & same
        m[z, 0] = sb
        m[z, 1] = sbi
        m[z, 2] = sb.T
        m[z, 3] = same
    return m


def make_consts():
    return {
        "cst_masks": _masks(),
        "cst_identf": np.eye(128, dtype=np.float32),
        "cst_invc64": _pool_invc(64),
        "cst_invc256": _pool_invc(256),
    }


WEIGHT_KEYS = ["ada_w", "ada_b", "ln_g", "ln_b", "pool_w", "pool_scale", "ffn_w1", "ffn_w3", "ffn_w2",
               "moe_router_w", "moe_router_b", "moe_w1", "moe_w3", "moe_w2"]


def run(inputs, layers=(0, 1, 2, 3), n_cores=4, xin_override=None):
    prog = Prog(list(layers))
    nc = prog.build()
    print("instructions:", prog.kb.nins, "sems:", len(prog.kb.sems))
    cst = make_consts()
    in_maps = []
    for cidx in range(n_cores):
        b = cidx % 4
        m = {}
        if xin_override is not None:
            m["xin"] = xin_override[b]
        else:
            m["xin"] = np.ascontiguousarray(np.concatenate([inputs["ctx"][b], inputs["x"][b]], axis=0))
        m["cvec"] = np.ascontiguousarray(np.stack([inputs["c"][b], inputs["c_ctx"]], axis=0))
        for k in prog.inp:
            if k in m:
                continue
            m[k] = cst[k] if k in cst else np.ascontiguousarray(inputs[k])
        in_maps.append(m)
    res = run_bass_kernel_spmd(nc, in_maps, core_ids=list(range(n_cores)))
    return res


def kernel(**inputs):
    inputs = {k: np.asarray(v) for k, v in inputs.items()}
    res = run(inputs)
    out = np.stack([res.results[b]["out"] for b in range(4)], axis=0)
    return out.astype(np.float32)
```

```python
import contextlib
import numpy as np
import concourse.bass as bass
import concourse.mybir as mybir
from concourse.bass_utils import run_bass_kernel_spmd

F32 = mybir.dt.float32
BF16 = mybir.dt.bfloat16
AF = mybir.ActivationFunctionType
ALU = mybir.AluOpType
AX = mybir.AxisListType

D = 1024
NT = 8448
TC = 256
DEPTH = 4
ALPHA = (2 * DEPTH) ** 0.25
LN_EPS = 1e-5
NSLOT = 8


class KB:
    EPOCH = 30000
    NDMA = 24

    def __init__(self, nc):
        self.nc = nc
        self._ctx = []
        self.engs = {'pe': nc.tensor, 'dve': nc.vector, 'act': nc.scalar, 'pool': nc.gpsimd, 'sp': nc.sync}
        self.sems = []
        self.esem = {}
        self.ecnt = {}
        for e in ('pe', 'dve', 'act', 'pool'):
            self.esem[e] = self._newsem('c_' + e)
            self.ecnt[e] = 0
        self.dsem = [self._newsem('d%d' % i) for i in range(self.NDMA)]
        self.dcnt = [0] * self.NDMA
        self.dnext = 0
        self.waited = {e: {} for e in self.engs}
        self.res = {}
        self.nins = 0
        self.allsems = {}
        self.excl = set()

    def _newsem(self, name):
        cm = self.nc.semaphore(name + '_%d' % len(self.sems))
        h = cm.__enter__()
        self._ctx.append(cm)
        self.sems.append(h)
        return len(self.sems) - 1

    def _r(self, key):
        r = self.res.get(key)
        if r is None:
            r = {'w': None, 'r': {}}
            self.res[key] = r
        return r

    def _waits(self, eng, reads, writes):
        need = {}

        def add(tok):
            if tok is None:
                return
            s, v = tok
            if need.get(s, 0) < v:
                need[s] = v
        for k in reads:
            add(self._r(k)['w'])
        for k in writes:
            r = self._r(k)
            add(r['w'])
            for s, v in r['r'].items():
                add((s, v))
        wd = self.waited[eng]
        for s, v in need.items():
            if wd.get(s, 0) >= v:
                continue
            self.engs[eng].wait_ge(self.sems[s], v)
            self.nins += 1
            wd[s] = v

    def _mark(self, tok, reads, writes):
        s, v = tok
        self.allsems[s] = v
        for k in writes:
            r = self._r(k)
            r['w'] = tok
            r['r'] = {}
        for k in reads:
            if k in writes:
                continue
            r = self._r(k)
            if r['r'].get(s, 0) < v:
                r['r'][s] = v

    def op(self, eng, fn, reads=(), writes=()):
        ex = [k for k in reads if k in self.excl and k not in writes]
        if ex:
            writes = list(writes) + ex
        self._waits(eng, reads, writes)
        ins = fn()
        if self.ecnt[eng] >= self.EPOCH:
            self.esem[eng] = self._newsem('c_' + eng)
            self.ecnt[eng] = 0
        s = self.esem[eng]
        ins.then_inc(self.sems[s], 1)
        self.ecnt[eng] += 1
        self.nins += 1
        tok = (s, self.ecnt[eng])
        self._mark(tok, reads, writes)
        return tok

    def dma(self, q, out, in_, reads=(), writes=(), **kw):
        j = self.dnext
        self.dnext = (self.dnext + 1) % self.NDMA
        wd = self.waited[q]
        if self.dcnt[j] > 0 and wd.get(self.dsem[j], 0) < 16 * self.dcnt[j]:
            self.engs[q].wait_ge(self.sems[self.dsem[j]], 16 * self.dcnt[j])
            wd[self.dsem[j]] = 16 * self.dcnt[j]
        self._waits(q, reads, writes)
        self.engs[q].dma_start(out=out, in_=in_, **kw).then_inc(self.sems[self.dsem[j]], 16)
        self.dcnt[j] += 1
        self.nins += 1
        tok = (self.dsem[j], 16 * self.dcnt[j])
        self._mark(tok, reads, writes)
        return tok

    def barrier(self):
        for e in self.engs:
            wd = self.waited[e]
            for s, v in self.allsems.items():
                if wd.get(s, 0) >= v:
                    continue
                self.engs[e].wait_ge(self.sems[s], v)
                self.nins += 1
                wd[s] = v
        self.res = {}


class Stage:
    _n = [0]

    def __init__(self, kb):
        self.kb = kb
        self.nc = kb.nc
        self.es = contextlib.ExitStack()
        Stage._n[0] += 1
        self.sid = Stage._n[0]

    def __enter__(self):
        self.es.__enter__()
        return self

    def sb(self, name, shape, dt):
        return self.es.enter_context(self.nc.sbuf_tensor('%s_s%d' % (name, self.sid), shape, dt))

    def ps(self, name, shape, dt):
        self.kb.excl.add(name)
        return self.es.enter_context(self.nc.psum_tensor('%s_s%d' % (name, self.sid), shape, dt))

    def __exit__(self, *a):
        self.kb.barrier()
        return self.es.__exit__(*a)


def tiles_of(total, w, start=0):
    out = []
    t = start
    while t < total:
        out.append((t, min(w, total - t)))
        t += w
    return out


class Prog:
    def __init__(self, layers, debug_outs=()):
        self.layers = layers
        nc = bass.Bass("TRN2", target_bir_lowering=False)
        self.nc = nc
        self.kb = KB(nc)
        self.inp = {}
        self.debug_outs = debug_outs

    def din(self, name, shape, dt=F32):
        t = self.nc.dram_tensor(name, list(shape), dt, kind="ExternalInput").ap()
        self.inp[name] = t
        return t

    def dscr(self, name, shape, dt=F32):
        return self.nc.dram_tensor(name, list(shape), dt, kind="Internal").ap()

    def dout(self, name, shape, dt=F32):
        return self.nc.dram_tensor(name, list(shape), dt, kind="ExternalOutput").ap()

    def bcast_load(self, st, name, row_ap, n, dt=F32):
        t = st.sb(name, [128, n], dt)
        self.kb.dma('sp', t[:], row_ap.partition_broadcast(128), writes=[name])
        return t

    def stage_mod(self, i):
        nc, kb = self.nc, self.kb
        with Stage(kb) as st:
            cT = st.sb("cT", [128, 8, 2], F32)
            sT = st.sb("sT", [128, 8, 2], F32)
            modsb = st.sb("modsb", [2, 6144], F32)
            adab = st.sb("adab", [2, 6144], F32)
            wst = [st.sb("wst%d" % j, [128, 8, 512], F32) for j in range(2)]
            pm_ = [st.ps("pm%d" % j, [128, 512], F32) for j in range(2)]
            pm = [t[0:2, :] for t in pm_]
            for r in range(2):
                kb.dma('sp', cT[:, :, r], self.cvec[r, :].rearrange("(k p) -> p k", p=128),
                       reads=['cT'] if r else [], writes=['cT'], allow_slow_non_contiguous=True)
                kb.dma('sp', adab[r:r + 1, :], self.ada_b[i:i + 1, :], reads=['adab'] if r else [], writes=['adab'])
            kb.op('act', lambda: nc.scalar.activation(out=sT[:], in_=cT[:], func=AF.Silu), reads=['cT'], writes=['sT'])
            for g in range(12):
                j = g % 2
                kb.dma('sp', wst[j][:], self.ada_w[i, :, g * 512:(g + 1) * 512].rearrange("(k p) n -> p k n", p=128),
                       writes=['wst%d' % j])

                def mm(j=j):
                    for k in range(8):
                        ins = nc.tensor.matmul(pm[j][:], lhsT=sT[:, k, :], rhs=wst[j][:, k, :], start=(k == 0), stop=(k == 7))
                    return ins
                kb.op('pe', mm, reads=['sT', 'wst%d' % j], writes=['pm%d' % j])
                kb.op('dve', lambda j=j, g=g: nc.vector.tensor_tensor(out=modsb[:, g * 512:(g + 1) * 512], in0=pm[j][:],
                                                                     in1=adab[:, g * 512:(g + 1) * 512], op=ALU.add),
                      reads=['pm%d' % j, 'adab', 'modsb'], writes=['modsb'])
            for c0 in (1024, 4096):
                kb.op('dve', lambda c0=c0: nc.vector.tensor_scalar(out=modsb[:, c0:c0 + 1024], in0=modsb[:, c0:c0 + 1024],
                                                                 scalar1=1.0, scalar2=None, op0=ALU.add),
                      reads=['modsb'], writes=['modsb'])
            kb.dma('sp', self.MODD[i], modsb[:], reads=['modsb'], writes=['MODD'])

    def mod_tiles(self, st, i, slots):
        out = {}
        for s in slots:
            for r in range(2):
                out[(s, r)] = self.bcast_load(st, "mod%d_%d" % (s, r), self.MODD[i, r, s * 1024:(s + 1) * 1024], 1024)
        return out

    def stage_prep(self, i, sub, router=None):
        nc, kb = self.nc, self.kb
        with Stage(kb) as st:
            sh_s, sc_s = (0, 1) if sub == 1 else (3, 4)
            md = self.mod_tiles(st, i, [sh_s, sc_s])
            identf = st.sb("identf", [128, 128], F32)
            kb.dma('sp', identf[:], self.cst_identf, writes=['identf'])
            NB = 2
            xt = [st.sb("xt%d" % j, [128, 1024], F32) for j in range(NB)]
            hf = [st.sb("hf%d" % j, [128, 1024], F32) for j in range(NB)]
            hTb = [st.sb("hTb%d" % j, [128, 8, 128], BF16) for j in range(NB)]
            pT = [st.ps("pT%d" % j, [128, 8, 128], F32) for j in range(NB)]
            if router is not None:
                e = router
                rw = st.sb("rw", [128, 8, 8], F32)
                kb.dma('sp', rw[:], self.moe_router_w[e].rearrange("(k p) n -> p k n", p=128), writes=['rw'])
                rb = self.bcast_load(st, "rb", self.moe_router_b[e, :], 8)
                hTf = [st.sb("hTf%d" % j, [128, 8, 128], F32) for j in range(NB)]
                pl_ = [st.ps("pl%d" % j, [128, 512], F32) for j in range(NB)]
                pl = [t[:, 0:8] for t in pl_]
                lg = [st.sb("lg%d" % j, [128, 8], F32) for j in range(NB)]
                m1 = [st.sb("m1_%d" % j, [128, 8], F32) for j in range(NB)]
                m2 = [st.sb("m2_%d" % j, [128, 8], F32) for j in range(NB)]
                l2 = [st.sb("l2_%d" % j, [128, 8], F32) for j in range(NB)]
                mx = [st.sb("mx%d" % j, [128, 4], F32) for j in range(NB)]
                gt = [st.sb("gt%d" % j, [128, 8], F32) for j in range(NB)]
            for ti in range(NT // 128):
                j = ti % NB
                isc = 1 if ti < TC // 128 else 0
                t0 = ti * 128
                X, H, HB, PT = 'xt%d' % j, 'hf%d' % j, 'hTb%d' % j, 'pT%d' % j
                kb.dma('sp', xt[j][:], self.XS[t0:t0 + 128, :], writes=[X])
                kb.op('dve', lambda j=j, isc=isc: nc.vector.tensor_tensor(out=hf[j][:], in0=xt[j][:], in1=md[(sc_s, isc)][:], op=ALU.mult),
                      reads=[X, 'mod%d_%d' % (sc_s, isc)], writes=[H])
                kb.op('dve', lambda j=j, isc=isc: nc.vector.tensor_tensor(out=hf[j][:], in0=hf[j][:], in1=md[(sh_s, isc)][:], op=ALU.add),
                      reads=[H, 'mod%d_%d' % (sh_s, isc)], writes=[H])

                def tr(j=j):
                    for k in range(8):
                        ins = nc.tensor.transpose(pT[j][:, k, :], hf[j][:, k * 128:(k + 1) * 128], identf[:])
                    return ins
                kb.op('pe', tr, reads=[H, 'identf'], writes=[PT])
                if router is None:
                    kb.op('act', lambda j=j: nc.scalar.copy(out=hTb[j][:], in_=pT[j][:]), reads=[PT], writes=[HB])
                else:
                    kb.op('dve', lambda j=j: nc.vector.tensor_copy(out=hTf[j][:], in_=pT[j][:]), reads=[PT], writes=['hTf%d' % j])
                    kb.op('act', lambda j=j: nc.scalar.copy(out=hTb[j][:], in_=hTf[j][:]), reads=['hTf%d' % j], writes=[HB])
                kb.dma('sp', self.HT[:, :, t0:t0 + 128], hTb[j][:], reads=[HB], writes=['HT%d' % ti])
                import os
                RD = int(os.environ.get('RD', '9'))
                if router is not None and RD >= 1:
                    HF, PL, LG = 'hTf%d' % j, 'pl%d' % j, 'lg%d' % j

                    def rmm(j=j):
                        for k in range(8):
                            ins = nc.tensor.matmul(pl[j][:], lhsT=hTf[j][:, k, :], rhs=rw[:, k, :], start=(k == 0), stop=(k == 7))
                        return ins
                    kb.op('pe', rmm, reads=[HF, 'rw'], writes=[PL])
                    G = 'g%d' % j
                    if RD < 2:
                        continue
                    kb.op('dve', lambda j=j: nc.vector.tensor_tensor(out=lg[j][:], in0=pl[j][:], in1=rb[:], op=ALU.add), reads=[PL, 'rb', G], writes=[G])
                    if RD < 3:
                        kb.dma('sp', self.GATES[t0:t0 + 128, :], lg[j][:], reads=[G], writes=['GATES%d' % ti])
                        continue
                    kb.op('dve', lambda j=j: nc.vector.tensor_reduce(out=mx[j][:, 0:1], in_=lg[j][:], axis=AX.X, op=ALU.max), reads=[G], writes=[G])
                    kb.op('dve', lambda j=j: nc.vector.tensor_scalar(out=m1[j][:], in0=lg[j][:], scalar1=mx[j][:, 0:1], scalar2=None, op0=ALU.is_equal), reads=[G], writes=[G])
                    kb.op('dve', lambda j=j: nc.vector.scalar_tensor_tensor(out=l2[j][:], in0=m1[j][:], scalar=-1e30, in1=lg[j][:], op0=ALU.mult, op1=ALU.add), reads=[G], writes=[G])
                    kb.op('dve', lambda j=j: nc.vector.tensor_reduce(out=mx[j][:, 1:2], in_=l2[j][:], axis=AX.X, op=ALU.max), reads=[G], writes=[G])
                    kb.op('dve', lambda j=j: nc.vector.tensor_scalar(out=m2[j][:], in0=l2[j][:], scalar1=mx[j][:, 1:2], scalar2=None, op0=ALU.is_equal), reads=[G], writes=[G])
                    kb.op('dve', lambda j=j: nc.vector.tensor_tensor(out=mx[j][:, 2:3], in0=mx[j][:, 0:1], in1=mx[j][:, 1:2], op=ALU.subtract), reads=[G], writes=[G])
                    kb.op('act', lambda j=j: nc.scalar.activation(out=mx[j][:, 2:3], in_=mx[j][:, 2:3], func=AF.Sigmoid), reads=[G], writes=[G])
                    kb.op('dve', lambda j=j: nc.vector.tensor_scalar(out=mx[j][:, 3:4], in0=mx[j][:, 2:3], scalar1=-1.0, scalar2=1.0, op0=ALU.mult, op1=ALU.add), reads=[G], writes=[G])
                    kb.op('dve', lambda j=j: nc.vector.tensor_scalar(out=gt[j][:], in0=m1[j][:], scalar1=mx[j][:, 2:3], scalar2=None, op0=ALU.mult), reads=[G], writes=[G])
                    kb.op('dve', lambda j=j: nc.vector.scalar_tensor_tensor(out=gt[j][:], in0=m2[j][:], scalar=mx[j][:, 3:4], in1=gt[j][:], op0=ALU.mult, op1=ALU.add), reads=[G], writes=[G])
                    kb.dma('sp', self.GATES[t0:t0 + 128, :], gt[j][:], reads=[G], writes=['GATES%d' % ti])

    def stage_ffnpass(self, w1, w3, w2, gate_col, first):
        nc, kb = self.nc, self.kb
        with Stage(kb) as st:
            w1b = st.sb("w1b", [128, 8, 1408], BF16)
            w3b = st.sb("w3b", [128, 8, 1408], BF16)
            w2b = st.sb("w2b", [128, 11, 1024], BF16)
            stg = [st.sb("stg%d" % j, [128, 1408], F32) for j in range(3)]
            n = 0
            for (dst, src, nk, key) in ((w1b, w1, 8, 'w1b'), (w3b, w3, 8, 'w3b'), (w2b, w2, 11, 'w2b')):
                width = src.shape[1]
                for k in range(nk):
                    j = n % 3
                    n += 1
                    kb.dma('sp', stg[j][:, 0:width], src[k * 128:(k + 1) * 128, :], writes=['stg%d' % j])
                    if n % 2:
                        kb.op('act', lambda dst=dst, k=k, j=j, width=width: nc.scalar.copy(out=dst[:, k, :], in_=stg[j][:, 0:width]),
                              reads=['stg%d' % j, key], writes=[key])
                    else:
                        kb.op('dve', lambda dst=dst, k=k, j=j, width=width: nc.vector.tensor_copy(out=dst[:, k, :], in_=stg[j][:, 0:width]),
                              reads=['stg%d' % j, key], writes=[key])
            hT = [st.sb("hT%d" % j, [128, 8, 512], BF16) for j in range(2)]
            act = [st.sb("act%d" % j, [128, 11, 512], BF16) for j in range(2)]
            sg = [st.sb("sg%d" % j, [128, 512], F32) for j in range(2)]
            pA = [st.ps("pA%d" % j, [128, 512], F32) for j in range(2)]
            pB = [st.ps("pB%d" % j, [128, 512], F32) for j in range(2)]
            pY = [st.ps("pY%d" % j, [128, 512], F32) for j in range(2)]
            yo = [st.sb("yo%d" % j, [128, 1024], F32) for j in range(2)]
            ya = [st.sb("ya%d" % j, [128, 1024], F32) for j in range(2)]
            gtl = [st.sb("gtl%d" % j, [128, 8], F32) for j in range(2)]
            nf = 0
            ny = 0
            for it, (t0, W) in enumerate(tiles_of(NT, 512)):
                j = it % 2
                kb.dma('sp', hT[j][:, :, 0:W], self.HT[:, :, t0:t0 + W], reads=['HT%d' % q for q in range(t0 // 128, (t0 + W) // 128)],
                       writes=['hT%d' % j])
                for f in range(11):
                    jf = nf % 2
                    nf += 1

                    def mmA(jf=jf, f=f, j=j, W=W, wb=w1b, pp=pA):
                        for k in range(8):
                            ins = nc.tensor.matmul(pp[jf][:, 0:W], lhsT=wb[:, k, f * 128:(f + 1) * 128], rhs=hT[j][:, k, 0:W],
                                                   start=(k == 0), stop=(k == 7))
                        return ins
                    kb.op('pe', mmA, reads=['hT%d' % j, 'w1b'], writes=['pA%d' % jf])
                    kb.op('pe', lambda jf=jf, f=f, j=j, W=W: mmA(jf, f, j, W, w3b, pB), reads=['hT%d' % j, 'w3b'], writes=['pB%d' % jf])
                    kb.op('act', lambda jf=jf, W=W: nc.scalar.activation(out=sg[jf][:, 0:W], in_=pA[jf][:, 0:W], func=AF.Silu),
                          reads=['pA%d' % jf], writes=['sg%d' % jf])
                    kb.op('dve', lambda jf=jf, f=f, j=j, W=W: nc.vector.tensor_tensor(out=act[j][:, f, 0:W], in0=sg[jf][:, 0:W], in1=pB[jf][:, 0:W], op=ALU.mult),
                          reads=['sg%d' % jf, 'pB%d' % jf, 'act%d' % j], writes=['act%d' % j])
                for sub in range(W // 128):
                    jy = ny % 2
                    ny += 1
                    ti = t0 // 128 + sub
                    ts = t0 + sub * 128
                    if not first:
                        kb.dma('sp', ya[jy][:], self.ACC[ts:ts + 128, :], reads=['ACC%d' % ti], writes=['ya%d' % jy])
                    if gate_col is not None:
                        kb.dma('sp', gtl[jy][:], self.GATES[ts:ts + 128, :], reads=['GATES%d' % ti], writes=['gtl%d' % jy])
                    for half in range(2):
                        jp = half

                        def mmY(jp=jp, half=half, sub=sub, j=j):
                            for f in range(11):
                                ins = nc.tensor.matmul(pY[jp][:], lhsT=act[j][:, f, sub * 128:(sub + 1) * 128],
                                                       rhs=w2b[:, f, half * 512:(half + 1) * 512], start=(f == 0), stop=(f == 10))
                            return ins
                        kb.op('pe', mmY, reads=['act%d' % j, 'w2b'], writes=['pY%d' % jp])
                        osl = slice(half * 512, (half + 1) * 512)
                        rd = ['pY%d' % jp, 'yo%d' % jy]
                        if gate_col is not None and not first:
                            kb.op('dve', lambda jp=jp, jy=jy, osl=osl: nc.vector.scalar_tensor_tensor(
                                out=yo[jy][:, osl], in0=pY[jp][:], scalar=gtl[jy][:, gate_col:gate_col + 1], in1=ya[jy][:, osl],
                                op0=ALU.mult, op1=ALU.add), reads=rd + ['gtl%d' % jy, 'ya%d' % jy], writes=['yo%d' % jy])
                        elif gate_col is not None:
                            kb.op('dve', lambda jp=jp, jy=jy, osl=osl: nc.vector.tensor_scalar(
                                out=yo[jy][:, osl], in0=pY[jp][:], scalar1=gtl[jy][:, gate_col:gate_col + 1], scalar2=None, op0=ALU.mult),
                                reads=rd + ['gtl%d' % jy], writes=['yo%d' % jy])
                        elif not first:
                            kb.op('dve', lambda jp=jp, jy=jy, osl=osl: nc.vector.tensor_tensor(
                                out=yo[jy][:, osl], in0=pY[jp][:], in1=ya[jy][:, osl], op=ALU.add),
                                reads=rd + ['ya%d' % jy], writes=['yo%d' % jy])
                        else:
                            kb.op('act', lambda jp=jp, jy=jy, osl=osl: nc.scalar.copy(out=yo[jy][:, osl], in_=pY[jp][:]),
                                  reads=rd, writes=['yo%d' % jy])
                    kb.dma('sp', self.ACC[ts:ts + 128, :], yo[jy][:], reads=['yo%d' % jy], writes=['ACC%d' % ti])

    def stage_finish(self, i, sub, out_final=None):
        nc, kb = self.nc, self.kb
        with Stage(kb) as st:
            gs = 2 if sub == 1 else 5
            md = self.mod_tiles(st, i, [gs])
            lng = self.bcast_load(st, "lng", self.ln_g[i, sub - 1, :], 1024)
            lnb = self.bcast_load(st, "lnb", self.ln_b[i, sub - 1, :], 1024)
            NB = 2
            xt = [st.sb("xt%d" % j, [128, 1024], F32) for j in range(NB)]
            at = [st.sb("at%d" % j, [128, 1024], F32) for j in range(NB)]
            stt = [st.sb("stt%d" % j, [128, 2, 6], F32) for j in range(NB)]
            mv = [st.sb("mv%d" % j, [128, 4], F32) for j in range(NB)]
            for ti in range(NT // 128):
                j = ti % NB
                isc = 1 if ti < TC // 128 else 0
                t0 = ti * 128
                X, A, S = 'xt%d' % j, 'at%d' % j, 'st%d' % j
                kb.dma('sp', xt[j][:], self.XS[t0:t0 + 128, :], writes=[X])
                kb.dma('sp', at[j][:], self.ACC[t0:t0 + 128, :], reads=['ACC%d' % ti], writes=[A])
                kb.op('dve', lambda j=j, isc=isc: nc.vector.tensor_tensor(out=at[j][:], in0=at[j][:], in1=md[(gs, isc)][:], op=ALU.mult),
                      reads=[A, 'mod%d_%d' % (gs, isc)], writes=[A])
                kb.op('dve', lambda j=j: nc.vector.scalar_tensor_tensor(out=xt[j][:], in0=xt[j][:], scalar=float(ALPHA), in1=at[j][:], op0=ALU.mult, op1=ALU.add),
                      reads=[X, A], writes=[X])

                def bn(j=j):
                    nc.vector.bn_stats(out=stt[j][:, 0, :], in_=xt[j][:, 0:512])
                    return nc.vector.bn_stats(out=stt[j][:, 1, :], in_=xt[j][:, 512:1024])
                kb.op('dve', bn, reads=[X, S], writes=[S])
                kb.op('dve', lambda j=j: nc.vector.bn_aggr(out=mv[j][:, 0:2], in_=stt[j][:]), reads=[S], writes=[S])
                kb.op('dve', lambda j=j: nc.vector.tensor_scalar(out=mv[j][:, 2:3], in0=mv[j][:, 1:2], scalar1=LN_EPS, scalar2=None, op0=ALU.add), reads=[S], writes=[S])
                kb.op('act', lambda j=j: nc.scalar.activation(out=mv[j][:, 2:3], in_=mv[j][:, 2:3], func=AF.Sqrt), reads=[S], writes=[S])
                kb.op('dve', lambda j=j: nc.vector.reciprocal(out=mv[j][:, 3:4], in_=mv[j][:, 2:3]), reads=[S], writes=[S])
                kb.op('dve', lambda j=j: nc.vector.tensor_scalar(out=xt[j][:], in0=xt[j][:], scalar1=mv[j][:, 0:1], scalar2=mv[j][:, 3:4],
                                                             op0=ALU.subtract, op1=ALU.mult), reads=[X, S], writes=[X])
                kb.op('dve', lambda j=j: nc.vector.tensor_tensor(out=xt[j][:], in0=xt[j][:], in1=lng[:], op=ALU.mult), reads=[X, 'lng'], writes=[X])
                kb.op('dve', lambda j=j: nc.vector.tensor_tensor(out=xt[j][:], in0=xt[j][:], in1=lnb[:], op=ALU.add), reads=[X, 'lnb'], writes=[X])
                kb.dma('sp', self.XS[t0:t0 + 128, :], xt[j][:], reads=[X], writes=['XS%d' % ti])
                if out_final is not None and ti >= TC // 128:
                    kb.dma('sp', out_final[t0 - TC:t0 - TC + 128, :], xt[j][:], reads=[X], writes=['OUT%d' % ti])

    def stage_pool(self, j_layer):
        nc, kb = self.nc, self.kb
        with Stage(kb) as st:
            pw = st.sb("pw", [128, 4, 2, 256], BF16)
            pst = [st.sb("pst%d" % j, [128, 256], F32) for j in range(2)]
            n = 0
            for gi in range(4):
                for kk in range(2):
                    j = n % 2
                    n += 1
                    kb.dma('sp', pst[j][:], self.pool_w[j_layer, gi, kk * 128:(kk + 1) * 128, :], writes=['pst%d' % j])
                    kb.op('dve', lambda gi=gi, kk=kk, j=j: nc.vector.tensor_copy(out=pw[:, gi, kk, :], in_=pst[j][:]), reads=['pst%d' % j, 'pw'], writes=['pw'])
            psc = self.bcast_load(st, "psc", self.pool_scale[j_layer, :], 1024)
            invc = st.sb("invc", [128, 4, 64], F32)
            kb.dma('sp', invc[:], self.cst_invc64, writes=['invc'])
            invcc = st.sb("invcc", [128, 4, 256], F32)
            kb.dma('sp', invcc[:], self.cst_invc256, writes=['invcc'])
            hT = [st.sb("hT%d" % j, [128, 8, 512], BF16) for j in range(2)]
            P = st.sb("P", [128, 8, 8, 80], F32)
            S2 = st.sb("S2", [128, 8, 8, 80], F32)
            S4 = st.sb("S4", [128, 8, 8, 80], F32)
            S8 = st.sb("S8", [128, 8, 8, 80], F32)
            S16 = st.sb("S16", [128, 8, 8, 80], F32)
            pl = [st.sb("pl%d" % j, [128, 8, 512], BF16) for j in range(2)]
            tmp = st.sb("tmp", [128, 2, 8, 64], F32)
            pY = [st.ps("pY%d" % j, [128, 1024], F32) for j in range(2)]
            yo = [st.sb("yo%d" % j, [128, 1024], F32) for j in range(2)]
            for b_ in (P, S2, S4, S8, S16):
                pass
            kb.op('pool', lambda: nc.gpsimd.memset(P[:], 0.0), writes=['P'])
            ny = 0
            for it, (t0, W) in enumerate([(0, 256)] + tiles_of(NT, 512, 256)):
                j = it % 2
                isc = (t0 == 0)
                kb.dma('sp', hT[j][:, :, 0:W], self.HT[:, :, t0:t0 + W], reads=['HT%d' % q for q in range(t0 // 128, (t0 + W) // 128)],
                       writes=['hT%d' % j])
                if isc:
                    Pv = P[:].rearrange("p k r c -> p k (r c)")[:, :, 0:272].rearrange("p k (r c) -> p k r c", r=1)
                    views = [b_[:].rearrange("p k r c -> p k (r c)")[:, :, 0:272].rearrange("p k (r c) -> p k r c", r=1) for b_ in (P, S2, S4, S8, S16)]
                    L = 256
                    R = 1
                else:
                    if it == 1:
                        kb.op('pool', lambda: nc.gpsimd.memset(P[:], 0.0), reads=['P'], writes=['P'])
                    views = [b_[:] for b_ in (P, S2, S4, S8, S16)]
                    L = 64
                    R = 8
                Pv, S2v, S4v, S8v, S16v = views
                LP = L + 16
                kb.op('act', lambda Pv=Pv, j=j, L=L, R=R, W=W: nc.scalar.copy(out=Pv[:, :, :, 8:8 + L], in_=hT[j][:, :, 0:W].rearrange("p k (r c) -> p k r c", r=R)),
                      reads=['hT%d' % j, 'P'], writes=['P'])
                kb.op('dve', lambda: nc.vector.tensor_tensor(out=S2v[:, :, :, 0:LP - 1], in0=Pv[:, :, :, 0:LP - 1], in1=Pv[:, :, :, 1:LP], op=ALU.add), reads=['P', 'S2'], writes=['S2'])
                kb.op('dve', lambda: nc.vector.tensor_tensor(out=S4v[:, :, :, 0:LP - 3], in0=S2v[:, :, :, 0:LP - 3], in1=S2v[:, :, :, 2:LP - 1], op=ALU.add), reads=['S2', 'S4'], writes=['S4'])
                kb.op('dve', lambda: nc.vector.tensor_tensor(out=S8v[:, :, :, 0:LP - 7], in0=S4v[:, :, :, 0:LP - 7], in1=S4v[:, :, :, 4:LP - 3], op=ALU.add), reads=['S4', 'S8'], writes=['S8'])
                kb.op('dve', lambda: nc.vector.tensor_tensor(out=S16v[:, :, :, 0:LP - 15], in0=S8v[:, :, :, 0:LP - 15], in1=S8v[:, :, :, 8:LP - 7], op=ALU.add), reads=['S8', 'S16'], writes=['S16'])
                for gi, (win, Sv, key) in enumerate(((2, S2v, 'S2'), (4, S4v, 'S4'), (8, S8v, 'S8'), (16, S16v, 'S16'))):
                    o = 8 - win // 2
                    ic = (invcc if isc else invc)
                    icv = ic[:, gi, :].unsqueeze(1).unsqueeze(1).to_broadcast([128, 2, R, L])
                    tv = tmp[:].rearrange("p k r c -> p k (r c)")[:, :, 0:W].rearrange("p k (r c) -> p k r c", r=R)
                    kb.op('dve', lambda Sv=Sv, gi=gi, o=o, icv=icv, tv=tv, L=L: nc.vector.tensor_tensor(out=tv, in0=Sv[:, 2 * gi:2 * gi + 2, :, o:o + L], in1=icv, op=ALU.mult),
                          reads=[key, 'invc', 'invcc', 'tmp'], writes=['tmp'])
                    kb.op('dve', lambda gi=gi, tv=tv, j=j, Pv=Pv, L=L, R=R, W=W: nc.vector.tensor_tensor(
                        out=pl[j][:, 2 * gi:2 * gi + 2, 0:W].rearrange("p k (r c) -> p k r c", r=R), in0=tv, in1=Pv[:, 2 * gi:2 * gi + 2, :, 8:8 + L], op=ALU.subtract),
                        reads=['tmp', 'P', 'pl%d' % j], writes=['pl%d' % j])
                for sub in range(W // 128):
                    jy = ny % 2
                    ny += 1
                    ti = t0 // 128 + sub
                    ts = t0 + sub * 128

                    def mm(jy=jy, sub=sub, j=j):
                        for gi in range(4):
                            for kk in range(2):
                                ins = nc.tensor.matmul(pY[jy][:, gi * 256:(gi + 1) * 256], lhsT=pl[j][:, 2 * gi + kk, sub * 128:(sub + 1) * 128],
                                                       rhs=pw[:, gi, kk, :], start=(kk == 0), stop=(kk == 1))
                        return ins
                    kb.op('pe', mm, reads=['pl%d' % j, 'pw'], writes=['pY%d' % jy])
                    kb.op('dve', lambda jy=jy: nc.vector.tensor_tensor(out=yo[jy][:], in0=pY[jy][:], in1=psc[:], op=ALU.mult), reads=['pY%d' % jy, 'psc', 'yo%d' % jy], writes=['yo%d' % jy])
                    kb.dma('sp', self.ACC[ts:ts + 128, :], yo[jy][:], reads=['yo%d' % jy], writes=['ACC%d' % ti])

    def scan_consts(self, st, z):
        kb = self.kb
        c = {}
        for nm in ('SB', 'SBI', 'SBT', 'BLK'):
            t = st.sb(nm, [128, 128], F32)
            kb.dma('sp', t[:], self.cst_masks[z, {'SB': 0, 'SBI': 1, 'SBT': 2, 'BLK': 3}[nm]], writes=[nm])
            c[nm] = t
        idf = st.sb("identf", [128, 128], F32)
        kb.dma('sp', idf[:], self.cst_identf, writes=['identf'])
        idb = st.sb("identb", [128, 128], BF16)
        kb.op('dve', lambda: self.nc.vector.tensor_copy(out=idb[:], in_=idf[:]), reads=['identf'], writes=['identb'])
        c['identf'] = idf
        c['identb'] = idb
        return c

    def head_scan(self, st, C, hid, dk, dv, pb, KAP, RM, BCt, KCt, V, KAPT, BPT, KPT, RMT, Dx, Di, DxT, wc, T, yout, opk, z, bufs, slot=0, ndt=BF16):
        nc, kb = self.nc, self.kb
        B = bufs
        par = slot
        ps = B['ps']

        def nps():
            return ps[slot], B['psk'][slot]
        sfx = '_%d' % par

        def S(name):
            return B[name][par], name + sfx
        idf, idb = C['identf'], C['identb']

        def mm_ev(name, lhsT, rhs, mul=None, mulkey=None, eng='dve', rows=128, cols=128, extra=None, reads=()):
            p, pk = nps()
            dst, dk_ = S(name)

            def f():
                ins = nc.tensor.matmul(p[0:rows, 0:cols], lhsT=lhsT, rhs=rhs, start=True, stop=(extra is None))
                if extra is not None:
                    for qi, (l2, r2) in enumerate(extra):
                        ins = nc.tensor.matmul(p[0:rows, 0:cols], lhsT=l2, rhs=r2, start=False, stop=(qi == len(extra) - 1))
                return ins
            kb.op('pe', f, reads=list(reads), writes=[pk])
            if mul is not None:
                kb.op('dve', lambda: nc.vector.tensor_tensor(out=dst[0:rows, 0:cols], in0=p[0:rows, 0:cols], in1=mul, op=ALU.mult),
                      reads=[pk, mulkey, dk_], writes=[dk_])
            elif eng == 'act':
                kb.op('act', lambda: nc.scalar.copy(out=dst[0:rows, 0:cols], in_=p[0:rows, 0:cols]), reads=[pk, dk_], writes=[dk_])
            else:
                kb.op('dve', lambda: nc.vector.tensor_copy(out=dst[0:rows, 0:cols], in_=p[0:rows, 0:cols]), reads=[pk, dk_], writes=[dk_])
            return dst, dk_
        OK = list(opk)
        N_, Nk = mm_ev('N', BPT, KAPT, mul=Dx[0], mulkey=Dx[1], reads=OK)
        yield
        NT_, NTk = mm_ev('NT', KAPT, BPT, mul=DxT[0], mulkey=DxT[1], reads=OK)
        yield
        BtT, BtTk = mm_ev('BtT', KPT, KAPT, mul=Dx[0], mulkey=Dx[1], reads=OK)
        yield
        AbT, AbTk = mm_ev('AbT', BPT, RMT, mul=Di[0], mulkey=Di[1], reads=OK)
        yield
        AkT, AkTk = mm_ev('AkT', KPT, RMT, mul=Di[0], mulkey=Di[1], reads=OK)
        yield
        R_, Rk = S('R')
        kb.op('dve', lambda: nc.vector.tensor_tensor(out=R_[:], in0=N_[:], in1=idf[:], op=ALU.add), reads=[Nk, 'identf', Rk], writes=[Rk])
        yield
        P, Pk, PT, PTk = N_, Nk, NT_, NTk
        for it in range(5):
            PT2, PT2k = mm_ev('PT%d' % (it % 2), P[:], PT[:], eng='act', reads=[Pk, PTk])
            yield
            if it < 4:
                P2, P2k = mm_ev('P%d' % (it % 2), PT[:], P[:], eng='act', reads=[Pk, PTk])
                yield
            R2_, R2k = S('R' if it % 2 else 'Rb')
            Rn, Rnk = mm_ev('Rb' if it % 2 == 0 else 'R', (idb if ndt == BF16 else idf)[:], R_[:], extra=[(PT2[:], R_[:])], eng='dve', reads=['identb', 'identf', PT2k, Rk])
            yield
            R_, Rk = Rn, Rnk
            if it < 4:
                P, Pk, PT, PTk = P2, P2k, PT2, PT2k
        if ndt == BF16:
            MTb, MTbk = R_, Rk
        else:
            MTb, MTbk = S('MTb')
            kb.op('act', lambda: nc.scalar.copy(out=MTb[:], in_=R_[:]), reads=[Rk, MTbk], writes=[MTbk])
            yield
        X0, X0k = mm_ev('X0', BtT[:], V, cols=dv, reads=[BtTk] + OK)
        yield
        U0, U0k = mm_ev('U0', MTb[:], X0[:, 0:dv], cols=dv, eng='act', reads=[MTbk, X0k])
        yield
        MK, MKk = mm_ev('MK', MTb[:], KAP, cols=dk, reads=[MTbk] + OK)
        yield
        RstT, RstTk = mm_ev('RstT', MK[:, 0:dk], AbT[:], rows=dk, extra=[(RM, idb[:])], eng='act', reads=[MKk, AbTk, 'identb'] + OK)
        yield
        Y0, Y0k = mm_ev('Y0', AbT[:], U0[:, 0:dv], cols=dv, extra=[(AkT[:], V)], reads=[AbTk, U0k, AkTk] + OK)
        yield
        Phi = {}
        Z0 = {}
        for c in range(2):
            rs = slice(c * 64, (c + 1) * 64)
            p, pk = nps()
            kb.op('pe', lambda p=p, rs=rs: nc.tensor.matmul(p[0:dk, 0:dk], lhsT=MK[rs, 0:dk], rhs=BCt[rs, :], start=True, stop=True), reads=[MKk] + OK, writes=[pk])
            yield
            dst, dkey = S('Phi%d' % c)
            kb.op('dve', lambda p=p, dst=dst, c=c: nc.vector.scalar_tensor_tensor(out=dst[0:dk, 0:dk], in0=idf[0:dk, 0:dk], scalar=wc[0][:, c:c + 1], in1=p[0:dk, 0:dk],
                                                                               op0=ALU.mult, op1=ALU.add), reads=[pk, 'identf', wc[1], dkey], writes=[dkey])
            Phi[c] = (dst, dkey)
            Z0[c] = mm_ev('Z0%d' % c, BCt[rs, :], U0[rs, 0:dv], rows=dk, cols=dv, extra=[(KCt[rs, :], V[rs, :])], eng='act', reads=[U0k] + OK)
            yield
        Tt, Tk = T
        for c in ((0, 1) if z == 0 else (1, 0)):
            rs = slice(c * 64, (c + 1) * 64)
            p, pk = nps()
            kb.op('pe', lambda p=p: nc.tensor.matmul(p[:, 0:dv], lhsT=RstT[0:dk, :], rhs=Tt[:], start=True, stop=True), reads=[RstTk, Tk], writes=[pk])
            yield
            kb.op('dve', lambda p=p, rs=rs: nc.vector.tensor_tensor(out=yout[0][rs, :], in0=p[rs, 0:dv], in1=Y0[rs, 0:dv], op=ALU.add), reads=[pk, Y0k, yout[1]], writes=[yout[1]])
            yield
            p2, pk2 = nps()

            def tm(p2=p2, c=c):
                nc.tensor.matmul(p2[0:dk, 0:dv], lhsT=Phi[c][0][0:dk, 0:dk], rhs=Tt[:], start=True, stop=False)
                return nc.tensor.matmul(p2[0:dk, 0:dv], lhsT=idf[0:dk, 0:dk], rhs=Z0[c][0][0:dk, 0:dv], start=False, stop=True)
            kb.op('pe', tm, reads=[Phi[c][1], Z0[c][1], Tk, 'identf'], writes=[pk2])
            yield
            kb.op('act', lambda p2=p2: nc.scalar.copy(out=Tt[:], in_=p2[0:dk, 0:dv]), reads=[pk2, Tk], writes=[Tk])
            yield

    def run_heads(self, gens):
        gens = list(gens)
        active = {}
        free = list(range(NSLOT))
        while gens or active:
            while gens and free:
                sl = free.pop(0)
                active[sl] = gens.pop(0)(sl)
            for sl in list(active.keys()):
                try:
                    next(active[sl])
                except StopIteration:
                    del active[sl]
                    free.append(sl)

    def head_bufs(self, st, ndt=BF16):
        B = {}
        for nm, dt in (('N', ndt), ('NT', ndt), ('R', ndt), ('Rb', ndt), ('P0', ndt), ('P1', ndt), ('PT0', ndt), ('PT1', ndt),
                       ('BtT', BF16), ('AbT', BF16), ('AkT', BF16), ('MTb', BF16), ('X0', BF16), ('U0', BF16), ('MK', BF16),
                       ('RstT', F32), ('Y0', F32), ('Phi0', F32), ('Phi1', F32), ('Z00', F32), ('Z01', F32)):
            B[nm] = [st.sb("%s_%d" % (nm, q), [128, 128], dt) for q in range(NSLOT)]
        B['ps'] = [st.ps("hps%d" % q, [128, 512], F32) for q in range(NSLOT - 2)]
        B['psk'] = ['hps%d' % q for q in range(NSLOT - 2)]
        return B

    def scan_order(self, z):
        nt = NT // 128
        nct = TC // 128
        if z == 0:
            return list(range(nt))
        return list(range(nct - 1, -1, -1)) + list(range(nt - 1, nct - 1, -1))

    def stage_rwkv_proj(self, jl):
        nc, kb = self.nc, self.kb
        with Stage(kb) as st:
            NCOL = 3072 + 384
            Wb = st.sb("Wb", [128, 8, NCOL], BF16)
            stg = [st.sb("stg%d" % j, [128, 1024], F32) for j in range(2)]
            n = 0
            srcs = [(self.rk_w_rkv[jl, 0], 0, 1024), (self.rk_w_rkv[jl, 1], 1024, 1024), (self.rk_w_rkv[jl, 2], 2048, 1024),
                    (self.rk_w1[jl, 0], 3072, 64), (self.rk_w1[jl, 1], 3136, 64), (self.rk_a1[jl, 0], 3200, 64), (self.rk_a1[jl, 1], 3264, 64),
                    (self.rk_g1[jl], 3328, 128)]
            for (src, c0, wd) in srcs:
                for k in range(8):
                    j = n % 2
                    n += 1
                    kb.dma('sp', stg[j][:, 0:wd], src[k * 128:(k + 1) * 128, :], writes=['stg%d' % j])
                    kb.op('act' if n % 2 else 'dve', (lambda k=k, j=j, c0=c0, wd=wd: nc.scalar.copy(out=Wb[:, k, c0:c0 + wd], in_=stg[j][:, 0:wd])) if n % 2 else
                          (lambda k=k, j=j, c0=c0, wd=wd: nc.vector.tensor_copy(out=Wb[:, k, c0:c0 + wd], in_=stg[j][:, 0:wd])), reads=['stg%d' % j, 'Wb'], writes=['Wb'])
            W2 = st.sb("W2", [128, 3, 1024], BF16)
            for q, srcl in enumerate(([self.rk_w2[jl, 0], self.rk_w2[jl, 1]], [self.rk_a2[jl, 0], self.rk_a2[jl, 1]], [self.rk_g2[jl]])):
                j = q % 2
                r0 = 0
                for si, src in enumerate(srcl):
                    rws = src.shape[0]
                    kb.dma('sp', stg[j][r0:r0 + rws, :], src, reads=['stg%d' % j] if si else [], writes=['stg%d' % j])
                    r0 += rws
                kb.op('dve', lambda q=q, j=j: nc.vector.tensor_copy(out=W2[:, q, :], in_=stg[j][:]), reads=['stg%d' % j, 'W2'], writes=['W2'])
            mu = st.sb("mu", [128, 6, 8], F32)
            for m in range(6):
                kb.dma('sp', mu[:, m, :], self.rk_mu[jl, m, :].rearrange("(k p) -> p k", p=128), reads=['mu'] if m else [], writes=['mu'], allow_slow_non_contiguous=True)
            HW_ = 512 + 128
            ht = st.sb("ht", [128, 8, HW_], BF16)
            xx = st.sb("xx", [128, 8, 512], BF16)
            xm = st.sb("xm", [128, 6, 8, 512], BF16)
            l1 = st.sb("l1", [128, 3, 512], BF16)
            pp = [st.ps("pp%d" % q, [128, 512], F32) for q in range(4)]
            ob = [st.sb("ob%d" % q, [128, 1024], F32) for q in range(4)]
            npp = [0]
            nob = [0]
            tiles = [(0, TC)] + tiles_of(NT, 512, TC)
            for it, (t0, W) in enumerate(tiles):
                isc = (t0 == 0)
                lo = t0 - 64
                hi = t0 + W + 64
                zl = isc or t0 == TC
                zr = isc or (t0 + W == NT)
                if zl:
                    kb.op('dve', lambda: nc.vector.memset(ht[:, :, 0:64], 0.0), reads=['ht'], writes=['ht'])
                if zr:
                    kb.op('dve', lambda W=W: nc.vector.memset(ht[:, :, 64 + W:128 + W], 0.0), reads=['ht'], writes=['ht'])
                a = t0 if zl else lo
                b = t0 + W if zr else hi
                kb.dma('sp', ht[:, :, 64 + (a - t0):64 + (b - t0)], self.HT[:, :, a:b], reads=['HT%d' % q for q in range(a // 128, (b + 127) // 128)] + ['ht'], writes=['ht'])
                for k in range(8):
                    if isc:
                        off = 63 if k < 4 else 65
                    else:
                        off = (63, 63, 65, 65, 0, 0, 128, 128)[k]
                    kb.op('dve', lambda k=k, off=off, W=W: nc.vector.tensor_tensor(out=xx[:, k, 0:W], in0=ht[:, k, off:off + W], in1=ht[:, k, 64:64 + W], op=ALU.subtract),
                          reads=['ht', 'xx'], writes=['xx'])
                    if not isc and k < 4:
                        col = 0 if k < 2 else 63
                        kb.op('dve', lambda k=k, col=col, W=W: nc.vector.tensor_scalar(
                            out=xx[:, k, 0:W].rearrange("p (r c) -> p r c", c=64)[:, :, col], in0=ht[:, k, 64:64 + W].rearrange("p (r c) -> p r c", c=64)[:, :, col],
                            scalar1=-1.0, scalar2=None, op0=ALU.mult), reads=['ht', 'xx'], writes=['xx'])
                for m in range(6):
                    for k in range(8):
                        eng = 'dve'
                        E = nc.vector if eng == 'dve' else nc.gpsimd
                        kb.op(eng, lambda m=m, k=k, W=W, E=E: E.scalar_tensor_tensor(out=xm[:, m, k, 0:W], in0=xx[:, k, 0:W], scalar=mu[:, m, k:k + 1], in1=ht[:, k, 64:64 + W],
                                                                                 op0=ALU.mult, op1=ALU.add), reads=['xx', 'ht', 'mu', 'xm%d' % m], writes=['xm%d' % m])
                for c, (m, fn) in enumerate(((1, AF.Tanh), (4, AF.Copy), (5, AF.Sigmoid))):
                    q = npp[0] % 4
                    npp[0] += 1

                    def f(q=q, c=c, m=m, W=W):
                        for k in range(8):
                            ins = nc.tensor.matmul(pp[q][:, 0:W], lhsT=Wb[:, k, 3072 + c * 128:3072 + (c + 1) * 128], rhs=xm[:, m, k, 0:W], start=(k == 0), stop=(k == 7))
                        return ins
                    kb.op('pe', f, reads=['Wb', 'xm%d' % m], writes=['pp%d' % q])
                    kb.op('act', lambda q=q, c=c, fn=fn, W=W: nc.scalar.activation(out=l1[:, c, 0:W], in_=pp[q][:, 0:W], func=fn), reads=['pp%d' % q, 'l1'], writes=['l1'])
                for sub in range(W // 128):
                    ts = t0 + sub * 128
                    ti = ts // 128
                    ss = slice(sub * 128, (sub + 1) * 128)
                    outs = [('RR', 0, 0, None), ('KK0', 2, 1024, None), ('VV', 3, 2048, None),
                            ('DL0', None, 0, (0, 0, 64)), ('DL1', None, 0, (0, 64, 128)), ('AL0', None, 1, (1, 0, 64)), ('AL1', None, 1, (1, 64, 128)), ('GG', None, 2, (2, 0, 128))]
                    for (nm, m, c0, l2) in outs:
                        o = nob[0] % 4
                        nob[0] += 1
                        for half in range(2):
                            q = npp[0] % 4
                            npp[0] += 1
                            if l2 is None:
                                def f(q=q, m=m, c0=c0, half=half, ss=ss):
                                    for k in range(8):
                                        ins = nc.tensor.matmul(pp[q][:], lhsT=xm[:, m, k, ss], rhs=Wb[:, k, c0 + half * 512:c0 + (half + 1) * 512], start=(k == 0), stop=(k == 7))
                                    return ins
                                kb.op('pe', f, reads=['Wb', 'xm%d' % m], writes=['pp%d' % q])
                            else:
                                c, r0, r1 = l2
                                kb.op('pe', lambda q=q, c=c, r0=r0, r1=r1, half=half, ss=ss: nc.tensor.matmul(pp[q][:], lhsT=l1[r0:r1, c, ss], rhs=W2[r0:r1, c, half * 512:(half + 1) * 512],
                                                                                                           start=True, stop=True), reads=['l1', 'W2'], writes=['pp%d' % q])
                            if half == 0:
                                kb.op('act', lambda q=q, o=o: nc.scalar.copy(out=ob[o][:, 0:512], in_=pp[q][:]), reads=['pp%d' % q, 'ob%d' % o], writes=['ob%d' % o])
                            else:
                                kb.op('dve', lambda q=q, o=o: nc.vector.tensor_copy(out=ob[o][:, 512:1024], in_=pp[q][:]), reads=['pp%d' % q, 'ob%d' % o], writes=['ob%d' % o])
                        kb.dma('sp', self.RK[nm][ts:ts + 128, :], ob[o][:], reads=['ob%d' % o], writes=['%s%d' % (nm, ti)])

    def stage_rwkv_scan(self, jl, z):
        nc, kb = self.nc, self.kb
        with Stage(kb) as st:
            C = self.scan_consts(st, z)
            B = self.head_bufs(st)
            w0 = self.bcast_load(st, "w0", self.rk_w0[jl, z, :], 1024)
            a0 = self.bcast_load(st, "a0", self.rk_a0[jl, z, :], 1024)
            k_k = self.bcast_load(st, "k_k", self.rk_k_k[jl, :], 1024)
            k_a = self.bcast_load(st, "k_a", self.rk_k_a[jl, :], 1024)
            T = [st.sb("T%d" % h, [64, 64], F32) for h in range(16)]
            for h in range(16):
                kb.op('dve', lambda h=h: nc.vector.memset(T[h][:], 0.0), writes=['T%d' % h])
            ld = {nm: st.sb("ld_" + nm, [128, 1024], F32) for nm in ('RR', 'KK0', 'VV', 'DL', 'AL')}
            f = {nm: st.sb("f_" + nm, [128, 1024], F32) for nm in ('sw', 'a', 'kk', 'kd', 'b', 'eG', 'enG', 'eGx', 'eRev', 'eTot', 'Gs', 't1')}
            sm = st.sb("sm", [128, 3, 16], F32)
            tb = {nm: st.sb("tb_" + nm, [128, 1024], BF16) for nm in ('KAP', 'RM', 'BC', 'KC', 'V', 'BP', 'KP')}
            fT = {nm: st.sb("fT_" + nm, [128, 8, 128], BF16) for nm in ('KAP', 'BP', 'KP', 'RM')}
            eTT = st.sb("eTT", [128, 8, 128], F32)
            wc = st.sb("wc", [64, 16, 2], F32)
            pG = [st.ps("pG%d" % q, [128, 512], F32) for q in range(2)]
            B['ps'] = B['ps'] + pG
            B['psk'] = B['psk'] + ['pG0', 'pG1']
            yt = st.sb("yt", [128, 1024], F32)
            DEC = 0.6065306597126334
            for ti in self.scan_order(z):
                t0 = ti * 128
                for nm, src in (('RR', self.RK['RR']), ('KK0', self.RK['KK0']), ('VV', self.RK['VV']), ('DL', self.RK['DL%d' % z]), ('AL', self.RK['AL%d' % z])):
                    kb.dma('sp', ld[nm][:], src[t0:t0 + 128, :], reads=['%s%d' % (nm if nm in ('RR', 'KK0', 'VV') else nm + str(z), ti)], writes=['ld_' + nm])
                V_ = nc.vector
                kb.op('dve', lambda: V_.tensor_tensor(out=f['t1'][:], in0=ld['DL'][:], in1=w0[:], op=ALU.add), reads=['ld_DL', 'w0', 'f_t1'], writes=['f_t1'])
                kb.op('act', lambda: nc.scalar.activation(out=f['sw'][:], in_=f['t1'][:], func=AF.Sigmoid), reads=['f_t1', 'f_sw'], writes=['f_sw'])
                kb.op('dve', lambda: V_.tensor_tensor(out=f['t1'][:], in0=ld['AL'][:], in1=a0[:], op=ALU.add), reads=['ld_AL', 'a0', 'f_t1'], writes=['f_t1'])
                kb.op('act', lambda: nc.scalar.activation(out=f['a'][:], in_=f['t1'][:], func=AF.Sigmoid), reads=['f_t1', 'f_a'], writes=['f_a'])
                kb.op('dve', lambda: V_.tensor_tensor(out=f['kk'][:], in0=ld['KK0'][:], in1=k_k[:], op=ALU.mult), reads=['ld_KK0', 'k_k', 'f_kk'], writes=['f_kk'])
                kb.op('dve', lambda: V_.tensor_tensor(out=f['t1'][:], in0=f['kk'][:], in1=f['kk'][:], op=ALU.mult), reads=['f_kk', 'f_t1'], writes=['f_t1'])
                kb.op('dve', lambda: V_.tensor_reduce(out=sm[:, 0, :], in_=f['t1'][:].rearrange("p (h n) -> p h n", n=64), axis=AX.X, op=ALU.add), reads=['f_t1', 'sm'], writes=['sm'])
                kb.op('act', lambda: nc.scalar.activation(out=sm[:, 1, :], in_=sm[:, 0, :], func=AF.Sqrt), reads=['sm'], writes=['sm'])
                kb.op('dve', lambda: V_.tensor_scalar(out=sm[:, 1, :], in0=sm[:, 1, :], scalar1=1e-12, scalar2=None, op0=ALU.max), reads=['sm'], writes=['sm'])
                kb.op('dve', lambda: V_.reciprocal(out=sm[:, 2, :], in_=sm[:, 1, :]), reads=['sm'], writes=['sm'])
                kb.op('dve', lambda: V_.tensor_tensor(out=f['kk'][:].rearrange("p (h n) -> p h n", n=64), in0=f['kk'][:].rearrange("p (h n) -> p h n", n=64),
                                                      in1=sm[:, 2, :].unsqueeze(2).to_broadcast([128, 16, 64]), op=ALU.mult), reads=['sm', 'f_kk'], writes=['f_kk'])
                kb.op('dve', lambda: V_.scalar_tensor_tensor(out=f['t1'][:], in0=f['a'][:], scalar=-1.0, in1=k_a[:], op0=ALU.add, op1=ALU.mult), reads=['f_a', 'k_a', 'f_t1'], writes=['f_t1'])
                kb.op('dve', lambda: V_.scalar_tensor_tensor(out=f['kd'][:], in0=f['t1'][:], scalar=1.0, in1=ld['KK0'][:], op0=ALU.add, op1=ALU.mult), reads=['f_t1', 'ld_KK0', 'f_kd'], writes=['f_kd'])
                kb.op('dve', lambda: V_.tensor_tensor(out=f['b'][:], in0=f['kk'][:], in1=f['a'][:], op=ALU.mult), reads=['f_kk', 'f_a', 'f_b'], writes=['f_b'])
                for half in range(2):
                    hs = slice(half * 512, (half + 1) * 512)
                    kb.op('pe', lambda hs=hs: nc.tensor.matmul(pG[0][:], lhsT=C['SBI'][:], rhs=f['sw'][:, hs], start=True, stop=True), reads=['SBI', 'f_sw'], writes=['pG0'])
                    kb.op('pe', lambda hs=hs: nc.tensor.matmul(pG[1][:], lhsT=C['BLK'][:], rhs=f['sw'][:, hs], start=True, stop=True), reads=['BLK', 'f_sw'], writes=['pG1'])
                    kb.op('act', lambda hs=hs: nc.scalar.activation(out=f['eG'][:, hs], in_=pG[0][:], func=AF.Exp, scale=-DEC), reads=['pG0', 'f_eG'], writes=['f_eG'])
                    kb.op('act', lambda hs=hs: nc.scalar.activation(out=f['enG'][:, hs], in_=pG[0][:], func=AF.Exp, scale=DEC), reads=['pG0', 'f_enG'], writes=['f_enG'])
                    kb.op('dve', lambda hs=hs: V_.tensor_copy(out=f['Gs'][:, hs], in_=pG[0][:]), reads=['pG0', 'f_Gs'], writes=['f_Gs'])
                    kb.op('act', lambda hs=hs: nc.scalar.activation(out=f['eTot'][:, hs], in_=pG[1][:], func=AF.Exp, scale=-DEC), reads=['pG1', 'f_eTot'], writes=['f_eTot'])
                    kb.op('dve', lambda hs=hs: V_.tensor_tensor(out=f['eRev'][:, hs], in0=pG[1][:], in1=f['Gs'][:, hs], op=ALU.subtract), reads=['pG1', 'f_Gs', 'f_eRev'], writes=['f_eRev'])
                kb.op('act', lambda: nc.scalar.activation(out=f['eRev'][:], in_=f['eRev'][:], func=AF.Exp, scale=-DEC), reads=['f_eRev'], writes=['f_eRev'])
                kb.op('dve', lambda: V_.tensor_tensor(out=f['eGx'][:], in0=f['Gs'][:], in1=f['sw'][:], op=ALU.subtract), reads=['f_Gs', 'f_sw', 'f_eGx'], writes=['f_eGx'])
                kb.op('act', lambda: nc.scalar.activation(out=f['eGx'][:], in_=f['eGx'][:], func=AF.Exp, scale=-DEC), reads=['f_eGx'], writes=['f_eGx'])
                kb.op('dve', lambda: V_.scalar_tensor_tensor(out=tb['KAP'][:], in0=f['kk'][:], scalar=-1.0, in1=f['eGx'][:], op0=ALU.mult, op1=ALU.mult), reads=['f_kk', 'f_eGx', 'tb_KAP'], writes=['tb_KAP'])
                kb.op('dve', lambda: nc.vector.tensor_tensor(out=tb['RM'][:], in0=ld['RR'][:], in1=f['eG'][:], op=ALU.mult), reads=['ld_RR', 'f_eG', 'tb_RM'], writes=['tb_RM'])
                kb.op('dve', lambda: V_.tensor_tensor(out=tb['BP'][:], in0=f['b'][:], in1=f['enG'][:], op=ALU.mult), reads=['f_b', 'f_enG', 'tb_BP'], writes=['tb_BP'])
                kb.op('dve', lambda: nc.vector.tensor_tensor(out=tb['KP'][:], in0=f['kd'][:], in1=f['enG'][:], op=ALU.mult), reads=['f_kd', 'f_enG', 'tb_KP'], writes=['tb_KP'])
                kb.op('dve', lambda: V_.tensor_tensor(out=tb['BC'][:], in0=f['b'][:], in1=f['eRev'][:], op=ALU.mult), reads=['f_b', 'f_eRev', 'tb_BC'], writes=['tb_BC'])
                kb.op('dve', lambda: nc.vector.tensor_tensor(out=tb['KC'][:], in0=f['kd'][:], in1=f['eRev'][:], op=ALU.mult), reads=['f_kd', 'f_eRev', 'tb_KC'], writes=['tb_KC'])
                kb.op('act', lambda: nc.scalar.copy(out=tb['V'][:], in_=ld['VV'][:]), reads=['ld_VV', 'tb_V'], writes=['tb_V'])
                for nm in ('KAP', 'BP', 'KP', 'RM'):
                    pT = pG[0][:].bitcast(BF16)[:, 0:1024].rearrange("p (k t) -> p k t", t=128)

                    def tr(nm=nm, pT=pT):
                        for k in range(8):
                            ins = nc.tensor.transpose(pT[:, k, :], tb[nm][:, k * 128:(k + 1) * 128], C['identb'][:])
                        return ins
                    kb.op('pe', tr, reads=['tb_' + nm, 'identb'], writes=['pG0'])
                    kb.op('act', lambda nm=nm, pT=pT: nc.scalar.copy(out=fT[nm][:], in_=pT), reads=['pG0', 'fT_' + nm], writes=['fT_' + nm])
                for half in range(2):
                    pT = pG[1][:].rearrange("p (k t) -> p k t", t=128)

                    def tr2(half=half, pT=pT):
                        for k in range(4):
                            kk_ = half * 4 + k
                            ins = nc.tensor.transpose(pT[:, k, :], f['eTot'][:, kk_ * 128:(kk_ + 1) * 128], C['identf'][:])
                        return ins
                    kb.op('pe', tr2, reads=['f_eTot', 'identf'], writes=['pG1'])
                    kb.op('dve', lambda half=half, pT=pT: V_.tensor_copy(out=eTT[:, half * 4:(half + 1) * 4, :], in_=pT), reads=['pG1', 'eTT'], writes=['eTT'])
                for par in range(2):
                    kb.op('dve', lambda par=par: V_.tensor_copy(out=wc[:, par::2, :], in_=eTT[par * 64:(par + 1) * 64, :, 0:128:64]), reads=['eTT', 'wc'], writes=['wc'])
                opk = ['tb_KAP', 'tb_RM', 'tb_BC', 'tb_KC', 'tb_V', 'fT_KAP', 'fT_BP', 'fT_KP', 'fT_RM']
                gens = []
                for h in range(16):
                    def mk(h):
                        cs = slice(h * 64, (h + 1) * 64)
                        pb = (h % 2) * 64
                        k8 = h // 2
                        return lambda sl: self.head_scan(st, C, h, 64, 64, pb, tb['KAP'][:, cs], tb['RM'][:, cs], tb['BC'][:, cs], tb['KC'][:, cs], tb['V'][:, cs],
                                   fT['KAP'][pb:pb + 64, k8, :], fT['BP'][pb:pb + 64, k8, :], fT['KP'][pb:pb + 64, k8, :], fT['RM'][pb:pb + 64, k8, :],
                                   (C['SB'][:], 'SB'), (C['SBI'][:], 'SBI'), (C['SBT'][:], 'SBT'), (wc[:, h, :], 'wc'), (T[h], 'T%d' % h), (yt[:, cs], 'yt%d' % h), opk, z, B, sl)
                    gens.append(mk(h))
                self.run_heads(gens)
                kb.dma('sp', self.YZ[z][t0:t0 + 128, :], yt[:], reads=['yt%d' % h for h in range(16)], writes=['YZ%d_%d' % (z, ti)])

    def stage_rwkv_out(self, jl):
        nc, kb = self.nc, self.kb
        with Stage(kb) as st:
            V_ = nc.vector
            idf = st.sb("identf", [128, 128], F32)
            kb.dma('sp', idf[:], self.cst_identf, writes=['identf'])
            idb = st.sb("identb", [128, 128], BF16)
            kb.op('dve', lambda: V_.tensor_copy(out=idb[:], in_=idf[:]), reads=['identf'], writes=['identb'])
            wo = st.sb("wo", [128, 8, 1024], BF16)
            stg = [st.sb("stg%d" % j, [128, 1024], F32) for j in range(2)]
            for k in range(8):
                j = k % 2
                kb.dma('sp', stg[j][:], self.rk_w_o[jl, k * 128:(k + 1) * 128, :], writes=['stg%d' % j])
                kb.op('dve', lambda k=k, j=j: V_.tensor_copy(out=wo[:, k, :], in_=stg[j][:]), reads=['stg%d' % j, 'wo'], writes=['wo'])
            bc = {}
            for nm, src in (('a00', self.rk_a0[jl, 0, :]), ('a01', self.rk_a0[jl, 1, :]), ('k_a', self.rk_k_a[jl, :]), ('r_k', self.rk_r_k[jl].rearrange("h n -> (h n)")),
                            ('lg', self.rk_lnx_g[jl, :]), ('lb', self.rk_lnx_b[jl, :])):
                bc[nm] = self.bcast_load(st, nm, src, 1024)
            ld = {nm: st.sb("ld_" + nm, [128, 1024], F32) for nm in ('Y0', 'Y1', 'RR', 'KK0', 'VV', 'AL0', 'AL1', 'GG')}
            t1 = st.sb("t1", [128, 1024], F32)
            t2 = st.sb("t2", [128, 1024], F32)
            zb = st.sb("zb", [128, 1024], BF16)
            zT = st.sb("zT", [128, 8, 128], BF16)
            sm = st.sb("sm", [128, 4, 16], F32)
            pT = st.ps("pT", [128, 512], F32)
            pO = [st.ps("pO%d" % q, [128, 512], F32) for q in range(2)]
            yo = st.sb("yo", [128, 1024], F32)
            H3 = lambda ap: ap.rearrange("p (h n) -> p h n", n=64)
            B3 = lambda ap: ap.unsqueeze(2).to_broadcast([128, 16, 64])
            for ti in range(NT // 128):
                t0 = ti * 128
                for nm, src, key in (('Y0', self.YZ[0], 'YZ0_%d' % ti), ('Y1', self.YZ[1], 'YZ1_%d' % ti), ('RR', self.RK['RR'], 'RR%d' % ti), ('KK0', self.RK['KK0'], 'KK0%d' % ti),
                                     ('VV', self.RK['VV'], 'VV%d' % ti), ('AL0', self.RK['AL0'], 'AL0%d' % ti), ('AL1', self.RK['AL1'], 'AL1%d' % ti), ('GG', self.RK['GG'], 'GG%d' % ti)):
                    kb.dma('sp', ld[nm][:], src[t0:t0 + 128, :], reads=[key], writes=['ld_' + nm])
                kb.op('dve', lambda: V_.tensor_tensor(out=t1[:], in0=ld['Y0'][:], in1=ld['Y1'][:], op=ALU.add), reads=['ld_Y0', 'ld_Y1', 't1'], writes=['t1'])
                kb.op('dve', lambda: V_.tensor_reduce(out=sm[:, 0, :], in_=H3(t1[:]), axis=AX.X, op=ALU.add), reads=['t1', 'sm'], writes=['sm'])
                kb.op('dve', lambda: V_.tensor_scalar(out=sm[:, 0, :], in0=sm[:, 0, :], scalar1=1.0 / 64, scalar2=None, op0=ALU.mult), reads=['sm'], writes=['sm'])
                kb.op('dve', lambda: V_.tensor_tensor(out=H3(t1[:]), in0=H3(t1[:]), in1=B3(sm[:, 0, :]), op=ALU.subtract), reads=['sm', 't1'], writes=['t1'])
                kb.op('dve', lambda: V_.tensor_tensor(out=t2[:], in0=t1[:], in1=t1[:], op=ALU.mult), reads=['t1', 't2'], writes=['t2'])
                kb.op('dve', lambda: V_.tensor_reduce(out=sm[:, 1, :], in_=H3(t2[:]), axis=AX.X, op=ALU.add), reads=['t2', 'sm'], writes=['sm'])
                kb.op('dve', lambda: V_.tensor_scalar(out=sm[:, 1, :], in0=sm[:, 1, :], scalar1=1.0 / 64, scalar2=64e-5, op0=ALU.mult, op1=ALU.add), reads=['sm'], writes=['sm'])
                kb.op('act', lambda: nc.scalar.activation(out=sm[:, 1, :], in_=sm[:, 1, :], func=AF.Sqrt), reads=['sm'], writes=['sm'])
                kb.op('dve', lambda: V_.reciprocal(out=sm[:, 2, :], in_=sm[:, 1, :]), reads=['sm'], writes=['sm'])
                kb.op('dve', lambda: V_.tensor_tensor(out=H3(t1[:]), in0=H3(t1[:]), in1=B3(sm[:, 2, :]), op=ALU.mult), reads=['sm', 't1'], writes=['t1'])
                kb.op('dve', lambda: V_.tensor_tensor(out=t1[:], in0=t1[:], in1=bc['lg'][:], op=ALU.mult), reads=['t1', 'lg'], writes=['t1'])
                kb.op('dve', lambda: V_.tensor_tensor(out=t1[:], in0=t1[:], in1=bc['lb'][:], op=ALU.add), reads=['t1', 'lb'], writes=['t1'])
                kb.op('dve', lambda: V_.tensor_tensor(out=t2[:], in0=ld['AL0'][:], in1=bc['a00'][:], op=ALU.add), reads=['ld_AL0', 'a00', 't2'], writes=['t2'])
                kb.op('act', lambda: nc.scalar.activation(out=t2[:], in_=t2[:], func=AF.Sigmoid), reads=['t2'], writes=['t2'])
                kb.op('dve', lambda: V_.tensor_tensor(out=ld['AL1'][:], in0=ld['AL1'][:], in1=bc['a01'][:], op=ALU.add), reads=['ld_AL1', 'a01'], writes=['ld_AL1'])
                kb.op('act', lambda: nc.scalar.activation(out=ld['AL1'][:], in_=ld['AL1'][:], func=AF.Sigmoid), reads=['ld_AL1'], writes=['ld_AL1'])
                kb.op('dve', lambda: V_.tensor_tensor(out=t2[:], in0=t2[:], in1=ld['AL1'][:], op=ALU.add), reads=['t2', 'ld_AL1'], writes=['t2'])
                kb.op('dve', lambda: V_.scalar_tensor_tensor(out=t2[:], in0=t2[:], scalar=-2.0, in1=bc['k_a'][:], op0=ALU.add, op1=ALU.mult), reads=['t2', 'k_a'], writes=['t2'])
                kb.op('dve', lambda: V_.scalar_tensor_tensor(out=t2[:], in0=t2[:], scalar=2.0, in1=ld['KK0'][:], op0=ALU.add, op1=ALU.mult), reads=['t2', 'ld_KK0'], writes=['t2'])
                kb.op('dve', lambda: V_.tensor_tensor(out=t2[:], in0=t2[:], in1=ld['RR'][:], op=ALU.mult), reads=['t2', 'ld_RR'], writes=['t2'])
                kb.op('dve', lambda: V_.tensor_tensor(out=t2[:], in0=t2[:], in1=bc['r_k'][:], op=ALU.mult), reads=['t2', 'r_k'], writes=['t2'])
                kb.op('dve', lambda: V_.tensor_reduce(out=sm[:, 3, :], in_=H3(t2[:]), axis=AX.X, op=ALU.add), reads=['t2', 'sm'], writes=['sm'])
                kb.op('dve', lambda: V_.tensor_tensor(out=H3(t2[:]), in0=H3(ld['VV'][:]), in1=B3(sm[:, 3, :]), op=ALU.mult), reads=['sm', 'ld_VV', 't2'], writes=['t2'])
                kb.op('dve', lambda: V_.tensor_tensor(out=t1[:], in0=t1[:], in1=t2[:], op=ALU.add), reads=['t1', 't2'], writes=['t1'])
                kb.op('dve', lambda: V_.tensor_tensor(out=zb[:], in0=t1[:], in1=ld['GG'][:], op=ALU.mult), reads=['t1', 'ld_GG', 'zb'], writes=['zb'])
                pTv = pT[:].bitcast(BF16)[:, 0:1024].rearrange("p (k t) -> p k t", t=128)

                def tr(pTv=pTv):
                    for k in range(8):
                        ins = nc.tensor.transpose(pTv[:, k, :], zb[:, k * 128:(k + 1) * 128], idb[:])
                    return ins
                kb.op('pe', tr, reads=['zb', 'identb'], writes=['pT'])
                kb.op('act', lambda pTv=pTv: nc.scalar.copy(out=zT[:], in_=pTv), reads=['pT', 'zT'], writes=['zT'])
                for half in range(2):
                    def mm(half=half):
                        for k in range(8):
                            ins = nc.tensor.matmul(pO[half][:], lhsT=zT[:, k, :], rhs=wo[:, k, half * 512:(half + 1) * 512], start=(k == 0), stop=(k == 7))
                        return ins
                    kb.op('pe', mm, reads=['zT', 'wo'], writes=['pO%d' % half])
                    kb.op('act' if half else 'dve', (lambda half=half: nc.scalar.copy(out=yo[:, 512:1024], in_=pO[1][:])) if half else
                          (lambda half=half: V_.tensor_copy(out=yo[:, 0:512], in_=pO[0][:])), reads=['pO%d' % half, 'yo'], writes=['yo'])
                kb.dma('sp', self.ACC[t0:t0 + 128, :], yo[:], reads=['yo'], writes=['ACC%d' % ti])

    def stage_rwkv(self, i, jl):
        self.stage_rwkv_proj(jl)
        for z in range(2):
            self.stage_rwkv_scan(jl, z)
        self.stage_rwkv_out(jl)

    def stage_gdn_proj(self, jl):
        nc, kb = self.nc, self.kb
        V_ = nc.vector
        for blk in range(4):
            with Stage(kb) as st:
                ntap = 4 if blk < 3 else 1
                ncol = 1024 if blk < 3 else 1056
                c0 = blk * 1024
                Wc = st.sb("Wc", [128, ntap, 8, ncol], BF16)
                stg = [st.sb("stg%d" % j, [128, 1056], F32) for j in range(2)]
                cw = [self.bcast_load(st, "cw%d" % tp, self.gdn_conv_w[jl, tp, c0:c0 + 1024], 1024) for tp in range(ntap)] if blk < 3 else None
                for k in range(8):
                    j = k % 2
                    kb.dma('sp', stg[j][:, 0:ncol], self.gdn_w_in[jl, k * 128:(k + 1) * 128, c0:c0 + ncol], writes=['stg%d' % j])
                    for tp in range(ntap):
                        if blk < 3:
                            kb.op('dve', lambda k=k, j=j, tp=tp: V_.tensor_tensor(out=Wc[:, tp, k, :], in0=stg[j][:, 0:1024], in1=cw[tp][:], op=ALU.mult), reads=['stg%d' % j, 'cw%d' % tp, 'Wc'], writes=['Wc'])
                        else:
                            kb.op('dve', lambda k=k, j=j: V_.tensor_copy(out=Wc[:, 0, k, :], in_=stg[j][:, 0:ncol]), reads=['stg%d' % j, 'Wc'], writes=['Wc'])
                ht = st.sb("ht", [128, 8, 640], BF16)
                pp = [st.ps("pp%d" % q, [128, 512], F32) for q in range(4)]
                ob = [st.sb("ob%d" % q, [128, 1056], F32) for q in range(2)]
                sq = st.sb("sq", [128, 1024], F32)
                sm = st.sb("sm", [128, 2, 8], F32)
                npp = [0]
                nob = [0]
                for it, (t0, W) in enumerate([(0, TC)] + tiles_of(NT, 512, TC)):
                    isc = (t0 == 0)
                    zl = isc or t0 == TC
                    zr = isc or (t0 + W == NT)
                    if zl:
                        kb.op('dve', lambda: V_.memset(ht[:, :, 0:64], 0.0), reads=['ht'], writes=['ht'])
                    if zr:
                        kb.op('dve', lambda W=W: V_.memset(ht[:, :, 64 + W:128 + W], 0.0), reads=['ht'], writes=['ht'])
                    a = t0 if zl else t0 - 64
                    b = t0 + W if zr else t0 + W + 64
                    kb.dma('sp', ht[:, :, 64 + (a - t0):64 + (b - t0)], self.HT[:, :, a:b], reads=['HT%d' % q for q in range(a // 128, (b + 127) // 128)] + ['ht'], writes=['ht'])
                    for sub in range(W // 128):
                        ts = t0 + sub * 128
                        ti = ts // 128
                        o = nob[0] % 2
                        nob[0] += 1
                        segs = [(0, 512), (512, 1024)] + ([(1024, 1056)] if blk == 3 else [])
                        for (s0, s1) in segs:
                            q = npp[0] % 4
                            npp[0] += 1

                            def f(q=q, s0=s0, s1=s1, sub=sub):
                                n_ = ntap * 8
                                i_ = 0
                                for tp in range(ntap):
                                    off = 64 + sub * 128 + (tp - 2 if blk < 3 else 0)
                                    for k in range(8):
                                        ins = nc.tensor.matmul(pp[q][:, 0:s1 - s0], lhsT=ht[:, k, off:off + 128], rhs=Wc[:, tp, k, s0:s1], start=(i_ == 0), stop=(i_ == n_ - 1))
                                        i_ += 1
                                return ins
                            kb.op('pe', f, reads=['ht', 'Wc'], writes=['pp%d' % q])
                            if blk < 3:
                                kb.op('act', lambda q=q, o=o, s0=s0, s1=s1: nc.scalar.activation(out=ob[o][:, s0:s1], in_=pp[q][:, 0:s1 - s0], func=AF.Silu), reads=['pp%d' % q, 'ob%d' % o], writes=['ob%d' % o])
                            else:
                                kb.op('act', lambda q=q, o=o, s0=s0, s1=s1: nc.scalar.copy(out=ob[o][:, s0:s1], in_=pp[q][:, 0:s1 - s0]), reads=['pp%d' % q, 'ob%d' % o], writes=['ob%d' % o])
                        if blk < 2:
                            O3 = ob[o][:, 0:1024].rearrange("p (h n) -> p h n", n=128)
                            kb.op('dve', lambda o=o: V_.tensor_tensor(out=sq[:], in0=ob[o][:, 0:1024], in1=ob[o][:, 0:1024], op=ALU.mult), reads=['ob%d' % o, 'sq'], writes=['sq'])
                            kb.op('dve', lambda: V_.tensor_reduce(out=sm[:, 0, :], in_=sq[:].rearrange("p (h n) -> p h n", n=128), axis=AX.X, op=ALU.add), reads=['sq', 'sm'], writes=['sm'])
                            kb.op('dve', lambda: V_.tensor_scalar(out=sm[:, 0, :], in0=sm[:, 0, :], scalar1=1e-6, scalar2=None, op0=ALU.add), reads=['sm'], writes=['sm'])
                            kb.op('act', lambda: nc.scalar.activation(out=sm[:, 0, :], in_=sm[:, 0, :], func=AF.Sqrt), reads=['sm'], writes=['sm'])
                            kb.op('dve', lambda: V_.reciprocal(out=sm[:, 1, :], in_=sm[:, 0, :]), reads=['sm'], writes=['sm'])
                            if blk == 0:
                                kb.op('dve', lambda: V_.tensor_scalar(out=sm[:, 1, :], in0=sm[:, 1, :], scalar1=128 ** -0.5, scalar2=None, op0=ALU.mult), reads=['sm'], writes=['sm'])
                            kb.op('dve', lambda O3=O3: V_.tensor_tensor(out=O3, in0=O3, in1=sm[:, 1, :].unsqueeze(2).to_broadcast([128, 8, 128]), op=ALU.mult), reads=['sm', 'ob%d' % o], writes=['ob%d' % o])
                        nm = ('GQ', 'GK', 'GV', 'GZ')[blk]
                        kb.dma('sp', self.GD[nm][ts:ts + 128, :], ob[o][:, 0:1024], reads=['ob%d' % o], writes=['%s%d' % (nm, ti)])
                        if blk == 3:
                            kb.dma('sp', self.GD['GAB'][ts:ts + 128, :], ob[o][:, 1024:1056], reads=['ob%d' % o], writes=['GAB%d' % ti])

    def stage_gdn_scan(self, jl, z):
        nc, kb = self.nc, self.kb
        V_ = nc.vector
        with Stage(kb) as st:
            C = self.scan_consts(st, z)
            B = self.head_bufs(st, F32)
            ones = st.sb("ones", [128, 128], F32)
            kb.op('dve', lambda: V_.memset(ones[:], 1.0), writes=['ones'])
            alog = self.bcast_load(st, "alog", self.gdn_a_log[jl, z, :], 8)
            dtb = self.bcast_load(st, "dtb", self.gdn_dt_bias[jl, z, :], 8)
            kb.op('act', lambda: nc.scalar.activation(out=alog[:], in_=alog[:], func=AF.Exp), reads=['alog'], writes=['alog'])
            T = [st.sb("T%d" % h, [128, 128], F32) for h in range(8)]
            for h in range(8):
                kb.op('dve', lambda h=h: V_.memset(T[h][:], 0.0), writes=['T%d' % h])
            ld = {nm: st.sb("ld_" + nm, [128, 1024], F32) for nm in ('GQ', 'GK', 'GV')}
            ab = st.sb("ab", [128, 32], F32)
            s8 = {nm: st.sb("s8_" + nm, [128, 8], F32) for nm in ('g', 'beta', 'G', 'Gx', 'eGx', 'eG', 'eRev', 'bk', 'bkg', 'bkr', 't', 'eTot', 'nG')}
            tb = {nm: st.sb("tb_" + nm, [128, 1024], BF16) for nm in ('KAP', 'RM', 'BC', 'KC', 'V', 'KU', 'BU', 'KD', 'QU')}
            fT = {nm: st.sb("fT_" + nm, [128, 8, 128], BF16) for nm in ('KU', 'BU', 'KD', 'QU')}
            wc = st.sb("wc", [128, 8, 2], F32)
            pG = [st.ps("pG%d" % q, [128, 512], F32) for q in range(2)]
            B['ps'] = B['ps'] + pG
            B['psk'] = B['psk'] + ['pG0', 'pG1']
            dg = [st.sb("dg%d" % q, [128, 128], F32) for q in range(NSLOT)]
            Dm = [{nm: st.sb("D_%s%d" % (nm, h), [128, 128], F32) for nm in ('x', 'i', 'xT')} for h in range(8)]
            yt = st.sb("yt", [128, 1024], F32)
            H3 = lambda ap: ap.rearrange("p (h n) -> p h n", n=128)
            B3 = lambda ap: ap.unsqueeze(2).to_broadcast([128, 8, 128])
            S8 = ['s8']
            for ti in self.scan_order(z):
                t0 = ti * 128
                for nm in ('GQ', 'GK', 'GV'):
                    kb.dma('sp', ld[nm][:], self.GD[nm][t0:t0 + 128, :], reads=['%s%d' % (nm, ti)], writes=['ld_' + nm])
                kb.dma('sp', ab[:], self.GD['GAB'][t0:t0 + 128, :], reads=['GAB%d' % ti], writes=['ab'])
                kb.op('dve', lambda: V_.tensor_tensor(out=s8['t'][:], in0=ab[:, z * 8:(z + 1) * 8], in1=dtb[:], op=ALU.add), reads=['ab', 'dtb'] + S8, writes=S8)
                kb.op('act', lambda: nc.scalar.activation(out=s8['t'][:], in_=s8['t'][:], func=AF.Exp), reads=S8, writes=S8)
                kb.op('act', lambda: nc.scalar.activation(out=s8['t'][:], in_=s8['t'][:], func=AF.Ln, bias=1.0), reads=S8, writes=S8)
                kb.op('dve', lambda: V_.scalar_tensor_tensor(out=s8['g'][:], in0=s8['t'][:], scalar=-1.0, in1=alog[:], op0=ALU.mult, op1=ALU.mult), reads=S8 + ['alog'], writes=S8)
                kb.op('act', lambda: nc.scalar.activation(out=s8['beta'][:], in_=ab[:, 16 + z * 8:16 + (z + 1) * 8], func=AF.Sigmoid), reads=['ab'] + S8, writes=S8)
                kb.op('pe', lambda: nc.tensor.matmul(pG[0][:, 0:8], lhsT=C['SBI'][:], rhs=s8['g'][:], start=True, stop=True), reads=['SBI'] + S8, writes=['pG0'])
                kb.op('pe', lambda: nc.tensor.matmul(pG[1][:, 0:8], lhsT=C['BLK'][:], rhs=s8['g'][:], start=True, stop=True), reads=['BLK'] + S8, writes=['pG1'])
                kb.op('dve', lambda: V_.tensor_copy(out=s8['G'][:], in_=pG[0][:, 0:8]), reads=['pG0'] + S8, writes=S8)
                kb.op('dve', lambda: V_.tensor_tensor(out=s8['Gx'][:], in0=s8['G'][:], in1=s8['g'][:], op=ALU.subtract), reads=S8, writes=S8)
                kb.op('dve', lambda: V_.tensor_tensor(out=s8['eRev'][:], in0=pG[1][:, 0:8], in1=s8['G'][:], op=ALU.subtract), reads=['pG1'] + S8, writes=S8)
                kb.op('act', lambda: nc.scalar.activation(out=s8['eTot'][:], in_=pG[1][:, 0:8], func=AF.Exp), reads=['pG1'] + S8, writes=S8)
                for nm_o, nm_i in (('eRev', 'eRev'), ('eGx', 'Gx'), ('eG', 'G'), ('bkg', 'g')):
                    kb.op('act', lambda nm_o=nm_o, nm_i=nm_i: nc.scalar.activation(out=s8[nm_o][:], in_=s8[nm_i][:], func=AF.Exp), reads=S8, writes=S8)
                kb.op('dve', lambda: V_.tensor_scalar(out=s8['nG'][:], in0=s8['G'][:], scalar1=-1.0, scalar2=None, op0=ALU.mult), reads=S8, writes=S8)
                kb.op('dve', lambda: V_.tensor_tensor(out=s8['bkg'][:], in0=s8['bkg'][:], in1=s8['beta'][:], op=ALU.mult), reads=S8, writes=S8)
                kb.op('dve', lambda: V_.tensor_tensor(out=s8['bk'][:], in0=s8['bkg'][:], in1=s8['eRev'][:], op=ALU.mult), reads=S8, writes=S8)
                kb.op('dve', lambda: V_.tensor_tensor(out=s8['bkr'][:], in0=s8['beta'][:], in1=s8['eRev'][:], op=ALU.mult), reads=S8, writes=S8)
                kb.op('dve', lambda: V_.tensor_scalar(out=s8['eGx'][:], in0=s8['eGx'][:], scalar1=-1.0, scalar2=None, op0=ALU.mult), reads=S8, writes=S8)
                K3 = H3(ld['GK'][:])
                for nm, sc in (('KAP', 'eGx'), ('BC', 'bk'), ('KC', 'bkr'), ('BU', 'bkg'), ('KD', 'beta')):
                    kb.op('dve', lambda nm=nm, sc=sc: V_.tensor_tensor(out=H3(tb[nm][:]), in0=K3, in1=B3(s8[sc][:]), op=ALU.mult), reads=S8 + ['ld_GK', 'tb_' + nm], writes=['tb_' + nm])
                kb.op('dve', lambda: V_.tensor_scalar(out=tb['KU'][:], in0=ld['GK'][:], scalar1=-1.0, scalar2=None, op0=ALU.mult), reads=['ld_GK', 'tb_KU'], writes=['tb_KU'])
                kb.op('dve', lambda: V_.tensor_tensor(out=H3(tb['RM'][:]), in0=H3(ld['GQ'][:]), in1=B3(s8['eG'][:]), op=ALU.mult), reads=S8 + ['ld_GQ', 'tb_RM'], writes=['tb_RM'])
                kb.op('act', lambda: nc.scalar.copy(out=tb['QU'][:], in_=ld['GQ'][:]), reads=['ld_GQ', 'tb_QU'], writes=['tb_QU'])
                kb.op('act', lambda: nc.scalar.copy(out=tb['V'][:], in_=ld['GV'][:]), reads=['ld_GV', 'tb_V'], writes=['tb_V'])
                for nm in ('KU', 'BU', 'KD', 'QU'):
                    pT = pG[0][:].bitcast(BF16)[:, 0:1024].rearrange("p (k t) -> p k t", t=128)

                    def tr(nm=nm, pT=pT):
                        for k in range(8):
                            ins = nc.tensor.transpose(pT[:, k, :], tb[nm][:, k * 128:(k + 1) * 128], C['identb'][:])
                        return ins
                    kb.op('pe', tr, reads=['tb_' + nm, 'identb'], writes=['pG0'])
                    kb.op('act', lambda nm=nm, pT=pT: nc.scalar.copy(out=fT[nm][:], in_=pT), reads=['pG0', 'fT_' + nm], writes=['fT_' + nm])
                for c in range(2):
                    kb.op('pe', lambda c=c: nc.tensor.matmul(pG[1][:, 0:8], lhsT=C['BLK'][c * 64:(c + 1) * 64, c * 64:c * 64 + 1].to_broadcast([64, 128]) if False else ones[c * 64:(c + 1) * 64, :],
                                                           rhs=s8['g'][c * 64:(c + 1) * 64, :], start=True, stop=True), reads=['ones'] + S8, writes=['pG1'])
                    kb.op('act', lambda c=c: nc.scalar.activation(out=wc[:, :, c], in_=pG[1][:, 0:8], func=AF.Exp), reads=['pG1', 'wc'], writes=['wc'])
                opk = ['tb_KAP', 'tb_RM', 'tb_BC', 'tb_KC', 'tb_V', 'fT_KU', 'fT_BU', 'fT_KD', 'fT_QU']
                gens = []
                for h in range(8):
                    def mk(h):
                        cs = slice(h * 128, (h + 1) * 128)
                        dmh = Dm[h]

                        def gen(sl):
                            pbk, pkey = B['ps'][sl], B['psk'][sl]
                            dgt, dgk = dg[sl], 'dg%d' % sl
                            for (src, dn0, sub_, mk_, flip) in (('G', 'i', 'G', 'SBI', False), ('Gx', 'x', 'G', 'SB', False), ('G', 'xT', 'Gx', 'SBT', True)):
                                d_ = dmh[dn0]
                                dn = dn0 + str(h)
                                kb.op('dve', lambda: V_.tensor_scalar(out=dgt[:], in0=C['identf'][:], scalar1=s8[src][:, h:h + 1], scalar2=None, op0=ALU.mult), reads=['identf', dgk] + S8, writes=[dgk])
                                kb.op('pe', lambda: nc.tensor.matmul(pbk[:, 0:128], lhsT=ones[:], rhs=dgt[:], start=True, stop=True), reads=['ones', dgk], writes=[pkey])
                                yield
                                if not flip:
                                    kb.op('dve', lambda: V_.tensor_scalar(out=d_[:], in0=pbk[:, 0:128], scalar1=s8[sub_][:, h:h + 1], scalar2=0.0, op0=ALU.subtract, op1=ALU.min),
                                          reads=[pkey, 'D_' + dn] + S8, writes=['D_' + dn])
                                else:
                                    kb.op('dve', lambda: V_.tensor_scalar(out=d_[:], in0=pbk[:, 0:128], scalar1=-1.0, scalar2=s8[sub_][:, h:h + 1], op0=ALU.mult, op1=ALU.add),
                                          reads=[pkey, 'D_' + dn] + S8, writes=['D_' + dn])
                                    kb.op('dve', lambda: V_.tensor_scalar(out=d_[:], in0=d_[:], scalar1=0.0, scalar2=None, op0=ALU.min), reads=['D_' + dn], writes=['D_' + dn])
                                yield
                                kb.op('act', lambda: nc.scalar.activation(out=d_[:], in_=d_[:], func=AF.Exp), reads=['D_' + dn], writes=['D_' + dn])
                                yield
                                kb.op('dve', lambda: V_.tensor_tensor(out=d_[:], in0=d_[:], in1=C[mk_][:], op=ALU.mult), reads=['D_' + dn, mk_], writes=['D_' + dn])
                                yield
                            yield from self.head_scan(st, C, h, 128, 128, 0, tb['KAP'][:, cs], tb['RM'][:, cs], tb['BC'][:, cs], tb['KC'][:, cs], tb['V'][:, cs],
                                   fT['KU'][:, h, :], fT['BU'][:, h, :], fT['KD'][:, h, :], fT['QU'][:, h, :],
                                   (dmh['x'][:], 'D_x%d' % h), (dmh['i'][:], 'D_i%d' % h), (dmh['xT'][:], 'D_xT%d' % h), (wc[:, h, :], 'wc'), (T[h], 'T%d' % h), (yt[:, cs], 'yt%d' % h), opk, z, B, sl, F32)
                        return gen
                    gens.append(mk(h))
                self.run_heads(gens)
                kb.dma('sp', self.YZ[z][t0:t0 + 128, :], yt[:], reads=['yt%d' % h for h in range(8)], writes=['YZ%d_%d' % (z, ti)])

    def stage_gdn_out(self, jl):
        nc, kb = self.nc, self.kb
        V_ = nc.vector
        with Stage(kb) as st:
            idf = st.sb("identf", [128, 128], F32)
            kb.dma('sp', idf[:], self.cst_identf, writes=['identf'])
            idb = st.sb("identb", [128, 128], BF16)
            kb.op('dve', lambda: V_.tensor_copy(out=idb[:], in_=idf[:]), reads=['identf'], writes=['identb'])
            wo = st.sb("wo", [128, 8, 1024], BF16)
            stg = [st.sb("stg%d" % j, [128, 1024], F32) for j in range(2)]
            for k in range(8):
                j = k % 2
                kb.dma('sp', stg[j][:], self.gdn_w_o[jl, k * 128:(k + 1) * 128, :], writes=['stg%d' % j])
                kb.op('dve', lambda k=k, j=j: V_.tensor_copy(out=wo[:, k, :], in_=stg[j][:]), reads=['stg%d' % j, 'wo'], writes=['wo'])
            nw = self.bcast_load(st, "nw", self.gdn_norm_w[jl, :], 128)
            ld = {nm: st.sb("ld_" + nm, [128, 1024], F32) for nm in ('Y0', 'Y1', 'GZ')}
            t2 = st.sb("t2", [128, 1024], F32)
            zb = st.sb("zb", [128, 1024], BF16)
            zT = st.sb("zT", [128, 8, 128], BF16)
            sm = st.sb("sm", [128, 2, 8], F32)
            pT = st.ps("pT", [128, 512], F32)
            pO = [st.ps("pO%d" % q, [128, 512], F32) for q in range(2)]
            yo = st.sb("yo", [128, 1024], F32)
            H3 = lambda ap: ap.rearrange("p (h n) -> p h n", n=128)
            for ti in range(NT // 128):
                t0 = ti * 128
                for nm, src, key in (('Y0', self.YZ[0], 'YZ0_%d' % ti), ('Y1', self.YZ[1], 'YZ1_%d' % ti), ('GZ', self.GD['GZ'], 'GZ%d' % ti)):
                    kb.dma('sp', ld[nm][:], src[t0:t0 + 128, :], reads=[key], writes=['ld_' + nm])
                kb.op('dve', lambda: V_.tensor_tensor(out=ld['Y0'][:], in0=ld['Y0'][:], in1=ld['Y1'][:], op=ALU.add), reads=['ld_Y0', 'ld_Y1'], writes=['ld_Y0'])
                kb.op('dve', lambda: V_.tensor_tensor(out=t2[:], in0=ld['Y0'][:], in1=ld['Y0'][:], op=ALU.mult), reads=['ld_Y0', 't2'], writes=['t2'])
                kb.op('dve', lambda: V_.tensor_reduce(out=sm[:, 0, :], in_=H3(t2[:]), axis=AX.X, op=ALU.add), reads=['t2', 'sm'], writes=['sm'])
                kb.op('dve', lambda: V_.tensor_scalar(out=sm[:, 0, :], in0=sm[:, 0, :], scalar1=1.0 / 128, scalar2=1e-6, op0=ALU.mult, op1=ALU.add), reads=['sm'], writes=['sm'])
                kb.op('act', lambda: nc.scalar.activation(out=sm[:, 0, :], in_=sm[:, 0, :], func=AF.Sqrt), reads=['sm'], writes=['sm'])
                kb.op('dve', lambda: V_.reciprocal(out=sm[:, 1, :], in_=sm[:, 0, :]), reads=['sm'], writes=['sm'])
                kb.op('dve', lambda: V_.tensor_tensor(out=H3(ld['Y0'][:]), in0=H3(ld['Y0'][:]), in1=sm[:, 1, :].unsqueeze(2).to_broadcast([128, 8, 128]), op=ALU.mult), reads=['sm', 'ld_Y0'], writes=['ld_Y0'])
                kb.op('dve', lambda: V_.tensor_tensor(out=H3(ld['Y0'][:]), in0=H3(ld['Y0'][:]), in1=nw[:].unsqueeze(1).to_broadcast([128, 8, 128]), op=ALU.mult), reads=['nw', 'ld_Y0'], writes=['ld_Y0'])
                kb.op('act', lambda: nc.scalar.activation(out=t2[:], in_=ld['GZ'][:], func=AF.Silu), reads=['ld_GZ', 't2'], writes=['t2'])
                kb.op('dve', lambda: V_.tensor_tensor(out=zb[:], in0=ld['Y0'][:], in1=t2[:], op=ALU.mult), reads=['ld_Y0', 't2', 'zb'], writes=['zb'])
                pTv = pT[:].bitcast(BF16)[:, 0:1024].rearrange("p (k t) -> p k t", t=128)

                def tr(pTv=pTv):
                    for k in range(8):
                        ins = nc.tensor.transpose(pTv[:, k, :], zb[:, k * 128:(k + 1) * 128], idb[:])
                    return ins
                kb.op('pe', tr, reads=['zb', 'identb'], writes=['pT'])
                kb.op('act', lambda pTv=pTv: nc.scalar.copy(out=zT[:], in_=pTv), reads=['pT', 'zT'], writes=['zT'])
                for half in range(2):
                    def mm(half=half):
                        for k in range(8):
                            ins = nc.tensor.matmul(pO[half][:], lhsT=zT[:, k, :], rhs=wo[:, k, half * 512:(half + 1) * 512], start=(k == 0), stop=(k == 7))
                        return ins
                    kb.op('pe', mm, reads=['zT', 'wo'], writes=['pO%d' % half])
                    kb.op('dve', lambda half=half: V_.tensor_copy(out=yo[:, half * 512:(half + 1) * 512], in_=pO[half][:]), reads=['pO%d' % half, 'yo'], writes=['yo'])
                kb.dma('sp', self.ACC[t0:t0 + 128, :], yo[:], reads=['yo'], writes=['ACC%d' % ti])

    def stage_gdn(self, i, jl):
        self.stage_gdn_proj(jl)
        for z in range(2):
            self.stage_gdn_scan(jl, z)
        self.stage_gdn_out(jl)

    def decls(self):
        nc, kb = self.nc, self.kb
        self.xin = self.din("xin", [NT, D])
        self.cvec = self.din("cvec", [2, D])
        self.ada_w = self.din("ada_w", [4, D, 6 * D])
        self.ada_b = self.din("ada_b", [4, 6 * D])
        self.ln_g = self.din("ln_g", [4, 2, D])
        self.ln_b = self.din("ln_b", [4, 2, D])
        self.pool_w = self.din("pool_w", [1, 4, 256, 256])
        self.pool_scale = self.din("pool_scale", [1, D])
        self.ffn_w1 = self.din("ffn_w1", [2, D, 2816])
        self.ffn_w3 = self.din("ffn_w3", [2, D, 2816])
        self.ffn_w2 = self.din("ffn_w2", [2, 2816, D])
        self.moe_router_w = self.din("moe_router_w", [2, D, 8])
        self.moe_router_b = self.din("moe_router_b", [2, 8])
        self.moe_w1 = self.din("moe_w1", [2, 8, D, 1408])
        self.moe_w3 = self.din("moe_w3", [2, 8, D, 1408])
        self.moe_w2 = self.din("moe_w2", [2, 8, 1408, D])
        for nm, shp in (("rk_mu", [2, 6, D]), ("rk_w_rkv", [2, 3, D, D]), ("rk_w0", [2, 2, D]), ("rk_w1", [2, 2, D, 64]), ("rk_w2", [2, 2, 64, D]),
                        ("rk_a0", [2, 2, D]), ("rk_a1", [2, 2, D, 64]), ("rk_a2", [2, 2, 64, D]), ("rk_g1", [2, D, 128]), ("rk_g2", [2, 128, D]),
                        ("rk_k_k", [2, D]), ("rk_k_a", [2, D]), ("rk_r_k", [2, 16, 64]), ("rk_lnx_g", [2, D]), ("rk_lnx_b", [2, D]), ("rk_w_o", [2, D, D])):
            setattr(self, nm, self.din(nm, shp))
        for nm, shp in (("gdn_w_in", [1, D, 4128]), ("gdn_conv_w", [1, 4, 3072]), ("gdn_a_log", [1, 2, 8]), ("gdn_dt_bias", [1, 2, 8]),
                        ("gdn_norm_w", [1, 128]), ("gdn_w_o", [1, D, D])):
            setattr(self, nm, self.din(nm, shp))
        self.GD = {nm: self.dscr("GD_" + nm, [NT, D]) for nm in ("GQ", "GK", "GV", "GZ")}
        self.GD["GAB"] = self.dscr("GD_GAB", [NT, 32])
        self.cst_masks = self.din("cst_masks", [2, 4, 128, 128])
        self.RK = {nm: self.dscr("RK_" + nm, [NT, D]) for nm in ("RR", "KK0", "VV", "DL0", "DL1", "AL0", "AL1", "GG")}
        self.YZ = [self.dscr("YZ%d" % z, [NT, D]) for z in range(2)]
        self.cst_identf = self.din("cst_identf", [128, 128])
        self.cst_invc64 = self.din("cst_invc64", [128, 4, 64])
        self.cst_invc256 = self.din("cst_invc256", [128, 4, 256])
        self.out = self.dout("out", [NT - TC, D])
        self.XS = self.dscr("XS", [NT, D])
        self.ACC = self.dscr("ACC", [NT, D])
        self.HT = self.dscr("HT", [128, 8, NT], BF16)
        self.GATES = self.dscr("GATES", [NT, 8])
        self.MODD = self.dscr("MODD", [4, 2, 6 * D])

    def stage_init(self):
        kb = self.kb
        for (t0, W) in tiles_of(NT, 128):
            kb.dma('sp', self.XS[t0:t0 + W, :], self.xin[t0:t0 + W, :], writes=['XSinit%d' % t0])
        kb.barrier()

    def build(self):
        nc, kb = self.nc, self.kb
        self.decls()
        self.stage_init()
        for i in self.layers:
            last = (i == self.layers[-1])
            self.stage_mod(i)
            self.stage_prep(i, 1)
            kind, jl = i % 3, i // 3
            if kind == 1:
                self.stage_pool(jl)
            elif kind == 0:
                self.stage_rwkv(i, jl)
            else:
                self.stage_gdn(i, jl)
            self.stage_finish(i, 1)
            e = i // 2
            if i % 2 == 0:
                self.stage_prep(i, 2)
                for hf in range(2):
                    sl = slice(hf * 1408, (hf + 1) * 1408)
                    self.stage_ffnpass(self.ffn_w1[e, :, sl], self.ffn_w3[e, :, sl], self.ffn_w2[e, sl, :], None, hf == 0)
            else:
                self.stage_prep(i, 2, router=e)
                for x in range(8):
                    self.stage_ffnpass(self.moe_w1[e, x], self.moe_w3[e, x], self.moe_w2[e, x], x, x == 0)
            self.stage_finish(i, 2, out_final=self.out if last else None)
        kb.barrier()
        return nc


def _pool_invc(L):
    t = np.arange(L)
    out = np.zeros((4, L), np.float32)
    for gi, w in enumerate((2, 4, 8, 16)):
        lo = np.clip(t - w // 2, 0, L)
        hi = np.clip(t + w // 2, 0, L)
        out[gi] = 1.0 / (hi - lo)
    return np.ascontiguousarray(np.broadcast_to(out[None], (128, 4, L))).astype(np.float32)


def _masks():
    s = np.arange(128)[:, None]
    t = np.arange(128)[None, :]
    same = (s // 64) == (t // 64)
    m = np.zeros((2, 4, 128, 128), np.float32)
    for z in range(2):
        sb = ((s < t) if z == 0 else (s > t)) & same
        sbi = ((s <= t) if z == 0 else (s >= t)) & same
        m[z, 0] = sb
        m[z, 1] = sbi
        m[z, 2] = sb.T
        m[z, 3] = same
    return m


def make_consts():
    return {
        "cst_masks": _masks(),
        "cst_identf": np.eye(128, dtype=np.float32),
        "cst_invc64": _pool_invc(64),
        "cst_invc256": _pool_invc(256),
    }


WEIGHT_KEYS = ["ada_w", "ada_b", "ln_g", "ln_b", "pool_w", "pool_scale", "ffn_w1", "ffn_w3", "ffn_w2",
               "moe_router_w", "moe_router_b", "moe_w1", "moe_w3", "moe_w2"]


def run(inputs, layers=(0, 1, 2, 3), n_cores=4, xin_override=None):
    prog = Prog(list(layers))
    nc = prog.build()
    print("instructions:", prog.kb.nins, "sems:", len(prog.kb.sems))
    cst = make_consts()
    in_maps = []
    for cidx in range(n_cores):
        b = cidx % 4
        m = {}
        if xin_override is not None:
            m["xin"] = xin_override[b]
        else:
            m["xin"] = np.ascontiguousarray(np.concatenate([inputs["ctx"][b], inputs["x"][b]], axis=0))
        m["cvec"] = np.ascontiguousarray(np.stack([inputs["c"][b], inputs["c_ctx"]], axis=0))
        for k in prog.inp:
            if k in m:
                continue
            m[k] = cst[k] if k in cst else np.ascontiguousarray(inputs[k])
        in_maps.append(m)
    res = run_bass_kernel_spmd(nc, in_maps, core_ids=list(range(n_cores)))
    return res


def kernel(**inputs):
    inputs = {k: np.asarray(v) for k, v in inputs.items()}
    res = run(inputs)
    out = np.stack([res.results[b]["out"] for b in range(4)], axis=0)
    return out.astype(np.float32)
```

```python
import contextlib
import numpy as np
import concourse.bass as bass
import concourse.mybir as mybir
from concourse.bass_utils import run_bass_kernel_spmd

F32 = mybir.dt.float32
BF16 = mybir.dt.bfloat16
AF = mybir.ActivationFunctionType
ALU = mybir.AluOpType
AX = mybir.AxisListType

D = 1024
NT = 8448
TC = 256
DEPTH = 4
ALPHA = (2 * DEPTH) ** 0.25
LN_EPS = 1e-5
NSLOT = 8


class KB:
    EPOCH = 30000
    NDMA = 24

    def __init__(self, nc):
        self.nc = nc
        self._ctx = []
        self.engs = {'pe': nc.tensor, 'dve': nc.vector, 'act': nc.scalar, 'pool': nc.gpsimd, 'sp': nc.sync}
        self.sems = []
        self.esem = {}
        self.ecnt = {}
        for e in ('pe', 'dve', 'act', 'pool'):
            self.esem[e] = self._newsem('c_' + e)
            self.ecnt[e] = 0
        self.dsem = [self._newsem('d%d' % i) for i in range(self.NDMA)]
        self.dcnt = [0] * self.NDMA
        self.dnext = 0
        self.waited = {e: {} for e in self.engs}
        self.res = {}
        self.nins = 0
        self.allsems = {}
        self.excl = set()

    def _newsem(self, name):
        cm = self.nc.semaphore(name + '_%d' % len(self.sems))
        h = cm.__enter__()
        self._ctx.append(cm)
        self.sems.append(h)
        return len(self.sems) - 1

    def _r(self, key):
        r = self.res.get(key)
        if r is None:
            r = {'w': None, 'r': {}}
            self.res[key] = r
        return r

    def _waits(self, eng, reads, writes):
        need = {}

        def add(tok):
            if tok is None:
                return
            s, v = tok
            if need.get(s, 0) < v:
                need[s] = v
        for k in reads:
            add(self._r(k)['w'])
        for k in writes:
            r = self._r(k)
            add(r['w'])
            for s, v in r['r'].items():
                add((s, v))
        wd = self.waited[eng]
        for s, v in need.items():
            if wd.get(s, 0) >= v:
                continue
            self.engs[eng].wait_ge(self.sems[s], v)
            self.nins += 1
            wd[s] = v

    def _mark(self, tok, reads, writes):
        s, v = tok
        self.allsems[s] = v
        for k in writes:
            r = self._r(k)
            r['w'] = tok
            r['r'] = {}
        for k in reads:
            if k in writes:
                continue
            r = self._r(k)
            if r['r'].get(s, 0) < v:
                r['r'][s] = v

    def op(self, eng, fn, reads=(), writes=()):
        ex = [k for k in reads if k in self.excl and k not in writes]
        if ex:
            writes = list(writes) + ex
        self._waits(eng, reads, writes)
        ins = fn()
        if self.ecnt[eng] >= self.EPOCH:
            self.esem[eng] = self._newsem('c_' + eng)
            self.ecnt[eng] = 0
        s = self.esem[eng]
        ins.then_inc(self.sems[s], 1)
        self.ecnt[eng] += 1
        self.nins += 1
        tok = (s, self.ecnt[eng])
        self._mark(tok, reads, writes)
        return tok

    def dma(self, q, out, in_, reads=(), writes=(), **kw):
        j = self.dnext
        self.dnext = (self.dnext + 1) % self.NDMA
        wd = self.waited[q]
        if self.dcnt[j] > 0 and wd.get(self.dsem[j], 0) < 16 * self.dcnt[j]:
            self.engs[q].wait_ge(self.sems[self.dsem[j]], 16 * self.dcnt[j])
            wd[self.dsem[j]] = 16 * self.dcnt[j]
        self._waits(q, reads, writes)
        self.engs[q].dma_start(out=out, in_=in_, **kw).then_inc(self.sems[self.dsem[j]], 16)
        self.dcnt[j] += 1
        self.nins += 1
        tok = (self.dsem[j], 16 * self.dcnt[j])
        self._mark(tok, reads, writes)
        return tok

    def barrier(self):
        for e in self.engs:
            wd = self.waited[e]
            for s, v in self.allsems.items():
                if wd.get(s, 0) >= v:
                    continue
                self.engs[e].wait_ge(self.sems[s], v)
                self.nins += 1
                wd[s] = v
        self.res = {}


class Stage:
    _n = [0]

    def __init__(self, kb):
        self.kb = kb
        self.nc = kb.nc
        self.es = contextlib.ExitStack()
        Stage._n[0] += 1
        self.sid = Stage._n[0]

    def __enter__(self):
        self.es.__enter__()
        return self

    def sb(self, name, shape, dt):
        return self.es.enter_context(self.nc.sbuf_tensor('%s_s%d' % (name, self.sid), shape, dt))

    def ps(self, name, shape, dt):
        self.kb.excl.add(name)
        return self.es.enter_context(self.nc.psum_tensor('%s_s%d' % (name, self.sid), shape, dt))

    def __exit__(self, *a):
        self.kb.barrier()
        return self.es.__exit__(*a)


def tiles_of(total, w, start=0):
    out = []
    t = start
    while t < total:
        out.append((t, min(w, total - t)))
        t += w
    return out


class Prog:
    def __init__(self, layers, debug_outs=()):
        self.layers = layers
        nc = bass.Bass("TRN2", target_bir_lowering=False)
        self.nc = nc
        self.kb = KB(nc)
        self.inp = {}
        self.debug_outs = debug_outs

    def din(self, name, shape, dt=F32):
        t = self.nc.dram_tensor(name, list(shape), dt, kind="ExternalInput").ap()
        self.inp[name] = t
        return t

    def dscr(self, name, shape, dt=F32):
        return self.nc.dram_tensor(name, list(shape), dt, kind="Internal").ap()

    def dout(self, name, shape, dt=F32):
        return self.nc.dram_tensor(name, list(shape), dt, kind="ExternalOutput").ap()

    def bcast_load(self, st, name, row_ap, n, dt=F32):
        t = st.sb(name, [128, n], dt)
        self.kb.dma('sp', t[:], row_ap.partition_broadcast(128), writes=[name])
        return t

    def stage_mod(self, i):
        nc, kb = self.nc, self.kb
        with Stage(kb) as st:
            cT = st.sb("cT", [128, 8, 2], F32)
            sT = st.sb("sT", [128, 8, 2], F32)
            modsb = st.sb("modsb", [2, 6144], F32)
            adab = st.sb("adab", [2, 6144], F32)
            wst = [st.sb("wst%d" % j, [128, 8, 512], F32) for j in range(2)]
            pm_ = [st.ps("pm%d" % j, [128, 512], F32) for j in range(2)]
            pm = [t[0:2, :] for t in pm_]
            for r in range(2):
                kb.dma('sp', cT[:, :, r], self.cvec[r, :].rearrange("(k p) -> p k", p=128),
                       reads=['cT'] if r else [], writes=['cT'], allow_slow_non_contiguous=True)
                kb.dma('sp', adab[r:r + 1, :], self.ada_b[i:i + 1, :], reads=['adab'] if r else [], writes=['adab'])
            kb.op('act', lambda: nc.scalar.activation(out=sT[:], in_=cT[:], func=AF.Silu), reads=['cT'], writes=['sT'])
            for g in range(12):
                j = g % 2
                kb.dma('sp', wst[j][:], self.ada_w[i, :, g * 512:(g + 1) * 512].rearrange("(k p) n -> p k n", p=128),
                       writes=['wst%d' % j])

                def mm(j=j):
                    for k in range(8):
                        ins = nc.tensor.matmul(pm[j][:], lhsT=sT[:, k, :], rhs=wst[j][:, k, :], start=(k == 0), stop=(k == 7))
                    return ins
                kb.op('pe', mm, reads=['sT', 'wst%d' % j], writes=['pm%d' % j])
                kb.op('dve', lambda j=j, g=g: nc.vector.tensor_tensor(out=modsb[:, g * 512:(g + 1) * 512], in0=pm[j][:],
                                                                     in1=adab[:, g * 512:(g + 1) * 512], op=ALU.add),
                      reads=['pm%d' % j, 'adab', 'modsb'], writes=['modsb'])
            for c0 in (1024, 4096):
                kb.op('dve', lambda c0=c0: nc.vector.tensor_scalar(out=modsb[:, c0:c0 + 1024], in0=modsb[:, c0:c0 + 1024],
                                                                 scalar1=1.0, scalar2=None, op0=ALU.add),
                      reads=['modsb'], writes=['modsb'])
            kb.dma('sp', self.MODD[i], modsb[:], reads=['modsb'], writes=['MODD'])

    def mod_tiles(self, st, i, slots):
        out = {}
        for s in slots:
            for r in range(2):
                out[(s, r)] = self.bcast_load(st, "mod%d_%d" % (s, r), self.MODD[i, r, s * 1024:(s + 1) * 1024], 1024)
        return out

    def stage_prep(self, i, sub, router=None):
        nc, kb = self.nc, self.kb
        with Stage(kb) as st:
            sh_s, sc_s = (0, 1) if sub == 1 else (3, 4)
            md = self.mod_tiles(st, i, [sh_s, sc_s])
            identf = st.sb("identf", [128, 128], F32)
            kb.dma('sp', identf[:], self.cst_identf, writes=['identf'])
            NB = 2
            xt = [st.sb("xt%d" % j, [128, 1024], F32) for j in range(NB)]
            hf = [st.sb("hf%d" % j, [128, 1024], F32) for j in range(NB)]
            hTb = [st.sb("hTb%d" % j, [128, 8, 128], BF16) for j in range(NB)]
            pT = [st.ps("pT%d" % j, [128, 8, 128], F32) for j in range(NB)]
            if router is not None:
                e = router
                rw = st.sb("rw", [128, 8, 8], F32)
                kb.dma('sp', rw[:], self.moe_router_w[e].rearrange("(k p) n -> p k n", p=128), writes=['rw'])
                rb = self.bcast_load(st, "rb", self.moe_router_b[e, :], 8)
                hTf = [st.sb("hTf%d" % j, [128, 8, 128], F32) for j in range(NB)]
                pl_ = [st.ps("pl%d" % j, [128, 512], F32) for j in range(NB)]
                pl = [t[:, 0:8] for t in pl_]
                lg = [st.sb("lg%d" % j, [128, 8], F32) for j in range(NB)]
                m1 = [st.sb("m1_%d" % j, [128, 8], F32) for j in range(NB)]
                m2 = [st.sb("m2_%d" % j, [128, 8], F32) for j in range(NB)]
                l2 = [st.sb("l2_%d" % j, [128, 8], F32) for j in range(NB)]
                mx = [st.sb("mx%d" % j, [128, 4], F32) for j in range(NB)]
                gt = [st.sb("gt%d" % j, [128, 8], F32) for j in range(NB)]
            def _ldx(tq):
                kb.dma('sp', xt[tq % NB][:], self.XS[tq * 128:(tq + 1) * 128, :], writes=['xt%d' % (tq % NB)])
            _ldx(0)
            for ti in range(NT // 128):
                j = ti % NB
                isc = 1 if ti < TC // 128 else 0
                t0 = ti * 128
                X, H, HB, PT = 'xt%d' % j, 'hf%d' % j, 'hTb%d' % j, 'pT%d' % j
                if ti + 1 < NT // 128:
                    _ldx(ti + 1)
                kb.op('dve', lambda j=j, isc=isc: nc.vector.tensor_tensor(out=hf[j][:], in0=xt[j][:], in1=md[(sc_s, isc)][:], op=ALU.mult),
                      reads=[X, 'mod%d_%d' % (sc_s, isc)], writes=[H])
                kb.op('dve', lambda j=j, isc=isc: nc.vector.tensor_tensor(out=hf[j][:], in0=hf[j][:], in1=md[(sh_s, isc)][:], op=ALU.add),
                      reads=[H, 'mod%d_%d' % (sh_s, isc)], writes=[H])

                def tr(j=j):
                    for k in range(8):
                        ins = nc.tensor.transpose(pT[j][:, k, :], hf[j][:, k * 128:(k + 1) * 128], identf[:])
                    return ins
                kb.op('pe', tr, reads=[H, 'identf'], writes=[PT])
                if router is None:
                    kb.op('act', lambda j=j: nc.scalar.copy(out=hTb[j][:], in_=pT[j][:]), reads=[PT], writes=[HB])
                else:
                    kb.op('dve', lambda j=j: nc.vector.tensor_copy(out=hTf[j][:], in_=pT[j][:]), reads=[PT], writes=['hTf%d' % j])
                    kb.op('act', lambda j=j: nc.scalar.copy(out=hTb[j][:], in_=hTf[j][:]), reads=['hTf%d' % j], writes=[HB])
                kb.dma('sp', self.HT[:, :, t0:t0 + 128], hTb[j][:], reads=[HB], writes=['HT%d' % ti])
                import os
                RD = int(os.environ.get('RD', '9'))
                if router is not None and RD >= 1:
                    HF, PL, LG = 'hTf%d' % j, 'pl%d' % j, 'lg%d' % j

                    def rmm(j=j):
                        for k in range(8):
                            ins = nc.tensor.matmul(pl[j][:], lhsT=hTf[j][:, k, :], rhs=rw[:, k, :], start=(k == 0), stop=(k == 7))
                        return ins
                    kb.op('pe', rmm, reads=[HF, 'rw'], writes=[PL])
                    G = 'g%d' % j
                    if RD < 2:
                        continue
                    kb.op('dve', lambda j=j: nc.vector.tensor_tensor(out=lg[j][:], in0=pl[j][:], in1=rb[:], op=ALU.add), reads=[PL, 'rb', G], writes=[G])
                    if RD < 3:
                        kb.dma('sp', self.GATES[t0:t0 + 128, :], lg[j][:], reads=[G], writes=['GATES%d' % ti])
                        continue
                    kb.op('dve', lambda j=j: nc.vector.tensor_reduce(out=mx[j][:, 0:1], in_=lg[j][:], axis=AX.X, op=ALU.max), reads=[G], writes=[G])
                    kb.op('dve', lambda j=j: nc.vector.tensor_scalar(out=m1[j][:], in0=lg[j][:], scalar1=mx[j][:, 0:1], scalar2=None, op0=ALU.is_equal), reads=[G], writes=[G])
                    kb.op('dve', lambda j=j: nc.vector.scalar_tensor_tensor(out=l2[j][:], in0=m1[j][:], scalar=-1e30, in1=lg[j][:], op0=ALU.mult, op1=ALU.add), reads=[G], writes=[G])
                    kb.op('dve', lambda j=j: nc.vector.tensor_reduce(out=mx[j][:, 1:2], in_=l2[j][:], axis=AX.X, op=ALU.max), reads=[G], writes=[G])
                    kb.op('dve', lambda j=j: nc.vector.tensor_scalar(out=m2[j][:], in0=l2[j][:], scalar1=mx[j][:, 1:2], scalar2=None, op0=ALU.is_equal), reads=[G], writes=[G])
                    kb.op('dve', lambda j=j: nc.vector.tensor_tensor(out=mx[j][:, 2:3], in0=mx[j][:, 0:1], in1=mx[j][:, 1:2], op=ALU.subtract), reads=[G], writes=[G])
                    kb.op('act', lambda j=j: nc.scalar.activation(out=mx[j][:, 2:3], in_=mx[j][:, 2:3], func=AF.Sigmoid), reads=[G], writes=[G])
                    kb.op('dve', lambda j=j: nc.vector.tensor_scalar(out=mx[j][:, 3:4], in0=mx[j][:, 2:3], scalar1=-1.0, scalar2=1.0, op0=ALU.mult, op1=ALU.add), reads=[G], writes=[G])
                    kb.op('dve', lambda j=j: nc.vector.tensor_scalar(out=gt[j][:], in0=m1[j][:], scalar1=mx[j][:, 2:3], scalar2=None, op0=ALU.mult), reads=[G], writes=[G])
                    kb.op('dve', lambda j=j: nc.vector.scalar_tensor_tensor(out=gt[j][:], in0=m2[j][:], scalar=mx[j][:, 3:4], in1=gt[j][:], op0=ALU.mult, op1=ALU.add), reads=[G], writes=[G])
                    kb.dma('sp', self.GATES[t0:t0 + 128, :], gt[j][:], reads=[G], writes=['GATES%d' % ti])

    def stage_ffnpass(self, w1, w3, w2, gate_col, first):
        nc, kb = self.nc, self.kb
        with Stage(kb) as st:
            w1b = st.sb("w1b", [128, 8, 1408], BF16)
            w3b = st.sb("w3b", [128, 8, 1408], BF16)
            w2b = st.sb("w2b", [128, 11, 1024], BF16)
            stg = [st.sb("stg%d" % j, [128, 1408], F32) for j in range(3)]
            n = 0
            for (dst, src, nk, key) in ((w1b, w1, 8, 'w1b'), (w3b, w3, 8, 'w3b'), (w2b, w2, 11, 'w2b')):
                width = src.shape[1]
                for k in range(nk):
                    j = n % 3
                    n += 1
                    kb.dma('sp', stg[j][:, 0:width], src[k * 128:(k + 1) * 128, :], writes=['stg%d' % j])
                    if n % 2:
                        kb.op('act', lambda dst=dst, k=k, j=j, width=width: nc.scalar.copy(out=dst[:, k, :], in_=stg[j][:, 0:width]),
                              reads=['stg%d' % j, key], writes=[key])
                    else:
                        kb.op('dve', lambda dst=dst, k=k, j=j, width=width: nc.vector.tensor_copy(out=dst[:, k, :], in_=stg[j][:, 0:width]),
                              reads=['stg%d' % j, key], writes=[key])
            hT = [st.sb("hT%d" % j, [128, 8, 512], BF16) for j in range(2)]
            act = [st.sb("act%d" % j, [128, 11, 512], BF16) for j in range(2)]
            sg = [st.sb("sg%d" % j, [128, 512], F32) for j in range(2)]
            pA = [st.ps("pA%d" % j, [128, 512], F32) for j in range(2)]
            pB = [st.ps("pB%d" % j, [128, 512], F32) for j in range(2)]
            pY = [st.ps("pY%d" % j, [128, 512], F32) for j in range(2)]
            yo = [st.sb("yo%d" % j, [128, 1024], F32) for j in range(2)]
            ya = [st.sb("ya%d" % j, [128, 1024], F32) for j in range(4)]
            gtl = [st.sb("gtl%d" % j, [128, 8], F32) for j in range(4)]
            nf = 0
            ny = 0
            _tl = tiles_of(NT, 512)

            def _ldh(itq):
                tq0, Wq = _tl[itq]
                kb.dma('sp', hT[itq % 2][:, :, 0:Wq], self.HT[:, :, tq0:tq0 + Wq], reads=['HT%d' % q for q in range(tq0 // 128, (tq0 + Wq) // 128)],
                       writes=['hT%d' % (itq % 2)])
            _ldh(0)
            for it, (t0, W) in enumerate(_tl):
                j = it % 2
                if it + 1 < len(_tl):
                    _ldh(it + 1)
                for sub in range(W // 128):
                    ts_ = t0 + sub * 128
                    if not first:
                        kb.dma('sp', ya[sub][:], self.ACC[ts_:ts_ + 128, :], reads=['ACC%d' % (ts_ // 128)], writes=['ya%d' % sub])
                    if gate_col is not None:
                        kb.dma('sp', gtl[sub][:], self.GATES[ts_:ts_ + 128, :], reads=['GATES%d' % (ts_ // 128)], writes=['gtl%d' % sub])
                for f in range(11):
                    jf = nf % 2
                    nf += 1

                    def mmA(jf=jf, f=f, j=j, W=W, wb=w1b, pp=pA):
                        for k in range(8):
                            ins = nc.tensor.matmul(pp[jf][:, 0:W], lhsT=wb[:, k, f * 128:(f + 1) * 128], rhs=hT[j][:, k, 0:W],
                                                   start=(k == 0), stop=(k == 7))
                        return ins
                    kb.op('pe', mmA, reads=['hT%d' % j, 'w1b'], writes=['pA%d' % jf])
                    kb.op('pe', lambda jf=jf, f=f, j=j, W=W: mmA(jf, f, j, W, w3b, pB), reads=['hT%d' % j, 'w3b'], writes=['pB%d' % jf])
                    kb.op('act', lambda jf=jf, W=W: nc.scalar.activation(out=sg[jf][:, 0:W], in_=pA[jf][:, 0:W], func=AF.Silu),
                          reads=['pA%d' % jf], writes=['sg%d' % jf])
                    kb.op('dve', lambda jf=jf, f=f, j=j, W=W: nc.vector.tensor_tensor(out=act[j][:, f, 0:W], in0=sg[jf][:, 0:W], in1=pB[jf][:, 0:W], op=ALU.mult),
                          reads=['sg%d' % jf, 'pB%d' % jf, 'act%d' % j], writes=['act%d' % j])
                for sub in range(W // 128):
                    jy = ny % 2
                    ny += 1
                    ti = t0 // 128 + sub
                    ts = t0 + sub * 128
                    for half in range(2):
                        jp = half

                        def mmY(jp=jp, half=half, sub=sub, j=j):
                            for f in range(11):
                                ins = nc.tensor.matmul(pY[jp][:], lhsT=act[j][:, f, sub * 128:(sub + 1) * 128],
                                                       rhs=w2b[:, f, half * 512:(half + 1) * 512], start=(f == 0), stop=(f == 10))
                            return ins
                        kb.op('pe', mmY, reads=['act%d' % j, 'w2b'], writes=['pY%d' % jp])
                        osl = slice(half * 512, (half + 1) * 512)
                        rd = ['pY%d' % jp, 'yo%d' % jy]
                        if gate_col is not None and not first:
                            kb.op('dve', lambda jp=jp, jy=jy, osl=osl, sub=sub: nc.vector.scalar_tensor_tensor(
                                out=yo[jy][:, osl], in0=pY[jp][:], scalar=gtl[sub][:, gate_col:gate_col + 1], in1=ya[sub][:, osl],
                                op0=ALU.mult, op1=ALU.add), reads=rd + ['gtl%d' % sub, 'ya%d' % sub], writes=['yo%d' % jy])
                        elif gate_col is not None:
                            kb.op('dve', lambda jp=jp, jy=jy, osl=osl, sub=sub: nc.vector.tensor_scalar(
                                out=yo[jy][:, osl], in0=pY[jp][:], scalar1=gtl[sub][:, gate_col:gate_col + 1], scalar2=None, op0=ALU.mult),
                                reads=rd + ['gtl%d' % sub], writes=['yo%d' % jy])
                        elif not first:
                            kb.op('dve', lambda jp=jp, jy=jy, osl=osl, sub=sub: nc.vector.tensor_tensor(
                                out=yo[jy][:, osl], in0=pY[jp][:], in1=ya[sub][:, osl], op=ALU.add),
                                reads=rd + ['ya%d' % sub], writes=['yo%d' % jy])
                        else:
                            kb.op('act', lambda jp=jp, jy=jy, osl=osl, sub=sub: nc.scalar.copy(out=yo[jy][:, osl], in_=pY[jp][:]),
                                  reads=rd, writes=['yo%d' % jy])
                    kb.dma('sp', self.ACC[ts:ts + 128, :], yo[jy][:], reads=['yo%d' % jy], writes=['ACC%d' % ti])

    def stage_finish(self, i, sub, out_final=None):
        nc, kb = self.nc, self.kb
        with Stage(kb) as st:
            gs = 2 if sub == 1 else 5
            md = self.mod_tiles(st, i, [gs])
            lng = self.bcast_load(st, "lng", self.ln_g[i, sub - 1, :], 1024)
            lnb = self.bcast_load(st, "lnb", self.ln_b[i, sub - 1, :], 1024)
            NB = 3
            xt = [st.sb("xt%d" % j, [128, 1024], F32) for j in range(NB)]
            at = [st.sb("at%d" % j, [128, 1024], F32) for j in range(NB)]
            stt = [st.sb("stt%d" % j, [128, 2, 6], F32) for j in range(NB)]
            mv = [st.sb("mv%d" % j, [128, 4], F32) for j in range(NB)]
            def _ld(tq):
                jq = tq % NB
                kb.dma('sp', xt[jq][:], self.XS[tq * 128:(tq + 1) * 128, :], writes=['xt%d' % jq])
                kb.dma('sp', at[jq][:], self.ACC[tq * 128:(tq + 1) * 128, :], reads=['ACC%d' % tq], writes=['at%d' % jq])
            _ld(0)
            for ti in range(NT // 128):
                j = ti % NB
                isc = 1 if ti < TC // 128 else 0
                t0 = ti * 128
                X, A, S = 'xt%d' % j, 'at%d' % j, 'st%d' % j
                if ti + 1 < NT // 128:
                    _ld(ti + 1)
                kb.op('dve', lambda j=j, isc=isc: nc.vector.tensor_tensor(out=at[j][:], in0=at[j][:], in1=md[(gs, isc)][:], op=ALU.mult),
                      reads=[A, 'mod%d_%d' % (gs, isc)], writes=[A])
                kb.op('dve', lambda j=j: nc.vector.scalar_tensor_tensor(out=xt[j][:], in0=xt[j][:], scalar=float(ALPHA), in1=at[j][:], op0=ALU.mult, op1=ALU.add),
                      reads=[X, A], writes=[X])

                def bn(j=j):
                    nc.vector.bn_stats(out=stt[j][:, 0, :], in_=xt[j][:, 0:512])
                    return nc.vector.bn_stats(out=stt[j][:, 1, :], in_=xt[j][:, 512:1024])
                kb.op('dve', bn, reads=[X, S], writes=[S])
                kb.op('dve', lambda j=j: nc.vector.bn_aggr(out=mv[j][:, 0:2], in_=stt[j][:]), reads=[S], writes=[S])
                kb.op('dve', lambda j=j: nc.vector.tensor_scalar(out=mv[j][:, 2:3], in0=mv[j][:, 1:2], scalar1=LN_EPS, scalar2=None, op0=ALU.add), reads=[S], writes=[S])
                kb.op('act', lambda j=j: nc.scalar.activation(out=mv[j][:, 2:3], in_=mv[j][:, 2:3], func=AF.Sqrt), reads=[S], writes=[S])
                kb.op('dve', lambda j=j: nc.vector.reciprocal(out=mv[j][:, 3:4], in_=mv[j][:, 2:3]), reads=[S], writes=[S])
                kb.op('dve', lambda j=j: nc.vector.tensor_scalar(out=xt[j][:], in0=xt[j][:], scalar1=mv[j][:, 0:1], scalar2=mv[j][:, 3:4],
                                                             op0=ALU.subtract, op1=ALU.mult), reads=[X, S], writes=[X])
                kb.op('dve', lambda j=j: nc.vector.tensor_tensor(out=xt[j][:], in0=xt[j][:], in1=lng[:], op=ALU.mult), reads=[X, 'lng'], writes=[X])
                kb.op('dve', lambda j=j: nc.vector.tensor_tensor(out=xt[j][:], in0=xt[j][:], in1=lnb[:], op=ALU.add), reads=[X, 'lnb'], writes=[X])
                kb.dma('sp', self.XS[t0:t0 + 128, :], xt[j][:], reads=[X], writes=['XS%d' % ti])
                if out_final is not None and ti >= TC // 128:
                    kb.dma('sp', out_final[t0 - TC:t0 - TC + 128, :], xt[j][:], reads=[X], writes=['OUT%d' % ti])

    def stage_pool(self, j_layer):
        nc, kb = self.nc, self.kb
        with Stage(kb) as st:
            pw = st.sb("pw", [128, 4, 2, 256], BF16)
            pst = [st.sb("pst%d" % j, [128, 256], F32) for j in range(2)]
            n = 0
            for gi in range(4):
                for kk in range(2):
                    j = n % 2
                    n += 1
                    kb.dma('sp', pst[j][:], self.pool_w[j_layer, gi, kk * 128:(kk + 1) * 128, :], writes=['pst%d' % j])
                    kb.op('dve', lambda gi=gi, kk=kk, j=j: nc.vector.tensor_copy(out=pw[:, gi, kk, :], in_=pst[j][:]), reads=['pst%d' % j, 'pw'], writes=['pw'])
            psc = self.bcast_load(st, "psc", self.pool_scale[j_layer, :], 1024)
            invc = st.sb("invc", [128, 4, 64], F32)
            kb.dma('sp', invc[:], self.cst_invc64, writes=['invc'])
            invcc = st.sb("invcc", [128, 4, 256], F32)
            kb.dma('sp', invcc[:], self.cst_invc256, writes=['invcc'])
            hT = [st.sb("hT%d" % j, [128, 8, 512], BF16) for j in range(2)]
            P = st.sb("P", [128, 8, 8, 80], F32)
            S2 = st.sb("S2", [128, 8, 8, 80], F32)
            S4 = st.sb("S4", [128, 8, 8, 80], F32)
            S8 = st.sb("S8", [128, 8, 8, 80], F32)
            S16 = st.sb("S16", [128, 8, 8, 80], F32)
            pl = [st.sb("pl%d" % j, [128, 8, 512], BF16) for j in range(2)]
            tmp = st.sb("tmp", [128, 2, 8, 64], F32)
            pY = [st.ps("pY%d" % j, [128, 1024], F32) for j in range(2)]
            yo = [st.sb("yo%d" % j, [128, 1024], F32) for j in range(2)]
            for b_ in (P, S2, S4, S8, S16):
                pass
            kb.op('pool', lambda: nc.gpsimd.memset(P[:], 0.0), writes=['P'])
            ny = 0
            for it, (t0, W) in enumerate([(0, 256)] + tiles_of(NT, 512, 256)):
                j = it % 2
                isc = (t0 == 0)
                kb.dma('sp', hT[j][:, :, 0:W], self.HT[:, :, t0:t0 + W], reads=['HT%d' % q for q in range(t0 // 128, (t0 + W) // 128)],
                       writes=['hT%d' % j])
                if isc:
                    Pv = P[:].rearrange("p k r c -> p k (r c)")[:, :, 0:272].rearrange("p k (r c) -> p k r c", r=1)
                    views = [b_[:].rearrange("p k r c -> p k (r c)")[:, :, 0:272].rearrange("p k (r c) -> p k r c", r=1) for b_ in (P, S2, S4, S8, S16)]
                    L = 256
                    R = 1
                else:
                    if it == 1:
                        kb.op('pool', lambda: nc.gpsimd.memset(P[:], 0.0), reads=['P'], writes=['P'])
                    views = [b_[:] for b_ in (P, S2, S4, S8, S16)]
                    L = 64
                    R = 8
                Pv, S2v, S4v, S8v, S16v = views
                LP = L + 16
                kb.op('act', lambda Pv=Pv, j=j, L=L, R=R, W=W: nc.scalar.copy(out=Pv[:, :, :, 8:8 + L], in_=hT[j][:, :, 0:W].rearrange("p k (r c) -> p k r c", r=R)),
                      reads=['hT%d' % j, 'P'], writes=['P'])
                kb.op('dve', lambda: nc.vector.tensor_tensor(out=S2v[:, :, :, 0:LP - 1], in0=Pv[:, :, :, 0:LP - 1], in1=Pv[:, :, :, 1:LP], op=ALU.add), reads=['P', 'S2'], writes=['S2'])
                kb.op('dve', lambda: nc.vector.tensor_tensor(out=S4v[:, :, :, 0:LP - 3], in0=S2v[:, :, :, 0:LP - 3], in1=S2v[:, :, :, 2:LP - 1], op=ALU.add), reads=['S2', 'S4'], writes=['S4'])
                kb.op('dve', lambda: nc.vector.tensor_tensor(out=S8v[:, :, :, 0:LP - 7], in0=S4v[:, :, :, 0:LP - 7], in1=S4v[:, :, :, 4:LP - 3], op=ALU.add), reads=['S4', 'S8'], writes=['S8'])
                kb.op('dve', lambda: nc.vector.tensor_tensor(out=S16v[:, :, :, 0:LP - 15], in0=S8v[:, :, :, 0:LP - 15], in1=S8v[:, :, :, 8:LP - 7], op=ALU.add), reads=['S8', 'S16'], writes=['S16'])
                for gi, (win, Sv, key) in enumerate(((2, S2v, 'S2'), (4, S4v, 'S4'), (8, S8v, 'S8'), (16, S16v, 'S16'))):
                    o = 8 - win // 2
                    ic = (invcc if isc else invc)
                    icv = ic[:, gi, :].unsqueeze(1).unsqueeze(1).to_broadcast([128, 2, R, L])
                    tv = tmp[:].rearrange("p k r c -> p k (r c)")[:, :, 0:W].rearrange("p k (r c) -> p k r c", r=R)
                    kb.op('dve', lambda Sv=Sv, gi=gi, o=o, icv=icv, tv=tv, L=L: nc.vector.tensor_tensor(out=tv, in0=Sv[:, 2 * gi:2 * gi + 2, :, o:o + L], in1=icv, op=ALU.mult),
                          reads=[key, 'invc', 'invcc', 'tmp'], writes=['tmp'])
                    kb.op('dve', lambda gi=gi, tv=tv, j=j, Pv=Pv, L=L, R=R, W=W: nc.vector.tensor_tensor(
                        out=pl[j][:, 2 * gi:2 * gi + 2, 0:W].rearrange("p k (r c) -> p k r c", r=R), in0=tv, in1=Pv[:, 2 * gi:2 * gi + 2, :, 8:8 + L], op=ALU.subtract),
                        reads=['tmp', 'P', 'pl%d' % j], writes=['pl%d' % j])
                for sub in range(W // 128):
                    jy = ny % 2
                    ny += 1
                    ti = t0 // 128 + sub
                    ts = t0 + sub * 128

                    def mm(jy=jy, sub=sub, j=j):
                        for gi in range(4):
                            for kk in range(2):
                                ins = nc.tensor.matmul(pY[jy][:, gi * 256:(gi + 1) * 256], lhsT=pl[j][:, 2 * gi + kk, sub * 128:(sub + 1) * 128],
                                                       rhs=pw[:, gi, kk, :], start=(kk == 0), stop=(kk == 1))
                        return ins
                    kb.op('pe', mm, reads=['pl%d' % j, 'pw'], writes=['pY%d' % jy])
                    kb.op('dve', lambda jy=jy: nc.vector.tensor_tensor(out=yo[jy][:], in0=pY[jy][:], in1=psc[:], op=ALU.mult), reads=['pY%d' % jy, 'psc', 'yo%d' % jy], writes=['yo%d' % jy])
                    kb.dma('sp', self.ACC[ts:ts + 128, :], yo[jy][:], reads=['yo%d' % jy], writes=['ACC%d' % ti])

    def scan_consts(self, st, z):
        kb = self.kb
        c = {}
        for nm in ('SB', 'SBI', 'SBT', 'BLK'):
            t = st.sb(nm, [128, 128], F32)
            kb.dma('sp', t[:], self.cst_masks[z, {'SB': 0, 'SBI': 1, 'SBT': 2, 'BLK': 3}[nm]], writes=[nm])
            c[nm] = t
        idf = st.sb("identf", [128, 128], F32)
        kb.dma('sp', idf[:], self.cst_identf, writes=['identf'])
        idb = st.sb("identb", [128, 128], BF16)
        kb.op('dve', lambda: self.nc.vector.tensor_copy(out=idb[:], in_=idf[:]), reads=['identf'], writes=['identb'])
        c['identf'] = idf
        c['identb'] = idb
        return c

    def head_scan(self, st, C, hid, dk, dv, pb, KAP, RM, BCt, KCt, V, KAPT, BPT, KPT, RMT, Dx, Di, DxT, wc, T, yout, opk, z, bufs, slot=0, ndt=BF16):
        nc, kb = self.nc, self.kb
        B = bufs
        par = slot
        ps = B['ps']

        def nps():
            return ps[slot], B['psk'][slot]
        sfx = '_%d' % par

        def S(name):
            return B[name][par], name + sfx
        idf, idb = C['identf'], C['identb']

        def mm_ev(name, lhsT, rhs, mul=None, mulkey=None, eng='dve', rows=128, cols=128, extra=None, reads=()):
            p, pk = nps()
            dst, dk_ = S(name)

            def f():
                ins = nc.tensor.matmul(p[0:rows, 0:cols], lhsT=lhsT, rhs=rhs, start=True, stop=(extra is None))
                if extra is not None:
                    for qi, (l2, r2) in enumerate(extra):
                        ins = nc.tensor.matmul(p[0:rows, 0:cols], lhsT=l2, rhs=r2, start=False, stop=(qi == len(extra) - 1))
                return ins
            kb.op('pe', f, reads=list(reads), writes=[pk])
            if mul is not None:
                kb.op('dve', lambda: nc.vector.tensor_tensor(out=dst[0:rows, 0:cols], in0=p[0:rows, 0:cols], in1=mul, op=ALU.mult),
                      reads=[pk, mulkey, dk_], writes=[dk_])
            elif eng == 'act':
                kb.op('act', lambda: nc.scalar.copy(out=dst[0:rows, 0:cols], in_=p[0:rows, 0:cols]), reads=[pk, dk_], writes=[dk_])
            else:
                kb.op('dve', lambda: nc.vector.tensor_copy(out=dst[0:rows, 0:cols], in_=p[0:rows, 0:cols]), reads=[pk, dk_], writes=[dk_])
            return dst, dk_
        OK = list(opk)
        N_, Nk = mm_ev('N', BPT, KAPT, mul=Dx[0], mulkey=Dx[1], reads=OK)
        yield
        NT_, NTk = mm_ev('NT', KAPT, BPT, mul=DxT[0], mulkey=DxT[1], reads=OK)
        yield
        BtT, BtTk = mm_ev('BtT', KPT, KAPT, mul=Dx[0], mulkey=Dx[1], reads=OK)
        yield
        AbT, AbTk = mm_ev('AbT', BPT, RMT, mul=Di[0], mulkey=Di[1], reads=OK)
        yield
        AkT, AkTk = mm_ev('AkT', KPT, RMT, mul=Di[0], mulkey=Di[1], reads=OK)
        yield
        R_, Rk = S('R')
        kb.op('dve', lambda: nc.vector.tensor_tensor(out=R_[:], in0=N_[:], in1=idf[:], op=ALU.add), reads=[Nk, 'identf', Rk], writes=[Rk])
        yield
        P, Pk, PT, PTk = N_, Nk, NT_, NTk
        for it in range(5):
            PT2, PT2k = mm_ev('PT%d' % (it % 2), P[:], PT[:], eng='act', reads=[Pk, PTk])
            yield
            if it < 4:
                P2, P2k = mm_ev('P%d' % (it % 2), PT[:], P[:], eng='act', reads=[Pk, PTk])
                yield
            R2_, R2k = S('R' if it % 2 else 'Rb')
            Rn, Rnk = mm_ev('Rb' if it % 2 == 0 else 'R', (idb if ndt == BF16 else idf)[:], R_[:], extra=[(PT2[:], R_[:])], eng='dve', reads=['identb', 'identf', PT2k, Rk])
            yield
            R_, Rk = Rn, Rnk
            if it < 4:
                P, Pk, PT, PTk = P2, P2k, PT2, PT2k
        if ndt == BF16:
            MTb, MTbk = R_, Rk
        else:
            MTb, MTbk = S('MTb')
            kb.op('act', lambda: nc.scalar.copy(out=MTb[:], in_=R_[:]), reads=[Rk, MTbk], writes=[MTbk])
            yield
        X0, X0k = mm_ev('X0', BtT[:], V, cols=dv, reads=[BtTk] + OK)
        yield
        U0, U0k = mm_ev('U0', MTb[:], X0[:, 0:dv], cols=dv, eng='act', reads=[MTbk, X0k])
        yield
        MK, MKk = mm_ev('MK', MTb[:], KAP, cols=dk, reads=[MTbk] + OK)
        yield
        RstT, RstTk = mm_ev('RstT', MK[:, 0:dk], AbT[:], rows=dk, extra=[(RM, idb[:])], eng='act', reads=[MKk, AbTk, 'identb'] + OK)
        yield
        Y0, Y0k = mm_ev('Y0', AbT[:], U0[:, 0:dv], cols=dv, extra=[(AkT[:], V)], reads=[AbTk, U0k, AkTk] + OK)
        yield
        Phi = {}
        Z0 = {}
        for c in range(2):
            rs = slice(c * 64, (c + 1) * 64)
            p, pk = nps()
            kb.op('pe', lambda p=p, rs=rs: nc.tensor.matmul(p[0:dk, 0:dk], lhsT=MK[rs, 0:dk], rhs=BCt[rs, :], start=True, stop=True), reads=[MKk] + OK, writes=[pk])
            yield
            dst, dkey = S('Phi%d' % c)
            kb.op('dve', lambda p=p, dst=dst, c=c: nc.vector.scalar_tensor_tensor(out=dst[0:dk, 0:dk], in0=idf[0:dk, 0:dk], scalar=wc[0][:, c:c + 1], in1=p[0:dk, 0:dk],
                                                                               op0=ALU.mult, op1=ALU.add), reads=[pk, 'identf', wc[1], dkey], writes=[dkey])
            Phi[c] = (dst, dkey)
            Z0[c] = mm_ev('Z0%d' % c, BCt[rs, :], U0[rs, 0:dv], rows=dk, cols=dv, extra=[(KCt[rs, :], V[rs, :])], eng='act', reads=[U0k] + OK)
            yield
        Tt, Tk = T
        for c in ((0, 1) if z == 0 else (1, 0)):
            rs = slice(c * 64, (c + 1) * 64)
            p, pk = nps()
            kb.op('pe', lambda p=p: nc.tensor.matmul(p[:, 0:dv], lhsT=RstT[0:dk, :], rhs=Tt[:], start=True, stop=True), reads=[RstTk, Tk], writes=[pk])
            yield
            kb.op('dve', lambda p=p, rs=rs: nc.vector.tensor_tensor(out=yout[0][rs, :], in0=p[rs, 0:dv], in1=Y0[rs, 0:dv], op=ALU.add), reads=[pk, Y0k, yout[1]], writes=[yout[1]])
            yield
            p2, pk2 = nps()

            def tm(p2=p2, c=c):
                nc.tensor.matmul(p2[0:dk, 0:dv], lhsT=Phi[c][0][0:dk, 0:dk], rhs=Tt[:], start=True, stop=False)
                return nc.tensor.matmul(p2[0:dk, 0:dv], lhsT=idf[0:dk, 0:dk], rhs=Z0[c][0][0:dk, 0:dv], start=False, stop=True)
            kb.op('pe', tm, reads=[Phi[c][1], Z0[c][1], Tk, 'identf'], writes=[pk2])
            yield
            kb.op('act', lambda p2=p2: nc.scalar.copy(out=Tt[:], in_=p2[0:dk, 0:dv]), reads=[pk2, Tk], writes=[Tk])
            yield

    def run_heads(self, gens):
        gens = list(gens)
        active = {}
        free = list(range(NSLOT))
        while gens or active:
            while gens and free:
                sl = free.pop(0)
                active[sl] = gens.pop(0)(sl)
            for sl in list(active.keys()):
                try:
                    next(active[sl])
                except StopIteration:
                    del active[sl]
                    free.append(sl)

    def run_pipeline(self, tiles):
        n = len(tiles)
        NS = NSLOT - 2
        op_done = [False] * n
        heads_left = [list(t[1]) for t in tiles]
        heads_active = [0] * n
        stored = [False] * n
        cur_op = None
        cur_op_idx = -1
        next_op = 0
        active = {}
        free = list(range(NS))
        head_tile = 0
        while not all(stored):
            progressed = False
            if cur_op is None and next_op < n and (next_op < 2 or (not heads_left[next_op - 2] and heads_active[next_op - 2] == 0 and stored[next_op - 2])):
                cur_op = tiles[next_op][0]()
                cur_op_idx = next_op
                next_op += 1
            if cur_op is not None:
                try:
                    next(cur_op)
                except StopIteration:
                    op_done[cur_op_idx] = True
                    cur_op = None
                progressed = True
            while free and head_tile < n and op_done[head_tile]:
                if heads_left[head_tile]:
                    sl = free.pop(0)
                    active[sl] = (head_tile, heads_left[head_tile].pop(0)(sl))
                    heads_active[head_tile] += 1
                else:
                    if heads_active[head_tile] == 0:
                        tiles[head_tile][2]()
                        stored[head_tile] = True
                        head_tile += 1
                        progressed = True
                    else:
                        break
            for sl in list(active.keys()):
                tix, g = active[sl]
                try:
                    next(g)
                except StopIteration:
                    del active[sl]
                    free.append(sl)
                    heads_active[tix] -= 1
                progressed = True
            assert progressed, "pipeline stuck"

    def head_bufs(self, st, ndt=BF16):
        B = {}
        for nm, dt in (('N', ndt), ('NT', ndt), ('R', ndt), ('Rb', ndt), ('P0', ndt), ('P1', ndt), ('PT0', ndt), ('PT1', ndt),
                       ('BtT', BF16), ('AbT', BF16), ('AkT', BF16), ('MTb', BF16), ('X0', BF16), ('U0', BF16), ('MK', BF16),
                       ('RstT', F32), ('Y0', F32), ('Phi0', F32), ('Phi1', F32), ('Z00', F32), ('Z01', F32)):
            B[nm] = [st.sb("%s_%d" % (nm, q), [128, 128], dt) for q in range(NSLOT)]
        B['ps'] = [st.ps("hps%d" % q, [128, 512], F32) for q in range(NSLOT - 2)]
        B['psk'] = ['hps%d' % q for q in range(NSLOT - 2)]
        return B

    def scan_order(self, z):
        nt = NT // 128
        nct = TC // 128
        if z == 0:
            return list(range(nt))
        return list(range(nct - 1, -1, -1)) + list(range(nt - 1, nct - 1, -1))

    def stage_rwkv_proj(self, jl):
        nc, kb = self.nc, self.kb
        with Stage(kb) as st:
            NCOL = 3072 + 384
            Wb = st.sb("Wb", [128, 8, NCOL], BF16)
            stg = [st.sb("stg%d" % j, [128, 1024], F32) for j in range(2)]
            n = 0
            srcs = [(self.rk_w_rkv[jl, 0], 0, 1024), (self.rk_w_rkv[jl, 1], 1024, 1024), (self.rk_w_rkv[jl, 2], 2048, 1024),
                    (self.rk_w1[jl, 0], 3072, 64), (self.rk_w1[jl, 1], 3136, 64), (self.rk_a1[jl, 0], 3200, 64), (self.rk_a1[jl, 1], 3264, 64),
                    (self.rk_g1[jl], 3328, 128)]
            for (src, c0, wd) in srcs:
                for k in range(8):
                    j = n % 2
                    n += 1
                    kb.dma('sp', stg[j][:, 0:wd], src[k * 128:(k + 1) * 128, :], writes=['stg%d' % j])
                    kb.op('act' if n % 2 else 'dve', (lambda k=k, j=j, c0=c0, wd=wd: nc.scalar.copy(out=Wb[:, k, c0:c0 + wd], in_=stg[j][:, 0:wd])) if n % 2 else
                          (lambda k=k, j=j, c0=c0, wd=wd: nc.vector.tensor_copy(out=Wb[:, k, c0:c0 + wd], in_=stg[j][:, 0:wd])), reads=['stg%d' % j, 'Wb'], writes=['Wb'])
            W2 = st.sb("W2", [128, 3, 1024], BF16)
            for q, srcl in enumerate(([self.rk_w2[jl, 0], self.rk_w2[jl, 1]], [self.rk_a2[jl, 0], self.rk_a2[jl, 1]], [self.rk_g2[jl]])):
                j = q % 2
                r0 = 0
                for si, src in enumerate(srcl):
                    rws = src.shape[0]
                    kb.dma('sp', stg[j][r0:r0 + rws, :], src, reads=['stg%d' % j] if si else [], writes=['stg%d' % j])
                    r0 += rws
                kb.op('dve', lambda q=q, j=j: nc.vector.tensor_copy(out=W2[:, q, :], in_=stg[j][:]), reads=['stg%d' % j, 'W2'], writes=['W2'])
            mu = st.sb("mu", [128, 6, 8], F32)
            for m in range(6):
                kb.dma('sp', mu[:, m, :], self.rk_mu[jl, m, :].rearrange("(k p) -> p k", p=128), reads=['mu'] if m else [], writes=['mu'], allow_slow_non_contiguous=True)
            HW_ = 512 + 128
            ht = st.sb("ht", [128, 8, HW_], BF16)
            xx = st.sb("xx", [128, 8, 512], BF16)
            xm = st.sb("xm", [128, 6, 8, 512], BF16)
            l1 = st.sb("l1", [128, 3, 512], BF16)
            pp = [st.ps("pp%d" % q, [128, 512], F32) for q in range(4)]
            ob = [st.sb("ob%d" % q, [128, 1024], F32) for q in range(4)]
            npp = [0]
            nob = [0]
            tiles = [(0, TC)] + tiles_of(NT, 512, TC)
            for it, (t0, W) in enumerate(tiles):
                isc = (t0 == 0)
                lo = t0 - 64
                hi = t0 + W + 64
                zl = isc or t0 == TC
                zr = isc or (t0 + W == NT)
                if zl:
                    kb.op('dve', lambda: nc.vector.memset(ht[:, :, 0:64], 0.0), reads=['ht'], writes=['ht'])
                if zr:
                    kb.op('dve', lambda W=W: nc.vector.memset(ht[:, :, 64 + W:128 + W], 0.0), reads=['ht'], writes=['ht'])
                a = t0 if zl else lo
                b = t0 + W if zr else hi
                kb.dma('sp', ht[:, :, 64 + (a - t0):64 + (b - t0)], self.HT[:, :, a:b], reads=['HT%d' % q for q in range(a // 128, (b + 127) // 128)] + ['ht'], writes=['ht'])
                for k in range(8):
                    if isc:
                        off = 63 if k < 4 else 65
                    else:
                        off = (63, 63, 65, 65, 0, 0, 128, 128)[k]
                    kb.op('dve', lambda k=k, off=off, W=W: nc.vector.tensor_tensor(out=xx[:, k, 0:W], in0=ht[:, k, off:off + W], in1=ht[:, k, 64:64 + W], op=ALU.subtract),
                          reads=['ht', 'xx'], writes=['xx'])
                    if not isc and k < 4:
                        col = 0 if k < 2 else 63
                        kb.op('dve', lambda k=k, col=col, W=W: nc.vector.tensor_scalar(
                            out=xx[:, k, 0:W].rearrange("p (r c) -> p r c", c=64)[:, :, col], in0=ht[:, k, 64:64 + W].rearrange("p (r c) -> p r c", c=64)[:, :, col],
                            scalar1=-1.0, scalar2=None, op0=ALU.mult), reads=['ht', 'xx'], writes=['xx'])
                for m in range(6):
                    for k in range(8):
                        eng = 'dve'
                        E = nc.vector if eng == 'dve' else nc.gpsimd
                        kb.op(eng, lambda m=m, k=k, W=W, E=E: E.scalar_tensor_tensor(out=xm[:, m, k, 0:W], in0=xx[:, k, 0:W], scalar=mu[:, m, k:k + 1], in1=ht[:, k, 64:64 + W],
                                                                                 op0=ALU.mult, op1=ALU.add), reads=['xx', 'ht', 'mu', 'xm%d' % m], writes=['xm%d' % m])
                for c, (m, fn) in enumerate(((1, AF.Tanh), (4, AF.Copy), (5, AF.Sigmoid))):
                    q = npp[0] % 4
                    npp[0] += 1

                    def f(q=q, c=c, m=m, W=W):
                        for k in range(8):
                            ins = nc.tensor.matmul(pp[q][:, 0:W], lhsT=Wb[:, k, 3072 + c * 128:3072 + (c + 1) * 128], rhs=xm[:, m, k, 0:W], start=(k == 0), stop=(k == 7))
                        return ins
                    kb.op('pe', f, reads=['Wb', 'xm%d' % m], writes=['pp%d' % q])
                    kb.op('act', lambda q=q, c=c, fn=fn, W=W: nc.scalar.activation(out=l1[:, c, 0:W], in_=pp[q][:, 0:W], func=fn), reads=['pp%d' % q, 'l1'], writes=['l1'])
                for sub in range(W // 128):
                    ts = t0 + sub * 128
                    ti = ts // 128
                    ss = slice(sub * 128, (sub + 1) * 128)
                    outs = [('RR', 0, 0, None), ('KK0', 2, 1024, None), ('VV', 3, 2048, None),
                            ('DL0', None, 0, (0, 0, 64)), ('DL1', None, 0, (0, 64, 128)), ('AL0', None, 1, (1, 0, 64)), ('AL1', None, 1, (1, 64, 128)), ('GG', None, 2, (2, 0, 128))]
                    for (nm, m, c0, l2) in outs:
                        o = nob[0] % 4
                        nob[0] += 1
                        for half in range(2):
                            q = npp[0] % 4
                            npp[0] += 1
                            if l2 is None:
                                def f(q=q, m=m, c0=c0, half=half, ss=ss):
                                    for k in range(8):
                                        ins = nc.tensor.matmul(pp[q][:], lhsT=xm[:, m, k, ss], rhs=Wb[:, k, c0 + half * 512:c0 + (half + 1) * 512], start=(k == 0), stop=(k == 7))
                                    return ins
                                kb.op('pe', f, reads=['Wb', 'xm%d' % m], writes=['pp%d' % q])
                            else:
                                c, r0, r1 = l2
                                kb.op('pe', lambda q=q, c=c, r0=r0, r1=r1, half=half, ss=ss: nc.tensor.matmul(pp[q][:], lhsT=l1[r0:r1, c, ss], rhs=W2[r0:r1, c, half * 512:(half + 1) * 512],
                                                                                                           start=True, stop=True), reads=['l1', 'W2'], writes=['pp%d' % q])
                            if half == 0:
                                kb.op('act', lambda q=q, o=o: nc.scalar.copy(out=ob[o][:, 0:512], in_=pp[q][:]), reads=['pp%d' % q, 'ob%d' % o], writes=['ob%d' % o])
                            else:
                                kb.op('dve', lambda q=q, o=o: nc.vector.tensor_copy(out=ob[o][:, 512:1024], in_=pp[q][:]), reads=['pp%d' % q, 'ob%d' % o], writes=['ob%d' % o])
                        kb.dma('sp', self.RK[nm][ts:ts + 128, :], ob[o][:], reads=['ob%d' % o], writes=['%s%d' % (nm, ti)])

    def stage_rwkv_scan(self, jl, z):
        nc, kb = self.nc, self.kb
        with Stage(kb) as st:
            C = self.scan_consts(st, z)
            B = self.head_bufs(st)
            w0 = self.bcast_load(st, "w0", self.rk_w0[jl, z, :], 1024)
            a0 = self.bcast_load(st, "a0", self.rk_a0[jl, z, :], 1024)
            k_k = self.bcast_load(st, "k_k", self.rk_k_k[jl, :], 1024)
            k_a = self.bcast_load(st, "k_a", self.rk_k_a[jl, :], 1024)
            T = [st.sb("T%d" % h, [64, 64], F32) for h in range(16)]
            for h in range(16):
                kb.op('dve', lambda h=h: nc.vector.memset(T[h][:], 0.0), writes=['T%d' % h])
            ld = {nm: st.sb("ld_" + nm, [128, 1024], F32) for nm in ('RR', 'KK0', 'VV', 'DL', 'AL')}
            f = {nm: st.sb("f_" + nm, [128, 1024], F32) for nm in ('sw', 'a', 'kk', 'kd', 'b', 'eG', 'enG', 'eGx', 'eRev', 'eTot', 'Gs', 't1')}
            sm = st.sb("sm", [128, 3, 16], F32)
            tb2 = [{nm: st.sb("tb_%s_%d" % (nm, q), [128, 1024], BF16) for nm in ('KAP', 'RM', 'BC', 'KC', 'V', 'BP', 'KP')} for q in range(2)]
            fT2 = [{nm: st.sb("fT_%s_%d" % (nm, q), [128, 8, 128], BF16) for nm in ('KAP', 'BP', 'KP', 'RM')} for q in range(2)]
            eTT = st.sb("eTT", [128, 8, 128], F32)
            wc2 = [st.sb("wc%d" % q, [64, 16, 2], F32) for q in range(2)]
            pG = [st.ps("pG%d" % q, [128, 512], F32) for q in range(2)]
            yt2 = [st.sb("yt%d" % q, [128, 1024], F32) for q in range(2)]
            DEC = 0.6065306597126334
            order = self.scan_order(z)

            def make_tile(idx, ti):
                q = idx % 2
                tb, fT, wc, yt = tb2[q], fT2[q], wc2[q], yt2[q]
                K_ = lambda nm: '%s#%d' % (nm, q)
                t0 = ti * 128

                def opgen():
                    t0 = ti * 128
                    for nm, src in (('RR', self.RK['RR']), ('KK0', self.RK['KK0']), ('VV', self.RK['VV']), ('DL', self.RK['DL%d' % z]), ('AL', self.RK['AL%d' % z])):
                        kb.dma('sp', ld[nm][:], src[t0:t0 + 128, :], reads=['%s%d' % (nm if nm in ('RR', 'KK0', 'VV') else nm + str(z), ti)], writes=['ld_' + nm])
                        yield
                    V_ = nc.vector
                    kb.op('dve', lambda: V_.tensor_tensor(out=f['t1'][:], in0=ld['DL'][:], in1=w0[:], op=ALU.add), reads=['ld_DL', 'w0', 'f_t1'], writes=['f_t1'])
                    yield
                    kb.op('act', lambda: nc.scalar.activation(out=f['sw'][:], in_=f['t1'][:], func=AF.Sigmoid), reads=['f_t1', 'f_sw'], writes=['f_sw'])
                    yield
                    kb.op('dve', lambda: V_.tensor_tensor(out=f['t1'][:], in0=ld['AL'][:], in1=a0[:], op=ALU.add), reads=['ld_AL', 'a0', 'f_t1'], writes=['f_t1'])
                    yield
                    kb.op('act', lambda: nc.scalar.activation(out=f['a'][:], in_=f['t1'][:], func=AF.Sigmoid), reads=['f_t1', 'f_a'], writes=['f_a'])
                    yield
                    kb.op('dve', lambda: V_.tensor_tensor(out=f['kk'][:], in0=ld['KK0'][:], in1=k_k[:], op=ALU.mult), reads=['ld_KK0', 'k_k', 'f_kk'], writes=['f_kk'])
                    yield
                    kb.op('dve', lambda: V_.tensor_tensor(out=f['t1'][:], in0=f['kk'][:], in1=f['kk'][:], op=ALU.mult), reads=['f_kk', 'f_t1'], writes=['f_t1'])
                    yield
                    kb.op('dve', lambda: V_.tensor_reduce(out=sm[:, 0, :], in_=f['t1'][:].rearrange("p (h n) -> p h n", n=64), axis=AX.X, op=ALU.add), reads=['f_t1', 'sm'], writes=['sm'])
                    yield
                    kb.op('act', lambda: nc.scalar.activation(out=sm[:, 1, :], in_=sm[:, 0, :], func=AF.Sqrt), reads=['sm'], writes=['sm'])
                    yield
                    kb.op('dve', lambda: V_.tensor_scalar(out=sm[:, 1, :], in0=sm[:, 1, :], scalar1=1e-12, scalar2=None, op0=ALU.max), reads=['sm'], writes=['sm'])
                    yield
                    kb.op('dve', lambda: V_.reciprocal(out=sm[:, 2, :], in_=sm[:, 1, :]), reads=['sm'], writes=['sm'])
                    yield
                    kb.op('dve', lambda: V_.tensor_tensor(out=f['kk'][:].rearrange("p (h n) -> p h n", n=64), in0=f['kk'][:].rearrange("p (h n) -> p h n", n=64),
                                                          in1=sm[:, 2, :].unsqueeze(2).to_broadcast([128, 16, 64]), op=ALU.mult), reads=['sm', 'f_kk'], writes=['f_kk'])
                    kb.op('dve', lambda: V_.scalar_tensor_tensor(out=f['t1'][:], in0=f['a'][:], scalar=-1.0, in1=k_a[:], op0=ALU.add, op1=ALU.mult), reads=['f_a', 'k_a', 'f_t1'], writes=['f_t1'])
                    yield
                    kb.op('dve', lambda: V_.scalar_tensor_tensor(out=f['kd'][:], in0=f['t1'][:], scalar=1.0, in1=ld['KK0'][:], op0=ALU.add, op1=ALU.mult), reads=['f_t1', 'ld_KK0', 'f_kd'], writes=['f_kd'])
                    yield
                    kb.op('dve', lambda: V_.tensor_tensor(out=f['b'][:], in0=f['kk'][:], in1=f['a'][:], op=ALU.mult), reads=['f_kk', 'f_a', 'f_b'], writes=['f_b'])
                    yield
                    for half in range(2):
                        hs = slice(half * 512, (half + 1) * 512)
                        kb.op('pe', lambda hs=hs: nc.tensor.matmul(pG[0][:], lhsT=C['SBI'][:], rhs=f['sw'][:, hs], start=True, stop=True), reads=['SBI', 'f_sw'], writes=['pG0'])
                        yield
                        kb.op('pe', lambda hs=hs: nc.tensor.matmul(pG[1][:], lhsT=C['BLK'][:], rhs=f['sw'][:, hs], start=True, stop=True), reads=['BLK', 'f_sw'], writes=['pG1'])
                        yield
                        kb.op('act', lambda hs=hs: nc.scalar.activation(out=f['eG'][:, hs], in_=pG[0][:], func=AF.Exp, scale=-DEC), reads=['pG0', 'f_eG'], writes=['f_eG'])
                        yield
                        kb.op('act', lambda hs=hs: nc.scalar.activation(out=f['enG'][:, hs], in_=pG[0][:], func=AF.Exp, scale=DEC), reads=['pG0', 'f_enG'], writes=['f_enG'])
                        yield
                        kb.op('dve', lambda hs=hs: V_.tensor_copy(out=f['Gs'][:, hs], in_=pG[0][:]), reads=['pG0', 'f_Gs'], writes=['f_Gs'])
                        yield
                        kb.op('act', lambda hs=hs: nc.scalar.activation(out=f['eTot'][:, hs], in_=pG[1][:], func=AF.Exp, scale=-DEC), reads=['pG1', 'f_eTot'], writes=['f_eTot'])
                        yield
                        kb.op('dve', lambda hs=hs: V_.tensor_tensor(out=f['eRev'][:, hs], in0=pG[1][:], in1=f['Gs'][:, hs], op=ALU.subtract), reads=['pG1', 'f_Gs', 'f_eRev'], writes=['f_eRev'])
                        yield
                    kb.op('act', lambda: nc.scalar.activation(out=f['eRev'][:], in_=f['eRev'][:], func=AF.Exp, scale=-DEC), reads=['f_eRev'], writes=['f_eRev'])
                    yield
                    kb.op('dve', lambda: V_.tensor_tensor(out=f['eGx'][:], in0=f['Gs'][:], in1=f['sw'][:], op=ALU.subtract), reads=['f_Gs', 'f_sw', 'f_eGx'], writes=['f_eGx'])
                    yield
                    kb.op('act', lambda: nc.scalar.activation(out=f['eGx'][:], in_=f['eGx'][:], func=AF.Exp, scale=-DEC), reads=['f_eGx'], writes=['f_eGx'])
                    yield
                    kb.op('dve', lambda: V_.scalar_tensor_tensor(out=tb['KAP'][:], in0=f['kk'][:], scalar=-1.0, in1=f['eGx'][:], op0=ALU.mult, op1=ALU.mult), reads=['f_kk', 'f_eGx', K_('tb_KAP')], writes=[K_('tb_KAP')])
                    yield
                    kb.op('dve', lambda: nc.vector.tensor_tensor(out=tb['RM'][:], in0=ld['RR'][:], in1=f['eG'][:], op=ALU.mult), reads=['ld_RR', 'f_eG', K_('tb_RM')], writes=[K_('tb_RM')])
                    yield
                    kb.op('dve', lambda: V_.tensor_tensor(out=tb['BP'][:], in0=f['b'][:], in1=f['enG'][:], op=ALU.mult), reads=['f_b', 'f_enG', K_('tb_BP')], writes=[K_('tb_BP')])
                    yield
                    kb.op('dve', lambda: nc.vector.tensor_tensor(out=tb['KP'][:], in0=f['kd'][:], in1=f['enG'][:], op=ALU.mult), reads=['f_kd', 'f_enG', K_('tb_KP')], writes=[K_('tb_KP')])
                    yield
                    kb.op('dve', lambda: V_.tensor_tensor(out=tb['BC'][:], in0=f['b'][:], in1=f['eRev'][:], op=ALU.mult), reads=['f_b', 'f_eRev', K_('tb_BC')], writes=[K_('tb_BC')])
                    yield
                    kb.op('dve', lambda: nc.vector.tensor_tensor(out=tb['KC'][:], in0=f['kd'][:], in1=f['eRev'][:], op=ALU.mult), reads=['f_kd', 'f_eRev', K_('tb_KC')], writes=[K_('tb_KC')])
                    yield
                    kb.op('act', lambda: nc.scalar.copy(out=tb['V'][:], in_=ld['VV'][:]), reads=['ld_VV', K_('tb_V')], writes=[K_('tb_V')])
                    yield
                    for nm in ('KAP', 'BP', 'KP', 'RM'):
                        pT = pG[0][:].bitcast(BF16)[:, 0:1024].rearrange("p (k t) -> p k t", t=128)

                        def tr(nm=nm, pT=pT):
                            for k in range(8):
                                ins = nc.tensor.transpose(pT[:, k, :], tb[nm][:, k * 128:(k + 1) * 128], C['identb'][:])
                            return ins
                        kb.op('pe', tr, reads=[K_('tb_' + nm), 'identb'], writes=['pG0'])
                        yield
                        kb.op('act', lambda nm=nm, pT=pT: nc.scalar.copy(out=fT[nm][:], in_=pT), reads=['pG0', K_('fT_' + nm)], writes=[K_('fT_' + nm)])
                        yield
                    for half in range(2):
                        pT = pG[1][:].rearrange("p (k t) -> p k t", t=128)

                        def tr2(half=half, pT=pT):
                            for k in range(4):
                                kk_ = half * 4 + k
                                ins = nc.tensor.transpose(pT[:, k, :], f['eTot'][:, kk_ * 128:(kk_ + 1) * 128], C['identf'][:])
                            return ins
                        kb.op('pe', tr2, reads=['f_eTot', 'identf'], writes=['pG1'])
                        yield
                        kb.op('dve', lambda half=half, pT=pT: V_.tensor_copy(out=eTT[:, half * 4:(half + 1) * 4, :], in_=pT), reads=['pG1', 'eTT'], writes=['eTT'])
                        yield
                    for par in range(2):
                        kb.op('dve', lambda par=par: V_.tensor_copy(out=wc[:, par::2, :], in_=eTT[par * 64:(par + 1) * 64, :, 0:128:64]), reads=['eTT', K_('wc')], writes=[K_('wc')])
                        yield

                    return
                    yield
                opk = [K_('tb_KAP'), K_('tb_RM'), K_('tb_BC'), K_('tb_KC'), K_('tb_V'), K_('fT_KAP'), K_('fT_BP'), K_('fT_KP'), K_('fT_RM')]
                gens = []
                for h in range(16):
                    def mk(h):
                        cs = slice(h * 64, (h + 1) * 64)
                        pb = (h % 2) * 64
                        k8 = h // 2
                        return lambda sl: self.head_scan(st, C, h, 64, 64, pb, tb['KAP'][:, cs], tb['RM'][:, cs], tb['BC'][:, cs], tb['KC'][:, cs], tb['V'][:, cs],
                                   fT['KAP'][pb:pb + 64, k8, :], fT['BP'][pb:pb + 64, k8, :], fT['KP'][pb:pb + 64, k8, :], fT['RM'][pb:pb + 64, k8, :],
                                   (C['SB'][:], 'SB'), (C['SBI'][:], 'SBI'), (C['SBT'][:], 'SBT'), (wc[:, h, :], K_('wc')), (T[h], 'T%d' % h), (yt[:, cs], K_('yt%d' % h)), opk, z, B, sl)
                    gens.append(mk(h))

                def store():
                    kb.dma('sp', self.YZ[z][t0:t0 + 128, :], yt[:], reads=[K_('yt%d' % h) for h in range(16)], writes=['YZ%d_%d' % (z, ti)])
                return opgen, gens, store
            self.run_pipeline([make_tile(idx, ti) for idx, ti in enumerate(order)])

    def stage_rwkv_out(self, jl):
        nc, kb = self.nc, self.kb
        with Stage(kb) as st:
            V_ = nc.vector
            idf = st.sb("identf", [128, 128], F32)
            kb.dma('sp', idf[:], self.cst_identf, writes=['identf'])
            idb = st.sb("identb", [128, 128], BF16)
            kb.op('dve', lambda: V_.tensor_copy(out=idb[:], in_=idf[:]), reads=['identf'], writes=['identb'])
            wo = st.sb("wo", [128, 8, 1024], BF16)
            stg = [st.sb("stg%d" % j, [128, 1024], F32) for j in range(2)]
            for k in range(8):
                j = k % 2
                kb.dma('sp', stg[j][:], self.rk_w_o[jl, k * 128:(k + 1) * 128, :], writes=['stg%d' % j])
                kb.op('dve', lambda k=k, j=j: V_.tensor_copy(out=wo[:, k, :], in_=stg[j][:]), reads=['stg%d' % j, 'wo'], writes=['wo'])
            bc = {}
            for nm, src in (('a00', self.rk_a0[jl, 0, :]), ('a01', self.rk_a0[jl, 1, :]), ('k_a', self.rk_k_a[jl, :]), ('r_k', self.rk_r_k[jl].rearrange("h n -> (h n)")),
                            ('lg', self.rk_lnx_g[jl, :]), ('lb', self.rk_lnx_b[jl, :])):
                bc[nm] = self.bcast_load(st, nm, src, 1024)
            ld = {nm: st.sb("ld_" + nm, [128, 1024], F32) for nm in ('Y0', 'Y1', 'RR', 'KK0', 'VV', 'AL0', 'AL1', 'GG')}
            t1 = st.sb("t1", [128, 1024], F32)
            t2 = st.sb("t2", [128, 1024], F32)
            zb = st.sb("zb", [128, 1024], BF16)
            zT = st.sb("zT", [128, 8, 128], BF16)
            sm = st.sb("sm", [128, 4, 16], F32)
            pT = st.ps("pT", [128, 512], F32)
            pO = [st.ps("pO%d" % q, [128, 512], F32) for q in range(2)]
            yo = st.sb("yo", [128, 1024], F32)
            H3 = lambda ap: ap.rearrange("p (h n) -> p h n", n=64)
            B3 = lambda ap: ap.unsqueeze(2).to_broadcast([128, 16, 64])
            for ti in range(NT // 128):
                t0 = ti * 128
                for nm, src, key in (('Y0', self.YZ[0], 'YZ0_%d' % ti), ('Y1', self.YZ[1], 'YZ1_%d' % ti), ('RR', self.RK['RR'], 'RR%d' % ti), ('KK0', self.RK['KK0'], 'KK0%d' % ti),
                                     ('VV', self.RK['VV'], 'VV%d' % ti), ('AL0', self.RK['AL0'], 'AL0%d' % ti), ('AL1', self.RK['AL1'], 'AL1%d' % ti), ('GG', self.RK['GG'], 'GG%d' % ti)):
                    kb.dma('sp', ld[nm][:], src[t0:t0 + 128, :], reads=[key], writes=['ld_' + nm])
                kb.op('dve', lambda: V_.tensor_tensor(out=t1[:], in0=ld['Y0'][:], in1=ld['Y1'][:], op=ALU.add), reads=['ld_Y0', 'ld_Y1', 't1'], writes=['t1'])
                kb.op('dve', lambda: V_.tensor_reduce(out=sm[:, 0, :], in_=H3(t1[:]), axis=AX.X, op=ALU.add), reads=['t1', 'sm'], writes=['sm'])
                kb.op('dve', lambda: V_.tensor_scalar(out=sm[:, 0, :], in0=sm[:, 0, :], scalar1=1.0 / 64, scalar2=None, op0=ALU.mult), reads=['sm'], writes=['sm'])
                kb.op('dve', lambda: V_.tensor_tensor(out=H3(t1[:]), in0=H3(t1[:]), in1=B3(sm[:, 0, :]), op=ALU.subtract), reads=['sm', 't1'], writes=['t1'])
                kb.op('dve', lambda: V_.tensor_tensor(out=t2[:], in0=t1[:], in1=t1[:], op=ALU.mult), reads=['t1', 't2'], writes=['t2'])
                kb.op('dve', lambda: V_.tensor_reduce(out=sm[:, 1, :], in_=H3(t2[:]), axis=AX.X, op=ALU.add), reads=['t2', 'sm'], writes=['sm'])
                kb.op('dve', lambda: V_.tensor_scalar(out=sm[:, 1, :], in0=sm[:, 1, :], scalar1=1.0 / 64, scalar2=64e-5, op0=ALU.mult, op1=ALU.add), reads=['sm'], writes=['sm'])
                kb.op('act', lambda: nc.scalar.activation(out=sm[:, 1, :], in_=sm[:, 1, :], func=AF.Sqrt), reads=['sm'], writes=['sm'])
                kb.op('dve', lambda: V_.reciprocal(out=sm[:, 2, :], in_=sm[:, 1, :]), reads=['sm'], writes=['sm'])
                kb.op('dve', lambda: V_.tensor_tensor(out=H3(t1[:]), in0=H3(t1[:]), in1=B3(sm[:, 2, :]), op=ALU.mult), reads=['sm', 't1'], writes=['t1'])
                kb.op('dve', lambda: V_.tensor_tensor(out=t1[:], in0=t1[:], in1=bc['lg'][:], op=ALU.mult), reads=['t1', 'lg'], writes=['t1'])
                kb.op('dve', lambda: V_.tensor_tensor(out=t1[:], in0=t1[:], in1=bc['lb'][:], op=ALU.add), reads=['t1', 'lb'], writes=['t1'])
                kb.op('dve', lambda: V_.tensor_tensor(out=t2[:], in0=ld['AL0'][:], in1=bc['a00'][:], op=ALU.add), reads=['ld_AL0', 'a00', 't2'], writes=['t2'])
                kb.op('act', lambda: nc.scalar.activation(out=t2[:], in_=t2[:], func=AF.Sigmoid), reads=['t2'], writes=['t2'])
                kb.op('dve', lambda: V_.tensor_tensor(out=ld['AL1'][:], in0=ld['AL1'][:], in1=bc['a01'][:], op=ALU.add), reads=['ld_AL1', 'a01'], writes=['ld_AL1'])
                kb.op('act', lambda: nc.scalar.activation(out=ld['AL1'][:], in_=ld['AL1'][:], func=AF.Sigmoid), reads=['ld_AL1'], writes=['ld_AL1'])
                kb.op('dve', lambda: V_.tensor_tensor(out=t2[:], in0=t2[:], in1=ld['AL1'][:], op=ALU.add), reads=['t2', 'ld_AL1'], writes=['t2'])
                kb.op('dve', lambda: V_.scalar_tensor_tensor(out=t2[:], in0=t2[:], scalar=-2.0, in1=bc['k_a'][:], op0=ALU.add, op1=ALU.mult), reads=['t2', 'k_a'], writes=['t2'])
                kb.op('dve', lambda: V_.scalar_tensor_tensor(out=t2[:], in0=t2[:], scalar=2.0, in1=ld['KK0'][:], op0=ALU.add, op1=ALU.mult), reads=['t2', 'ld_KK0'], writes=['t2'])
                kb.op('dve', lambda: V_.tensor_tensor(out=t2[:], in0=t2[:], in1=ld['RR'][:], op=ALU.mult), reads=['t2', 'ld_RR'], writes=['t2'])
                kb.op('dve', lambda: V_.tensor_tensor(out=t2[:], in0=t2[:], in1=bc['r_k'][:], op=ALU.mult), reads=['t2', 'r_k'], writes=['t2'])
                kb.op('dve', lambda: V_.tensor_reduce(out=sm[:, 3, :], in_=H3(t2[:]), axis=AX.X, op=ALU.add), reads=['t2', 'sm'], writes=['sm'])
                kb.op('dve', lambda: V_.tensor_tensor(out=H3(t2[:]), in0=H3(ld['VV'][:]), in1=B3(sm[:, 3, :]), op=ALU.mult), reads=['sm', 'ld_VV', 't2'], writes=['t2'])
                kb.op('dve', lambda: V_.tensor_tensor(out=t1[:], in0=t1[:], in1=t2[:], op=ALU.add), reads=['t1', 't2'], writes=['t1'])
                kb.op('dve', lambda: V_.tensor_tensor(out=zb[:], in0=t1[:], in1=ld['GG'][:], op=ALU.mult), reads=['t1', 'ld_GG', 'zb'], writes=['zb'])
                pTv = pT[:].bitcast(BF16)[:, 0:1024].rearrange("p (k t) -> p k t", t=128)

                def tr(pTv=pTv):
                    for k in range(8):
                        ins = nc.tensor.transpose(pTv[:, k, :], zb[:, k * 128:(k + 1) * 128], idb[:])
                    return ins
                kb.op('pe', tr, reads=['zb', 'identb'], writes=['pT'])
                kb.op('act', lambda pTv=pTv: nc.scalar.copy(out=zT[:], in_=pTv), reads=['pT', 'zT'], writes=['zT'])
                for half in range(2):
                    def mm(half=half):
                        for k in range(8):
                            ins = nc.tensor.matmul(pO[half][:], lhsT=zT[:, k, :], rhs=wo[:, k, half * 512:(half + 1) * 512], start=(k == 0), stop=(k == 7))
                        return ins
                    kb.op('pe', mm, reads=['zT', 'wo'], writes=['pO%d' % half])
                    kb.op('act' if half else 'dve', (lambda half=half: nc.scalar.copy(out=yo[:, 512:1024], in_=pO[1][:])) if half else
                          (lambda half=half: V_.tensor_copy(out=yo[:, 0:512], in_=pO[0][:])), reads=['pO%d' % half, 'yo'], writes=['yo'])
                kb.dma('sp', self.ACC[t0:t0 + 128, :], yo[:], reads=['yo'], writes=['ACC%d' % ti])

    def stage_rwkv(self, i, jl):
        self.stage_rwkv_proj(jl)
        for z in range(2):
            self.stage_rwkv_scan(jl, z)
        self.stage_rwkv_out(jl)

    def stage_gdn_proj(self, jl):
        nc, kb = self.nc, self.kb
        V_ = nc.vector
        for blk in range(4):
            with Stage(kb) as st:
                ntap = 4 if blk < 3 else 1
                ncol = 1024 if blk < 3 else 1056
                c0 = blk * 1024
                Wc = st.sb("Wc", [128, ntap, 8, ncol], BF16)
                stg = [st.sb("stg%d" % j, [128, 1056], F32) for j in range(2)]
                cw = [self.bcast_load(st, "cw%d" % tp, self.gdn_conv_w[jl, tp, c0:c0 + 1024], 1024) for tp in range(ntap)] if blk < 3 else None
                for k in range(8):
                    j = k % 2
                    kb.dma('sp', stg[j][:, 0:ncol], self.gdn_w_in[jl, k * 128:(k + 1) * 128, c0:c0 + ncol], writes=['stg%d' % j])
                    for tp in range(ntap):
                        if blk < 3:
                            kb.op('dve', lambda k=k, j=j, tp=tp: V_.tensor_tensor(out=Wc[:, tp, k, :], in0=stg[j][:, 0:1024], in1=cw[tp][:], op=ALU.mult), reads=['stg%d' % j, 'cw%d' % tp, 'Wc'], writes=['Wc'])
                        else:
                            kb.op('dve', lambda k=k, j=j: V_.tensor_copy(out=Wc[:, 0, k, :], in_=stg[j][:, 0:ncol]), reads=['stg%d' % j, 'Wc'], writes=['Wc'])
                ht = st.sb("ht", [128, 8, 640], BF16)
                pp = [st.ps("pp%d" % q, [128, 512], F32) for q in range(4)]
                ob = [st.sb("ob%d" % q, [128, 1056], F32) for q in range(2)]
                sq = st.sb("sq", [128, 1024], F32)
                sm = st.sb("sm", [128, 2, 8], F32)
                npp = [0]
                nob = [0]
                for it, (t0, W) in enumerate([(0, TC)] + tiles_of(NT, 512, TC)):
                    isc = (t0 == 0)
                    zl = isc or t0 == TC
                    zr = isc or (t0 + W == NT)
                    if zl:
                        kb.op('dve', lambda: V_.memset(ht[:, :, 0:64], 0.0), reads=['ht'], writes=['ht'])
                    if zr:
                        kb.op('dve', lambda W=W: V_.memset(ht[:, :, 64 + W:128 + W], 0.0), reads=['ht'], writes=['ht'])
                    a = t0 if zl else t0 - 64
                    b = t0 + W if zr else t0 + W + 64
                    kb.dma('sp', ht[:, :, 64 + (a - t0):64 + (b - t0)], self.HT[:, :, a:b], reads=['HT%d' % q for q in range(a // 128, (b + 127) // 128)] + ['ht'], writes=['ht'])
                    for sub in range(W // 128):
                        ts = t0 + sub * 128
                        ti = ts // 128
                        o = nob[0] % 2
                        nob[0] += 1
                        segs = [(0, 512), (512, 1024)] + ([(1024, 1056)] if blk == 3 else [])
                        for (s0, s1) in segs:
                            q = npp[0] % 4
                            npp[0] += 1

                            def f(q=q, s0=s0, s1=s1, sub=sub):
                                n_ = ntap * 8
                                i_ = 0
                                for tp in range(ntap):
                                    off = 64 + sub * 128 + (tp - 2 if blk < 3 else 0)
                                    for k in range(8):
                                        ins = nc.tensor.matmul(pp[q][:, 0:s1 - s0], lhsT=ht[:, k, off:off + 128], rhs=Wc[:, tp, k, s0:s1], start=(i_ == 0), stop=(i_ == n_ - 1))
                                        i_ += 1
                                return ins
                            kb.op('pe', f, reads=['ht', 'Wc'], writes=['pp%d' % q])
                            if blk < 3:
                                kb.op('act', lambda q=q, o=o, s0=s0, s1=s1: nc.scalar.activation(out=ob[o][:, s0:s1], in_=pp[q][:, 0:s1 - s0], func=AF.Silu), reads=['pp%d' % q, 'ob%d' % o], writes=['ob%d' % o])
                            else:
                                kb.op('act', lambda q=q, o=o, s0=s0, s1=s1: nc.scalar.copy(out=ob[o][:, s0:s1], in_=pp[q][:, 0:s1 - s0]), reads=['pp%d' % q, 'ob%d' % o], writes=['ob%d' % o])
                        if blk < 2:
                            O3 = ob[o][:, 0:1024].rearrange("p (h n) -> p h n", n=128)
                            kb.op('dve', lambda o=o: V_.tensor_tensor(out=sq[:], in0=ob[o][:, 0:1024], in1=ob[o][:, 0:1024], op=ALU.mult), reads=['ob%d' % o, 'sq'], writes=['sq'])
                            kb.op('dve', lambda: V_.tensor_reduce(out=sm[:, 0, :], in_=sq[:].rearrange("p (h n) -> p h n", n=128), axis=AX.X, op=ALU.add), reads=['sq', 'sm'], writes=['sm'])
                            kb.op('dve', lambda: V_.tensor_scalar(out=sm[:, 0, :], in0=sm[:, 0, :], scalar1=1e-6, scalar2=None, op0=ALU.add), reads=['sm'], writes=['sm'])
                            kb.op('act', lambda: nc.scalar.activation(out=sm[:, 0, :], in_=sm[:, 0, :], func=AF.Sqrt), reads=['sm'], writes=['sm'])
                            kb.op('dve', lambda: V_.reciprocal(out=sm[:, 1, :], in_=sm[:, 0, :]), reads=['sm'], writes=['sm'])
                            if blk == 0:
                                kb.op('dve', lambda: V_.tensor_scalar(out=sm[:, 1, :], in0=sm[:, 1, :], scalar1=128 ** -0.5, scalar2=None, op0=ALU.mult), reads=['sm'], writes=['sm'])
                            kb.op('dve', lambda O3=O3: V_.tensor_tensor(out=O3, in0=O3, in1=sm[:, 1, :].unsqueeze(2).to_broadcast([128, 8, 128]), op=ALU.mult), reads=['sm', 'ob%d' % o], writes=['ob%d' % o])
                        nm = ('GQ', 'GK', 'GV', 'GZ')[blk]
                        kb.dma('sp', self.GD[nm][ts:ts + 128, :], ob[o][:, 0:1024], reads=['ob%d' % o], writes=['%s%d' % (nm, ti)])
                        if blk == 3:
                            kb.dma('sp', self.GD['GAB'][ts:ts + 128, :], ob[o][:, 1024:1056], reads=['ob%d' % o], writes=['GAB%d' % ti])

    def stage_gdn_scan(self, jl, z):
        nc, kb = self.nc, self.kb
        V_ = nc.vector
        with Stage(kb) as st:
            C = self.scan_consts(st, z)
            B = self.head_bufs(st, F32)
            ones = st.sb("ones", [128, 128], F32)
            kb.op('dve', lambda: V_.memset(ones[:], 1.0), writes=['ones'])
            alog = self.bcast_load(st, "alog", self.gdn_a_log[jl, z, :], 8)
            dtb = self.bcast_load(st, "dtb", self.gdn_dt_bias[jl, z, :], 8)
            kb.op('act', lambda: nc.scalar.activation(out=alog[:], in_=alog[:], func=AF.Exp), reads=['alog'], writes=['alog'])
            T = [st.sb("T%d" % h, [128, 128], F32) for h in range(8)]
            for h in range(8):
                kb.op('dve', lambda h=h: V_.memset(T[h][:], 0.0), writes=['T%d' % h])
            ld = {nm: st.sb("ld_" + nm, [128, 1024], F32) for nm in ('GQ', 'GK', 'GV')}
            ab = st.sb("ab", [128, 32], F32)
            s82 = [{nm: st.sb("s8_%s_%d" % (nm, q), [128, 8], F32) for nm in ('g', 'beta', 'G', 'Gx', 'eGx', 'eG', 'eRev', 'bk', 'bkg', 'bkr', 't', 'eTot', 'nG')} for q in range(2)]
            tb2 = [{nm: st.sb("tb_%s_%d" % (nm, q), [128, 1024], BF16) for nm in ('KAP', 'RM', 'BC', 'KC', 'V', 'KU', 'BU', 'KD', 'QU')} for q in range(2)]
            fT2 = [{nm: st.sb("fT_%s_%d" % (nm, q), [128, 8, 128], BF16) for nm in ('KU', 'BU', 'KD', 'QU')} for q in range(2)]
            wc2 = [st.sb("wc%d" % q, [128, 8, 2], F32) for q in range(2)]
            pG = [st.ps("pG%d" % q, [128, 512], F32) for q in range(2)]
            dg = [st.sb("dg%d" % q, [128, 128], F32) for q in range(NSLOT)]
            Dm = [{nm: st.sb("D_%s%d" % (nm, h), [128, 128], F32) for nm in ('x', 'i', 'xT')} for h in range(8)]
            yt2 = [st.sb("yt%d" % q, [128, 1024], F32) for q in range(2)]
            H3 = lambda ap: ap.rearrange("p (h n) -> p h n", n=128)
            B3 = lambda ap: ap.unsqueeze(2).to_broadcast([128, 8, 128])
            order = self.scan_order(z)

            def make_tile(idx, ti):
                q = idx % 2
                tb, fT, wc, yt, s8 = tb2[q], fT2[q], wc2[q], yt2[q], s82[q]
                K_ = lambda nm: '%s#%d' % (nm, q)
                S8 = [K_('s8')]
                t0 = ti * 128

                def opgen():
                    t0 = ti * 128
                    for nm in ('GQ', 'GK', 'GV'):
                        kb.dma('sp', ld[nm][:], self.GD[nm][t0:t0 + 128, :], reads=['%s%d' % (nm, ti)], writes=['ld_' + nm])
                        yield
                    kb.dma('sp', ab[:], self.GD['GAB'][t0:t0 + 128, :], reads=['GAB%d' % ti], writes=['ab'])
                    yield
                    kb.op('dve', lambda: V_.tensor_tensor(out=s8['t'][:], in0=ab[:, z * 8:(z + 1) * 8], in1=dtb[:], op=ALU.add), reads=['ab', 'dtb'] + S8, writes=S8)
                    yield
                    kb.op('act', lambda: nc.scalar.activation(out=s8['t'][:], in_=s8['t'][:], func=AF.Exp), reads=S8, writes=S8)
                    yield
                    kb.op('act', lambda: nc.scalar.activation(out=s8['t'][:], in_=s8['t'][:], func=AF.Ln, bias=1.0), reads=S8, writes=S8)
                    yield
                    kb.op('dve', lambda: V_.scalar_tensor_tensor(out=s8['g'][:], in0=s8['t'][:], scalar=-1.0, in1=alog[:], op0=ALU.mult, op1=ALU.mult), reads=S8 + ['alog'], writes=S8)
                    yield
                    kb.op('act', lambda: nc.scalar.activation(out=s8['beta'][:], in_=ab[:, 16 + z * 8:16 + (z + 1) * 8], func=AF.Sigmoid), reads=['ab'] + S8, writes=S8)
                    yield
                    kb.op('pe', lambda: nc.tensor.matmul(pG[0][:, 0:8], lhsT=C['SBI'][:], rhs=s8['g'][:], start=True, stop=True), reads=['SBI'] + S8, writes=['pG0'])
                    yield
                    kb.op('pe', lambda: nc.tensor.matmul(pG[1][:, 0:8], lhsT=C['BLK'][:], rhs=s8['g'][:], start=True, stop=True), reads=['BLK'] + S8, writes=['pG1'])
                    yield
                    kb.op('dve', lambda: V_.tensor_copy(out=s8['G'][:], in_=pG[0][:, 0:8]), reads=['pG0'] + S8, writes=S8)
                    yield
                    kb.op('dve', lambda: V_.tensor_tensor(out=s8['Gx'][:], in0=s8['G'][:], in1=s8['g'][:], op=ALU.subtract), reads=S8, writes=S8)
                    yield
                    kb.op('dve', lambda: V_.tensor_tensor(out=s8['eRev'][:], in0=pG[1][:, 0:8], in1=s8['G'][:], op=ALU.subtract), reads=['pG1'] + S8, writes=S8)
                    yield
                    kb.op('act', lambda: nc.scalar.activation(out=s8['eTot'][:], in_=pG[1][:, 0:8], func=AF.Exp), reads=['pG1'] + S8, writes=S8)
                    yield
                    for nm_o, nm_i in (('eRev', 'eRev'), ('eGx', 'Gx'), ('eG', 'G'), ('bkg', 'g')):
                        kb.op('act', lambda nm_o=nm_o, nm_i=nm_i: nc.scalar.activation(out=s8[nm_o][:], in_=s8[nm_i][:], func=AF.Exp), reads=S8, writes=S8)
                        yield
                    kb.op('dve', lambda: V_.tensor_scalar(out=s8['nG'][:], in0=s8['G'][:], scalar1=-1.0, scalar2=None, op0=ALU.mult), reads=S8, writes=S8)
                    yield
                    kb.op('dve', lambda: V_.tensor_tensor(out=s8['bkg'][:], in0=s8['bkg'][:], in1=s8['beta'][:], op=ALU.mult), reads=S8, writes=S8)
                    yield
                    kb.op('dve', lambda: V_.tensor_tensor(out=s8['bk'][:], in0=s8['bkg'][:], in1=s8['eRev'][:], op=ALU.mult), reads=S8, writes=S8)
                    yield
                    kb.op('dve', lambda: V_.tensor_tensor(out=s8['bkr'][:], in0=s8['beta'][:], in1=s8['eRev'][:], op=ALU.mult), reads=S8, writes=S8)
                    yield
                    kb.op('dve', lambda: V_.tensor_scalar(out=s8['eGx'][:], in0=s8['eGx'][:], scalar1=-1.0, scalar2=None, op0=ALU.mult), reads=S8, writes=S8)
                    yield
                    K3 = H3(ld['GK'][:])
                    for nm, sc in (('KAP', 'eGx'), ('BC', 'bk'), ('KC', 'bkr'), ('BU', 'bkg'), ('KD', 'beta')):
                        kb.op('dve', lambda nm=nm, sc=sc: V_.tensor_tensor(out=H3(tb[nm][:]), in0=K3, in1=B3(s8[sc][:]), op=ALU.mult), reads=S8 + ['ld_GK', K_('tb_' + nm)], writes=[K_('tb_' + nm)])
                        yield
                    kb.op('dve', lambda: V_.tensor_scalar(out=tb['KU'][:], in0=ld['GK'][:], scalar1=-1.0, scalar2=None, op0=ALU.mult), reads=['ld_GK', K_('tb_KU')], writes=[K_('tb_KU')])
                    yield
                    kb.op('dve', lambda: V_.tensor_tensor(out=H3(tb['RM'][:]), in0=H3(ld['GQ'][:]), in1=B3(s8['eG'][:]), op=ALU.mult), reads=S8 + ['ld_GQ', K_('tb_RM')], writes=[K_('tb_RM')])
                    yield
                    kb.op('act', lambda: nc.scalar.copy(out=tb['QU'][:], in_=ld['GQ'][:]), reads=['ld_GQ', K_('tb_QU')], writes=[K_('tb_QU')])
                    yield
                    kb.op('act', lambda: nc.scalar.copy(out=tb['V'][:], in_=ld['GV'][:]), reads=['ld_GV', K_('tb_V')], writes=[K_('tb_V')])
                    yield
                    for nm in ('KU', 'BU', 'KD', 'QU'):
                        pT = pG[0][:].bitcast(BF16)[:, 0:1024].rearrange("p (k t) -> p k t", t=128)

                        def tr(nm=nm, pT=pT):
                            for k in range(8):
                                ins = nc.tensor.transpose(pT[:, k, :], tb[nm][:, k * 128:(k + 1) * 128], C['identb'][:])
                            return ins
                        kb.op('pe', tr, reads=[K_('tb_' + nm), 'identb'], writes=['pG0'])
                        yield
                        kb.op('act', lambda nm=nm, pT=pT: nc.scalar.copy(out=fT[nm][:], in_=pT), reads=['pG0', K_('fT_' + nm)], writes=[K_('fT_' + nm)])
                        yield
                    for c in range(2):
                        kb.op('pe', lambda c=c: nc.tensor.matmul(pG[1][:, 0:8], lhsT=C['BLK'][c * 64:(c + 1) * 64, c * 64:c * 64 + 1].to_broadcast([64, 128]) if False else ones[c * 64:(c + 1) * 64, :],
                                                               rhs=s8['g'][c * 64:(c + 1) * 64, :], start=True, stop=True), reads=['ones'] + S8, writes=['pG1'])
                        kb.op('act', lambda c=c: nc.scalar.activation(out=wc[:, :, c], in_=pG[1][:, 0:8], func=AF.Exp), reads=['pG1', K_('wc')], writes=[K_('wc')])
                        yield

                    return
                    yield
                opk = [K_('tb_KAP'), K_('tb_RM'), K_('tb_BC'), K_('tb_KC'), K_('tb_V'), K_('fT_KU'), K_('fT_BU'), K_('fT_KD'), K_('fT_QU')]
                gens = []
                for h in range(8):
                    def mk(h):
                        cs = slice(h * 128, (h + 1) * 128)
                        dmh = Dm[h]

                        def gen(sl):
                            pbk, pkey = B['ps'][sl], B['psk'][sl]
                            dgt, dgk = dg[sl], 'dg%d' % sl
                            for (src, dn0, sub_, mk_, flip) in (('G', 'i', 'G', 'SBI', False), ('Gx', 'x', 'G', 'SB', False), ('G', 'xT', 'Gx', 'SBT', True)):
                                d_ = dmh[dn0]
                                dn = dn0 + str(h)
                                kb.op('dve', lambda: V_.tensor_scalar(out=dgt[:], in0=C['identf'][:], scalar1=s8[src][:, h:h + 1], scalar2=None, op0=ALU.mult), reads=['identf', dgk] + S8, writes=[dgk])
                                kb.op('pe', lambda: nc.tensor.matmul(pbk[:, 0:128], lhsT=ones[:], rhs=dgt[:], start=True, stop=True), reads=['ones', dgk], writes=[pkey])
                                yield
                                if not flip:
                                    kb.op('dve', lambda: V_.tensor_scalar(out=d_[:], in0=pbk[:, 0:128], scalar1=s8[sub_][:, h:h + 1], scalar2=0.0, op0=ALU.subtract, op1=ALU.min),
                                          reads=[pkey, 'D_' + dn] + S8, writes=['D_' + dn])
                                else:
                                    kb.op('dve', lambda: V_.tensor_scalar(out=d_[:], in0=pbk[:, 0:128], scalar1=-1.0, scalar2=s8[sub_][:, h:h + 1], op0=ALU.mult, op1=ALU.add),
                                          reads=[pkey, 'D_' + dn] + S8, writes=['D_' + dn])
                                    kb.op('dve', lambda: V_.tensor_scalar(out=d_[:], in0=d_[:], scalar1=0.0, scalar2=None, op0=ALU.min), reads=['D_' + dn], writes=['D_' + dn])
                                yield
                                kb.op('act', lambda: nc.scalar.activation(out=d_[:], in_=d_[:], func=AF.Exp), reads=['D_' + dn], writes=['D_' + dn])
                                yield
                                kb.op('dve', lambda: V_.tensor_tensor(out=d_[:], in0=d_[:], in1=C[mk_][:], op=ALU.mult), reads=['D_' + dn, mk_], writes=['D_' + dn])
                                yield
                            yield from self.head_scan(st, C, h, 128, 128, 0, tb['KAP'][:, cs], tb['RM'][:, cs], tb['BC'][:, cs], tb['KC'][:, cs], tb['V'][:, cs],
                                   fT['KU'][:, h, :], fT['BU'][:, h, :], fT['KD'][:, h, :], fT['QU'][:, h, :],
                                   (dmh['x'][:], 'D_x%d' % h), (dmh['i'][:], 'D_i%d' % h), (dmh['xT'][:], 'D_xT%d' % h), (wc[:, h, :], K_('wc')), (T[h], 'T%d' % h), (yt[:, cs], K_('yt%d' % h)), opk, z, B, sl, F32)
                        return gen
                    gens.append(mk(h))

                def store():
                    kb.dma('sp', self.YZ[z][t0:t0 + 128, :], yt[:], reads=[K_('yt%d' % h) for h in range(8)], writes=['YZ%d_%d' % (z, ti)])
                return opgen, gens, store
            self.run_pipeline([make_tile(idx, ti) for idx, ti in enumerate(order)])

    def stage_gdn_out(self, jl):
        nc, kb = self.nc, self.kb
        V_ = nc.vector
        with Stage(kb) as st:
            idf = st.sb("identf", [128, 128], F32)
            kb.dma('sp', idf[:], self.cst_identf, writes=['identf'])
            idb = st.sb("identb", [128, 128], BF16)
            kb.op('dve', lambda: V_.tensor_copy(out=idb[:], in_=idf[:]), reads=['identf'], writes=['identb'])
            wo = st.sb("wo", [128, 8, 1024], BF16)
            stg = [st.sb("stg%d" % j, [128, 1024], F32) for j in range(2)]
            for k in range(8):
                j = k % 2
                kb.dma('sp', stg[j][:], self.gdn_w_o[jl, k * 128:(k + 1) * 128, :], writes=['stg%d' % j])
                kb.op('dve', lambda k=k, j=j: V_.tensor_copy(out=wo[:, k, :], in_=stg[j][:]), reads=['stg%d' % j, 'wo'], writes=['wo'])
            nw = self.bcast_load(st, "nw", self.gdn_norm_w[jl, :], 128)
            ld = {nm: st.sb("ld_" + nm, [128, 1024], F32) for nm in ('Y0', 'Y1', 'GZ')}
            t2 = st.sb("t2", [128, 1024], F32)
            zb = st.sb("zb", [128, 1024], BF16)
            zT = st.sb("zT", [128, 8, 128], BF16)
            sm = st.sb("sm", [128, 2, 8], F32)
            pT = st.ps("pT", [128, 512], F32)
            pO = [st.ps("pO%d" % q, [128, 512], F32) for q in range(2)]
            yo = st.sb("yo", [128, 1024], F32)
            H3 = lambda ap: ap.rearrange("p (h n) -> p h n", n=128)
            for ti in range(NT // 128):
                t0 = ti * 128
                for nm, src, key in (('Y0', self.YZ[0], 'YZ0_%d' % ti), ('Y1', self.YZ[1], 'YZ1_%d' % ti), ('GZ', self.GD['GZ'], 'GZ%d' % ti)):
                    kb.dma('sp', ld[nm][:], src[t0:t0 + 128, :], reads=[key], writes=['ld_' + nm])
                kb.op('dve', lambda: V_.tensor_tensor(out=ld['Y0'][:], in0=ld['Y0'][:], in1=ld['Y1'][:], op=ALU.add), reads=['ld_Y0', 'ld_Y1'], writes=['ld_Y0'])
                kb.op('dve', lambda: V_.tensor_tensor(out=t2[:], in0=ld['Y0'][:], in1=ld['Y0'][:], op=ALU.mult), reads=['ld_Y0', 't2'], writes=['t2'])
                kb.op('dve', lambda: V_.tensor_reduce(out=sm[:, 0, :], in_=H3(t2[:]), axis=AX.X, op=ALU.add), reads=['t2', 'sm'], writes=['sm'])
                kb.op('dve', lambda: V_.tensor_scalar(out=sm[:, 0, :], in0=sm[:, 0, :], scalar1=1.0 / 128, scalar2=1e-6, op0=ALU.mult, op1=ALU.add), reads=['sm'], writes=['sm'])
                kb.op('act', lambda: nc.scalar.activation(out=sm[:, 0, :], in_=sm[:, 0, :], func=AF.Sqrt), reads=['sm'], writes=['sm'])
                kb.op('dve', lambda: V_.reciprocal(out=sm[:, 1, :], in_=sm[:, 0, :]), reads=['sm'], writes=['sm'])
                kb.op('dve', lambda: V_.tensor_tensor(out=H3(ld['Y0'][:]), in0=H3(ld['Y0'][:]), in1=sm[:, 1, :].unsqueeze(2).to_broadcast([128, 8, 128]), op=ALU.mult), reads=['sm', 'ld_Y0'], writes=['ld_Y0'])
                kb.op('dve', lambda: V_.tensor_tensor(out=H3(ld['Y0'][:]), in0=H3(ld['Y0'][:]), in1=nw[:].unsqueeze(1).to_broadcast([128, 8, 128]), op=ALU.mult), reads=['nw', 'ld_Y0'], writes=['ld_Y0'])
                kb.op('act', lambda: nc.scalar.activation(out=t2[:], in_=ld['GZ'][:], func=AF.Silu), reads=['ld_GZ', 't2'], writes=['t2'])
                kb.op('dve', lambda: V_.tensor_tensor(out=zb[:], in0=ld['Y0'][:], in1=t2[:], op=ALU.mult), reads=['ld_Y0', 't2', 'zb'], writes=['zb'])
                pTv = pT[:].bitcast(BF16)[:, 0:1024].rearrange("p (k t) -> p k t", t=128)

                def tr(pTv=pTv):
                    for k in range(8):
                        ins = nc.tensor.transpose(pTv[:, k, :], zb[:, k * 128:(k + 1) * 128], idb[:])
                    return ins
                kb.op('pe', tr, reads=['zb', 'identb'], writes=['pT'])
                kb.op('act', lambda pTv=pTv: nc.scalar.copy(out=zT[:], in_=pTv), reads=['pT', 'zT'], writes=['zT'])
                for half in range(2):
                    def mm(half=half):
                        for k in range(8):
                            ins = nc.tensor.matmul(pO[half][:], lhsT=zT[:, k, :], rhs=wo[:, k, half * 512:(half + 1) * 512], start=(k == 0), stop=(k == 7))
                        return ins
                    kb.op('pe', mm, reads=['zT', 'wo'], writes=['pO%d' % half])
                    kb.op('dve', lambda half=half: V_.tensor_copy(out=yo[:, half * 512:(half + 1) * 512], in_=pO[half][:]), reads=['pO%d' % half, 'yo'], writes=['yo'])
                kb.dma('sp', self.ACC[t0:t0 + 128, :], yo[:], reads=['yo'], writes=['ACC%d' % ti])

    def stage_gdn(self, i, jl):
        self.stage_gdn_proj(jl)
        for z in range(2):
            self.stage_gdn_scan(jl, z)
        self.stage_gdn_out(jl)

    def decls(self):
        nc, kb = self.nc, self.kb
        self.xin = self.din("xin", [NT, D])
        self.cvec = self.din("cvec", [2, D])
        self.ada_w = self.din("ada_w", [4, D, 6 * D])
        self.ada_b = self.din("ada_b", [4, 6 * D])
        self.ln_g = self.din("ln_g", [4, 2, D])
        self.ln_b = self.din("ln_b", [4, 2, D])
        self.pool_w = self.din("pool_w", [1, 4, 256, 256])
        self.pool_scale = self.din("pool_scale", [1, D])
        self.ffn_w1 = self.din("ffn_w1", [2, D, 2816])
        self.ffn_w3 = self.din("ffn_w3", [2, D, 2816])
        self.ffn_w2 = self.din("ffn_w2", [2, 2816, D])
        self.moe_router_w = self.din("moe_router_w", [2, D, 8])
        self.moe_router_b = self.din("moe_router_b", [2, 8])
        self.moe_w1 = self.din("moe_w1", [2, 8, D, 1408])
        self.moe_w3 = self.din("moe_w3", [2, 8, D, 1408])
        self.moe_w2 = self.din("moe_w2", [2, 8, 1408, D])
        for nm, shp in (("rk_mu", [2, 6, D]), ("rk_w_rkv", [2, 3, D, D]), ("rk_w0", [2, 2, D]), ("rk_w1", [2, 2, D, 64]), ("rk_w2", [2, 2, 64, D]),
                        ("rk_a0", [2, 2, D]), ("rk_a1", [2, 2, D, 64]), ("rk_a2", [2, 2, 64, D]), ("rk_g1", [2, D, 128]), ("rk_g2", [2, 128, D]),
                        ("rk_k_k", [2, D]), ("rk_k_a", [2, D]), ("rk_r_k", [2, 16, 64]), ("rk_lnx_g", [2, D]), ("rk_lnx_b", [2, D]), ("rk_w_o", [2, D, D])):
            setattr(self, nm, self.din(nm, shp))
        for nm, shp in (("gdn_w_in", [1, D, 4128]), ("gdn_conv_w", [1, 4, 3072]), ("gdn_a_log", [1, 2, 8]), ("gdn_dt_bias", [1, 2, 8]),
                        ("gdn_norm_w", [1, 128]), ("gdn_w_o", [1, D, D])):
            setattr(self, nm, self.din(nm, shp))
        self.GD = {nm: self.dscr("GD_" + nm, [NT, D]) for nm in ("GQ", "GK", "GV", "GZ")}
        self.GD["GAB"] = self.dscr("GD_GAB", [NT, 32])
        self.cst_masks = self.din("cst_masks", [2, 4, 128, 128])
        self.RK = {nm: self.dscr("RK_" + nm, [NT, D]) for nm in ("RR", "KK0", "VV", "DL0", "DL1", "AL0", "AL1", "GG")}
        self.YZ = [self.dscr("YZ%d" % z, [NT, D]) for z in range(2)]
        self.cst_identf = self.din("cst_identf", [128, 128])
        self.cst_invc64 = self.din("cst_invc64", [128, 4, 64])
        self.cst_invc256 = self.din("cst_invc256", [128, 4, 256])
        self.out = self.dout("out", [NT - TC, D])
        self.XS = self.dscr("XS", [NT, D])
        self.ACC = self.dscr("ACC", [NT, D])
        self.HT = self.dscr("HT", [128, 8, NT], BF16)
        self.GATES = self.dscr("GATES", [NT, 8])
        self.MODD = self.dscr("MODD", [4, 2, 6 * D])

    def stage_init(self):
        kb = self.kb
        for (t0, W) in tiles_of(NT, 128):
            kb.dma('sp', self.XS[t0:t0 + W, :], self.xin[t0:t0 + W, :], writes=['XSinit%d' % t0])
        kb.barrier()

    def build(self):
        nc, kb = self.nc, self.kb
        self.decls()
        self.stage_init()
        for i in self.layers:
            last = (i == self.layers[-1])
            self.stage_mod(i)
            self.stage_prep(i, 1)
            kind, jl = i % 3, i // 3
            if kind == 1:
                self.stage_pool(jl)
            elif kind == 0:
                self.stage_rwkv(i, jl)
            else:
                self.stage_gdn(i, jl)
            self.stage_finish(i, 1)
            e = i // 2
            if i % 2 == 0:
                self.stage_prep(i, 2)
                for hf in range(2):
                    sl = slice(hf * 1408, (hf + 1) * 1408)
                    self.stage_ffnpass(self.ffn_w1[e, :, sl], self.ffn_w3[e, :, sl], self.ffn_w2[e, sl, :], None, hf == 0)
            else:
                self.stage_prep(i, 2, router=e)
                for x in range(8):
                    self.stage_ffnpass(self.moe_w1[e, x], self.moe_w3[e, x], self.moe_w2[e, x], x, x == 0)
            self.stage_finish(i, 2, out_final=self.out if last else None)
        kb.barrier()
        return nc


def _pool_invc(L):
    t = np.arange(L)
    out = np.zeros((4, L), np.float32)
    for gi, w in enumerate((2, 4, 8, 16)):
        lo = np.clip(t - w // 2, 0, L)
        hi = np.clip(t + w // 2, 0, L)
        out[gi] = 1.0 / (hi - lo)
    return np.ascontiguousarray(np.broadcast_to(out[None], (128, 4, L))).astype(np.float32)


def _masks():
    s = np.arange(128)[:, None]
    t = np.arange(128)[None, :]
    same = (s // 64) == (t // 64)
    m = np.zeros((2, 4, 128, 128), np.float32)
    for z in range(2):
        sb = ((s < t) if z == 0 else (s > t)) & same
        sbi = ((s <= t) if z == 0 else (s >= t)) & same
        m[z, 0] = sb
        m[z, 1] = sbi
        m[z, 2] = sb.T
        m[z, 3] = same
    return m


def make_consts():
    return {
        "cst_masks": _masks(),
        "cst_identf": np.eye(128, dtype=np.float32),
        "cst_invc64": _pool_invc(64),
        "cst_invc256": _pool_invc(256),
    }


WEIGHT_KEYS = ["ada_w", "ada_b", "ln_g", "ln_b", "pool_w", "pool_scale", "ffn_w1", "ffn_w3", "ffn_w2",
               "moe_router_w", "moe_router_b", "moe_w1", "moe_w3", "moe_w2"]


def run(inputs, layers=(0, 1, 2, 3), n_cores=4, xin_override=None):
    prog = Prog(list(layers))
    nc = prog.build()
    print("instructions:", prog.kb.nins, "sems:", len(prog.kb.sems))
    cst = make_consts()
    in_maps = []
    for cidx in range(n_cores):
        b = cidx % 4
        m = {}
        if xin_override is not None:
            m["xin"] = xin_override[b]
        else:
            m["xin"] = np.ascontiguousarray(np.concatenate([inputs["ctx"][b], inputs["x"][b]], axis=0))
        m["cvec"] = np.ascontiguousarray(np.stack([inputs["c"][b], inputs["c_ctx"]], axis=0))
        for k in prog.inp:
            if k in m:
                continue
            m[k] = cst[k] if k in cst else np.ascontiguousarray(inputs[k])
        in_maps.append(m)
    res = run_bass_kernel_spmd(nc, in_maps, core_ids=list(range(n_cores)))
    return res


def kernel(**inputs):
    inputs = {k: np.asarray(v) for k, v in inputs.items()}
    res = run(inputs)
    out = np.stack([res.results[b]["out"] for b in range(4)], axis=0)
    return out.astype(np.float32)
```

```python
import contextlib
import numpy as np
import concourse.bass as bass
import concourse.mybir as mybir
from concourse.bass_utils import run_bass_kernel_spmd

F32 = mybir.dt.float32
BF16 = mybir.dt.bfloat16
AF = mybir.ActivationFunctionType
ALU = mybir.AluOpType
AX = mybir.AxisListType

D = 1024
NT = 8448
TC = 256
DEPTH = 4
ALPHA = (2 * DEPTH) ** 0.25
LN_EPS = 1e-5
NSLOT = 8


class KB:
    EPOCH = 30000
    NDMA = 24

    def __init__(self, nc):
        self.nc = nc
        self._ctx = []
        self.engs = {'pe': nc.tensor, 'dve': nc.vector, 'act': nc.scalar, 'pool': nc.gpsimd, 'sp': nc.sync}
        self.sems = []
        self.esem = {}
        self.ecnt = {}
        for e in ('pe', 'dve', 'act', 'pool'):
            self.esem[e] = self._newsem('c_' + e)
            self.ecnt[e] = 0
        self.dsem = [self._newsem('d%d' % i) for i in range(self.NDMA)]
        self.dcnt = [0] * self.NDMA
        self.dnext = 0
        self.waited = {e: {} for e in self.engs}
        self.res = {}
        self.nins = 0
        self.allsems = {}
        self.excl = set()

    def _newsem(self, name):
        cm = self.nc.semaphore(name + '_%d' % len(self.sems))
        h = cm.__enter__()
        self._ctx.append(cm)
        self.sems.append(h)
        return len(self.sems) - 1

    def _r(self, key):
        r = self.res.get(key)
        if r is None:
            r = {'w': None, 'r': {}}
            self.res[key] = r
        return r

    def _waits(self, eng, reads, writes):
        need = {}

        def add(tok):
            if tok is None:
                return
            s, v = tok
            if need.get(s, 0) < v:
                need[s] = v
        for k in reads:
            add(self._r(k)['w'])
        for k in writes:
            r = self._r(k)
            add(r['w'])
            for s, v in r['r'].items():
                add((s, v))
        wd = self.waited[eng]
        for s, v in need.items():
            if wd.get(s, 0) >= v:
                continue
            self.engs[eng].wait_ge(self.sems[s], v)
            self.nins += 1
            wd[s] = v

    def _mark(self, tok, reads, writes):
        s, v = tok
        self.allsems[s] = v
        for k in writes:
            r = self._r(k)
            r['w'] = tok
            r['r'] = {}
        for k in reads:
            if k in writes:
                continue
            r = self._r(k)
            if r['r'].get(s, 0) < v:
                r['r'][s] = v

    def op(self, eng, fn, reads=(), writes=()):
        ex = [k for k in reads if k in self.excl and k not in writes]
        if ex:
            writes = list(writes) + ex
        self._waits(eng, reads, writes)
        ins = fn()
        if self.ecnt[eng] >= self.EPOCH:
            self.esem[eng] = self._newsem('c_' + eng)
            self.ecnt[eng] = 0
        s = self.esem[eng]
        ins.then_inc(self.sems[s], 1)
        self.ecnt[eng] += 1
        self.nins += 1
        tok = (s, self.ecnt[eng])
        self._mark(tok, reads, writes)
        return tok

    def dma(self, q, out, in_, reads=(), writes=(), **kw):
        j = self.dnext
        self.dnext = (self.dnext + 1) % self.NDMA
        wd = self.waited[q]
        if self.dcnt[j] > 0 and wd.get(self.dsem[j], 0) < 16 * self.dcnt[j]:
            self.engs[q].wait_ge(self.sems[self.dsem[j]], 16 * self.dcnt[j])
            wd[self.dsem[j]] = 16 * self.dcnt[j]
        self._waits(q, reads, writes)
        self.engs[q].dma_start(out=out, in_=in_, **kw).then_inc(self.sems[self.dsem[j]], 16)
        self.dcnt[j] += 1
        self.nins += 1
        tok = (self.dsem[j], 16 * self.dcnt[j])
        self._mark(tok, reads, writes)
        return tok

    def barrier(self):
        for e in self.engs:
            wd = self.waited[e]
            for s, v in self.allsems.items():
                if wd.get(s, 0) >= v:
                    continue
                self.engs[e].wait_ge(self.sems[s], v)
                self.nins += 1
                wd[s] = v
        self.res = {}


class Stage:
    _n = [0]

    def __init__(self, kb):
        self.kb = kb
        self.nc = kb.nc
        self.es = contextlib.ExitStack()
        Stage._n[0] += 1
        self.sid = Stage._n[0]

    def __enter__(self):
        self.es.__enter__()
        return self

    def sb(self, name, shape, dt):
        return self.es.enter_context(self.nc.sbuf_tensor('%s_s%d' % (name, self.sid), shape, dt))

    def ps(self, name, shape, dt):
        self.kb.excl.add(name)
        return self.es.enter_context(self.nc.psum_tensor('%s_s%d' % (name, self.sid), shape, dt))

    def __exit__(self, *a):
        self.kb.barrier()
        return self.es.__exit__(*a)


def tiles_of(total, w, start=0):
    out = []
    t = start
    while t < total:
        out.append((t, min(w, total - t)))
        t += w
    return out


class Prog:
    def __init__(self, layers, debug_outs=()):
        self.layers = layers
        nc = bass.Bass("TRN2", target_bir_lowering=False)
        self.nc = nc
        self.kb = KB(nc)
        self.inp = {}
        self.debug_outs = debug_outs

    def din(self, name, shape, dt=F32):
        t = self.nc.dram_tensor(name, list(shape), dt, kind="ExternalInput").ap()
        self.inp[name] = t
        return t

    def dscr(self, name, shape, dt=F32):
        return self.nc.dram_tensor(name, list(shape), dt, kind="Internal").ap()

    def dout(self, name, shape, dt=F32):
        return self.nc.dram_tensor(name, list(shape), dt, kind="ExternalOutput").ap()

    def bcast_load(self, st, name, row_ap, n, dt=F32):
        t = st.sb(name, [128, n], dt)
        self.kb.dma('sp', t[:], row_ap.partition_broadcast(128), writes=[name])
        return t

    def stage_mod(self, i):
        nc, kb = self.nc, self.kb
        with Stage(kb) as st:
            cT = st.sb("cT", [128, 8, 2], F32)
            sT = st.sb("sT", [128, 8, 2], F32)
            modsb = st.sb("modsb", [2, 6144], F32)
            adab = st.sb("adab", [2, 6144], F32)
            wst = [st.sb("wst%d" % j, [128, 8, 512], F32) for j in range(2)]
            pm_ = [st.ps("pm%d" % j, [128, 512], F32) for j in range(2)]
            pm = [t[0:2, :] for t in pm_]
            for r in range(2):
                kb.dma('sp', cT[:, :, r], self.cvec[r, :].rearrange("(k p) -> p k", p=128),
                       reads=['cT'] if r else [], writes=['cT'], allow_slow_non_contiguous=True)
                kb.dma('sp', adab[r:r + 1, :], self.ada_b[i:i + 1, :], reads=['adab'] if r else [], writes=['adab'])
            kb.op('act', lambda: nc.scalar.activation(out=sT[:], in_=cT[:], func=AF.Silu), reads=['cT'], writes=['sT'])
            for g in range(12):
                j = g % 2
                kb.dma('sp', wst[j][:], self.ada_w[i, :, g * 512:(g + 1) * 512].rearrange("(k p) n -> p k n", p=128),
                       writes=['wst%d' % j])

                def mm(j=j):
                    for k in range(8):
                        ins = nc.tensor.matmul(pm[j][:], lhsT=sT[:, k, :], rhs=wst[j][:, k, :], start=(k == 0), stop=(k == 7))
                    return ins
                kb.op('pe', mm, reads=['sT', 'wst%d' % j], writes=['pm%d' % j])
                kb.op('dve', lambda j=j, g=g: nc.vector.tensor_tensor(out=modsb[:, g * 512:(g + 1) * 512], in0=pm[j][:],
                                                                     in1=adab[:, g * 512:(g + 1) * 512], op=ALU.add),
                      reads=['pm%d' % j, 'adab', 'modsb'], writes=['modsb'])
            for c0 in (1024, 4096):
                kb.op('dve', lambda c0=c0: nc.vector.tensor_scalar(out=modsb[:, c0:c0 + 1024], in0=modsb[:, c0:c0 + 1024],
                                                                 scalar1=1.0, scalar2=None, op0=ALU.add),
                      reads=['modsb'], writes=['modsb'])
            kb.dma('sp', self.MODD[i], modsb[:], reads=['modsb'], writes=['MODD'])

    def mod_tiles(self, st, i, slots):
        out = {}
        for s in slots:
            for r in range(2):
                out[(s, r)] = self.bcast_load(st, "mod%d_%d" % (s, r), self.MODD[i, r, s * 1024:(s + 1) * 1024], 1024)
        return out

    def stage_prep(self, i, sub, router=None):
        nc, kb = self.nc, self.kb
        with Stage(kb) as st:
            sh_s, sc_s = (0, 1) if sub == 1 else (3, 4)
            md = self.mod_tiles(st, i, [sh_s, sc_s])
            identf = st.sb("identf", [128, 128], F32)
            kb.dma('sp', identf[:], self.cst_identf, writes=['identf'])
            NB = 2
            xt = [st.sb("xt%d" % j, [128, 1024], F32) for j in range(NB)]
            hf = [st.sb("hf%d" % j, [128, 1024], F32) for j in range(NB)]
            hTb = [st.sb("hTb%d" % j, [128, 8, 128], BF16) for j in range(NB)]
            pT = [st.ps("pT%d" % j, [128, 8, 128], F32) for j in range(NB)]
            if router is not None:
                e = router
                rw = st.sb("rw", [128, 8, 8], F32)
                kb.dma('sp', rw[:], self.moe_router_w[e].rearrange("(k p) n -> p k n", p=128), writes=['rw'])
                rb = self.bcast_load(st, "rb", self.moe_router_b[e, :], 8)
                hTf = [st.sb("hTf%d" % j, [128, 8, 128], F32) for j in range(NB)]
                pl_ = [st.ps("pl%d" % j, [128, 512], F32) for j in range(NB)]
                pl = [t[:, 0:8] for t in pl_]
                lg = [st.sb("lg%d" % j, [128, 8], F32) for j in range(NB)]
                m1 = [st.sb("m1_%d" % j, [128, 8], F32) for j in range(NB)]
                m2 = [st.sb("m2_%d" % j, [128, 8], F32) for j in range(NB)]
                l2 = [st.sb("l2_%d" % j, [128, 8], F32) for j in range(NB)]
                mx = [st.sb("mx%d" % j, [128, 4], F32) for j in range(NB)]
                gt = [st.sb("gt%d" % j, [128, 8], F32) for j in range(NB)]
            def _ldx(tq):
                kb.dma('sp', xt[tq % NB][:], self.XS[tq * 128:(tq + 1) * 128, :], writes=['xt%d' % (tq % NB)])
            _ldx(0)
            for ti in range(NT // 128):
                j = ti % NB
                isc = 1 if ti < TC // 128 else 0
                t0 = ti * 128
                X, H, HB, PT = 'xt%d' % j, 'hf%d' % j, 'hTb%d' % j, 'pT%d' % j
                if ti + 1 < NT // 128:
                    _ldx(ti + 1)
                kb.op('dve', lambda j=j, isc=isc: nc.vector.tensor_tensor(out=hf[j][:], in0=xt[j][:], in1=md[(sc_s, isc)][:], op=ALU.mult),
                      reads=[X, 'mod%d_%d' % (sc_s, isc)], writes=[H])
                kb.op('dve', lambda j=j, isc=isc: nc.vector.tensor_tensor(out=hf[j][:], in0=hf[j][:], in1=md[(sh_s, isc)][:], op=ALU.add),
                      reads=[H, 'mod%d_%d' % (sh_s, isc)], writes=[H])

                def tr(j=j):
                    for k in range(8):
                        ins = nc.tensor.transpose(pT[j][:, k, :], hf[j][:, k * 128:(k + 1) * 128], identf[:])
                    return ins
                kb.op('pe', tr, reads=[H, 'identf'], writes=[PT])
                if router is None:
                    kb.op('act', lambda j=j: nc.scalar.copy(out=hTb[j][:], in_=pT[j][:]), reads=[PT], writes=[HB])
                else:
                    kb.op('dve', lambda j=j: nc.vector.tensor_copy(out=hTf[j][:], in_=pT[j][:]), reads=[PT], writes=['hTf%d' % j])
                    kb.op('act', lambda j=j: nc.scalar.copy(out=hTb[j][:], in_=hTf[j][:]), reads=['hTf%d' % j], writes=[HB])
                kb.dma('sp', self.HT[:, :, t0:t0 + 128], hTb[j][:], reads=[HB], writes=['HT%d' % ti])
                import os
                RD = int(os.environ.get('RD', '9'))
                if router is not None and RD >= 1:
                    HF, PL, LG = 'hTf%d' % j, 'pl%d' % j, 'lg%d' % j

                    def rmm(j=j):
                        for k in range(8):
                            ins = nc.tensor.matmul(pl[j][:], lhsT=hTf[j][:, k, :], rhs=rw[:, k, :], start=(k == 0), stop=(k == 7))
                        return ins
                    kb.op('pe', rmm, reads=[HF, 'rw'], writes=[PL])
                    G = 'g%d' % j
                    if RD < 2:
                        continue
                    kb.op('dve', lambda j=j: nc.vector.tensor_tensor(out=lg[j][:], in0=pl[j][:], in1=rb[:], op=ALU.add), reads=[PL, 'rb', G], writes=[G])
                    if RD < 3:
                        kb.dma('sp', self.GATES[t0:t0 + 128, :], lg[j][:], reads=[G], writes=['GATES%d' % ti])
                        continue
                    kb.op('dve', lambda j=j: nc.vector.tensor_reduce(out=mx[j][:, 0:1], in_=lg[j][:], axis=AX.X, op=ALU.max), reads=[G], writes=[G])
                    kb.op('dve', lambda j=j: nc.vector.tensor_scalar(out=m1[j][:], in0=lg[j][:], scalar1=mx[j][:, 0:1], scalar2=None, op0=ALU.is_equal), reads=[G], writes=[G])
                    kb.op('dve', lambda j=j: nc.vector.scalar_tensor_tensor(out=l2[j][:], in0=m1[j][:], scalar=-1e30, in1=lg[j][:], op0=ALU.mult, op1=ALU.add), reads=[G], writes=[G])
                    kb.op('dve', lambda j=j: nc.vector.tensor_reduce(out=mx[j][:, 1:2], in_=l2[j][:], axis=AX.X, op=ALU.max), reads=[G], writes=[G])
                    kb.op('dve', lambda j=j: nc.vector.tensor_scalar(out=m2[j][:], in0=l2[j][:], scalar1=mx[j][:, 1:2], scalar2=None, op0=ALU.is_equal), reads=[G], writes=[G])
                    kb.op('dve', lambda j=j: nc.vector.tensor_tensor(out=mx[j][:, 2:3], in0=mx[j][:, 0:1], in1=mx[j][:, 1:2], op=ALU.subtract), reads=[G], writes=[G])
                    kb.op('act', lambda j=j: nc.scalar.activation(out=mx[j][:, 2:3], in_=mx[j][:, 2:3], func=AF.Sigmoid), reads=[G], writes=[G])
                    kb.op('dve', lambda j=j: nc.vector.tensor_scalar(out=mx[j][:, 3:4], in0=mx[j][:, 2:3], scalar1=-1.0, scalar2=1.0, op0=ALU.mult, op1=ALU.add), reads=[G], writes=[G])
                    kb.op('dve', lambda j=j: nc.vector.tensor_scalar(out=gt[j][:], in0=m1[j][:], scalar1=mx[j][:, 2:3], scalar2=None, op0=ALU.mult), reads=[G], writes=[G])
                    kb.op('dve', lambda j=j: nc.vector.scalar_tensor_tensor(out=gt[j][:], in0=m2[j][:], scalar=mx[j][:, 3:4], in1=gt[j][:], op0=ALU.mult, op1=ALU.add), reads=[G], writes=[G])
                    kb.dma('sp', self.GATES[t0:t0 + 128, :], gt[j][:], reads=[G], writes=['GATES%d' % ti])

    def stage_ffnpass(self, w1, w3, w2, gate_col, first):
        nc, kb = self.nc, self.kb
        with Stage(kb) as st:
            w1b = st.sb("w1b", [128, 8, 1408], BF16)
            w3b = st.sb("w3b", [128, 8, 1408], BF16)
            w2b = st.sb("w2b", [128, 11, 1024], BF16)
            stg = [st.sb("stg%d" % j, [128, 1408], F32) for j in range(3)]
            n = 0
            for (dst, src, nk, key) in ((w1b, w1, 8, 'w1b'), (w3b, w3, 8, 'w3b'), (w2b, w2, 11, 'w2b')):
                width = src.shape[1]
                for k in range(nk):
                    j = n % 3
                    n += 1
                    kb.dma('sp', stg[j][:, 0:width], src[k * 128:(k + 1) * 128, :], writes=['stg%d' % j])
                    if n % 2:
                        kb.op('act', lambda dst=dst, k=k, j=j, width=width: nc.scalar.copy(out=dst[:, k, :], in_=stg[j][:, 0:width]),
                              reads=['stg%d' % j, key], writes=[key])
                    else:
                        kb.op('dve', lambda dst=dst, k=k, j=j, width=width: nc.vector.tensor_copy(out=dst[:, k, :], in_=stg[j][:, 0:width]),
                              reads=['stg%d' % j, key], writes=[key])
            hT = [st.sb("hT%d" % j, [128, 8, 512], BF16) for j in range(2)]
            act = [st.sb("act%d" % j, [128, 11, 512], BF16) for j in range(2)]
            sg = [st.sb("sg%d" % j, [128, 512], F32) for j in range(2)]
            pA = [st.ps("pA%d" % j, [128, 512], F32) for j in range(2)]
            pB = [st.ps("pB%d" % j, [128, 512], F32) for j in range(2)]
            pY = [st.ps("pY%d" % j, [128, 512], F32) for j in range(2)]
            yo = [st.sb("yo%d" % j, [128, 1024], F32) for j in range(2)]
            ya = [st.sb("ya%d" % j, [128, 1024], F32) for j in range(4)]
            gtl = [st.sb("gtl%d" % j, [128, 8], F32) for j in range(4)]
            nf = 0
            ny = 0
            _tl = tiles_of(NT, 512)

            def _ldh(itq):
                tq0, Wq = _tl[itq]
                kb.dma('sp', hT[itq % 2][:, :, 0:Wq], self.HT[:, :, tq0:tq0 + Wq], reads=['HT%d' % q for q in range(tq0 // 128, (tq0 + Wq) // 128)],
                       writes=['hT%d' % (itq % 2)])
            _ldh(0)
            for it, (t0, W) in enumerate(_tl):
                j = it % 2
                if it + 1 < len(_tl):
                    _ldh(it + 1)
                for sub in range(W // 128):
                    ts_ = t0 + sub * 128
                    if not first:
                        kb.dma('sp', ya[sub][:], self.ACC[ts_:ts_ + 128, :], reads=['ACC%d' % (ts_ // 128)], writes=['ya%d' % sub])
                    if gate_col is not None:
                        kb.dma('sp', gtl[sub][:], self.GATES[ts_:ts_ + 128, :], reads=['GATES%d' % (ts_ // 128)], writes=['gtl%d' % sub])
                for f in range(11):
                    jf = nf % 2
                    nf += 1

                    def mmA(jf=jf, f=f, j=j, W=W, wb=w1b, pp=pA):
                        for k in range(8):
                            ins = nc.tensor.matmul(pp[jf][:, 0:W], lhsT=wb[:, k, f * 128:(f + 1) * 128], rhs=hT[j][:, k, 0:W],
                                                   start=(k == 0), stop=(k == 7))
                        return ins
                    kb.op('pe', mmA, reads=['hT%d' % j, 'w1b'], writes=['pA%d' % jf])
                    kb.op('pe', lambda jf=jf, f=f, j=j, W=W: mmA(jf, f, j, W, w3b, pB), reads=['hT%d' % j, 'w3b'], writes=['pB%d' % jf])
                    kb.op('act', lambda jf=jf, W=W: nc.scalar.activation(out=sg[jf][:, 0:W], in_=pA[jf][:, 0:W], func=AF.Silu),
                          reads=['pA%d' % jf], writes=['sg%d' % jf])
                    kb.op('dve', lambda jf=jf, f=f, j=j, W=W: nc.vector.tensor_tensor(out=act[j][:, f, 0:W], in0=sg[jf][:, 0:W], in1=pB[jf][:, 0:W], op=ALU.mult),
                          reads=['sg%d' % jf, 'pB%d' % jf, 'act%d' % j], writes=['act%d' % j])
                for sub in range(W // 128):
                    jy = ny % 2
                    ny += 1
                    ti = t0 // 128 + sub
                    ts = t0 + sub * 128
                    for half in range(2):
                        jp = half

                        def mmY(jp=jp, half=half, sub=sub, j=j):
                            for f in range(11):
                                ins = nc.tensor.matmul(pY[jp][:], lhsT=act[j][:, f, sub * 128:(sub + 1) * 128],
                                                       rhs=w2b[:, f, half * 512:(half + 1) * 512], start=(f == 0), stop=(f == 10))
                            return ins
                        kb.op('pe', mmY, reads=['act%d' % j, 'w2b'], writes=['pY%d' % jp])
                        osl = slice(half * 512, (half + 1) * 512)
                        rd = ['pY%d' % jp, 'yo%d' % jy]
                        if gate_col is not None and not first:
                            kb.op('dve', lambda jp=jp, jy=jy, osl=osl, sub=sub: nc.vector.scalar_tensor_tensor(
                                out=yo[jy][:, osl], in0=pY[jp][:], scalar=gtl[sub][:, gate_col:gate_col + 1], in1=ya[sub][:, osl],
                                op0=ALU.mult, op1=ALU.add), reads=rd + ['gtl%d' % sub, 'ya%d' % sub], writes=['yo%d' % jy])
                        elif gate_col is not None:
                            kb.op('dve', lambda jp=jp, jy=jy, osl=osl, sub=sub: nc.vector.tensor_scalar(
                                out=yo[jy][:, osl], in0=pY[jp][:], scalar1=gtl[sub][:, gate_col:gate_col + 1], scalar2=None, op0=ALU.mult),
                                reads=rd + ['gtl%d' % sub], writes=['yo%d' % jy])
                        elif not first:
                            kb.op('dve', lambda jp=jp, jy=jy, osl=osl, sub=sub: nc.vector.tensor_tensor(
                                out=yo[jy][:, osl], in0=pY[jp][:], in1=ya[sub][:, osl], op=ALU.add),
                                reads=rd + ['ya%d' % sub], writes=['yo%d' % jy])
                        else:
                            kb.op('act', lambda jp=jp, jy=jy, osl=osl, sub=sub: nc.scalar.copy(out=yo[jy][:, osl], in_=pY[jp][:]),
                                  reads=rd, writes=['yo%d' % jy])
                    kb.dma('sp', self.ACC[ts:ts + 128, :], yo[jy][:], reads=['yo%d' % jy], writes=['ACC%d' % ti])

    def stage_finish(self, i, sub, out_final=None):
        nc, kb = self.nc, self.kb
        with Stage(kb) as st:
            gs = 2 if sub == 1 else 5
            md = self.mod_tiles(st, i, [gs])
            lng = self.bcast_load(st, "lng", self.ln_g[i, sub - 1, :], 1024)
            lnb = self.bcast_load(st, "lnb", self.ln_b[i, sub - 1, :], 1024)
            NB = 3
            xt = [st.sb("xt%d" % j, [128, 1024], F32) for j in range(NB)]
            at = [st.sb("at%d" % j, [128, 1024], F32) for j in range(NB)]
            stt = [st.sb("stt%d" % j, [128, 2, 6], F32) for j in range(NB)]
            mv = [st.sb("mv%d" % j, [128, 4], F32) for j in range(NB)]
            def _ld(tq):
                jq = tq % NB
                kb.dma('sp', xt[jq][:], self.XS[tq * 128:(tq + 1) * 128, :], writes=['xt%d' % jq])
                kb.dma('sp', at[jq][:], self.ACC[tq * 128:(tq + 1) * 128, :], reads=['ACC%d' % tq], writes=['at%d' % jq])
            _ld(0)
            for ti in range(NT // 128):
                j = ti % NB
                isc = 1 if ti < TC // 128 else 0
                t0 = ti * 128
                X, A, S = 'xt%d' % j, 'at%d' % j, 'st%d' % j
                if ti + 1 < NT // 128:
                    _ld(ti + 1)
                kb.op('dve', lambda j=j, isc=isc: nc.vector.tensor_tensor(out=at[j][:], in0=at[j][:], in1=md[(gs, isc)][:], op=ALU.mult),
                      reads=[A, 'mod%d_%d' % (gs, isc)], writes=[A])
                kb.op('dve', lambda j=j: nc.vector.scalar_tensor_tensor(out=xt[j][:], in0=xt[j][:], scalar=float(ALPHA), in1=at[j][:], op0=ALU.mult, op1=ALU.add),
                      reads=[X, A], writes=[X])

                def bn(j=j):
                    nc.vector.bn_stats(out=stt[j][:, 0, :], in_=xt[j][:, 0:512])
                    return nc.vector.bn_stats(out=stt[j][:, 1, :], in_=xt[j][:, 512:1024])
                kb.op('dve', bn, reads=[X, S], writes=[S])
                kb.op('dve', lambda j=j: nc.vector.bn_aggr(out=mv[j][:, 0:2], in_=stt[j][:]), reads=[S], writes=[S])
                kb.op('dve', lambda j=j: nc.vector.tensor_scalar(out=mv[j][:, 2:3], in0=mv[j][:, 1:2], scalar1=LN_EPS, scalar2=None, op0=ALU.add), reads=[S], writes=[S])
                kb.op('act', lambda j=j: nc.scalar.activation(out=mv[j][:, 2:3], in_=mv[j][:, 2:3], func=AF.Sqrt), reads=[S], writes=[S])
                kb.op('dve', lambda j=j: nc.vector.reciprocal(out=mv[j][:, 3:4], in_=mv[j][:, 2:3]), reads=[S], writes=[S])
                kb.op('dve', lambda j=j: nc.vector.tensor_scalar(out=xt[j][:], in0=xt[j][:], scalar1=mv[j][:, 0:1], scalar2=mv[j][:, 3:4],
                                                             op0=ALU.subtract, op1=ALU.mult), reads=[X, S], writes=[X])
                kb.op('dve', lambda j=j: nc.vector.tensor_tensor(out=xt[j][:], in0=xt[j][:], in1=lng[:], op=ALU.mult), reads=[X, 'lng'], writes=[X])
                kb.op('dve', lambda j=j: nc.vector.tensor_tensor(out=xt[j][:], in0=xt[j][:], in1=lnb[:], op=ALU.add), reads=[X, 'lnb'], writes=[X])
                kb.dma('sp', self.XS[t0:t0 + 128, :], xt[j][:], reads=[X], writes=['XS%d' % ti])
                if out_final is not None and ti >= TC // 128:
                    kb.dma('sp', out_final[t0 - TC:t0 - TC + 128, :], xt[j][:], reads=[X], writes=['OUT%d' % ti])

    def stage_pool(self, j_layer):
        nc, kb = self.nc, self.kb
        with Stage(kb) as st:
            pw = st.sb("pw", [128, 4, 2, 256], BF16)
            pst = [st.sb("pst%d" % j, [128, 256], F32) for j in range(2)]
            n = 0
            for gi in range(4):
                for kk in range(2):
                    j = n % 2
                    n += 1
                    kb.dma('sp', pst[j][:], self.pool_w[j_layer, gi, kk * 128:(kk + 1) * 128, :], writes=['pst%d' % j])
                    kb.op('dve', lambda gi=gi, kk=kk, j=j: nc.vector.tensor_copy(out=pw[:, gi, kk, :], in_=pst[j][:]), reads=['pst%d' % j, 'pw'], writes=['pw'])
            psc = self.bcast_load(st, "psc", self.pool_scale[j_layer, :], 1024)
            invc = st.sb("invc", [128, 4, 64], F32)
            kb.dma('sp', invc[:], self.cst_invc64, writes=['invc'])
            invcc = st.sb("invcc", [128, 4, 256], F32)
            kb.dma('sp', invcc[:], self.cst_invc256, writes=['invcc'])
            hT = [st.sb("hT%d" % j, [128, 8, 512], BF16) for j in range(2)]
            P = st.sb("P", [128, 8, 8, 80], F32)
            S2 = st.sb("S2", [128, 8, 8, 80], F32)
            S4 = st.sb("S4", [128, 8, 8, 80], F32)
            S8 = st.sb("S8", [128, 8, 8, 80], F32)
            S16 = st.sb("S16", [128, 8, 8, 80], F32)
            pl = [st.sb("pl%d" % j, [128, 8, 512], BF16) for j in range(2)]
            tmp = st.sb("tmp", [128, 2, 8, 64], F32)
            pY = [st.ps("pY%d" % j, [128, 1024], F32) for j in range(2)]
            yo = [st.sb("yo%d" % j, [128, 1024], F32) for j in range(2)]
            for b_ in (P, S2, S4, S8, S16):
                pass
            kb.op('pool', lambda: nc.gpsimd.memset(P[:], 0.0), writes=['P'])
            ny = 0
            for it, (t0, W) in enumerate([(0, 256)] + tiles_of(NT, 512, 256)):
                j = it % 2
                isc = (t0 == 0)
                kb.dma('sp', hT[j][:, :, 0:W], self.HT[:, :, t0:t0 + W], reads=['HT%d' % q for q in range(t0 // 128, (t0 + W) // 128)],
                       writes=['hT%d' % j])
                if isc:
                    Pv = P[:].rearrange("p k r c -> p k (r c)")[:, :, 0:272].rearrange("p k (r c) -> p k r c", r=1)
                    views = [b_[:].rearrange("p k r c -> p k (r c)")[:, :, 0:272].rearrange("p k (r c) -> p k r c", r=1) for b_ in (P, S2, S4, S8, S16)]
                    L = 256
                    R = 1
                else:
                    if it == 1:
                        kb.op('pool', lambda: nc.gpsimd.memset(P[:], 0.0), reads=['P'], writes=['P'])
                    views = [b_[:] for b_ in (P, S2, S4, S8, S16)]
                    L = 64
                    R = 8
                Pv, S2v, S4v, S8v, S16v = views
                LP = L + 16
                kb.op('act', lambda Pv=Pv, j=j, L=L, R=R, W=W: nc.scalar.copy(out=Pv[:, :, :, 8:8 + L], in_=hT[j][:, :, 0:W].rearrange("p k (r c) -> p k r c", r=R)),
                      reads=['hT%d' % j, 'P'], writes=['P'])
                kb.op('dve', lambda: nc.vector.tensor_tensor(out=S2v[:, :, :, 0:LP - 1], in0=Pv[:, :, :, 0:LP - 1], in1=Pv[:, :, :, 1:LP], op=ALU.add), reads=['P', 'S2'], writes=['S2'])
                kb.op('dve', lambda: nc.vector.tensor_tensor(out=S4v[:, :, :, 0:LP - 3], in0=S2v[:, :, :, 0:LP - 3], in1=S2v[:, :, :, 2:LP - 1], op=ALU.add), reads=['S2', 'S4'], writes=['S4'])
                kb.op('dve', lambda: nc.vector.tensor_tensor(out=S8v[:, :, :, 0:LP - 7], in0=S4v[:, :, :, 0:LP - 7], in1=S4v[:, :, :, 4:LP - 3], op=ALU.add), reads=['S4', 'S8'], writes=['S8'])
                kb.op('dve', lambda: nc.vector.tensor_tensor(out=S16v[:, :, :, 0:LP - 15], in0=S8v[:, :, :, 0:LP - 15], in1=S8v[:, :, :, 8:LP - 7], op=ALU.add), reads=['S8', 'S16'], writes=['S16'])
                for gi, (win, Sv, key) in enumerate(((2, S2v, 'S2'), (4, S4v, 'S4'), (8, S8v, 'S8'), (16, S16v, 'S16'))):
                    o = 8 - win // 2
                    ic = (invcc if isc else invc)
                    icv = ic[:, gi, :].unsqueeze(1).unsqueeze(1).to_broadcast([128, 2, R, L])
                    tv = tmp[:].rearrange("p k r c -> p k (r c)")[:, :, 0:W].rearrange("p k (r c) -> p k r c", r=R)
                    kb.op('dve', lambda Sv=Sv, gi=gi, o=o, icv=icv, tv=tv, L=L: nc.vector.tensor_tensor(out=tv, in0=Sv[:, 2 * gi:2 * gi + 2, :, o:o + L], in1=icv, op=ALU.mult),
                          reads=[key, 'invc', 'invcc', 'tmp'], writes=['tmp'])
                    kb.op('dve', lambda gi=gi, tv=tv, j=j, Pv=Pv, L=L, R=R, W=W: nc.vector.tensor_tensor(
                        out=pl[j][:, 2 * gi:2 * gi + 2, 0:W].rearrange("p k (r c) -> p k r c", r=R), in0=tv, in1=Pv[:, 2 * gi:2 * gi + 2, :, 8:8 + L], op=ALU.subtract),
                        reads=['tmp', 'P', 'pl%d' % j], writes=['pl%d' % j])
                for sub in range(W // 128):
                    jy = ny % 2
                    ny += 1
                    ti = t0 // 128 + sub
                    ts = t0 + sub * 128

                    def mm(jy=jy, sub=sub, j=j):
                        for gi in range(4):
                            for kk in range(2):
                                ins = nc.tensor.matmul(pY[jy][:, gi * 256:(gi + 1) * 256], lhsT=pl[j][:, 2 * gi + kk, sub * 128:(sub + 1) * 128],
                                                       rhs=pw[:, gi, kk, :], start=(kk == 0), stop=(kk == 1))
                        return ins
                    kb.op('pe', mm, reads=['pl%d' % j, 'pw'], writes=['pY%d' % jy])
                    kb.op('dve', lambda jy=jy: nc.vector.tensor_tensor(out=yo[jy][:], in0=pY[jy][:], in1=psc[:], op=ALU.mult), reads=['pY%d' % jy, 'psc', 'yo%d' % jy], writes=['yo%d' % jy])
                    kb.dma('sp', self.ACC[ts:ts + 128, :], yo[jy][:], reads=['yo%d' % jy], writes=['ACC%d' % ti])

    def scan_consts(self, st, z):
        kb = self.kb
        c = {}
        for nm in ('SB', 'SBI', 'SBT', 'BLK'):
            t = st.sb(nm, [128, 128], F32)
            kb.dma('sp', t[:], self.cst_masks[z, {'SB': 0, 'SBI': 1, 'SBT': 2, 'BLK': 3}[nm]], writes=[nm])
            c[nm] = t
        idf = st.sb("identf", [128, 128], F32)
        kb.dma('sp', idf[:], self.cst_identf, writes=['identf'])
        idb = st.sb("identb", [128, 128], BF16)
        kb.op('dve', lambda: self.nc.vector.tensor_copy(out=idb[:], in_=idf[:]), reads=['identf'], writes=['identb'])
        c['identf'] = idf
        c['identb'] = idb
        return c

    def head_scan(self, st, C, hid, dk, dv, pb, KAP, RM, BCt, KCt, V, KAPT, BPT, KPT, RMT, Dx, Di, DxT, wc, T, yout, opk, z, bufs, slot=0, ndt=BF16):
        nc, kb = self.nc, self.kb
        B = bufs
        par = slot
        ps = B['ps']

        def nps():
            return ps[slot], B['psk'][slot]
        sfx = '_%d' % par

        def S(name):
            return B[name][par], name + sfx
        idf, idb = C['identf'], C['identb']

        def mm_ev(name, lhsT, rhs, mul=None, mulkey=None, eng='dve', rows=128, cols=128, extra=None, reads=()):
            p, pk = nps()
            dst, dk_ = S(name)

            def f():
                ins = nc.tensor.matmul(p[0:rows, 0:cols], lhsT=lhsT, rhs=rhs, start=True, stop=(extra is None))
                if extra is not None:
                    for qi, (l2, r2) in enumerate(extra):
                        ins = nc.tensor.matmul(p[0:rows, 0:cols], lhsT=l2, rhs=r2, start=False, stop=(qi == len(extra) - 1))
                return ins
            kb.op('pe', f, reads=list(reads), writes=[pk])
            if mul is not None:
                kb.op('dve', lambda: nc.vector.tensor_tensor(out=dst[0:rows, 0:cols], in0=p[0:rows, 0:cols], in1=mul, op=ALU.mult),
                      reads=[pk, mulkey, dk_], writes=[dk_])
            elif eng == 'act':
                kb.op('act', lambda: nc.scalar.copy(out=dst[0:rows, 0:cols], in_=p[0:rows, 0:cols]), reads=[pk, dk_], writes=[dk_])
            else:
                kb.op('dve', lambda: nc.vector.tensor_copy(out=dst[0:rows, 0:cols], in_=p[0:rows, 0:cols]), reads=[pk, dk_], writes=[dk_])
            return dst, dk_
        OK = list(opk)
        N_, Nk = mm_ev('N', BPT, KAPT, mul=Dx[0], mulkey=Dx[1], reads=OK)
        yield
        NT_, NTk = mm_ev('NT', KAPT, BPT, mul=DxT[0], mulkey=DxT[1], reads=OK)
        yield
        BtT, BtTk = mm_ev('BtT', KPT, KAPT, mul=Dx[0], mulkey=Dx[1], reads=OK)
        yield
        AbT, AbTk = mm_ev('AbT', BPT, RMT, mul=Di[0], mulkey=Di[1], reads=OK)
        yield
        AkT, AkTk = mm_ev('AkT', KPT, RMT, mul=Di[0], mulkey=Di[1], reads=OK)
        yield
        R_, Rk = S('R')
        kb.op('dve', lambda: nc.vector.tensor_tensor(out=R_[:], in0=N_[:], in1=idf[:], op=ALU.add), reads=[Nk, 'identf', Rk], writes=[Rk])
        yield
        P, Pk, PT, PTk = N_, Nk, NT_, NTk
        for it in range(5):
            PT2, PT2k = mm_ev('PT%d' % (it % 2), P[:], PT[:], eng='act', reads=[Pk, PTk])
            yield
            if it < 4:
                P2, P2k = mm_ev('P%d' % (it % 2), PT[:], P[:], eng='act', reads=[Pk, PTk])
                yield
            R2_, R2k = S('R' if it % 2 else 'Rb')
            Rn, Rnk = mm_ev('Rb' if it % 2 == 0 else 'R', (idb if ndt == BF16 else idf)[:], R_[:], extra=[(PT2[:], R_[:])], eng='dve', reads=['identb', 'identf', PT2k, Rk])
            yield
            R_, Rk = Rn, Rnk
            if it < 4:
                P, Pk, PT, PTk = P2, P2k, PT2, PT2k
        if ndt == BF16:
            MTb, MTbk = R_, Rk
        else:
            MTb, MTbk = S('MTb')
            kb.op('act', lambda: nc.scalar.copy(out=MTb[:], in_=R_[:]), reads=[Rk, MTbk], writes=[MTbk])
            yield
        X0, X0k = mm_ev('X0', BtT[:], V, cols=dv, reads=[BtTk] + OK)
        yield
        U0, U0k = mm_ev('U0', MTb[:], X0[:, 0:dv], cols=dv, eng='act', reads=[MTbk, X0k])
        yield
        MK, MKk = mm_ev('MK', MTb[:], KAP, cols=dk, reads=[MTbk] + OK)
        yield
        RstT, RstTk = mm_ev('RstT', MK[:, 0:dk], AbT[:], rows=dk, extra=[(RM, idb[:])], eng='act', reads=[MKk, AbTk, 'identb'] + OK)
        yield
        Y0, Y0k = mm_ev('Y0', AbT[:], U0[:, 0:dv], cols=dv, extra=[(AkT[:], V)], reads=[AbTk, U0k, AkTk] + OK)
        yield
        Phi = {}
        Z0 = {}
        for c in range(2):
            rs = slice(c * 64, (c + 1) * 64)
            p, pk = nps()
            kb.op('pe', lambda p=p, rs=rs: nc.tensor.matmul(p[0:dk, 0:dk], lhsT=MK[rs, 0:dk], rhs=BCt[rs, :], start=True, stop=True), reads=[MKk] + OK, writes=[pk])
            yield
            dst, dkey = S('Phi%d' % c)
            kb.op('dve', lambda p=p, dst=dst, c=c: nc.vector.scalar_tensor_tensor(out=dst[0:dk, 0:dk], in0=idf[0:dk, 0:dk], scalar=wc[0][:, c:c + 1], in1=p[0:dk, 0:dk],
                                                                               op0=ALU.mult, op1=ALU.add), reads=[pk, 'identf', wc[1], dkey], writes=[dkey])
            Phi[c] = (dst, dkey)
            Z0[c] = mm_ev('Z0%d' % c, BCt[rs, :], U0[rs, 0:dv], rows=dk, cols=dv, extra=[(KCt[rs, :], V[rs, :])], eng='act', reads=[U0k] + OK)
            yield
        Tt, Tk = T
        for c in ((0, 1) if z == 0 else (1, 0)):
            rs = slice(c * 64, (c + 1) * 64)
            p, pk = nps()
            kb.op('pe', lambda p=p: nc.tensor.matmul(p[:, 0:dv], lhsT=RstT[0:dk, :], rhs=Tt[:], start=True, stop=True), reads=[RstTk, Tk], writes=[pk])
            yield
            kb.op('dve', lambda p=p, rs=rs: nc.vector.tensor_tensor(out=yout[0][rs, :], in0=p[rs, 0:dv], in1=Y0[rs, 0:dv], op=ALU.add), reads=[pk, Y0k, yout[1]], writes=[yout[1]])
            yield
            p2, pk2 = nps()

            def tm(p2=p2, c=c):
                nc.tensor.matmul(p2[0:dk, 0:dv], lhsT=Phi[c][0][0:dk, 0:dk], rhs=Tt[:], start=True, stop=False)
                return nc.tensor.matmul(p2[0:dk, 0:dv], lhsT=idf[0:dk, 0:dk], rhs=Z0[c][0][0:dk, 0:dv], start=False, stop=True)
            kb.op('pe', tm, reads=[Phi[c][1], Z0[c][1], Tk, 'identf'], writes=[pk2])
            yield
            kb.op('act', lambda p2=p2: nc.scalar.copy(out=Tt[:], in_=p2[0:dk, 0:dv]), reads=[pk2, Tk], writes=[Tk])
            yield

    def run_heads(self, gens):
        gens = list(gens)
        active = {}
        free = list(range(NSLOT))
        while gens or active:
            while gens and free:
                sl = free.pop(0)
                active[sl] = gens.pop(0)(sl)
            for sl in list(active.keys()):
                try:
                    next(active[sl])
                except StopIteration:
                    del active[sl]
                    free.append(sl)

    def run_pipeline(self, tiles):
        n = len(tiles)
        NS = NSLOT - 2
        op_done = [False] * n
        heads_left = [list(t[1]) for t in tiles]
        heads_active = [0] * n
        stored = [False] * n
        cur_op = None
        cur_op_idx = -1
        next_op = 0
        active = {}
        free = list(range(NS))
        head_tile = 0
        while not all(stored):
            progressed = False
            if cur_op is None and next_op < n and (next_op < 2 or (not heads_left[next_op - 2] and heads_active[next_op - 2] == 0 and stored[next_op - 2])):
                cur_op = tiles[next_op][0]()
                cur_op_idx = next_op
                next_op += 1
            if cur_op is not None:
                try:
                    next(cur_op)
                except StopIteration:
                    op_done[cur_op_idx] = True
                    cur_op = None
                progressed = True
            while free and head_tile < n and op_done[head_tile]:
                if heads_left[head_tile]:
                    sl = free.pop(0)
                    active[sl] = (head_tile, heads_left[head_tile].pop(0)(sl))
                    heads_active[head_tile] += 1
                else:
                    if heads_active[head_tile] == 0:
                        tiles[head_tile][2]()
                        stored[head_tile] = True
                        head_tile += 1
                        progressed = True
                    else:
                        break
            for sl in list(active.keys()):
                tix, g = active[sl]
                try:
                    next(g)
                except StopIteration:
                    del active[sl]
                    free.append(sl)
                    heads_active[tix] -= 1
                progressed = True
            assert progressed, "pipeline stuck"

    def head_bufs(self, st, ndt=BF16):
        B = {}
        for nm, dt in (('N', ndt), ('NT', ndt), ('R', ndt), ('Rb', ndt), ('P0', ndt), ('P1', ndt), ('PT0', ndt), ('PT1', ndt),
                       ('BtT', BF16), ('AbT', BF16), ('AkT', BF16), ('MTb', BF16), ('X0', BF16), ('U0', BF16), ('MK', BF16),
                       ('RstT', F32), ('Y0', F32), ('Phi0', F32), ('Phi1', F32), ('Z00', F32), ('Z01', F32)):
            B[nm] = [st.sb("%s_%d" % (nm, q), [128, 128], dt) for q in range(NSLOT)]
        B['ps'] = [st.ps("hps%d" % q, [128, 512], F32) for q in range(NSLOT - 2)]
        B['psk'] = ['hps%d' % q for q in range(NSLOT - 2)]
        return B

    def scan_order(self, z):
        nt = NT // 128
        nct = TC // 128
        if z == 0:
            return list(range(nt))
        return list(range(nct - 1, -1, -1)) + list(range(nt - 1, nct - 1, -1))

    def stage_rwkv_proj(self, jl):
        nc, kb = self.nc, self.kb
        with Stage(kb) as st:
            NCOL = 3072 + 384
            Wb = st.sb("Wb", [128, 8, NCOL], BF16)
            stg = [st.sb("stg%d" % j, [128, 1024], F32) for j in range(2)]
            n = 0
            srcs = [(self.rk_w_rkv[jl, 0], 0, 1024), (self.rk_w_rkv[jl, 1], 1024, 1024), (self.rk_w_rkv[jl, 2], 2048, 1024),
                    (self.rk_w1[jl, 0], 3072, 64), (self.rk_w1[jl, 1], 3136, 64), (self.rk_a1[jl, 0], 3200, 64), (self.rk_a1[jl, 1], 3264, 64),
                    (self.rk_g1[jl], 3328, 128)]
            for (src, c0, wd) in srcs:
                for k in range(8):
                    j = n % 2
                    n += 1
                    kb.dma('sp', stg[j][:, 0:wd], src[k * 128:(k + 1) * 128, :], writes=['stg%d' % j])
                    kb.op('act' if n % 2 else 'dve', (lambda k=k, j=j, c0=c0, wd=wd: nc.scalar.copy(out=Wb[:, k, c0:c0 + wd], in_=stg[j][:, 0:wd])) if n % 2 else
                          (lambda k=k, j=j, c0=c0, wd=wd: nc.vector.tensor_copy(out=Wb[:, k, c0:c0 + wd], in_=stg[j][:, 0:wd])), reads=['stg%d' % j, 'Wb'], writes=['Wb'])
            W2 = st.sb("W2", [128, 3, 1024], BF16)
            for q, srcl in enumerate(([self.rk_w2[jl, 0], self.rk_w2[jl, 1]], [self.rk_a2[jl, 0], self.rk_a2[jl, 1]], [self.rk_g2[jl]])):
                j = q % 2
                r0 = 0
                for si, src in enumerate(srcl):
                    rws = src.shape[0]
                    kb.dma('sp', stg[j][r0:r0 + rws, :], src, reads=['stg%d' % j] if si else [], writes=['stg%d' % j])
                    r0 += rws
                kb.op('dve', lambda q=q, j=j: nc.vector.tensor_copy(out=W2[:, q, :], in_=stg[j][:]), reads=['stg%d' % j, 'W2'], writes=['W2'])
            mu = st.sb("mu", [128, 6, 8], F32)
            for m in range(6):
                kb.dma('sp', mu[:, m, :], self.rk_mu[jl, m, :].rearrange("(k p) -> p k", p=128), reads=['mu'] if m else [], writes=['mu'], allow_slow_non_contiguous=True)
            HW_ = 512 + 128
            ht = st.sb("ht", [128, 8, HW_], BF16)
            xx = st.sb("xx", [128, 8, 512], BF16)
            xm = st.sb("xm", [128, 6, 8, 512], BF16)
            l1 = st.sb("l1", [128, 3, 512], BF16)
            pp = [st.ps("pp%d" % q, [128, 512], F32) for q in range(4)]
            ob = [st.sb("ob%d" % q, [128, 1024], F32) for q in range(4)]
            npp = [0]
            nob = [0]
            tiles = [(0, TC)] + tiles_of(NT, 512, TC)
            for it, (t0, W) in enumerate(tiles):
                isc = (t0 == 0)
                lo = t0 - 64
                hi = t0 + W + 64
                zl = isc or t0 == TC
                zr = isc or (t0 + W == NT)
                if zl:
                    kb.op('dve', lambda: nc.vector.memset(ht[:, :, 0:64], 0.0), reads=['ht'], writes=['ht'])
                if zr:
                    kb.op('dve', lambda W=W: nc.vector.memset(ht[:, :, 64 + W:128 + W], 0.0), reads=['ht'], writes=['ht'])
                a = t0 if zl else lo
                b = t0 + W if zr else hi
                kb.dma('sp', ht[:, :, 64 + (a - t0):64 + (b - t0)], self.HT[:, :, a:b], reads=['HT%d' % q for q in range(a // 128, (b + 127) // 128)] + ['ht'], writes=['ht'])
                for k in range(8):
                    if isc:
                        off = 63 if k < 4 else 65
                    else:
                        off = (63, 63, 65, 65, 0, 0, 128, 128)[k]
                    kb.op('dve', lambda k=k, off=off, W=W: nc.vector.tensor_tensor(out=xx[:, k, 0:W], in0=ht[:, k, off:off + W], in1=ht[:, k, 64:64 + W], op=ALU.subtract),
                          reads=['ht', 'xx'], writes=['xx'])
                    if not isc and k < 4:
                        col = 0 if k < 2 else 63
                        kb.op('dve', lambda k=k, col=col, W=W: nc.vector.tensor_scalar(
                            out=xx[:, k, 0:W].rearrange("p (r c) -> p r c", c=64)[:, :, col], in0=ht[:, k, 64:64 + W].rearrange("p (r c) -> p r c", c=64)[:, :, col],
                            scalar1=-1.0, scalar2=None, op0=ALU.mult), reads=['ht', 'xx'], writes=['xx'])
                for m in range(6):
                    for k in range(8):
                        eng = 'dve'
                        E = nc.vector if eng == 'dve' else nc.gpsimd
                        kb.op(eng, lambda m=m, k=k, W=W, E=E: E.scalar_tensor_tensor(out=xm[:, m, k, 0:W], in0=xx[:, k, 0:W], scalar=mu[:, m, k:k + 1], in1=ht[:, k, 64:64 + W],
                                                                                 op0=ALU.mult, op1=ALU.add), reads=['xx', 'ht', 'mu', 'xm%d' % m], writes=['xm%d' % m])
                for c, (m, fn) in enumerate(((1, AF.Tanh), (4, AF.Copy), (5, AF.Sigmoid))):
                    q = npp[0] % 4
                    npp[0] += 1

                    def f(q=q, c=c, m=m, W=W):
                        for k in range(8):
                            ins = nc.tensor.matmul(pp[q][:, 0:W], lhsT=Wb[:, k, 3072 + c * 128:3072 + (c + 1) * 128], rhs=xm[:, m, k, 0:W], start=(k == 0), stop=(k == 7))
                        return ins
                    kb.op('pe', f, reads=['Wb', 'xm%d' % m], writes=['pp%d' % q])
                    kb.op('act', lambda q=q, c=c, fn=fn, W=W: nc.scalar.activation(out=l1[:, c, 0:W], in_=pp[q][:, 0:W], func=fn), reads=['pp%d' % q, 'l1'], writes=['l1'])
                for sub in range(W // 128):
                    ts = t0 + sub * 128
                    ti = ts // 128
                    ss = slice(sub * 128, (sub + 1) * 128)
                    outs = [('RR', 0, 0, None), ('KK0', 2, 1024, None), ('VV', 3, 2048, None),
                            ('DL0', None, 0, (0, 0, 64)), ('DL1', None, 0, (0, 64, 128)), ('AL0', None, 1, (1, 0, 64)), ('AL1', None, 1, (1, 64, 128)), ('GG', None, 2, (2, 0, 128))]
                    for (nm, m, c0, l2) in outs:
                        o = nob[0] % 4
                        nob[0] += 1
                        for half in range(2):
                            q = npp[0] % 4
                            npp[0] += 1
                            if l2 is None:
                                def f(q=q, m=m, c0=c0, half=half, ss=ss):
                                    for k in range(8):
                                        ins = nc.tensor.matmul(pp[q][:], lhsT=xm[:, m, k, ss], rhs=Wb[:, k, c0 + half * 512:c0 + (half + 1) * 512], start=(k == 0), stop=(k == 7))
                                    return ins
                                kb.op('pe', f, reads=['Wb', 'xm%d' % m], writes=['pp%d' % q])
                            else:
                                c, r0, r1 = l2
                                kb.op('pe', lambda q=q, c=c, r0=r0, r1=r1, half=half, ss=ss: nc.tensor.matmul(pp[q][:], lhsT=l1[r0:r1, c, ss], rhs=W2[r0:r1, c, half * 512:(half + 1) * 512],
                                                                                                           start=True, stop=True), reads=['l1', 'W2'], writes=['pp%d' % q])
                            if half == 0:
                                kb.op('act', lambda q=q, o=o: nc.scalar.copy(out=ob[o][:, 0:512], in_=pp[q][:]), reads=['pp%d' % q, 'ob%d' % o], writes=['ob%d' % o])
                            else:
                                kb.op('dve', lambda q=q, o=o: nc.vector.tensor_copy(out=ob[o][:, 512:1024], in_=pp[q][:]), reads=['pp%d' % q, 'ob%d' % o], writes=['ob%d' % o])
                        kb.dma('sp', self.RK[nm][ts:ts + 128, :], ob[o][:], reads=['ob%d' % o], writes=['%s%d' % (nm, ti)])

    def stage_rwkv_scan(self, jl, z):
        nc, kb = self.nc, self.kb
        with Stage(kb) as st:
            C = self.scan_consts(st, z)
            B = self.head_bufs(st)
            w0 = self.bcast_load(st, "w0", self.rk_w0[jl, z, :], 1024)
            a0 = self.bcast_load(st, "a0", self.rk_a0[jl, z, :], 1024)
            k_k = self.bcast_load(st, "k_k", self.rk_k_k[jl, :], 1024)
            k_a = self.bcast_load(st, "k_a", self.rk_k_a[jl, :], 1024)
            T = [st.sb("T%d" % h, [64, 64], F32) for h in range(16)]
            for h in range(16):
                kb.op('dve', lambda h=h: nc.vector.memset(T[h][:], 0.0), writes=['T%d' % h])
            ld = {nm: st.sb("ld_" + nm, [128, 1024], F32) for nm in ('RR', 'KK0', 'VV', 'DL', 'AL')}
            f = {nm: st.sb("f_" + nm, [128, 1024], F32) for nm in ('sw', 'a', 'kk', 'kd', 'b', 'eG', 'enG', 'eGx', 'eRev', 'eTot', 'Gs', 't1')}
            sm = st.sb("sm", [128, 3, 16], F32)
            tb2 = [{nm: st.sb("tb_%s_%d" % (nm, q), [128, 1024], BF16) for nm in ('KAP', 'RM', 'BC', 'KC', 'V', 'BP', 'KP')} for q in range(2)]
            fT2 = [{nm: st.sb("fT_%s_%d" % (nm, q), [128, 8, 128], BF16) for nm in ('KAP', 'BP', 'KP', 'RM')} for q in range(2)]
            eTT = st.sb("eTT", [128, 8, 128], F32)
            wc2 = [st.sb("wc%d" % q, [64, 16, 2], F32) for q in range(2)]
            pG = [st.ps("pG%d" % q, [128, 512], F32) for q in range(2)]
            yt2 = [st.sb("yt%d" % q, [128, 1024], F32) for q in range(2)]
            DEC = 0.6065306597126334
            order = self.scan_order(z)

            def make_tile(idx, ti):
                q = idx % 2
                tb, fT, wc, yt = tb2[q], fT2[q], wc2[q], yt2[q]
                K_ = lambda nm: '%s#%d' % (nm, q)
                t0 = ti * 128

                def opgen():
                    t0 = ti * 128
                    for nm, src in (('RR', self.RK['RR']), ('KK0', self.RK['KK0']), ('VV', self.RK['VV']), ('DL', self.RK['DL%d' % z]), ('AL', self.RK['AL%d' % z])):
                        kb.dma('sp', ld[nm][:], src[t0:t0 + 128, :], reads=['%s%d' % (nm if nm in ('RR', 'KK0', 'VV') else nm + str(z), ti)], writes=['ld_' + nm])
                        yield
                    V_ = nc.vector
                    kb.op('dve', lambda: V_.tensor_tensor(out=f['t1'][:], in0=ld['DL'][:], in1=w0[:], op=ALU.add), reads=['ld_DL', 'w0', 'f_t1'], writes=['f_t1'])
                    yield
                    kb.op('act', lambda: nc.scalar.activation(out=f['sw'][:], in_=f['t1'][:], func=AF.Sigmoid), reads=['f_t1', 'f_sw'], writes=['f_sw'])
                    yield
                    kb.op('dve', lambda: V_.tensor_tensor(out=f['t1'][:], in0=ld['AL'][:], in1=a0[:], op=ALU.add), reads=['ld_AL', 'a0', 'f_t1'], writes=['f_t1'])
                    yield
                    kb.op('act', lambda: nc.scalar.activation(out=f['a'][:], in_=f['t1'][:], func=AF.Sigmoid), reads=['f_t1', 'f_a'], writes=['f_a'])
                    yield
                    kb.op('dve', lambda: V_.tensor_tensor(out=f['kk'][:], in0=ld['KK0'][:], in1=k_k[:], op=ALU.mult), reads=['ld_KK0', 'k_k', 'f_kk'], writes=['f_kk'])
                    yield
                    kb.op('dve', lambda: V_.tensor_tensor(out=f['t1'][:], in0=f['kk'][:], in1=f['kk'][:], op=ALU.mult), reads=['f_kk', 'f_t1'], writes=['f_t1'])
                    yield
                    kb.op('dve', lambda: V_.tensor_reduce(out=sm[:, 0, :], in_=f['t1'][:].rearrange("p (h n) -> p h n", n=64), axis=AX.X, op=ALU.add), reads=['f_t1', 'sm'], writes=['sm'])
                    yield
                    kb.op('act', lambda: nc.scalar.activation(out=sm[:, 1, :], in_=sm[:, 0, :], func=AF.Sqrt), reads=['sm'], writes=['sm'])
                    yield
                    kb.op('dve', lambda: V_.tensor_scalar(out=sm[:, 1, :], in0=sm[:, 1, :], scalar1=1e-12, scalar2=None, op0=ALU.max), reads=['sm'], writes=['sm'])
                    yield
                    kb.op('dve', lambda: V_.reciprocal(out=sm[:, 2, :], in_=sm[:, 1, :]), reads=['sm'], writes=['sm'])
                    yield
                    kb.op('dve', lambda: V_.tensor_tensor(out=f['kk'][:].rearrange("p (h n) -> p h n", n=64), in0=f['kk'][:].rearrange("p (h n) -> p h n", n=64),
                                                          in1=sm[:, 2, :].unsqueeze(2).to_broadcast([128, 16, 64]), op=ALU.mult), reads=['sm', 'f_kk'], writes=['f_kk'])
                    kb.op('dve', lambda: V_.scalar_tensor_tensor(out=f['t1'][:], in0=f['a'][:], scalar=-1.0, in1=k_a[:], op0=ALU.add, op1=ALU.mult), reads=['f_a', 'k_a', 'f_t1'], writes=['f_t1'])
                    yield
                    kb.op('dve', lambda: V_.scalar_tensor_tensor(out=f['kd'][:], in0=f['t1'][:], scalar=1.0, in1=ld['KK0'][:], op0=ALU.add, op1=ALU.mult), reads=['f_t1', 'ld_KK0', 'f_kd'], writes=['f_kd'])
                    yield
                    kb.op('dve', lambda: V_.tensor_tensor(out=f['b'][:], in0=f['kk'][:], in1=f['a'][:], op=ALU.mult), reads=['f_kk', 'f_a', 'f_b'], writes=['f_b'])
                    yield
                    for half in range(2):
                        hs = slice(half * 512, (half + 1) * 512)
                        kb.op('pe', lambda hs=hs: nc.tensor.matmul(pG[0][:], lhsT=C['SBI'][:], rhs=f['sw'][:, hs], start=True, stop=True), reads=['SBI', 'f_sw'], writes=['pG0'])
                        yield
                        kb.op('pe', lambda hs=hs: nc.tensor.matmul(pG[1][:], lhsT=C['BLK'][:], rhs=f['sw'][:, hs], start=True, stop=True), reads=['BLK', 'f_sw'], writes=['pG1'])
                        yield
                        kb.op('act', lambda hs=hs: nc.scalar.activation(out=f['eG'][:, hs], in_=pG[0][:], func=AF.Exp, scale=-DEC), reads=['pG0', 'f_eG'], writes=['f_eG'])
                        yield
                        kb.op('act', lambda hs=hs: nc.scalar.activation(out=f['enG'][:, hs], in_=pG[0][:], func=AF.Exp, scale=DEC), reads=['pG0', 'f_enG'], writes=['f_enG'])
                        yield
                        kb.op('dve', lambda hs=hs: V_.tensor_copy(out=f['Gs'][:, hs], in_=pG[0][:]), reads=['pG0', 'f_Gs'], writes=['f_Gs'])
                        yield
                        kb.op('act', lambda hs=hs: nc.scalar.activation(out=f['eTot'][:, hs], in_=pG[1][:], func=AF.Exp, scale=-DEC), reads=['pG1', 'f_eTot'], writes=['f_eTot'])
                        yield
                        kb.op('dve', lambda hs=hs: V_.tensor_tensor(out=f['eRev'][:, hs], in0=pG[1][:], in1=f['Gs'][:, hs], op=ALU.subtract), reads=['pG1', 'f_Gs', 'f_eRev'], writes=['f_eRev'])
                        yield
                    kb.op('act', lambda: nc.scalar.activation(out=f['eRev'][:], in_=f['eRev'][:], func=AF.Exp, scale=-DEC), reads=['f_eRev'], writes=['f_eRev'])
                    yield
                    kb.op('dve', lambda: V_.tensor_tensor(out=f['eGx'][:], in0=f['Gs'][:], in1=f['sw'][:], op=ALU.subtract), reads=['f_Gs', 'f_sw', 'f_eGx'], writes=['f_eGx'])
                    yield
                    kb.op('act', lambda: nc.scalar.activation(out=f['eGx'][:], in_=f['eGx'][:], func=AF.Exp, scale=-DEC), reads=['f_eGx'], writes=['f_eGx'])
                    yield
                    kb.op('dve', lambda: V_.scalar_tensor_tensor(out=tb['KAP'][:], in0=f['kk'][:], scalar=-1.0, in1=f['eGx'][:], op0=ALU.mult, op1=ALU.mult), reads=['f_kk', 'f_eGx', K_('tb_KAP')], writes=[K_('tb_KAP')])
                    yield
                    kb.op('dve', lambda: nc.vector.tensor_tensor(out=tb['RM'][:], in0=ld['RR'][:], in1=f['eG'][:], op=ALU.mult), reads=['ld_RR', 'f_eG', K_('tb_RM')], writes=[K_('tb_RM')])
                    yield
                    kb.op('dve', lambda: V_.tensor_tensor(out=tb['BP'][:], in0=f['b'][:], in1=f['enG'][:], op=ALU.mult), reads=['f_b', 'f_enG', K_('tb_BP')], writes=[K_('tb_BP')])
                    yield
                    kb.op('dve', lambda: nc.vector.tensor_tensor(out=tb['KP'][:], in0=f['kd'][:], in1=f['enG'][:], op=ALU.mult), reads=['f_kd', 'f_enG', K_('tb_KP')], writes=[K_('tb_KP')])
                    yield
                    kb.op('dve', lambda: V_.tensor_tensor(out=tb['BC'][:], in0=f['b'][:], in1=f['eRev'][:], op=ALU.mult), reads=['f_b', 'f_eRev', K_('tb_BC')], writes=[K_('tb_BC')])
                    yield
                    kb.op('dve', lambda: nc.vector.tensor_tensor(out=tb['KC'][:], in0=f['kd'][:], in1=f['eRev'][:], op=ALU.mult), reads=['f_kd', 'f_eRev', K_('tb_KC')], writes=[K_('tb_KC')])
                    yield
                    kb.op('act', lambda: nc.scalar.copy(out=tb['V'][:], in_=ld['VV'][:]), reads=['ld_VV', K_('tb_V')], writes=[K_('tb_V')])
                    yield
                    for nm in ('KAP', 'BP', 'KP', 'RM'):
                        pT = pG[0][:].bitcast(BF16)[:, 0:1024].rearrange("p (k t) -> p k t", t=128)

                        def tr(nm=nm, pT=pT):
                            for k in range(8):
                                ins = nc.tensor.transpose(pT[:, k, :], tb[nm][:, k * 128:(k + 1) * 128], C['identb'][:])
                            return ins
                        kb.op('pe', tr, reads=[K_('tb_' + nm), 'identb'], writes=['pG0'])
                        yield
                        kb.op('act', lambda nm=nm, pT=pT: nc.scalar.copy(out=fT[nm][:], in_=pT), reads=['pG0', K_('fT_' + nm)], writes=[K_('fT_' + nm)])
                        yield
                    for half in range(2):
                        pT = pG[1][:].rearrange("p (k t) -> p k t", t=128)

                        def tr2(half=half, pT=pT):
                            for k in range(4):
                                kk_ = half * 4 + k
                                ins = nc.tensor.transpose(pT[:, k, :], f['eTot'][:, kk_ * 128:(kk_ + 1) * 128], C['identf'][:])
                            return ins
                        kb.op('pe', tr2, reads=['f_eTot', 'identf'], writes=['pG1'])
                        yield
                        kb.op('dve', lambda half=half, pT=pT: V_.tensor_copy(out=eTT[:, half * 4:(half + 1) * 4, :], in_=pT), reads=['pG1', 'eTT'], writes=['eTT'])
                        yield
                    for par in range(2):
                        kb.op('dve', lambda par=par: V_.tensor_copy(out=wc[:, par::2, :], in_=eTT[par * 64:(par + 1) * 64, :, 0:128:64]), reads=['eTT', K_('wc')], writes=[K_('wc')])
                        yield

                    return
                    yield
                opk = [K_('tb_KAP'), K_('tb_RM'), K_('tb_BC'), K_('tb_KC'), K_('tb_V'), K_('fT_KAP'), K_('fT_BP'), K_('fT_KP'), K_('fT_RM')]
                gens = []
                for h in range(16):
                    def mk(h):
                        cs = slice(h * 64, (h + 1) * 64)
                        pb = (h % 2) * 64
                        k8 = h // 2
                        return lambda sl: self.head_scan(st, C, h, 64, 64, pb, tb['KAP'][:, cs], tb['RM'][:, cs], tb['BC'][:, cs], tb['KC'][:, cs], tb['V'][:, cs],
                                   fT['KAP'][pb:pb + 64, k8, :], fT['BP'][pb:pb + 64, k8, :], fT['KP'][pb:pb + 64, k8, :], fT['RM'][pb:pb + 64, k8, :],
                                   (C['SB'][:], 'SB'), (C['SBI'][:], 'SBI'), (C['SBT'][:], 'SBT'), (wc[:, h, :], K_('wc')), (T[h], 'T%d' % h), (yt[:, cs], K_('yt%d' % h)), opk, z, B, sl)
                    gens.append(mk(h))

                def store():
                    kb.dma('sp', self.YZ[z][t0:t0 + 128, :], yt[:], reads=[K_('yt%d' % h) for h in range(16)], writes=['YZ%d_%d' % (z, ti)])
                return opgen, gens, store
            self.run_pipeline([make_tile(idx, ti) for idx, ti in enumerate(order)])

    def stage_rwkv_out(self, jl):
        nc, kb = self.nc, self.kb
        with Stage(kb) as st:
            V_ = nc.vector
            idf = st.sb("identf", [128, 128], F32)
            kb.dma('sp', idf[:], self.cst_identf, writes=['identf'])
            idb = st.sb("identb", [128, 128], BF16)
            kb.op('dve', lambda: V_.tensor_copy(out=idb[:], in_=idf[:]), reads=['identf'], writes=['identb'])
            wo = st.sb("wo", [128, 8, 1024], BF16)
            stg = [st.sb("stg%d" % j, [128, 1024], F32) for j in range(2)]
            for k in range(8):
                j = k % 2
                kb.dma('sp', stg[j][:], self.rk_w_o[jl, k * 128:(k + 1) * 128, :], writes=['stg%d' % j])
                kb.op('dve', lambda k=k, j=j: V_.tensor_copy(out=wo[:, k, :], in_=stg[j][:]), reads=['stg%d' % j, 'wo'], writes=['wo'])
            bc = {}
            for nm, src in (('a00', self.rk_a0[jl, 0, :]), ('a01', self.rk_a0[jl, 1, :]), ('k_a', self.rk_k_a[jl, :]), ('r_k', self.rk_r_k[jl].rearrange("h n -> (h n)")),
                            ('lg', self.rk_lnx_g[jl, :]), ('lb', self.rk_lnx_b[jl, :])):
                bc[nm] = self.bcast_load(st, nm, src, 1024)
            ld2 = [{nm: st.sb("ld_%s_%d" % (nm, _q), [128, 1024], F32) for nm in ('Y0', 'Y1', 'RR', 'KK0', 'VV', 'AL0', 'AL1', 'GG')} for _q in range(2)]
            t1 = st.sb("t1", [128, 1024], F32)
            t2 = st.sb("t2", [128, 1024], F32)
            zb = st.sb("zb", [128, 1024], BF16)
            zT = st.sb("zT", [128, 8, 128], BF16)
            sm = st.sb("sm", [128, 4, 16], F32)
            pT = st.ps("pT", [128, 512], F32)
            pO = [st.ps("pO%d" % q, [128, 512], F32) for q in range(2)]
            yo = st.sb("yo", [128, 1024], F32)
            H3 = lambda ap: ap.rearrange("p (h n) -> p h n", n=64)
            B3 = lambda ap: ap.unsqueeze(2).to_broadcast([128, 16, 64])
            def _ldall(ti):
                t0 = ti * 128
                for nm, src, key in (('Y0', self.YZ[0], 'YZ0_%d' % ti), ('Y1', self.YZ[1], 'YZ1_%d' % ti), ('RR', self.RK['RR'], 'RR%d' % ti), ('KK0', self.RK['KK0'], 'KK0%d' % ti),
                                     ('VV', self.RK['VV'], 'VV%d' % ti), ('AL0', self.RK['AL0'], 'AL0%d' % ti), ('AL1', self.RK['AL1'], 'AL1%d' % ti), ('GG', self.RK['GG'], 'GG%d' % ti)):
                    kb.dma('sp', ld2[ti % 2][nm][:], src[t0:t0 + 128, :], reads=[key], writes=['ld_%s#%d' % (nm, ti % 2)])
            _ldall(0)
            for ti in range(NT // 128):
                t0 = ti * 128
                ld = ld2[ti % 2]
                LK = lambda n_, _p=ti % 2: '%s#%d' % (n_, _p)
                if ti + 1 < NT // 128:
                    _ldall(ti + 1)
                kb.op('dve', lambda: V_.tensor_tensor(out=t1[:], in0=ld['Y0'][:], in1=ld['Y1'][:], op=ALU.add), reads=[LK('ld_Y0'), LK('ld_Y1'), 't1'], writes=['t1'])
                kb.op('dve', lambda: V_.tensor_reduce(out=sm[:, 0, :], in_=H3(t1[:]), axis=AX.X, op=ALU.add), reads=['t1', 'sm'], writes=['sm'])
                kb.op('dve', lambda: V_.tensor_scalar(out=sm[:, 0, :], in0=sm[:, 0, :], scalar1=1.0 / 64, scalar2=None, op0=ALU.mult), reads=['sm'], writes=['sm'])
                kb.op('dve', lambda: V_.tensor_tensor(out=H3(t1[:]), in0=H3(t1[:]), in1=B3(sm[:, 0, :]), op=ALU.subtract), reads=['sm', 't1'], writes=['t1'])
                kb.op('dve', lambda: V_.tensor_tensor(out=t2[:], in0=t1[:], in1=t1[:], op=ALU.mult), reads=['t1', 't2'], writes=['t2'])
                kb.op('dve', lambda: V_.tensor_reduce(out=sm[:, 1, :], in_=H3(t2[:]), axis=AX.X, op=ALU.add), reads=['t2', 'sm'], writes=['sm'])
                kb.op('dve', lambda: V_.tensor_scalar(out=sm[:, 1, :], in0=sm[:, 1, :], scalar1=1.0 / 64, scalar2=64e-5, op0=ALU.mult, op1=ALU.add), reads=['sm'], writes=['sm'])
                kb.op('act', lambda: nc.scalar.activation(out=sm[:, 1, :], in_=sm[:, 1, :], func=AF.Sqrt), reads=['sm'], writes=['sm'])
                kb.op('dve', lambda: V_.reciprocal(out=sm[:, 2, :], in_=sm[:, 1, :]), reads=['sm'], writes=['sm'])
                kb.op('dve', lambda: V_.tensor_tensor(out=H3(t1[:]), in0=H3(t1[:]), in1=B3(sm[:, 2, :]), op=ALU.mult), reads=['sm', 't1'], writes=['t1'])
                kb.op('dve', lambda: V_.tensor_tensor(out=t1[:], in0=t1[:], in1=bc['lg'][:], op=ALU.mult), reads=['t1', 'lg'], writes=['t1'])
                kb.op('dve', lambda: V_.tensor_tensor(out=t1[:], in0=t1[:], in1=bc['lb'][:], op=ALU.add), reads=['t1', 'lb'], writes=['t1'])
                kb.op('dve', lambda: V_.tensor_tensor(out=t2[:], in0=ld['AL0'][:], in1=bc['a00'][:], op=ALU.add), reads=[LK('ld_AL0'), 'a00', 't2'], writes=['t2'])
                kb.op('act', lambda: nc.scalar.activation(out=t2[:], in_=t2[:], func=AF.Sigmoid), reads=['t2'], writes=['t2'])
                kb.op('dve', lambda: V_.tensor_tensor(out=ld['AL1'][:], in0=ld['AL1'][:], in1=bc['a01'][:], op=ALU.add), reads=[LK('ld_AL1'), 'a01'], writes=[LK('ld_AL1')])
                kb.op('act', lambda: nc.scalar.activation(out=ld['AL1'][:], in_=ld['AL1'][:], func=AF.Sigmoid), reads=[LK('ld_AL1')], writes=[LK('ld_AL1')])
                kb.op('dve', lambda: V_.tensor_tensor(out=t2[:], in0=t2[:], in1=ld['AL1'][:], op=ALU.add), reads=['t2', LK('ld_AL1')], writes=['t2'])
                kb.op('dve', lambda: V_.scalar_tensor_tensor(out=t2[:], in0=t2[:], scalar=-2.0, in1=bc['k_a'][:], op0=ALU.add, op1=ALU.mult), reads=['t2', 'k_a'], writes=['t2'])
                kb.op('dve', lambda: V_.scalar_tensor_tensor(out=t2[:], in0=t2[:], scalar=2.0, in1=ld['KK0'][:], op0=ALU.add, op1=ALU.mult), reads=['t2', LK('ld_KK0')], writes=['t2'])
                kb.op('dve', lambda: V_.tensor_tensor(out=t2[:], in0=t2[:], in1=ld['RR'][:], op=ALU.mult), reads=['t2', LK('ld_RR')], writes=['t2'])
                kb.op('dve', lambda: V_.tensor_tensor(out=t2[:], in0=t2[:], in1=bc['r_k'][:], op=ALU.mult), reads=['t2', 'r_k'], writes=['t2'])
                kb.op('dve', lambda: V_.tensor_reduce(out=sm[:, 3, :], in_=H3(t2[:]), axis=AX.X, op=ALU.add), reads=['t2', 'sm'], writes=['sm'])
                kb.op('dve', lambda: V_.tensor_tensor(out=H3(t2[:]), in0=H3(ld['VV'][:]), in1=B3(sm[:, 3, :]), op=ALU.mult), reads=['sm', LK('ld_VV'), 't2'], writes=['t2'])
                kb.op('dve', lambda: V_.tensor_tensor(out=t1[:], in0=t1[:], in1=t2[:], op=ALU.add), reads=['t1', 't2'], writes=['t1'])
                kb.op('dve', lambda: V_.tensor_tensor(out=zb[:], in0=t1[:], in1=ld['GG'][:], op=ALU.mult), reads=['t1', LK('ld_GG'), 'zb'], writes=['zb'])
                pTv = pT[:].bitcast(BF16)[:, 0:1024].rearrange("p (k t) -> p k t", t=128)

                def tr(pTv=pTv):
                    for k in range(8):
                        ins = nc.tensor.transpose(pTv[:, k, :], zb[:, k * 128:(k + 1) * 128], idb[:])
                    return ins
                kb.op('pe', tr, reads=['zb', 'identb'], writes=['pT'])
                kb.op('act', lambda pTv=pTv: nc.scalar.copy(out=zT[:], in_=pTv), reads=['pT', 'zT'], writes=['zT'])
                for half in range(2):
                    def mm(half=half):
                        for k in range(8):
                            ins = nc.tensor.matmul(pO[half][:], lhsT=zT[:, k, :], rhs=wo[:, k, half * 512:(half + 1) * 512], start=(k == 0), stop=(k == 7))
                        return ins
                    kb.op('pe', mm, reads=['zT', 'wo'], writes=['pO%d' % half])
                    kb.op('act' if half else 'dve', (lambda half=half: nc.scalar.copy(out=yo[:, 512:1024], in_=pO[1][:])) if half else
                          (lambda half=half: V_.tensor_copy(out=yo[:, 0:512], in_=pO[0][:])), reads=['pO%d' % half, 'yo'], writes=['yo'])
                kb.dma('sp', self.ACC[t0:t0 + 128, :], yo[:], reads=['yo'], writes=['ACC%d' % ti])

    def stage_rwkv(self, i, jl):
        self.stage_rwkv_proj(jl)
        for z in range(2):
            self.stage_rwkv_scan(jl, z)
        self.stage_rwkv_out(jl)

    def stage_gdn_proj(self, jl):
        nc, kb = self.nc, self.kb
        V_ = nc.vector
        for blk in range(4):
            with Stage(kb) as st:
                ntap = 4 if blk < 3 else 1
                ncol = 1024 if blk < 3 else 1056
                c0 = blk * 1024
                Wc = st.sb("Wc", [128, ntap, 8, ncol], BF16)
                stg = [st.sb("stg%d" % j, [128, 1056], F32) for j in range(2)]
                cw = [self.bcast_load(st, "cw%d" % tp, self.gdn_conv_w[jl, tp, c0:c0 + 1024], 1024) for tp in range(ntap)] if blk < 3 else None
                for k in range(8):
                    j = k % 2
                    kb.dma('sp', stg[j][:, 0:ncol], self.gdn_w_in[jl, k * 128:(k + 1) * 128, c0:c0 + ncol], writes=['stg%d' % j])
                    for tp in range(ntap):
                        if blk < 3:
                            kb.op('dve', lambda k=k, j=j, tp=tp: V_.tensor_tensor(out=Wc[:, tp, k, :], in0=stg[j][:, 0:1024], in1=cw[tp][:], op=ALU.mult), reads=['stg%d' % j, 'cw%d' % tp, 'Wc'], writes=['Wc'])
                        else:
                            kb.op('dve', lambda k=k, j=j: V_.tensor_copy(out=Wc[:, 0, k, :], in_=stg[j][:, 0:ncol]), reads=['stg%d' % j, 'Wc'], writes=['Wc'])
                ht = st.sb("ht", [128, 8, 640], BF16)
                pp = [st.ps("pp%d" % q, [128, 512], F32) for q in range(4)]
                ob = [st.sb("ob%d" % q, [128, 1056], F32) for q in range(2)]
                sq = st.sb("sq", [128, 1024], F32)
                sm = st.sb("sm", [128, 2, 8], F32)
                npp = [0]
                nob = [0]
                for it, (t0, W) in enumerate([(0, TC)] + tiles_of(NT, 512, TC)):
                    isc = (t0 == 0)
                    zl = isc or t0 == TC
                    zr = isc or (t0 + W == NT)
                    if zl:
                        kb.op('dve', lambda: V_.memset(ht[:, :, 0:64], 0.0), reads=['ht'], writes=['ht'])
                    if zr:
                        kb.op('dve', lambda W=W: V_.memset(ht[:, :, 64 + W:128 + W], 0.0), reads=['ht'], writes=['ht'])
                    a = t0 if zl else t0 - 64
                    b = t0 + W if zr else t0 + W + 64
                    kb.dma('sp', ht[:, :, 64 + (a - t0):64 + (b - t0)], self.HT[:, :, a:b], reads=['HT%d' % q for q in range(a // 128, (b + 127) // 128)] + ['ht'], writes=['ht'])
                    for sub in range(W // 128):
                        ts = t0 + sub * 128
                        ti = ts // 128
                        o = nob[0] % 2
                        nob[0] += 1
                        segs = [(0, 512), (512, 1024)] + ([(1024, 1056)] if blk == 3 else [])
                        for (s0, s1) in segs:
                            q = npp[0] % 4
                            npp[0] += 1

                            def f(q=q, s0=s0, s1=s1, sub=sub):
                                n_ = ntap * 8
                                i_ = 0
                                for tp in range(ntap):
                                    off = 64 + sub * 128 + (tp - 2 if blk < 3 else 0)
                                    for k in range(8):
                                        ins = nc.tensor.matmul(pp[q][:, 0:s1 - s0], lhsT=ht[:, k, off:off + 128], rhs=Wc[:, tp, k, s0:s1], start=(i_ == 0), stop=(i_ == n_ - 1))
                                        i_ += 1
                                return ins
                            kb.op('pe', f, reads=['ht', 'Wc'], writes=['pp%d' % q])
                            if blk < 3:
                                kb.op('act', lambda q=q, o=o, s0=s0, s1=s1: nc.scalar.activation(out=ob[o][:, s0:s1], in_=pp[q][:, 0:s1 - s0], func=AF.Silu), reads=['pp%d' % q, 'ob%d' % o], writes=['ob%d' % o])
                            else:
                                kb.op('act', lambda q=q, o=o, s0=s0, s1=s1: nc.scalar.copy(out=ob[o][:, s0:s1], in_=pp[q][:, 0:s1 - s0]), reads=['pp%d' % q, 'ob%d' % o], writes=['ob%d' % o])
                        if blk < 2:
                            O3 = ob[o][:, 0:1024].rearrange("p (h n) -> p h n", n=128)
                            kb.op('dve', lambda o=o: V_.tensor_tensor(out=sq[:], in0=ob[o][:, 0:1024], in1=ob[o][:, 0:1024], op=ALU.mult), reads=['ob%d' % o, 'sq'], writes=['sq'])
                            kb.op('dve', lambda: V_.tensor_reduce(out=sm[:, 0, :], in_=sq[:].rearrange("p (h n) -> p h n", n=128), axis=AX.X, op=ALU.add), reads=['sq', 'sm'], writes=['sm'])
                            kb.op('dve', lambda: V_.tensor_scalar(out=sm[:, 0, :], in0=sm[:, 0, :], scalar1=1e-6, scalar2=None, op0=ALU.add), reads=['sm'], writes=['sm'])
                            kb.op('act', lambda: nc.scalar.activation(out=sm[:, 0, :], in_=sm[:, 0, :], func=AF.Sqrt), reads=['sm'], writes=['sm'])
                            kb.op('dve', lambda: V_.reciprocal(out=sm[:, 1, :], in_=sm[:, 0, :]), reads=['sm'], writes=['sm'])
                            if blk == 0:
                                kb.op('dve', lambda: V_.tensor_scalar(out=sm[:, 1, :], in0=sm[:, 1, :], scalar1=128 ** -0.5, scalar2=None, op0=ALU.mult), reads=['sm'], writes=['sm'])
                            kb.op('dve', lambda O3=O3: V_.tensor_tensor(out=O3, in0=O3, in1=sm[:, 1, :].unsqueeze(2).to_broadcast([128, 8, 128]), op=ALU.mult), reads=['sm', 'ob%d' % o], writes=['ob%d' % o])
                        nm = ('GQ', 'GK', 'GV', 'GZ')[blk]
                        kb.dma('sp', self.GD[nm][ts:ts + 128, :], ob[o][:, 0:1024], reads=['ob%d' % o], writes=['%s%d' % (nm, ti)])
                        if blk == 3:
                            kb.dma('sp', self.GD['GAB'][ts:ts + 128, :], ob[o][:, 1024:1056], reads=['ob%d' % o], writes=['GAB%d' % ti])

    def stage_gdn_scan(self, jl, z):
        nc, kb = self.nc, self.kb
        V_ = nc.vector
        with Stage(kb) as st:
            C = self.scan_consts(st, z)
            B = self.head_bufs(st, F32)
            ones = st.sb("ones", [128, 128], F32)
            kb.op('dve', lambda: V_.memset(ones[:], 1.0), writes=['ones'])
            alog = self.bcast_load(st, "alog", self.gdn_a_log[jl, z, :], 8)
            dtb = self.bcast_load(st, "dtb", self.gdn_dt_bias[jl, z, :], 8)
            kb.op('act', lambda: nc.scalar.activation(out=alog[:], in_=alog[:], func=AF.Exp), reads=['alog'], writes=['alog'])
            T = [st.sb("T%d" % h, [128, 128], F32) for h in range(8)]
            for h in range(8):
                kb.op('dve', lambda h=h: V_.memset(T[h][:], 0.0), writes=['T%d' % h])
            ld = {nm: st.sb("ld_" + nm, [128, 1024], F32) for nm in ('GQ', 'GK', 'GV')}
            ab = st.sb("ab", [128, 32], F32)
            s82 = [{nm: st.sb("s8_%s_%d" % (nm, q), [128, 8], F32) for nm in ('g', 'beta', 'G', 'Gx', 'eGx', 'eG', 'eRev', 'bk', 'bkg', 'bkr', 't', 'eTot', 'nG')} for q in range(2)]
            tb2 = [{nm: st.sb("tb_%s_%d" % (nm, q), [128, 1024], BF16) for nm in ('KAP', 'RM', 'BC', 'KC', 'V', 'KU', 'BU', 'KD', 'QU')} for q in range(2)]
            fT2 = [{nm: st.sb("fT_%s_%d" % (nm, q), [128, 8, 128], BF16) for nm in ('KU', 'BU', 'KD', 'QU')} for q in range(2)]
            wc2 = [st.sb("wc%d" % q, [128, 8, 2], F32) for q in range(2)]
            pG = [st.ps("pG%d" % q, [128, 512], F32) for q in range(2)]
            dg = [st.sb("dg%d" % q, [128, 128], F32) for q in range(NSLOT)]
            Dm = [{nm: st.sb("D_%s%d" % (nm, h), [128, 128], F32) for nm in ('x', 'i', 'xT')} for h in range(8)]
            yt2 = [st.sb("yt%d" % q, [128, 1024], F32) for q in range(2)]
            H3 = lambda ap: ap.rearrange("p (h n) -> p h n", n=128)
            B3 = lambda ap: ap.unsqueeze(2).to_broadcast([128, 8, 128])
            order = self.scan_order(z)

            def make_tile(idx, ti):
                q = idx % 2
                tb, fT, wc, yt, s8 = tb2[q], fT2[q], wc2[q], yt2[q], s82[q]
                K_ = lambda nm: '%s#%d' % (nm, q)
                S8 = [K_('s8')]
                t0 = ti * 128

                def opgen():
                    t0 = ti * 128
                    for nm in ('GQ', 'GK', 'GV'):
                        kb.dma('sp', ld[nm][:], self.GD[nm][t0:t0 + 128, :], reads=['%s%d' % (nm, ti)], writes=['ld_' + nm])
                        yield
                    kb.dma('sp', ab[:], self.GD['GAB'][t0:t0 + 128, :], reads=['GAB%d' % ti], writes=['ab'])
                    yield
                    kb.op('dve', lambda: V_.tensor_tensor(out=s8['t'][:], in0=ab[:, z * 8:(z + 1) * 8], in1=dtb[:], op=ALU.add), reads=['ab', 'dtb'] + S8, writes=S8)
                    yield
                    kb.op('act', lambda: nc.scalar.activation(out=s8['t'][:], in_=s8['t'][:], func=AF.Exp), reads=S8, writes=S8)
                    yield
                    kb.op('act', lambda: nc.scalar.activation(out=s8['t'][:], in_=s8['t'][:], func=AF.Ln, bias=1.0), reads=S8, writes=S8)
                    yield
                    kb.op('dve', lambda: V_.scalar_tensor_tensor(out=s8['g'][:], in0=s8['t'][:], scalar=-1.0, in1=alog[:], op0=ALU.mult, op1=ALU.mult), reads=S8 + ['alog'], writes=S8)
                    yield
                    kb.op('act', lambda: nc.scalar.activation(out=s8['beta'][:], in_=ab[:, 16 + z * 8:16 + (z + 1) * 8], func=AF.Sigmoid), reads=['ab'] + S8, writes=S8)
                    yield
                    kb.op('pe', lambda: nc.tensor.matmul(pG[0][:, 0:8], lhsT=C['SBI'][:], rhs=s8['g'][:], start=True, stop=True), reads=['SBI'] + S8, writes=['pG0'])
                    yield
                    kb.op('pe', lambda: nc.tensor.matmul(pG[1][:, 0:8], lhsT=C['BLK'][:], rhs=s8['g'][:], start=True, stop=True), reads=['BLK'] + S8, writes=['pG1'])
                    yield
                    kb.op('dve', lambda: V_.tensor_copy(out=s8['G'][:], in_=pG[0][:, 0:8]), reads=['pG0'] + S8, writes=S8)
                    yield
                    kb.op('dve', lambda: V_.tensor_tensor(out=s8['Gx'][:], in0=s8['G'][:], in1=s8['g'][:], op=ALU.subtract), reads=S8, writes=S8)
                    yield
                    kb.op('dve', lambda: V_.tensor_tensor(out=s8['eRev'][:], in0=pG[1][:, 0:8], in1=s8['G'][:], op=ALU.subtract), reads=['pG1'] + S8, writes=S8)
                    yield
                    kb.op('act', lambda: nc.scalar.activation(out=s8['eTot'][:], in_=pG[1][:, 0:8], func=AF.Exp), reads=['pG1'] + S8, writes=S8)
                    yield
                    for nm_o, nm_i in (('eRev', 'eRev'), ('eGx', 'Gx'), ('eG', 'G'), ('bkg', 'g')):
                        kb.op('act', lambda nm_o=nm_o, nm_i=nm_i: nc.scalar.activation(out=s8[nm_o][:], in_=s8[nm_i][:], func=AF.Exp), reads=S8, writes=S8)
                        yield
                    kb.op('dve', lambda: V_.tensor_scalar(out=s8['nG'][:], in0=s8['G'][:], scalar1=-1.0, scalar2=None, op0=ALU.mult), reads=S8, writes=S8)
                    yield
                    kb.op('dve', lambda: V_.tensor_tensor(out=s8['bkg'][:], in0=s8['bkg'][:], in1=s8['beta'][:], op=ALU.mult), reads=S8, writes=S8)
                    yield
                    kb.op('dve', lambda: V_.tensor_tensor(out=s8['bk'][:], in0=s8['bkg'][:], in1=s8['eRev'][:], op=ALU.mult), reads=S8, writes=S8)
                    yield
                    kb.op('dve', lambda: V_.tensor_tensor(out=s8['bkr'][:], in0=s8['beta'][:], in1=s8['eRev'][:], op=ALU.mult), reads=S8, writes=S8)
                    yield
                    kb.op('dve', lambda: V_.tensor_scalar(out=s8['eGx'][:], in0=s8['eGx'][:], scalar1=-1.0, scalar2=None, op0=ALU.mult), reads=S8, writes=S8)
                    yield
                    K3 = H3(ld['GK'][:])
                    for nm, sc in (('KAP', 'eGx'), ('BC', 'bk'), ('KC', 'bkr'), ('BU', 'bkg'), ('KD', 'beta')):
                        kb.op('dve', lambda nm=nm, sc=sc: V_.tensor_tensor(out=H3(tb[nm][:]), in0=K3, in1=B3(s8[sc][:]), op=ALU.mult), reads=S8 + ['ld_GK', K_('tb_' + nm)], writes=[K_('tb_' + nm)])
                        yield
                    kb.op('dve', lambda: V_.tensor_scalar(out=tb['KU'][:], in0=ld['GK'][:], scalar1=-1.0, scalar2=None, op0=ALU.mult), reads=['ld_GK', K_('tb_KU')], writes=[K_('tb_KU')])
                    yield
                    kb.op('dve', lambda: V_.tensor_tensor(out=H3(tb['RM'][:]), in0=H3(ld['GQ'][:]), in1=B3(s8['eG'][:]), op=ALU.mult), reads=S8 + ['ld_GQ', K_('tb_RM')], writes=[K_('tb_RM')])
                    yield
                    kb.op('act', lambda: nc.scalar.copy(out=tb['QU'][:], in_=ld['GQ'][:]), reads=['ld_GQ', K_('tb_QU')], writes=[K_('tb_QU')])
                    yield
                    kb.op('act', lambda: nc.scalar.copy(out=tb['V'][:], in_=ld['GV'][:]), reads=['ld_GV', K_('tb_V')], writes=[K_('tb_V')])
                    yield
                    for nm in ('KU', 'BU', 'KD', 'QU'):
                        pT = pG[0][:].bitcast(BF16)[:, 0:1024].rearrange("p (k t) -> p k t", t=128)

                        def tr(nm=nm, pT=pT):
                            for k in range(8):
                                ins = nc.tensor.transpose(pT[:, k, :], tb[nm][:, k * 128:(k + 1) * 128], C['identb'][:])
                            return ins
                        kb.op('pe', tr, reads=[K_('tb_' + nm), 'identb'], writes=['pG0'])
                        yield
                        kb.op('act', lambda nm=nm, pT=pT: nc.scalar.copy(out=fT[nm][:], in_=pT), reads=['pG0', K_('fT_' + nm)], writes=[K_('fT_' + nm)])
                        yield
                    for c in range(2):
                        kb.op('pe', lambda c=c: nc.tensor.matmul(pG[1][:, 0:8], lhsT=C['BLK'][c * 64:(c + 1) * 64, c * 64:c * 64 + 1].to_broadcast([64, 128]) if False else ones[c * 64:(c + 1) * 64, :],
                                                               rhs=s8['g'][c * 64:(c + 1) * 64, :], start=True, stop=True), reads=['ones'] + S8, writes=['pG1'])
                        kb.op('act', lambda c=c: nc.scalar.activation(out=wc[:, :, c], in_=pG[1][:, 0:8], func=AF.Exp), reads=['pG1', K_('wc')], writes=[K_('wc')])
                        yield

                    return
                    yield
                opk = [K_('tb_KAP'), K_('tb_RM'), K_('tb_BC'), K_('tb_KC'), K_('tb_V'), K_('fT_KU'), K_('fT_BU'), K_('fT_KD'), K_('fT_QU')]
                gens = []
                for h in range(8):
                    def mk(h):
                        cs = slice(h * 128, (h + 1) * 128)
                        dmh = Dm[h]

                        def gen(sl):
                            pbk, pkey = B['ps'][sl], B['psk'][sl]
                            dgt, dgk = dg[sl], 'dg%d' % sl
                            for (src, dn0, sub_, mk_, flip) in (('G', 'i', 'G', 'SBI', False), ('Gx', 'x', 'G', 'SB', False), ('G', 'xT', 'Gx', 'SBT', True)):
                                d_ = dmh[dn0]
                                dn = dn0 + str(h)
                                kb.op('dve', lambda: V_.tensor_scalar(out=dgt[:], in0=C['identf'][:], scalar1=s8[src][:, h:h + 1], scalar2=None, op0=ALU.mult), reads=['identf', dgk] + S8, writes=[dgk])
                                kb.op('pe', lambda: nc.tensor.matmul(pbk[:, 0:128], lhsT=ones[:], rhs=dgt[:], start=True, stop=True), reads=['ones', dgk], writes=[pkey])
                                yield
                                if not flip:
                                    kb.op('dve', lambda: V_.tensor_scalar(out=d_[:], in0=pbk[:, 0:128], scalar1=s8[sub_][:, h:h + 1], scalar2=0.0, op0=ALU.subtract, op1=ALU.min),
                                          reads=[pkey, 'D_' + dn] + S8, writes=['D_' + dn])
                                else:
                                    kb.op('dve', lambda: V_.tensor_scalar(out=d_[:], in0=pbk[:, 0:128], scalar1=-1.0, scalar2=s8[sub_][:, h:h + 1], op0=ALU.mult, op1=ALU.add),
                                          reads=[pkey, 'D_' + dn] + S8, writes=['D_' + dn])
                                    kb.op('dve', lambda: V_.tensor_scalar(out=d_[:], in0=d_[:], scalar1=0.0, scalar2=None, op0=ALU.min), reads=['D_' + dn], writes=['D_' + dn])
                                yield
                                kb.op('act', lambda: nc.scalar.activation(out=d_[:], in_=d_[:], func=AF.Exp), reads=['D_' + dn], writes=['D_' + dn])
                                yield
                                kb.op('dve', lambda: V_.tensor_tensor(out=d_[:], in0=d_[:], in1=C[mk_][:], op=ALU.mult), reads=['D_' + dn, mk_], writes=['D_' + dn])
                                yield
                            yield from self.head_scan(st, C, h, 128, 128, 0, tb['KAP'][:, cs], tb['RM'][:, cs], tb['BC'][:, cs], tb['KC'][:, cs], tb['V'][:, cs],
                                   fT['KU'][:, h, :], fT['BU'][:, h, :], fT['KD'][:, h, :], fT['QU'][:, h, :],
                                   (dmh['x'][:], 'D_x%d' % h), (dmh['i'][:], 'D_i%d' % h), (dmh['xT'][:], 'D_xT%d' % h), (wc[:, h, :], K_('wc')), (T[h], 'T%d' % h), (yt[:, cs], K_('yt%d' % h)), opk, z, B, sl, F32)
                        return gen
                    gens.append(mk(h))

                def store():
                    kb.dma('sp', self.YZ[z][t0:t0 + 128, :], yt[:], reads=[K_('yt%d' % h) for h in range(8)], writes=['YZ%d_%d' % (z, ti)])
                return opgen, gens, store
            self.run_pipeline([make_tile(idx, ti) for idx, ti in enumerate(order)])

    def stage_gdn_out(self, jl):
        nc, kb = self.nc, self.kb
        V_ = nc.vector
        with Stage(kb) as st:
            idf = st.sb("identf", [128, 128], F32)
            kb.dma('sp', idf[:], self.cst_identf, writes=['identf'])
            idb = st.sb("identb", [128, 128], BF16)
            kb.op('dve', lambda: V_.tensor_copy(out=idb[:], in_=idf[:]), reads=['identf'], writes=['identb'])
            wo = st.sb("wo", [128, 8, 1024], BF16)
            stg = [st.sb("stg%d" % j, [128, 1024], F32) for j in range(2)]
            for k in range(8):
                j = k % 2
                kb.dma('sp', stg[j][:], self.gdn_w_o[jl, k * 128:(k + 1) * 128, :], writes=['stg%d' % j])
                kb.op('dve', lambda k=k, j=j: V_.tensor_copy(out=wo[:, k, :], in_=stg[j][:]), reads=['stg%d' % j, 'wo'], writes=['wo'])
            nw = self.bcast_load(st, "nw", self.gdn_norm_w[jl, :], 128)
            ld2 = [{nm: st.sb("ld_%s_%d" % (nm, _q), [128, 1024], F32) for nm in ('Y0', 'Y1', 'GZ')} for _q in range(2)]
            t2 = st.sb("t2", [128, 1024], F32)
            zb = st.sb("zb", [128, 1024], BF16)
            zT = st.sb("zT", [128, 8, 128], BF16)
            sm = st.sb("sm", [128, 2, 8], F32)
            pT = st.ps("pT", [128, 512], F32)
            pO = [st.ps("pO%d" % q, [128, 512], F32) for q in range(2)]
            yo = st.sb("yo", [128, 1024], F32)
            H3 = lambda ap: ap.rearrange("p (h n) -> p h n", n=128)
            def _ldall(ti):
                t0 = ti * 128
                for nm, src, key in (('Y0', self.YZ[0], 'YZ0_%d' % ti), ('Y1', self.YZ[1], 'YZ1_%d' % ti), ('GZ', self.GD['GZ'], 'GZ%d' % ti)):
                    kb.dma('sp', ld2[ti % 2][nm][:], src[t0:t0 + 128, :], reads=[key], writes=['ld_%s#%d' % (nm, ti % 2)])
            _ldall(0)
            for ti in range(NT // 128):
                t0 = ti * 128
                ld = ld2[ti % 2]
                LK = lambda n_, _p=ti % 2: '%s#%d' % (n_, _p)
                if ti + 1 < NT // 128:
                    _ldall(ti + 1)
                kb.op('dve', lambda: V_.tensor_tensor(out=ld['Y0'][:], in0=ld['Y0'][:], in1=ld['Y1'][:], op=ALU.add), reads=[LK('ld_Y0'), LK('ld_Y1')], writes=[LK('ld_Y0')])
                kb.op('dve', lambda: V_.tensor_tensor(out=t2[:], in0=ld['Y0'][:], in1=ld['Y0'][:], op=ALU.mult), reads=[LK('ld_Y0'), 't2'], writes=['t2'])
                kb.op('dve', lambda: V_.tensor_reduce(out=sm[:, 0, :], in_=H3(t2[:]), axis=AX.X, op=ALU.add), reads=['t2', 'sm'], writes=['sm'])
                kb.op('dve', lambda: V_.tensor_scalar(out=sm[:, 0, :], in0=sm[:, 0, :], scalar1=1.0 / 128, scalar2=1e-6, op0=ALU.mult, op1=ALU.add), reads=['sm'], writes=['sm'])
                kb.op('act', lambda: nc.scalar.activation(out=sm[:, 0, :], in_=sm[:, 0, :], func=AF.Sqrt), reads=['sm'], writes=['sm'])
                kb.op('dve', lambda: V_.reciprocal(out=sm[:, 1, :], in_=sm[:, 0, :]), reads=['sm'], writes=['sm'])
                kb.op('dve', lambda: V_.tensor_tensor(out=H3(ld['Y0'][:]), in0=H3(ld['Y0'][:]), in1=sm[:, 1, :].unsqueeze(2).to_broadcast([128, 8, 128]), op=ALU.mult), reads=['sm', LK('ld_Y0')], writes=[LK('ld_Y0')])
                kb.op('dve', lambda: V_.tensor_tensor(out=H3(ld['Y0'][:]), in0=H3(ld['Y0'][:]), in1=nw[:].unsqueeze(1).to_broadcast([128, 8, 128]), op=ALU.mult), reads=['nw', LK('ld_Y0')], writes=[LK('ld_Y0')])
                kb.op('act', lambda: nc.scalar.activation(out=t2[:], in_=ld['GZ'][:], func=AF.Silu), reads=[LK('ld_GZ'), 't2'], writes=['t2'])
                kb.op('dve', lambda: V_.tensor_tensor(out=zb[:], in0=ld['Y0'][:], in1=t2[:], op=ALU.mult), reads=[LK('ld_Y0'), 't2', 'zb'], writes=['zb'])
                pTv = pT[:].bitcast(BF16)[:, 0:1024].rearrange("p (k t) -> p k t", t=128)

                def tr(pTv=pTv):
                    for k in range(8):
                        ins = nc.tensor.transpose(pTv[:, k, :], zb[:, k * 128:(k + 1) * 128], idb[:])
                    return ins
                kb.op('pe', tr, reads=['zb', 'identb'], writes=['pT'])
                kb.op('act', lambda pTv=pTv: nc.scalar.copy(out=zT[:], in_=pTv), reads=['pT', 'zT'], writes=['zT'])
                for half in range(2):
                    def mm(half=half):
                        for k in range(8):
                            ins = nc.tensor.matmul(pO[half][:], lhsT=zT[:, k, :], rhs=wo[:, k, half * 512:(half + 1) * 512], start=(k == 0), stop=(k == 7))
                        return ins
                    kb.op('pe', mm, reads=['zT', 'wo'], writes=['pO%d' % half])
                    kb.op('dve', lambda half=half: V_.tensor_copy(out=yo[:, half * 512:(half + 1) * 512], in_=pO[half][:]), reads=['pO%d' % half, 'yo'], writes=['yo'])
                kb.dma('sp', self.ACC[t0:t0 + 128, :], yo[:], reads=['yo'], writes=['ACC%d' % ti])

    def stage_gdn(self, i, jl):
        self.stage_gdn_proj(jl)
        for z in range(2):
            self.stage_gdn_scan(jl, z)
        self.stage_gdn_out(jl)

    def decls(self):
        nc, kb = self.nc, self.kb
        self.xin = self.din("xin", [NT, D])
        self.cvec = self.din("cvec", [2, D])
        self.ada_w = self.din("ada_w", [4, D, 6 * D])
        self.ada_b = self.din("ada_b", [4, 6 * D])
        self.ln_g = self.din("ln_g", [4, 2, D])
        self.ln_b = self.din("ln_b", [4, 2, D])
        self.pool_w = self.din("pool_w", [1, 4, 256, 256])
        self.pool_scale = self.din("pool_scale", [1, D])
        self.ffn_w1 = self.din("ffn_w1", [2, D, 2816])
        self.ffn_w3 = self.din("ffn_w3", [2, D, 2816])
        self.ffn_w2 = self.din("ffn_w2", [2, 2816, D])
        self.moe_router_w = self.din("moe_router_w", [2, D, 8])
        self.moe_router_b = self.din("moe_router_b", [2, 8])
        self.moe_w1 = self.din("moe_w1", [2, 8, D, 1408])
        self.moe_w3 = self.din("moe_w3", [2, 8, D, 1408])
        self.moe_w2 = self.din("moe_w2", [2, 8, 1408, D])
        for nm, shp in (("rk_mu", [2, 6, D]), ("rk_w_rkv", [2, 3, D, D]), ("rk_w0", [2, 2, D]), ("rk_w1", [2, 2, D, 64]), ("rk_w2", [2, 2, 64, D]),
                        ("rk_a0", [2, 2, D]), ("rk_a1", [2, 2, D, 64]), ("rk_a2", [2, 2, 64, D]), ("rk_g1", [2, D, 128]), ("rk_g2", [2, 128, D]),
                        ("rk_k_k", [2, D]), ("rk_k_a", [2, D]), ("rk_r_k", [2, 16, 64]), ("rk_lnx_g", [2, D]), ("rk_lnx_b", [2, D]), ("rk_w_o", [2, D, D])):
            setattr(self, nm, self.din(nm, shp))
        for nm, shp in (("gdn_w_in", [1, D, 4128]), ("gdn_conv_w", [1, 4, 3072]), ("gdn_a_log", [1, 2, 8]), ("gdn_dt_bias", [1, 2, 8]),
                        ("gdn_norm_w", [1, 128]), ("gdn_w_o", [1, D, D])):
            setattr(self, nm, self.din(nm, shp))
        self.GD = {nm: self.dscr("GD_" + nm, [NT, D]) for nm in ("GQ", "GK", "GV", "GZ")}
        self.GD["GAB"] = self.dscr("GD_GAB", [NT, 32])
        self.cst_masks = self.din("cst_masks", [2, 4, 128, 128])
        self.RK = {nm: self.dscr("RK_" + nm, [NT, D]) for nm in ("RR", "KK0", "VV", "DL0", "DL1", "AL0", "AL1", "GG")}
        self.YZ = [self.dscr("YZ%d" % z, [NT, D]) for z in range(2)]
        self.cst_identf = self.din("cst_identf", [128, 128])
        self.cst_invc64 = self.din("cst_invc64", [128, 4, 64])
        self.cst_invc256 = self.din("cst_invc256", [128, 4, 256])
        self.out = self.dout("out", [NT - TC, D])
        self.XS = self.dscr("XS", [NT, D])
        self.ACC = self.dscr("ACC", [NT, D])
        self.HT = self.dscr("HT", [128, 8, NT], BF16)
        self.GATES = self.dscr("GATES", [NT, 8])
        self.MODD = self.dscr("MODD", [4, 2, 6 * D])

    def stage_init(self):
        kb = self.kb
        for (t0, W) in tiles_of(NT, 128):
            kb.dma('sp', self.XS[t0:t0 + W, :], self.xin[t0:t0 + W, :], writes=['XSinit%d' % t0])
        kb.barrier()

    def build(self):
        nc, kb = self.nc, self.kb
        self.decls()
        self.stage_init()
        for i in self.layers:
            last = (i == self.layers[-1])
            self.stage_mod(i)
            self.stage_prep(i, 1)
            kind, jl = i % 3, i // 3
            if kind == 1:
                self.stage_pool(jl)
            elif kind == 0:
                self.stage_rwkv(i, jl)
            else:
                self.stage_gdn(i, jl)
            self.stage_finish(i, 1)
            e = i // 2
            if i % 2 == 0:
                self.stage_prep(i, 2)
                for hf in range(2):
                    sl = slice(hf * 1408, (hf + 1) * 1408)
                    self.stage_ffnpass(self.ffn_w1[e, :, sl], self.ffn_w3[e, :, sl], self.ffn_w2[e, sl, :], None, hf == 0)
            else:
                self.stage_prep(i, 2, router=e)
                for x in range(8):
                    self.stage_ffnpass(self.moe_w1[e, x], self.moe_w3[e, x], self.moe_w2[e, x], x, x == 0)
            self.stage_finish(i, 2, out_final=self.out if last else None)
        kb.barrier()
        return nc


def _pool_invc(L):
    t = np.arange(L)
    out = np.zeros((4, L), np.float32)
    for gi, w in enumerate((2, 4, 8, 16)):
        lo = np.clip(t - w // 2, 0, L)
        hi = np.clip(t + w // 2, 0, L)
        out[gi] = 1.0 / (hi - lo)
    return np.ascontiguousarray(np.broadcast_to(out[None], (128, 4, L))).astype(np.float32)


def _masks():
    s = np.arange(128)[:, None]
    t = np.arange(128)[None, :]
    same = (s // 64) == (t // 64)
    m = np.zeros((2, 4, 128, 128), np.float32)
    for z in range(2):
        sb = ((s < t) if z == 0 else (s > t)) & same
        sbi = ((s <= t) if z == 0 else (s >= t)) & same
        m[z, 0] = sb
        m[z, 1] = sbi
        m[z, 2] = sb.T
        m[z, 3] = same
    return m


def make_consts():
    return {
        "cst_masks": _masks(),
        "cst_identf": np.eye(128, dtype=np.float32),
        "cst_invc64": _pool_invc(64),
        "cst_invc256": _pool_invc(256),
    }


WEIGHT_KEYS = ["ada_w", "ada_b", "ln_g", "ln_b", "pool_w", "pool_scale", "ffn_w1", "ffn_w3", "ffn_w2",
               "moe_router_w", "moe_router_b", "moe_w1", "moe_w3", "moe_w2"]


def run(inputs, layers=(0, 1, 2, 3), n_cores=4, xin_override=None):
    prog = Prog(list(layers))
    nc = prog.build()
    print("instructions:", prog.kb.nins, "sems:", len(prog.kb.sems))
    cst = make_consts()
    in_maps = []
    for cidx in range(n_cores):
        b = cidx % 4
        m = {}
        if xin_override is not None:
            m["xin"] = xin_override[b]
        else:
            m["xin"] = np.ascontiguousarray(np.concatenate([inputs["ctx"][b], inputs["x"][b]], axis=0))
        m["cvec"] = np.ascontiguousarray(np.stack([inputs["c"][b], inputs["c_ctx"]], axis=0))
        for k in prog.inp:
            if k in m:
                continue
            m[k] = cst[k] if k in cst else np.ascontiguousarray(inputs[k])
        in_maps.append(m)
    res = run_bass_kernel_spmd(nc, in_maps, core_ids=list(range(n_cores)))
    return res


def kernel(**inputs):
    inputs = {k: np.asarray(v) for k, v in inputs.items()}
    res = run(inputs)
    out = np.stack([res.results[b]["out"] for b in range(4)], axis=0)
    return out.astype(np.float32)
```
